# Optimizing a Trainium2 kernel written in Bass

```python
import math
import jax, jax.numpy as jnp
from jax import lax
import numpy as np

D_MODEL = 1024
BATCH = 1
SEQ = 16384
DEPTH = 2

D_MIX = D_MODEL
N_GROUPS = 4
GROUP_W = D_MIX // N_GROUPS

A_HEADS = 4
A_QK_DIM = GROUP_W // A_HEADS // 2
A_V_DIM = 2 * A_QK_DIM

B_HEADS = 4
B_HEAD_DIM = GROUP_W // B_HEADS
MOBA_BLOCK = 256
MOBA_TOPK = 3

C_HEADS = 4
C_V_DIM = GROUP_W // C_HEADS
C_K_DIM = C_V_DIM // 2
GLA_RANK = 16
GLA_TAU = 16.0
GLA_CHUNK = 64

D_WIDTH = GROUP_W
D_BLOCKS = 4
D_BLOCK_DIM = D_WIDTH // D_BLOCKS
CONV_W = 4
LRU_C = 8.0

N_EXPERTS = 32
TOP_K = 4
D_FF = D_MODEL
SWIGLU_ALPHA = 1.702
SWIGLU_LIMIT = 7.0
MOE_BLOCK = 128

Q_BLOCK = 128
EPS = 1e-6

A_Q = A_HEADS * 2 * A_QK_DIM
A_K = A_HEADS * 2 * A_QK_DIM
A_V = A_HEADS * A_V_DIM
B_Q = GROUP_W
B_K = GROUP_W
B_V = GROUP_W
C_Q = C_HEADS * C_K_DIM
C_K = C_HEADS * C_K_DIM
C_V = C_HEADS * C_V_DIM
C_G = GLA_RANK
C_R = C_HEADS * C_V_DIM
D_X = D_WIDTH
D_G = D_WIDTH
D_IN = A_Q + A_K + A_V + B_Q + B_K + B_V + C_Q + C_K + C_V + C_G + C_R + D_X + D_G

kernel_name = "hybrid_parallel_heads_moe_block"


def rms_norm(x, gain):
    xf = x.astype(jnp.float32)
    y = xf * lax.rsqrt(jnp.mean(xf * xf, axis=-1, keepdims=True) + EPS)
    return (y * gain.astype(jnp.float32)).astype(x.dtype)


def to_heads(t, n):
    b, s, f = t.shape
    return t.reshape(b, s, n, f // n).transpose(0, 2, 1, 3)


def merge_heads(t):
    b, h, s, d = t.shape
    return t.transpose(0, 2, 1, 3).reshape(b, s, h * d)


def split_columns(proj):
    sizes = (A_Q, A_K, A_V, B_Q, B_K, B_V, C_Q, C_K, C_V, C_G, C_R, D_X, D_G)
    out, off = [], 0
    for sz in sizes:
        out.append(proj[..., off:off + sz])
        off += sz
    return out


def diff_attention(q, k, v, q_gain, k_gain, lam_q1, lam_k1, lam_q2, lam_k2, out_gain, layer_idx):
    bsz, s, _ = q.shape
    q = rms_norm(q.reshape(bsz, s, A_HEADS, 2, A_QK_DIM), q_gain).transpose(0, 2, 3, 1, 4)
    k = rms_norm(k.reshape(bsz, s, A_HEADS, 2, A_QK_DIM), k_gain).transpose(0, 2, 3, 1, 4)
    v = to_heads(v, A_HEADS)
    lam_init = 0.8 - 0.6 * math.exp(-0.3 * layer_idx)
    lam = (jnp.exp(jnp.sum(lam_q1.astype(jnp.float32) * lam_k1.astype(jnp.float32)))
           - jnp.exp(jnp.sum(lam_q2.astype(jnp.float32) * lam_k2.astype(jnp.float32))) + lam_init)
    scale = A_QK_DIM ** -0.5
    nq = s // Q_BLOCK
    qb = q.reshape(bsz, A_HEADS, 2, nq, Q_BLOCK, A_QK_DIM).transpose(3, 0, 1, 2, 4, 5)
    kpos = jnp.arange(s)

    def block(args):
        j, qj = args
        qpos = j * Q_BLOCK + jnp.arange(Q_BLOCK)
        sc = jnp.einsum('bhiqd,bhikd->bhiqk', qj, k).astype(jnp.float32) * scale
        sc = jnp.where(kpos[None, :] <= qpos[:, None], sc, -jnp.inf)
        p = jax.nn.softmax(sc, axis=-1)
        w = p[:, :, 0] - lam * p[:, :, 1]
        return jnp.einsum('bhqk,bhkd->bhqd', w.astype(v.dtype), v)

    o = lax.map(block, (jnp.arange(nq), qb))
    o = o.transpose(1, 2, 0, 3, 4).reshape(bsz, A_HEADS, s, A_V_DIM)
    o = rms_norm(o, out_gain) * (1.0 - lam_init)
    return merge_heads(o)


def moba_attention(q, k, v, q_gain, k_gain):
    bsz, s, _ = q.shape
    q = rms_norm(to_heads(q, B_HEADS), q_gain)
    k = rms_norm(to_heads(k, B_HEADS), k_gain)
    v = to_heads(v, B_HEADS)
    nb = -(-s // MOBA_BLOCK)
    pad = nb * MOBA_BLOCK - s
    k_p = jnp.pad(k, ((0, 0), (0, 0), (0, pad), (0, 0)))
    v_p = jnp.pad(v, ((0, 0), (0, 0), (0, pad), (0, 0)))
    kb = k_p.reshape(bsz, B_HEADS, nb, MOBA_BLOCK, B_HEAD_DIM)
    vb = v_p.reshape(bsz, B_HEADS, nb, MOBA_BLOCK, B_HEAD_DIM)
    k_mean = jnp.mean(kb.astype(jnp.float32), axis=3)
    topk = min(MOBA_TOPK, nb)
    scale = B_HEAD_DIM ** -0.5
    nq = s // Q_BLOCK
    qb = q.reshape(bsz, B_HEADS, nq, Q_BLOCK, B_HEAD_DIM).transpose(2, 0, 1, 3, 4)
    b_idx = jnp.arange(bsz)[:, None, None, None]
    h_idx = jnp.arange(B_HEADS)[None, :, None, None]
    blk_ids = jnp.arange(nb)

    def block(args):
        j, qj = args
        qpos = j * Q_BLOCK + jnp.arange(Q_BLOCK)
        own = (j * Q_BLOCK) // MOBA_BLOCK
        gate = jnp.einsum('bhqd,bhnd->bhqn', qj.astype(jnp.float32), k_mean)
        gate = jnp.where(blk_ids < own, gate, -jnp.inf)
        _, sel = lax.top_k(gate, topk)
        valid = sel < own
        k_sel = kb[b_idx, h_idx, sel]
        v_sel = vb[b_idx, h_idx, sel]
        s_sel = jnp.einsum('bhqd,bhqnkd->bhqnk', qj, k_sel).astype(jnp.float32) * scale
        s_sel = jnp.where(valid[..., None], s_sel, -jnp.inf).reshape(bsz, B_HEADS, Q_BLOCK, topk * MOBA_BLOCK)
        k_own = lax.dynamic_slice_in_dim(k_p, own * MOBA_BLOCK, MOBA_BLOCK, axis=2)
        v_own = lax.dynamic_slice_in_dim(v_p, own * MOBA_BLOCK, MOBA_BLOCK, axis=2)
        kpos = own * MOBA_BLOCK + jnp.arange(MOBA_BLOCK)
        s_own = jnp.einsum('bhqd,bhkd->bhqk', qj, k_own).astype(jnp.float32) * scale
        s_own = jnp.where(kpos[None, :] <= qpos[:, None], s_own, -jnp.inf)
        p = jax.nn.softmax(jnp.concatenate([s_sel, s_own], axis=-1), axis=-1).astype(v.dtype)
        p_sel = p[..., :topk * MOBA_BLOCK].reshape(bsz, B_HEADS, Q_BLOCK, topk, MOBA_BLOCK)
        p_own = p[..., topk * MOBA_BLOCK:]
        return (jnp.einsum('bhqnk,bhqnkd->bhqd', p_sel, v_sel)
                + jnp.einsum('bhqk,bhkd->bhqd', p_own, v_own))

    o = lax.map(block, (jnp.arange(nq), qb))
    o = o.transpose(1, 2, 0, 3, 4).reshape(bsz, B_HEADS, s, B_HEAD_DIM)
    return merge_heads(o)


def gla(q, k, v, g_low, r, w_g2, b_g, out_gain):
    bsz, s, _ = q.shape
    dt = q.dtype
    f32 = jnp.float32
    q = to_heads(q, C_HEADS).astype(f32) * (C_K_DIM ** -0.5)
    k = to_heads(k, C_HEADS).astype(f32)
    v = to_heads(v, C_HEADS).astype(f32)
    g = jax.nn.log_sigmoid((g_low @ w_g2 + b_g).astype(f32)) / GLA_TAU
    g = to_heads(g, C_HEADS)
    nc = s // GLA_CHUNK

    def chunks(t):
        return t.reshape(bsz, C_HEADS, nc, GLA_CHUNK, t.shape[-1]).transpose(2, 0, 1, 3, 4)

    causal = jnp.tril(jnp.ones((GLA_CHUNK, GLA_CHUNK), bool))

    def step(state, inp):
        qc, kc, vc, gc = inp
        bcum = jnp.cumsum(gc, axis=2)
        o_inter = jnp.einsum('bhcd,bhde->bhce', qc * jnp.exp(bcum), state)
        diff = bcum[:, :, :, None, :] - bcum[:, :, None, :, :]
        decay = jnp.exp(jnp.where(causal[:, :, None], diff, -jnp.inf))
        att = jnp.einsum('bhid,bhjd,bhijd->bhij', qc, kc, decay)
        o_intra = jnp.einsum('bhij,bhje->bhie', att, vc)
        b_last = bcum[:, :, -1:, :]
        state = (jnp.exp(b_last[:, :, 0, :])[..., None] * state
                 + jnp.einsum('bhcd,bhce->bhde', kc * jnp.exp(b_last - bcum), vc))
        return state, o_inter + o_intra

    state0 = jnp.zeros((bsz, C_HEADS, C_K_DIM, C_V_DIM), f32)
    _, o = lax.scan(step, state0, (chunks(q), chunks(k), chunks(v), chunks(g)))
    o = o.transpose(1, 2, 0, 3, 4).reshape(bsz, C_HEADS, s, C_V_DIM)
    o = merge_heads(rms_norm(o, out_gain)) * jax.nn.silu(r.astype(f32))
    return o.astype(dt)


def rg_lru(xb, gate_in, conv_w, conv_b, w_a, b_a, w_x, b_x, lam):
    bsz, s, cw = xb.shape
    f32 = jnp.float32
    xc = lax.conv_general_dilated(xb, conv_w[:, None, :], window_strides=(1,),
                                  padding=[(CONV_W - 1, 0)],
                                  dimension_numbers=('NWC', 'WIO', 'NWC'),
                                  feature_group_count=cw) + conv_b
    xs = xc.reshape(bsz, s, D_BLOCKS, D_BLOCK_DIM)
    r_gate = jax.nn.sigmoid((jnp.einsum('bsnd,nde->bsne', xs, w_a).reshape(bsz, s, cw) + b_a).astype(f32))
    i_gate = jax.nn.sigmoid((jnp.einsum('bsnd,nde->bsne', xs, w_x).reshape(bsz, s, cw) + b_x).astype(f32))
    log_a = LRU_C * r_gate * jax.nn.log_sigmoid(lam.astype(f32))
    a = jnp.exp(log_a)
    bt = jnp.sqrt(-jnp.expm1(2.0 * log_a)) * (i_gate * xc.astype(f32))

    def combine(c1, c2):
        a1, b1 = c1
        a2, b2 = c2
        return a1 * a2, a2 * b1 + b2

    _, h = lax.associative_scan(combine, (a, bt), axis=1)
    return (h * jax.nn.gelu(gate_in.astype(f32))).astype(xb.dtype)


def hybrid_mixer(h, w_in, w_out, a_q_gain, a_k_gain, a_lam_q1, a_lam_k1, a_lam_q2, a_lam_k2,
                 a_out_gain, b_q_gain, b_k_gain, c_w_g2, c_b_g, c_out_gain, d_conv_w, d_conv_b,
                 d_w_a, d_b_a, d_w_x, d_b_x, d_lambda, layer_idx):
    proj = h @ w_in
    aq, ak, av, bq, bk, bv, cq, ck, cv, cg, cr, dx, dg = split_columns(proj)
    o_a = diff_attention(aq, ak, av, a_q_gain, a_k_gain, a_lam_q1, a_lam_k1, a_lam_q2, a_lam_k2,
                         a_out_gain, layer_idx)
    o_b = moba_attention(bq, bk, bv, b_q_gain, b_k_gain)
    o_c = gla(cq, ck, cv, cg, cr, c_w_g2, c_b_g, c_out_gain)
    o_d = rg_lru(dx, dg, d_conv_w, d_conv_b, d_w_a, d_b_a, d_w_x, d_b_x, d_lambda)
    y = jnp.concatenate([o_a.astype(h.dtype), o_b.astype(h.dtype), o_c, o_d], axis=-1)
    return y @ w_out


def moe(h, router_w, router_b, w_up, b_up, w_down, b_down):
    bsz, s, d = h.shape
    t = bsz * s
    xt = h.reshape(t, d)
    logits = (xt @ router_w + router_b).astype(jnp.float32)
    top_vals, top_idx = lax.top_k(logits, TOP_K)
    gates = jax.nn.softmax(top_vals, axis=-1)
    n = t * TOP_K
    e_flat = top_idx.reshape(n)
    tok_flat = jnp.arange(n, dtype=jnp.int32) // TOP_K
    g_flat = gates.reshape(n)
    order = jnp.argsort(e_flat)
    e_sorted = e_flat[order]
    tok_sorted = tok_flat[order]
    g_sorted = g_flat[order]
    counts = jax.ops.segment_sum(jnp.ones((n,), jnp.int32), e_flat, num_segments=N_EXPERTS)
    starts = jnp.cumsum(counts) - counts
    padded = (counts + MOE_BLOCK - 1) // MOE_BLOCK * MOE_BLOCK
    pad_ends = jnp.cumsum(padded)
    pad_starts = pad_ends - padded
    dest = pad_starts[e_sorted] + (jnp.arange(n, dtype=jnp.int32) - starts[e_sorted])
    n_rows = -(-n // MOE_BLOCK) * MOE_BLOCK + N_EXPERTS * MOE_BLOCK
    n_blocks = n_rows // MOE_BLOCK
    row_tok = jnp.full((n_rows,), t, jnp.int32).at[dest].set(tok_sorted)
    row_gate = jnp.zeros((n_rows,), jnp.float32).at[dest].set(g_sorted)
    blk_expert = jnp.minimum(jnp.searchsorted(pad_ends, jnp.arange(n_blocks) * MOE_BLOCK, side='right'),
                             N_EXPERTS - 1)
    x_pad = jnp.concatenate([xt, jnp.zeros((1, d), xt.dtype)], axis=0)
    x_rows = x_pad[row_tok].reshape(n_blocks, MOE_BLOCK, d)

    def expert_block(args):
        e, xb = args
        hu = xb @ w_up[e] + b_up[e]
        g = jnp.minimum(hu[:, :D_FF], SWIGLU_LIMIT)
        lin = jnp.clip(hu[:, D_FF:], -SWIGLU_LIMIT, SWIGLU_LIMIT)
        glu = g * jax.nn.sigmoid(SWIGLU_ALPHA * g)
        return ((lin + 1.0) * glu) @ w_down[e] + b_down[e]

    y_rows = lax.map(expert_block, (blk_expert, x_rows)).reshape(n_rows, d)
    y = jax.ops.segment_sum(y_rows * row_gate[:, None].astype(y_rows.dtype), row_tok, num_segments=t + 1)[:t]
    return y.reshape(bsz, s, d)


def setup_inputs(seed: int = 0) -> dict:
    key = jax.random.key(seed)
    ks = iter(jax.random.split(key, 40))
    L = DEPTH
    f32 = jnp.float32

    def nrm(shape, scale):
        return jax.random.normal(next(ks), shape, f32) * scale

    def gain(shape):
        return 1.0 + nrm(shape, 0.05)

    x = nrm((BATCH, SEQ, D_MODEL), 1.0)
    c = nrm((BATCH, D_MODEL), 1.0)
    ada_w = nrm((L, D_MODEL, 6 * D_MODEL), 0.5 * D_MODEL ** -0.5)
    ada_b = nrm((L, 6 * D_MODEL), 0.02)
    norm1_g = gain((L, D_MODEL))
    norm2_g = gain((L, D_MODEL))
    w_in = nrm((L, D_MODEL, D_IN), D_MODEL ** -0.5)
    w_out = nrm((L, D_MIX, D_MODEL), D_MIX ** -0.5)
    a_q_gain = gain((L, A_QK_DIM))
    a_k_gain = gain((L, A_QK_DIM))
    a_lam_q1 = nrm((L, A_QK_DIM), 0.1)
    a_lam_k1 = nrm((L, A_QK_DIM), 0.1)
    a_lam_q2 = nrm((L, A_QK_DIM), 0.1)
    a_lam_k2 = nrm((L, A_QK_DIM), 0.1)
    a_out_gain = gain((L, A_V_DIM))
    b_q_gain = gain((L, B_HEAD_DIM))
    b_k_gain = gain((L, B_HEAD_DIM))
    c_w_g2 = nrm((L, GLA_RANK, C_HEADS * C_K_DIM), GLA_RANK ** -0.5)
    c_b_g = nrm((L, C_HEADS * C_K_DIM), 0.1)
    c_out_gain = gain((L, C_V_DIM))
    d_conv_w = nrm((L, CONV_W, D_WIDTH), CONV_W ** -0.5)
    d_conv_b = nrm((L, D_WIDTH), 0.02)
    d_w_a = nrm((L, D_BLOCKS, D_BLOCK_DIM, D_BLOCK_DIM), D_BLOCK_DIM ** -0.5)
    d_b_a = nrm((L, D_WIDTH), 0.02)
    d_w_x = nrm((L, D_BLOCKS, D_BLOCK_DIM, D_BLOCK_DIM), D_BLOCK_DIM ** -0.5)
    d_b_x = nrm((L, D_WIDTH), 0.02)
    u = jax.random.uniform(next(ks), (L, D_WIDTH), f32, 0.9, 0.999)
    p = u ** (1.0 / LRU_C)
    d_lambda = jnp.log(p) - jnp.log1p(-p)
    router_w = nrm((L, D_MODEL, N_EXPERTS), D_MODEL ** -0.5)
    router_b = nrm((L, N_EXPERTS), 0.01)
    exp_w_up = nrm((L, N_EXPERTS, D_MODEL, 2 * D_FF), D_MODEL ** -0.5)
    exp_b_up = nrm((L, N_EXPERTS, 2 * D_FF), 0.01)
    exp_w_down = nrm((L, N_EXPERTS, D_FF, D_MODEL), D_FF ** -0.5)
    exp_b_down = nrm((L, N_EXPERTS, D_MODEL), 0.01)
    return {"x": x, "c": c, "ada_w": ada_w, "ada_b": ada_b, "norm1_g": norm1_g, "norm2_g": norm2_g,
            "w_in": w_in, "w_out": w_out, "a_q_gain": a_q_gain, "a_k_gain": a_k_gain,
            "a_lam_q1": a_lam_q1, "a_lam_k1": a_lam_k1, "a_lam_q2": a_lam_q2, "a_lam_k2": a_lam_k2,
            "a_out_gain": a_out_gain, "b_q_gain": b_q_gain, "b_k_gain": b_k_gain,
            "c_w_g2": c_w_g2, "c_b_g": c_b_g, "c_out_gain": c_out_gain,
            "d_conv_w": d_conv_w, "d_conv_b": d_conv_b, "d_w_a": d_w_a, "d_b_a": d_b_a,
            "d_w_x": d_w_x, "d_b_x": d_b_x, "d_lambda": d_lambda,
            "router_w": router_w, "router_b": router_b, "exp_w_up": exp_w_up, "exp_b_up": exp_b_up,
            "exp_w_down": exp_w_down, "exp_b_down": exp_b_down}


def reference(x, c, ada_w, ada_b, norm1_g, norm2_g, w_in, w_out, a_q_gain, a_k_gain,
              a_lam_q1, a_lam_k1, a_lam_q2, a_lam_k2, a_out_gain, b_q_gain, b_k_gain,
              c_w_g2, c_b_g, c_out_gain, d_conv_w, d_conv_b, d_w_a, d_b_a, d_w_x, d_b_x, d_lambda,
              router_w, router_b, exp_w_up, exp_b_up, exp_w_down, exp_b_down):
    cond = jax.nn.silu(c)
    for l in range(DEPTH):
        mod = cond @ ada_w[l] + ada_b[l]
        sh1, sc1, g1, sh2, sc2, g2 = jnp.split(mod, 6, axis=-1)
        h = rms_norm(x, norm1_g[l]) * (1.0 + sc1[:, None, :]) + sh1[:, None, :]
        y = hybrid_mixer(h, w_in[l], w_out[l], a_q_gain[l], a_k_gain[l], a_lam_q1[l], a_lam_k1[l],
                         a_lam_q2[l], a_lam_k2[l], a_out_gain[l], b_q_gain[l], b_k_gain[l],
                         c_w_g2[l], c_b_g[l], c_out_gain[l], d_conv_w[l], d_conv_b[l],
                         d_w_a[l], d_b_a[l], d_w_x[l], d_b_x[l], d_lambda[l], l)
        x = x + g1[:, None, :] * y
        h = rms_norm(x, norm2_g[l]) * (1.0 + sc2[:, None, :]) + sh2[:, None, :]
        y = moe(h, router_w[l], router_b[l], exp_w_up[l], exp_b_up[l], exp_w_down[l], exp_b_down[l])
        x = x + g2[:, None, :] * y
    return x
```

```python
import contextlib, math
import numpy as np
import ml_dtypes
import concourse.bass as bass
import concourse.mybir as mybir
from concourse.bass_utils import run_bass_kernel_spmd


F32 = mybir.dt.float32
BF16 = mybir.dt.bfloat16
I32 = mybir.dt.int32
AF = mybir.ActivationFunctionType
ALU = mybir.AluOpType
AX = mybir.AxisListType


class Buf:
    def __init__(self, t, name):
        self.t = t
        self.name = name
        self.w = None
        self.r = {}

    def __getitem__(self, idx):
        return self.t[idx]


class Ctx:
    NDS = 8

    def __init__(self):
        self.nc = bass.Bass("TRN2", target_bir_lowering=False)
        nc = self.nc
        self.es = contextlib.ExitStack()
        self.E = {"pe": nc.tensor, "act": nc.scalar, "dve": nc.vector, "pool": nc.gpsimd, "sp": nc.sync}
        self.sems = {}
        self.cnt = {}
        for e in ("pe", "act", "dve", "pool"):
            self.sems[e] = self.es.enter_context(nc.semaphore("s_" + e))
            self.cnt[e] = 0
        self.dq = {}
        for q in ("sp", "pool", "act"):
            ss = []
            for i in range(self.NDS):
                k = "d_%s%d" % (q, i)
                self.sems[k] = self.es.enter_context(nc.semaphore(k))
                ss.append(k)
            self.dq[q] = [ss, 0]
        self.seen = {e: {} for e in self.E}
        self.nbuf = 0
        self.ninstr = 0

    def sbuf(self, shape, dt, name=None):
        self.nbuf += 1
        name = "sb_" + (name or "%d" % self.nbuf)
        t = self.es.enter_context(self.nc.sbuf_tensor(name, list(shape), dt))
        return Buf(t, name)

    def psum(self, shape, dt, name=None):
        self.nbuf += 1
        name = name or "ps%d" % self.nbuf
        t = self.es.enter_context(self.nc.psum_tensor(name, list(shape), dt))
        return Buf(t, name)

    def dram(self, name, shape, dt, kind):
        t = self.nc.dram_tensor(name, list(shape), dt, kind=kind).ap()
        return Buf(t, name)

    def _wait(self, eng, tok):
        if tok is None:
            return
        k, v = tok
        if self.seen[eng].get(k, 0) >= v:
            return
        self.E[eng].wait_ge(self.sems[k], v)
        self.seen[eng][k] = v
        self.ninstr += 1

    def _deps(self, eng, reads, writes, acc=False):
        for b in reads:
            self._wait(eng, b.w)
        for b in writes:
            if not (acc and b.w is not None and b.w[0] == eng == "pe"):
                self._wait(eng, b.w)
            for k, v in b.r.items():
                self._wait(eng, (k, v))

    def _mark(self, tok, reads, writes):
        k, v = tok
        for b in reads:
            if b.r.get(k, 0) < v:
                b.r[k] = v
        for b in writes:
            b.w = tok
            b.r = {}

    def op(self, eng, fn, reads=(), writes=(), acc=False):
        self._deps(eng, reads, writes, acc)
        ins = fn(self.E[eng])
        self.cnt[eng] += 1
        ins.then_inc(self.sems[eng], 1)
        self.ninstr += 1
        self._mark((eng, self.cnt[eng]), reads, writes)
        return ins

    def dma(self, q, out, in_, reads=(), writes=(), **kw):
        ss, j = self.dq[q]
        k = ss[j % self.NDS]
        rnd = j // self.NDS
        if rnd > 0:
            self._wait(q, (k, 16 * rnd))
        self._deps(q, reads, writes)
        ins = self.E[q].dma_start(out=out, in_=in_, **kw)
        ins.then_inc(self.sems[k], 16)
        self.dq[q][1] = j + 1
        self.ninstr += 1
        self._mark((k, 16 * (rnd + 1)), reads, writes)
        return ins

    def finish(self, bufs, eng="sp"):
        for b in bufs:
            self._wait(eng, b.w)

    def close(self):
        self.es.close()


def _dq_tokens(self):
    toks = [(e, self.cnt[e]) for e in ("pe", "act", "dve", "pool")]
    for q, (ss, j) in self.dq.items():
        for i, k in enumerate(ss):
            n = (j - i + self.NDS - 1) // self.NDS if j > i else 0
            if n > 0:
                toks.append((k, 16 * n))
    return toks


def _barrier(self):
    toks = _dq_tokens(self)
    for e in ("pe", "act", "dve", "pool", "sp"):
        for tk in toks:
            self._wait(e, tk)


def _push(self):
    self._outer = getattr(self, "_outer", [])
    self._outer.append(self.es)
    self.es = contextlib.ExitStack()


def _pop(self):
    _barrier(self)
    self.es.close()
    self.es = self._outer.pop()


Ctx.barrier = _barrier
Ctx.push = _push
Ctx.pop = _pop


T = 2048
NT = T // 128
D = 1024
DIN = 2832
EPS = 1e-6

def build_A():
    c = Ctx(); nc = c.nc
    inp = lambda n, s, d=F32: c.dram(n, s, d, "ExternalInput")
    outp = lambda n, s, d=F32: c.dram(n, s, d, "ExternalOutput")
    x = inp("x", [T, D]); cT = inp("cT", [128, 8]); adaw = inp("adaw", [D, 2048])
    adabT = inp("adabT", [128, 16]); n1gT = inp("n1gT", [128, 8]); w_in = inp("w_in", [D, DIN])
    gains = inp("gains", [128, 8])
    wg2 = inp("wg2", [16, 128]); ident_d = inp("ident", [128, 128]); bd32_d = inp("bd32", [128, 128]); bd64_d = inp("bd64", [128, 128])
    aqT = outp("aqT", [256, T], BF16); akT = outp("akT", [256, T], BF16)
    bqT = outp("bqT", [256, T], BF16); bkT = outp("bkT", [256, T], BF16)
    bqT32 = outp("bqT32", [256, T]); bkm = outp("bkm", [256, T // 256])
    cqT = outp("cqT", [128, T]); ckT = outp("ckT", [128, T]); gT = outp("gT", [128, T])
    dxT = outp("dxT", [256, T]); dgT = outp("dgT", [256, T])
    av = outp("av", [T, 256], BF16); bv = outp("bv", [T, 256], BF16)
    cv = outp("cv", [T, 256]); cr = outp("cr", [T, 256])

    ident = c.sbuf([128, 128], F32); bd32 = c.sbuf([128, 128], F32); bd64 = c.sbuf([128, 128], F32)
    gn = c.sbuf([128, 8], F32); cond = c.sbuf([128, 8], F32); adab = c.sbuf([128, 16], F32); n1g = c.sbuf([128, 8], F32)
    wg2s = c.sbuf([16, 128], F32)
    epsT = c.sbuf([128, 1], F32); oneT = c.sbuf([128, 1], F32)
    for sb, dr in ((ident, ident_d), (bd32, bd32_d), (bd64, bd64_d), (gn, gains), (cond, cT), (adab, adabT), (n1g, n1gT), (wg2s, wg2)):
        c.dma("sp", sb[:], dr[:], writes=[sb])
    c.op("dve", lambda e: e.memset(epsT[:], EPS), writes=[epsT])
    c.op("dve", lambda e: e.memset(oneT[:], 1.0), writes=[oneT])
    c.op("act", lambda e: e.activation(cond[:], cond[:], AF.Silu), reads=[cond], writes=[cond])

    PS = [c.psum([128, 512], F32, "psb%d" % i) for i in range(8)]

    adaw_v = adaw.t.rearrange("(k p) n -> p k n", p=128)
    wst = [c.sbuf([128, 8, 512], F32, "adst%d" % i) for i in range(2)]
    modps = PS[0]
    for jj in range(4):
        st = wst[jj % 2]
        c.dma("sp", st[:], adaw_v[:, :, jj * 512:(jj + 1) * 512], writes=[st])
        for j4 in range(4):
            j = jj * 4 + j4
            for k in range(8):
                c.op("pe", lambda e, k=k, j=j, j4=j4, st=st: e.matmul(modps[:, j:j + 1], st[:, k, j4 * 128:(j4 + 1) * 128], cond[:, k:k + 1],
                                                        start=(k == 0), stop=(k == 7)), reads=[st, cond], writes=[modps], acc=True)
    mod = c.sbuf([128, 16], F32)
    c.op("dve", lambda e: e.tensor_tensor(mod[:], modps[:, 0:16], adab[:], ALU.add), reads=[modps, adab], writes=[mod])
    a1 = c.sbuf([128, 8], F32)
    c.op("dve", lambda e: e.scalar_tensor_tensor(a1[:], mod[:, 8:16], 1.0, n1g[:], ALU.add, ALU.mult), reads=[mod, n1g], writes=[a1])

    wb = c.sbuf([128, 8, DIN], BF16, "wb")
    wbk = [Buf(None, "wbk%d" % k) for k in range(8)]
    win_v = w_in.t.rearrange("(k p) n -> p k n", p=128)
    wstage = [c.sbuf([128, DIN], F32, "wstage%d" % i) for i in range(2)]
    for k in range(8):
        st = wstage[k % 2]
        c.dma("sp", st[:], win_v[:, k, :], writes=[st])
        eng = "dve" if k % 2 == 0 else "pool"
        c.op(eng, lambda e, k=k, st=st: e.tensor_copy(wb[:, k, :], st[:]), reads=[st], writes=[wbk[k]])

    hT = c.sbuf([128, 8, T], BF16, "hT")
    hTt = [Buf(None, "hTt%d" % t) for t in range(NT)]
    xts = [c.sbuf([128, D], F32, "xt%d" % i) for i in range(2)]
    junk = c.sbuf([128, D], BF16, "junk")
    stat = [c.sbuf([128, 2], F32, "stat%d" % i) for i in range(2)]
    for t in range(NT):
        xt = xts[t % 2]; stt = stat[t % 2]
        c.dma("sp", xt[:], x[t * 128:(t + 1) * 128, :], writes=[xt])
        c.op("act", lambda e: e.activation(junk[:], xt[:], AF.Square, accum_out=stt[:, 0:1]), reads=[xt], writes=[junk, stt])
        c.op("act", lambda e: e.activation(stt[:, 1:2], stt[:, 0:1], AF.Sqrt, bias=epsT[:], scale=1.0 / D), reads=[stt, epsT], writes=[stt])
        c.op("dve", lambda e: e.reciprocal(stt[:, 1:2], stt[:, 1:2]), reads=[stt], writes=[stt])
        c.op("dve", lambda e: e.tensor_scalar(xt[:], xt[:], stt[:, 1:2], None, ALU.mult), reads=[xt, stt], writes=[xt])
        for half in range(2):
            ps = PS[1 + half]
            for kk in range(4):
                k = half * 4 + kk
                c.op("pe", lambda e, k=k, kk=kk, ps=ps: e.transpose(ps[:, kk * 128:(kk + 1) * 128], xt[:, k * 128:(k + 1) * 128], ident[:]),
                     reads=[xt, ident], writes=[ps], acc=True)
            for kk in range(4):
                k = half * 4 + kk
                if kk % 2 == 0:
                    c.op("dve", lambda e, k=k, kk=kk, ps=ps: e.tensor_scalar(hT[:, k, t * 128:(t + 1) * 128], ps[:, kk * 128:(kk + 1) * 128],
                                                                 a1[:, k:k + 1], mod[:, k:k + 1], ALU.mult, ALU.add),
                         reads=[ps, a1, mod], writes=[hTt[t]])
                else:
                    c.op("act", lambda e, k=k, kk=kk, ps=ps: e.activation(hT[:, k, t * 128:(t + 1) * 128], ps[:, kk * 128:(kk + 1) * 128],
                                                              AF.Identity, bias=mod[:, k:k + 1], scale=a1[:, k:k + 1]),
                         reads=[ps, a1, mod], writes=[hTt[t]])

    pi = [0]
    def nextps():
        pi[0] += 1
        return PS[3 + pi[0] % 5]
    wball = wbk
    def proj_fm(col0, ncols, g):
        ps = nextps()
        for k in range(8):
            c.op("pe", lambda e, k=k: e.matmul(ps[0:ncols, :], wb[:, k, col0:col0 + ncols], hT[:, k, g * 512:(g + 1) * 512],
                                               start=(k == 0), stop=(k == 7)),
                 reads=[wball[k]] + hTt[g * 4:(g + 1) * 4], writes=[ps], acc=True)
        return ps

    sqb = [c.sbuf([128, 512], F32, "sq%d" % i) for i in range(2)]
    rsb = [c.sbuf([128, 512], F32, "rs%d" % i) for i in range(2)]
    ob16 = [c.sbuf([128, 512], BF16, "ob16_%d" % i) for i in range(3)]
    ob32 = [c.sbuf([128, 512], F32, "ob32_%d" % i) for i in range(3)]
    kms = c.sbuf([128, 2, NT // 2], F32, "kms")
    kmsb = [Buf(None, "kmsb%d" % i) for i in range(2)]
    ctr = [0]
    def normed(ps, bd, inv_d, gcol, dst16, dst32=None, kmchunk=None, g=None, rows=None):
        i = ctr[0]; ctr[0] += 1
        sq = sqb[i % 2]; rs = rsb[i % 2]; o16 = ob16[i % 3]
        c.op("act", lambda e: e.activation(sq[:], ps[:], AF.Square), reads=[ps], writes=[sq])
        ps2 = nextps()
        c.op("pe", lambda e: e.matmul(ps2[:], bd[:], sq[:], start=True, stop=True), reads=[bd, sq], writes=[ps2])
        c.op("act", lambda e: e.activation(rs[:], ps2[:], AF.Sqrt, bias=epsT[:], scale=inv_d), reads=[ps2, epsT], writes=[rs])
        c.op("dve", lambda e: e.reciprocal(rs[:], rs[:]), reads=[rs], writes=[rs])
        c.op("dve", lambda e: e.scalar_tensor_tensor(o16[:], ps[:], gn[:, gcol:gcol + 1], rs[:], ALU.mult, ALU.mult), reads=[ps, gn, rs], writes=[o16])
        c.dma("pool", dst16[rows, g * 512:(g + 1) * 512], o16[:], reads=[o16], writes=[dst16])
        if dst32 is not None or kmchunk is not None:
            o32 = ob32[i % 3]
            c.op("dve", lambda e: e.scalar_tensor_tensor(o32[:], ps[:], gn[:, gcol:gcol + 1], rs[:], ALU.mult, ALU.mult), reads=[ps, gn, rs], writes=[o32])
            if dst32 is not None:
                c.dma("pool", dst32[rows, g * 512:(g + 1) * 512], o32[:], reads=[o32], writes=[dst32])
            if kmchunk is not None:
                c.op("dve", lambda e: e.tensor_reduce(kms[:, kmchunk, g * 2:(g + 1) * 2], o32[:].rearrange("p (b t) -> p b t", t=256), AX.X, ALU.add),
                     reads=[o32], writes=[kmsb[kmchunk]])

    def raw32(ps, nrows, dst, rows, g):
        i = ctr[0]; ctr[0] += 1
        o32 = ob32[i % 3]
        c.op("act" if i % 2 else "dve", (lambda e: e.activation(o32[0:nrows, :], ps[0:nrows, :], AF.Copy)) if i % 2 else
             (lambda e: e.tensor_copy(o32[0:nrows, :], ps[0:nrows, :])), reads=[ps], writes=[o32])
        c.dma("pool", dst[rows, g * 512:(g + 1) * 512], o32[0:nrows, :], reads=[o32], writes=[dst])
        return o32

    lt = [c.sbuf([128, 512], F32, "lt%d" % i) for i in range(3)]
    tm16 = [c.sbuf([128, 512], BF16, "tm16_%d" % i) for i in range(2)]
    tm32 = [c.sbuf([128, 512], F32, "tm32_%d" % i) for i in range(2)]
    cgs = c.sbuf([16, 512], F32, "cgs")
    for g in range(T // 512):
        for ch in range(2):
            rows = slice(ch * 128, (ch + 1) * 128)
            normed(proj_fm(0 + ch * 128, 128, g), bd32, 1.0 / 32, 0, aqT, g=g, rows=rows)
            normed(proj_fm(256 + ch * 128, 128, g), bd32, 1.0 / 32, 1, akT, g=g, rows=rows)
            normed(proj_fm(768 + ch * 128, 128, g), bd64, 1.0 / 64, 2, bqT, dst32=bqT32, g=g, rows=rows)
            normed(proj_fm(1024 + ch * 128, 128, g), bd64, 1.0 / 64, 3, bkT, kmchunk=ch, g=g, rows=rows)
            raw32(proj_fm(2320 + ch * 128, 128, g), 128, dxT, rows, g)
            raw32(proj_fm(2576 + ch * 128, 128, g), 128, dgT, rows, g)
        raw32(proj_fm(1536, 128, g), 128, cqT, slice(0, 128), g)
        raw32(proj_fm(1664, 128, g), 128, ckT, slice(0, 128), g)
        psg = proj_fm(2048, 16, g)
        c.op("dve", lambda e: e.tensor_copy(cgs[:], psg[0:16, :]), reads=[psg], writes=[cgs])
        psz = nextps()
        c.op("pe", lambda e: e.matmul(psz[:], wg2s[:], cgs[:], start=True, stop=True), reads=[wg2s, cgs], writes=[psz])
        z, az, m = lt
        c.op("dve", lambda e: e.tensor_scalar(z[:], psz[:], gn[:, 4:5], None, ALU.add), reads=[psz, gn], writes=[z])
        c.op("act", lambda e: e.activation(az[:], z[:], AF.Abs), reads=[z], writes=[az])
        c.op("act", lambda e: e.activation(az[:], az[:], AF.Exp, scale=-1.0), reads=[az], writes=[az])
        c.op("act", lambda e: e.activation(az[:], az[:], AF.Ln, bias=oneT[:], scale=1.0), reads=[az, oneT], writes=[az])
        c.op("dve", lambda e: e.tensor_scalar(m[:], z[:], 0.0, None, ALU.min), reads=[z], writes=[m])
        c.op("dve", lambda e: e.tensor_tensor(m[:], m[:], az[:], ALU.subtract), reads=[m, az], writes=[m])
        c.op("dve", lambda e: e.tensor_scalar(m[:], m[:], 1.0 / 16, None, ALU.mult), reads=[m], writes=[m])
        c.dma("pool", gT[:, g * 512:(g + 1) * 512], m[:], reads=[m], writes=[gT])
        for tt in range(4):
            t = g * 4 + tt
            ps = nextps()
            for (o, col0) in ((0, 512), (256, 1280)):
                for k in range(8):
                    c.op("pe", lambda e, k=k, o=o, col0=col0: e.matmul(ps[:, o:o + 256], hT[:, k, t * 128:(t + 1) * 128], wb[:, k, col0:col0 + 256],
                                                                    start=(k == 0), stop=(k == 7)), reads=[wball[k], hTt[t]], writes=[ps], acc=True)
            o16 = tm16[t % 2]
            c.op("act", lambda e: e.activation(o16[:], ps[:], AF.Copy), reads=[ps], writes=[o16])
            c.dma("pool", av[t * 128:(t + 1) * 128, :], o16[:, 0:256], reads=[o16], writes=[av])
            c.dma("pool", bv[t * 128:(t + 1) * 128, :], o16[:, 256:512], reads=[o16], writes=[bv])
            ps = nextps()
            for (o, col0) in ((0, 1792), (256, 2064)):
                for k in range(8):
                    c.op("pe", lambda e, k=k, o=o, col0=col0: e.matmul(ps[:, o:o + 256], hT[:, k, t * 128:(t + 1) * 128], wb[:, k, col0:col0 + 256],
                                                                    start=(k == 0), stop=(k == 7)), reads=[wball[k], hTt[t]], writes=[ps], acc=True)
            o32 = tm32[t % 2]
            c.op("dve", lambda e: e.tensor_copy(o32[:], ps[:]), reads=[ps], writes=[o32])
            c.dma("pool", cv[t * 128:(t + 1) * 128, :], o32[:, 0:256], reads=[o32], writes=[cv])
            c.dma("pool", cr[t * 128:(t + 1) * 128, :], o32[:, 256:512], reads=[o32], writes=[cr])
    for ch in range(2):
        c.dma("pool", bkm[ch * 128:(ch + 1) * 128, :], kms[:, ch, :], reads=[kmsb[ch]], writes=[bkm])
    outs = [aqT, akT, bqT, bkT, bqT32, bkm, cqT, ckT, gT, dxT, dgT, av, bv, cv, cr]
    c.finish(outs, "pool")
    c.close()
    return c


S = 16384
BIG = 1.0e9
BIGB = 30000.0

def build_B(do_rg=True, do_gla=True, do_diff=True, do_moba=True, NG=32):
    c = Ctx(); nc = c.nc
    inp = lambda n, s, d=F32: c.dram(n, s, d, "ExternalInput")
    outp = lambda n, s, d=F32: c.dram(n, s, d, "ExternalOutput")
    dq = inp("dq", [32, S], BF16); dk = inp("dk", [32, S], BF16); dv = inp("dv", [128, 128, 65], BF16)
    mq = inp("mq", [64, S], BF16); mk = inp("mk", [64, S // 2], BF16); mv = inp("mv", [128, 64, 65], BF16)
    mq32 = inp("mq32", [64, S]); mkm = inp("mkm", [64, 64]); par_d = inp("par", [128, 2]); bmask_d = inp("bmask", [128, 1024], BF16)
    cmask_d = inp("cmask", [128, 4, 512], BF16); Z_d = inp("Z", [32, 32, 128], BF16); iota_d = inp("iota", [128, 64]); ident_d = inp("ident", [128, 128])
    gq = inp("gq", [32, S]); gk = inp("gk", [32, S]); gg = inp("gg", [32, S]); gv = inp("gv", [64, 256, 64])
    rmask_d = inp("rmask", [32, 2048]); tri8_d = inp("tri8", [64, 512])
    rx = inp("rx", [64, S + 3]); rg = inp("rg", [64, S]); rw_d = inp("rw", [64, 8]); rwa_d = inp("rwa", [64, 64]); rwx_d = inp("rwx", [64, 64])
    oa = outp("oa", [65, S]); ob = outp("ob", [65, S]); oc = outp("oc", [256, 64, 64]); od = outp("od", [64, S])

    PS1 = [c.psum([128, 512], F32, "ps1_%d" % i) for i in range(4)]
    PS2 = [c.psum([128, 1024], F32, "ps2_%d" % i) for i in range(2)]
    ident = c.sbuf([128, 128], F32, "ident_sb")
    c.dma("sp", ident[:], ident_d[:], writes=[ident])
    oneT = c.sbuf([128, 1], F32, "oneT")
    c.op("dve", lambda e: e.memset(oneT[:], 1.0), writes=[oneT])

    if do_rg:
        c.push()
        PW = 2048
        rw = c.sbuf([64, 8], F32, "rw"); rwa = c.sbuf([64, 64], F32, "rwa"); rwx = c.sbuf([64, 64], F32, "rwx")
        for sb, dr in ((rw, rw_d), (rwa, rwa_d), (rwx, rwx_d)):
            c.dma("sp", sb[:], dr[:], writes=[sb])
        cl = c.sbuf([64, 2], F32, "cl")
        c.op("act", lambda e: e.activation(cl[:, 0:1], rw[:, 7:8], AF.Exp, scale=-1.0), reads=[rw], writes=[cl])
        c.op("act", lambda e: e.activation(cl[:, 0:1], cl[:, 0:1], AF.Ln, bias=oneT[0:64, :], scale=1.0), reads=[cl, oneT], writes=[cl])
        c.op("dve", lambda e: e.tensor_scalar(cl[:, 1:2], cl[:, 0:1], -8.0, None, ALU.mult), reads=[cl], writes=[cl])
        xin = [c.sbuf([64, PW + 3], F32, "xin%d" % i) for i in range(2)]
        gin = [c.sbuf([64, PW], F32, "gin%d" % i) for i in range(2)]
        xc = c.sbuf([64, PW], F32, "xc"); rgt = c.sbuf([64, PW], F32, "rgt"); igt = c.sbuf([64, PW], F32, "igt")
        aa = c.sbuf([64, PW], F32, "aa"); bt = c.sbuf([64, PW], F32, "bt")
        hh = [c.sbuf([64, PW], F32, "hh%d" % i) for i in range(2)]
        uu = c.sbuf([64, PW], F32, "uu")
        for pc in range(S // PW):
            lo = pc * PW
            xi = xin[pc % 2]; gi = gin[pc % 2]; h = hh[pc % 2]; hp = hh[(pc + 1) % 2]
            c.dma("sp", xi[:], rx[:, lo:lo + PW + 3], writes=[xi])
            c.dma("sp", gi[:], rg[:, lo:lo + PW], writes=[gi])
            c.op("dve", lambda e: e.tensor_scalar(xc[:], xi[:, 3:PW + 3], rw[:, 3:4], rw[:, 4:5], ALU.mult, ALU.add), reads=[xi, rw], writes=[xc])
            for j in range(3):
                c.op("dve", lambda e, j=j: e.scalar_tensor_tensor(xc[:], xi[:, j:PW + j], rw[:, j:j + 1], xc[:], ALU.mult, ALU.add), reads=[xi, rw, xc], writes=[xc])
            for grp in range(PW // 512):
                sl = slice(grp * 512, (grp + 1) * 512)
                p1 = PS1[0]; p2 = PS1[1]
                c.op("pe", lambda e: e.matmul(p1[0:64, :], rwa[:], xc[:, sl], start=True, stop=True), reads=[rwa, xc], writes=[p1])
                c.op("pe", lambda e: e.matmul(p2[0:64, :], rwx[:], xc[:, sl], start=True, stop=True), reads=[rwx, xc], writes=[p2])
                c.op("act", lambda e: e.activation(rgt[:, sl], p1[0:64, :], AF.Sigmoid, bias=rw[:, 5:6], scale=1.0), reads=[p1, rw], writes=[rgt])
                c.op("act", lambda e: e.activation(igt[:, sl], p2[0:64, :], AF.Sigmoid, bias=rw[:, 6:7], scale=1.0), reads=[p2, rw], writes=[igt])
            c.op("act", lambda e: e.activation(aa[:], rgt[:], AF.Exp, scale=cl[:, 1:2]), reads=[rgt, cl], writes=[aa])
            c.op("act", lambda e: e.activation(bt[:], aa[:], AF.Square), reads=[aa], writes=[bt])
            c.op("dve", lambda e: e.tensor_scalar(bt[:], bt[:], -1.0, 1.0, ALU.mult, ALU.add), reads=[bt], writes=[bt])
            c.op("act", lambda e: e.activation(bt[:], bt[:], AF.Sqrt), reads=[bt], writes=[bt])
            c.op("dve", lambda e: e.tensor_tensor(bt[:], bt[:], igt[:], ALU.mult), reads=[bt, igt], writes=[bt])
            c.op("dve", lambda e: e.tensor_tensor(bt[:], bt[:], xc[:], ALU.mult), reads=[bt, xc], writes=[bt])
            if pc == 0:
                c.op("dve", lambda e: e.tensor_tensor_scan(h[:], aa[:], bt[:], 0.0, ALU.mult, ALU.add), reads=[aa, bt], writes=[h])
            else:
                c.op("dve", lambda e: e.tensor_tensor_scan(h[:], aa[:], bt[:], hp[:, PW - 1:PW], ALU.mult, ALU.add), reads=[aa, bt, hp], writes=[h])
            c.op("dve", lambda e: e.tensor_tensor(uu[:], gi[:], gi[:], ALU.mult), reads=[gi], writes=[uu])
            c.op("dve", lambda e: e.tensor_scalar(uu[:], uu[:], 0.044715, 1.0, ALU.mult, ALU.add), reads=[uu], writes=[uu])
            c.op("dve", lambda e: e.tensor_tensor(uu[:], uu[:], gi[:], ALU.mult), reads=[uu, gi], writes=[uu])
            c.op("act", lambda e: e.activation(uu[:], uu[:], AF.Sigmoid, scale=1.5957691216057308), reads=[uu], writes=[uu])
            c.op("dve", lambda e: e.tensor_tensor(uu[:], uu[:], gi[:], ALU.mult), reads=[uu, gi], writes=[uu])
            c.op("dve", lambda e: e.tensor_tensor(uu[:], uu[:], h[:], ALU.mult), reads=[uu, h], writes=[uu])
            c.dma("pool", od[:, lo:lo + PW], uu[:], reads=[uu], writes=[od])
        c.pop()

    if do_gla:
        c.push()
        PW = 2048; NCH = PW // 64
        rmask = c.sbuf([32, PW], F32, "rmask"); tri8 = c.sbuf([64, 512], F32, "tri8")
        c.dma("sp", rmask[:], rmask_d[:], writes=[rmask]); c.dma("sp", tri8[:], tri8_d[:], writes=[tri8])
        qs = [c.sbuf([32, PW], F32, "gq%d" % i) for i in range(2)]
        ks = [c.sbuf([32, PW], F32, "gk%d" % i) for i in range(2)]
        gs = [c.sbuf([32, PW], F32, "gg%d" % i) for i in range(2)]
        vs = [c.sbuf([64, NCH, 64], F32, "gv%d" % i) for i in range(2)]
        bcum = c.sbuf([32, PW], F32, "bcum"); eb = c.sbuf([32, PW], F32, "eb"); qe = c.sbuf([32, PW], F32, "qe")
        ke = c.sbuf([32, PW], F32, "ke"); kl = c.sbuf([32, PW], F32, "kl"); dec = c.sbuf([32, NCH], F32, "dec")
        attT = c.sbuf([64, NCH, 64], F32, "attT"); klT = c.sbuf([64, 256], F32, "klT")
        U = c.sbuf([32, NCH, 64], F32, "U"); Sall = c.sbuf([32, NCH + 1, 64], F32, "Sall")
        osb = [c.sbuf([64, 8, 64], F32, "gosb%d" % i) for i in range(2)]
        c.op("dve", lambda e: e.memset(Sall[:, 0, :], 0.0), writes=[Sall])
        for pc in range(S // PW):
            lo = pc * PW
            q = qs[pc % 2]; k = ks[pc % 2]; g = gs[pc % 2]; v = vs[pc % 2]
            c.dma("sp", q[:], gq[:, lo:lo + PW], writes=[q]); c.dma("sp", k[:], gk[:, lo:lo + PW], writes=[k])
            c.dma("sp", g[:], gg[:, lo:lo + PW], writes=[g]); c.dma("sp", v[:], gv[:, pc * NCH:(pc + 1) * NCH, :], writes=[v])
            c.op("dve", lambda e: e.tensor_tensor_scan(bcum[:], rmask[:], g[:], 0.0, ALU.mult, ALU.add), reads=[rmask, g], writes=[bcum])
            c.op("act", lambda e: e.activation(eb[:], bcum[:], AF.Exp), reads=[bcum], writes=[eb])
            c.op("dve", lambda e: e.scalar_tensor_tensor(qe[:], q[:], 32.0 ** -0.5, eb[:], ALU.mult, ALU.mult), reads=[q, eb], writes=[qe])
            c.op("act", lambda e: e.activation(eb[:], bcum[:], AF.Exp, scale=-1.0), reads=[bcum], writes=[eb])
            c.op("dve", lambda e: e.tensor_tensor(ke[:], k[:], eb[:], ALU.mult), reads=[k, eb], writes=[ke])
            bc3 = bcum[:].rearrange("p (c t) -> p c t", t=64)
            c.op("act", lambda e: e.activation(dec[:], bc3[:, :, 63], AF.Exp), reads=[bcum], writes=[dec])
            for cc in range(NCH):
                c.op("dve", lambda e, cc=cc: e.tensor_scalar(kl[:, cc * 64:(cc + 1) * 64], ke[:, cc * 64:(cc + 1) * 64], dec[:, cc:cc + 1], None, ALU.mult),
                     reads=[ke, dec], writes=[kl])
            for grp in range(NCH // 8):
                pA, pT_, pU = PS1[0], PS1[1], PS1[2]
                for cc in range(8):
                    ch = grp * 8 + cc; sl = slice(ch * 64, (ch + 1) * 64)
                    c.op("pe", lambda e, cc=cc, sl=sl: e.matmul(pA[0:64, cc * 64:(cc + 1) * 64], ke[:, sl], qe[:, sl], start=True, stop=True),
                         reads=[ke, qe], writes=[pA], acc=True)
                c.op("dve", lambda e: e.tensor_tensor(attT[:, grp * 8:(grp + 1) * 8, :].rearrange("p c t -> p (c t)"), pA[0:64, :], tri8[:], ALU.mult),
                     reads=[pA, tri8], writes=[attT])
                for cc in range(8):
                    ch = grp * 8 + cc; sl = slice(ch * 64, (ch + 1) * 64)
                    c.op("pe", lambda e, cc=cc, sl=sl: e.transpose(pT_[0:64, cc * 32:(cc + 1) * 32], kl[:, sl], ident[0:32, 0:32]),
                         reads=[kl, ident], writes=[pT_], acc=True)
                c.op("act", lambda e: e.activation(klT[:], pT_[0:64, 0:256], AF.Copy), reads=[pT_], writes=[klT])
                for cc in range(8):
                    ch = grp * 8 + cc
                    c.op("pe", lambda e, cc=cc, ch=ch: e.matmul(pU[0:32, cc * 64:(cc + 1) * 64], klT[:, cc * 32:(cc + 1) * 32], v[:, ch, :], start=True, stop=True),
                         reads=[klT, v], writes=[pU], acc=True)
                c.op("dve", lambda e: e.tensor_copy(U[:, grp * 8:(grp + 1) * 8, :].rearrange("p c t -> p (c t)"), pU[0:32, :]), reads=[pU], writes=[U])
            for e_ in range(64):
                c.op("dve", lambda e, e_=e_: e.tensor_tensor_scan(Sall[:, 1:NCH + 1, e_], dec[:], U[:, :, e_], Sall[:, 0, e_:e_ + 1], ALU.mult, ALU.add),
                     reads=[dec, U, Sall], writes=[Sall])
            for grp in range(NCH // 8):
                pO = PS1[3]
                for cc in range(8):
                    ch = grp * 8 + cc; sl = slice(ch * 64, (ch + 1) * 64)
                    c.op("pe", lambda e, cc=cc, ch=ch: e.matmul(pO[0:64, cc * 64:(cc + 1) * 64], attT[:, ch, :], v[:, ch, :], start=True, stop=False),
                         reads=[attT, v], writes=[pO], acc=True)
                    c.op("pe", lambda e, cc=cc, ch=ch, sl=sl: e.matmul(pO[0:64, cc * 64:(cc + 1) * 64], qe[:, sl], Sall[:, ch, :], start=False, stop=True),
                         reads=[qe, Sall], writes=[pO], acc=True)
                o = osb[grp % 2]
                c.op("act", lambda e: e.activation(o[:].rearrange("p c t -> p (c t)"), pO[0:64, :], AF.Copy), reads=[pO], writes=[o])
                c0 = pc * NCH + grp * 8
                c.dma("pool", oc[c0:c0 + 8, :, :].rearrange("c p e -> p c e"), o[:], reads=[o], writes=[oc])
            c.op("dve", lambda e: e.tensor_copy(Sall[:, 0, :], Sall[:, NCH, :]), reads=[Sall], writes=[Sall])
        c.pop()

    c.push()
    qT = c.sbuf([64, S], BF16, "qT"); kT = c.sbuf([64, S], BF16, "kT"); V = c.sbuf([128, 128, 65], BF16, "V")
    cmask = c.sbuf([128, 4, 512], BF16, "cmask"); bmask = c.sbuf([128, 1024], BF16, "bmask")
    pTs = [c.sbuf([128, 1024], BF16, "pT%d" % i) for i in range(3)]
    osbs = [c.sbuf([65, 512], F32, "osb%d" % i) for i in range(2)]
    c.dma("sp", cmask[:], cmask_d[:], writes=[cmask]); c.dma("sp", bmask[:], bmask_d[:], writes=[bmask])
    step = [0]

    def attend(g, pairs, Kd, scale, masks, out_d, extra=None):
        pO = PS1[g % 2]
        n = len(pairs)
        for pi_, (j0, j1) in enumerate(pairs):
            sps = PS2[step[0] % 2]; pT = pTs[step[0] % 3]; step[0] += 1
            for hh, j in enumerate((j0, j1)):
                if extra is None:
                    c.op("pe", lambda e, hh=hh, j=j: e.matmul(sps[:, hh * 512:(hh + 1) * 512], kT[0:Kd, j * 128:(j + 1) * 128], qT[0:Kd, g * 512:(g + 1) * 512],
                                                             start=True, stop=True), reads=[kT, qT], writes=[sps], acc=True)
                else:
                    zl, biasT = extra(pi_)
                    c.op("pe", lambda e, hh=hh, j=j: e.matmul(sps[:, hh * 512:(hh + 1) * 512], kT[0:Kd, j * 128:(j + 1) * 128], qT[0:Kd, g * 512:(g + 1) * 512],
                                                             start=True, stop=False), reads=[kT, qT], writes=[sps], acc=True)
                    c.op("pe", lambda e, hh=hh, zl=zl, biasT=biasT: e.matmul(sps[:, hh * 512:(hh + 1) * 512], zl, biasT[:], start=False, stop=True),
                         reads=[Zb, biasT], writes=[sps], acc=True)
            c.op("act", lambda e: e.activation(pT[:], sps[:], AF.Exp, scale=scale), reads=[sps], writes=[pT])
            if pi_ in masks:
                mk_ap, mk_buf = masks[pi_]
                c.op("dve", lambda e: e.tensor_tensor(pT[:], pT[:], mk_ap, ALU.mult), reads=[pT, mk_buf], writes=[pT])
            for hh, j in enumerate((j0, j1)):
                c.op("pe", lambda e, hh=hh, j=j: e.matmul(pO[0:65, :], V[:, j, :], pT[:, hh * 512:(hh + 1) * 512],
                                                         start=(pi_ == 0 and hh == 0), stop=(pi_ == n - 1 and hh == 1)),
                     reads=[V, pT], writes=[pO], acc=True)
        o = osbs[g % 2]
        c.op("dve", lambda e: e.tensor_copy(o[:], pO[0:65, :]), reads=[pO], writes=[o])
        c.dma("pool", out_d[:, g * 512:(g + 1) * 512], o[:], reads=[o], writes=[out_d])

    if do_diff:
        c.dma("sp", qT[0:32, :], dq[:], writes=[qT]); c.dma("sp", kT[0:32, :], dk[:], writes=[kT]); c.dma("sp", V[:], dv[:], writes=[V])
        cm2 = cmask[:].rearrange("p d q -> p (d q)")
        for g in range(NG):
            npair = 2 * (g + 1)
            pairs = [(2 * i, 2 * i + 1) for i in range(npair)]
            masks = {npair - 2: (cm2[:, 0:1024], cmask), npair - 1: (cm2[:, 1024:2048], cmask)}
            attend(g, pairs, 32, 32.0 ** -0.5, masks, oa)

    if do_moba:
        Zb = c.sbuf([32, 32, 128], BF16, "Zb"); iota = c.sbuf([128, 64], F32, "iota"); par = c.sbuf([128, 2], F32, "par")
        km = c.sbuf([64, 64], F32, "km")
        c.dma("sp", Zb[:], Z_d[:], writes=[Zb]); c.dma("sp", iota[:], iota_d[:], writes=[iota]); c.dma("sp", par[:], par_d[:], writes=[par])
        c.dma("sp", km[:], mkm[:], writes=[km])
        c.dma("sp", qT[0:64, :], mq[:], writes=[qT]); c.dma("sp", kT[0:64, 0:S // 2], mk[:], writes=[kT]); c.dma("sp", V[:, 0:64, :], mv[:], writes=[V])
        q32s = [c.sbuf([64, 512], F32, "q32_%d" % i) for i in range(2)]
        biasTs = [c.sbuf([32, 512], BF16, "biasT%d" % i) for i in range(2)]
        W = {n: [c.sbuf([128, 64], F32, "mw_%s%d" % (n, i)) for i in range(2)] for n in ("lt", "t1", "gm", "sel", "eq")}
        top8s = [c.sbuf([128, 8], F32, "top8_%d" % i) for i in range(2)]
        bps = [c.sbuf([128, 32], F32, "bp%d" % i) for i in range(2)]
        for g in range(NG):
            q32 = q32s[g % 2]; biasT = biasTs[g % 2]
            c.dma("sp", q32[:], mq32[:, g * 512:(g + 1) * 512], writes=[q32])
            pB = PS1[2]
            for qi in range(4):
                qt = 4 * g + qi; own = float(qt // 2); i2 = qt % 2
                lt, t1, gm, sel, eq, top8, bp = W["lt"][i2], W["t1"][i2], W["gm"][i2], W["sel"][i2], W["eq"][i2], top8s[i2], bps[i2]
                pG = PS1[3]
                c.op("pe", lambda e, qi=qi: e.matmul(pG[:, 0:64], q32[:, qi * 128:(qi + 1) * 128], km[:], start=True, stop=True), reads=[q32, km], writes=[pG])
                c.op("dve", lambda e: e.tensor_single_scalar(lt[:], iota[:], own, ALU.is_lt), reads=[iota], writes=[lt])
                c.op("dve", lambda e: e.tensor_scalar(t1[:], lt[:], -1.0, BIG, ALU.add, ALU.mult), reads=[lt], writes=[t1])
                c.op("dve", lambda e: e.tensor_tensor(gm[:], pG[:, 0:64], lt[:], ALU.mult), reads=[pG, lt], writes=[gm])
                c.op("dve", lambda e: e.tensor_tensor(gm[:], gm[:], t1[:], ALU.add), reads=[gm, t1], writes=[gm])
                c.op("dve", lambda e: e.max(top8[:], gm[:]), reads=[gm], writes=[top8])
                c.op("dve", lambda e: e.tensor_scalar(sel[:], gm[:], top8[:, 2:3], None, ALU.is_ge), reads=[gm, top8], writes=[sel])
                c.op("dve", lambda e: e.tensor_tensor(sel[:], sel[:], lt[:], ALU.mult), reads=[sel, lt], writes=[sel])
                c.op("dve", lambda e: e.tensor_single_scalar(eq[:], iota[:], own, ALU.is_equal), reads=[iota], writes=[eq])
                c.op("dve", lambda e: e.tensor_tensor(sel[:], sel[:], eq[:], ALU.add), reads=[sel, eq], writes=[sel])
                c.op("dve", lambda e: e.tensor_scalar(sel[:], sel[:], -1.0, BIGB, ALU.add, ALU.mult), reads=[sel], writes=[sel])
                s3 = sel[:].rearrange("p (m two) -> p m two", two=2)
                c.op("dve", lambda e: e.tensor_scalar(bp[:], s3[:, :, 0], par[:, 0:1], None, ALU.mult), reads=[sel, par], writes=[bp])
                c.op("dve", lambda e: e.scalar_tensor_tensor(bp[:], s3[:, :, 1], par[:, 1:2], bp[:], ALU.mult, ALU.add), reads=[sel, par, bp], writes=[bp])
                c.op("pe", lambda e, qi=qi: e.transpose(pB[0:32, qi * 128:(qi + 1) * 128], bp[:], ident[:]), reads=[bp, ident], writes=[pB], acc=True)
            c.op("act", lambda e: e.activation(biasT[:], pB[0:32, :], AF.Copy), reads=[pB], writes=[biasT])
            pairs = [(2 * m, 2 * m + 1) for m in range(g + 1)]
            masks = {g: (bmask[:], bmask)}
            attend(g, pairs, 64, 64.0 ** -0.5, masks, ob, extra=lambda m, biasT=biasT: (Zb[:, m, :], biasT))
    c.pop()
    c.finish([oa, ob, oc, od], "pool")
    c.close()
    return c


T = 2048
NT = T // 128
D = 1024
EPS = 1e-6
NE = 32

def build_C(n_exp=NE):
    c = Ctx(); nc = c.nc
    inp = lambda n, s, d=F32: c.dram(n, s, d, "ExternalInput")
    x = inp("x", [T, D]); oaT = inp("oaT", [T, 8, 65]); obT = inp("obT", [T, 8, 65]); ocT = inp("ocT", [T, 256]); crT = inp("crT", [T, 256]); odT = inp("odT", [T, 256])
    cTb = inp("cTb", [128, 8, 128]); adaw = inp("adaw", [D, 6144]); adabB = inp("adabB", [128, 2, 1024]); adabT2 = inp("adabT2", [128, 16])
    lamv = inp("lamv", [128, 4, 32]); lamc_d = inp("lamc", [128, 2]); aog_d = inp("aog", [128, 64]); cog_d = inp("cog", [128, 64]); n2gT = inp("n2gT", [128, 8])
    w_out = inp("w_out", [D, D]); rwT = inp("rwT", [128, 8, 32]); rbB = inp("rbB", [128, 32]); ident_d = inp("ident", [128, 128])
    wup = inp("wup", [NE, D, 2 * D]); bupT = inp("bupT", [128, NE, 16]); wdn = inp("wdn", [NE, D, D]); bdn = inp("bdn", [NE, D])
    out = c.dram("out", [T, D], F32, "ExternalOutput")
    outt = [Buf(out.t, "out%d" % t) for t in range(NT)]

    PS = [c.psum([128, 512], F32, "psb%d" % i) for i in range(8)]
    ident = c.sbuf([128, 128], F32, "ident"); epsT = c.sbuf([128, 1], F32, "epsT")
    c.dma("sp", ident[:], ident_d[:], writes=[ident])
    c.op("dve", lambda e: e.memset(epsT[:], EPS), writes=[epsT])
    g2b = c.sbuf([128, D], F32, "g2b")
    h2T = c.sbuf([128, 8, T], BF16, "h2T"); h2Tt = [Buf(None, "h2Tt%d" % t) for t in range(NT)]
    Gall = c.sbuf([128, NT, NE], F32, "Gall"); Gt = [Buf(None, "Gt%d" % t) for t in range(NT)]
    GT = c.sbuf([32, T], F32, "GT"); GTt = [Buf(None, "GTt%d" % t) for t in range(NT)]

    c.push()
    cb = c.sbuf([128, 8, 128], F32, "cb")
    c.dma("sp", cb[:], cTb[:], writes=[cb])
    c.op("act", lambda e: e.activation(cb[:], cb[:], AF.Silu), reads=[cb], writes=[cb])
    adaw_v = adaw.t.rearrange("(k p) n -> p k n", p=128)
    wst = [c.sbuf([128, 8, 512], F32, "adst%d" % i) for i in range(2)]
    g1b = c.sbuf([128, D], F32, "g1b"); abB = c.sbuf([128, 2, D], F32, "abB")
    c.dma("sp", abB[:], adabB[:], writes=[abB])
    si = 0
    for gi, (dst, col0) in enumerate(((g1b, 2048), (g2b, 5120))):
        for hf in range(2):
            st = wst[si % 2]; si += 1
            c.dma("sp", st[:], adaw_v[:, :, col0 + hf * 512:col0 + (hf + 1) * 512], writes=[st])
            ps = PS[hf]
            for k in range(8):
                c.op("pe", lambda e, k=k, st=st, ps=ps: e.matmul(ps[:], cb[:, k, :], st[:, k, :], start=(k == 0), stop=(k == 7)), reads=[cb, st], writes=[ps], acc=True)
            c.op("dve", lambda e, ps=ps, dst=dst, gi=gi, hf=hf: e.tensor_tensor(dst[:, hf * 512:(hf + 1) * 512], ps[:], abB[:, gi, hf * 512:(hf + 1) * 512], ALU.add),
                 reads=[ps, abB], writes=[dst])
    modps = PS[2]
    adab2 = c.sbuf([128, 16], F32, "adab2"); n2g = c.sbuf([128, 8], F32, "n2g")
    c.dma("sp", adab2[:], adabT2[:], writes=[adab2]); c.dma("sp", n2g[:], n2gT[:], writes=[n2g])
    for jj in range(4):
        st = wst[si % 2]; si += 1
        c.dma("sp", st[:], adaw_v[:, :, 3072 + jj * 512:3072 + (jj + 1) * 512], writes=[st])
        for j4 in range(4):
            j = jj * 4 + j4
            for k in range(8):
                c.op("pe", lambda e, k=k, j=j, j4=j4, st=st: e.matmul(modps[:, j:j + 1], st[:, k, j4 * 128:(j4 + 1) * 128], cb[:, k, 0:1],
                                                                  start=(k == 0), stop=(k == 7)), reads=[st, cb], writes=[modps], acc=True)
    mod2 = c.sbuf([128, 16], F32, "mod2"); a2 = c.sbuf([128, 8], F32, "a2")
    c.op("dve", lambda e: e.tensor_tensor(mod2[:], modps[:, 0:16], adab2[:], ALU.add), reads=[modps, adab2], writes=[mod2])
    c.op("dve", lambda e: e.scalar_tensor_tensor(a2[:], mod2[:, 8:16], 1.0, n2g[:], ALU.add, ALU.mult), reads=[mod2, n2g], writes=[a2])
    lv = c.sbuf([128, 4, 32], F32, "lv"); lsm = c.sbuf([128, 4], F32, "lsm"); nlam = c.sbuf([128, 1], F32, "nlam")
    c.dma("sp", lv[:], lamv[:], writes=[lv])
    c.op("dve", lambda e: e.tensor_tensor(lv[:, 0, :], lv[:, 0, :], lv[:, 1, :], ALU.mult), reads=[lv], writes=[lv])
    c.op("dve", lambda e: e.tensor_tensor(lv[:, 2, :], lv[:, 2, :], lv[:, 3, :], ALU.mult), reads=[lv], writes=[lv])
    c.op("dve", lambda e: e.tensor_reduce(lsm[:, 0:1], lv[:, 0, :], AX.X, ALU.add), reads=[lv], writes=[lsm])
    c.op("dve", lambda e: e.tensor_reduce(lsm[:, 1:2], lv[:, 2, :], AX.X, ALU.add), reads=[lv], writes=[lsm])
    c.op("act", lambda e: e.activation(lsm[:, 2:4], lsm[:, 0:2], AF.Exp), reads=[lsm], writes=[lsm])
    c.op("dve", lambda e: e.tensor_tensor(nlam[:], lsm[:, 3:4], lsm[:, 2:3], ALU.subtract), reads=[lsm], writes=[nlam])
    lamc = c.sbuf([128, 2], F32, "lamc")
    c.dma("sp", lamc[:], lamc_d[:], writes=[lamc])
    c.op("dve", lambda e: e.tensor_scalar(nlam[:], nlam[:], lamc[:, 0:1], None, ALU.subtract), reads=[nlam, lamc], writes=[nlam])
    aog = c.sbuf([128, 64], F32, "aog"); cog = c.sbuf([128, 64], F32, "cog")
    c.dma("sp", aog[:], aog_d[:], writes=[aog]); c.dma("sp", cog[:], cog_d[:], writes=[cog])
    c.op("dve", lambda e: e.tensor_scalar(aog[:], aog[:], lamc[:, 1:2], None, ALU.mult), reads=[aog, lamc], writes=[aog])
    wob = c.sbuf([128, 8, D], BF16, "wob"); wobk = [Buf(None, "wobk%d" % k) for k in range(8)]
    wo_v = w_out.t.rearrange("(k p) n -> p k n", p=128)
    wos = [c.sbuf([128, D], F32, "wos%d" % i) for i in range(2)]
    for k in range(8):
        st = wos[k % 2]
        c.dma("sp", st[:], wo_v[:, k, :], writes=[st])
        c.op("pool", lambda e, k=k, st=st: e.tensor_copy(wob[:, k, :], st[:]), reads=[st], writes=[wobk[k]])
    rw = c.sbuf([128, 8, 32], F32, "rw"); rb = c.sbuf([128, 32], F32, "rb")
    c.dma("sp", rw[:], rwT[:], writes=[rw]); c.dma("sp", rb[:], rbB[:], writes=[rb])

    oas = [c.sbuf([128, 8, 65], F32, "oas%d" % i) for i in range(2)]; obs = [c.sbuf([128, 8, 65], F32, "obs%d" % i) for i in range(2)]
    ocs = [c.sbuf([128, 256], F32, "ocs%d" % i) for i in range(2)]; crs = [c.sbuf([128, 256], F32, "crs%d" % i) for i in range(2)]
    ys = [c.sbuf([128, D], F32, "ys%d" % i) for i in range(2)]; xs = [c.sbuf([128, D], F32, "xs%d" % i) for i in range(2)]
    rs8 = c.sbuf([128, 8], F32, "rs8"); on = c.sbuf([128, 8, 64], F32, "on"); od_ = c.sbuf([128, 256], F32, "od_"); sq = c.sbuf([128, 256], F32, "sq")
    st4 = c.sbuf([128, 4], F32, "st4"); osum = c.sbuf([128, 4, 65], F32, "osum")
    yT = [c.sbuf([128, 8, 128], BF16, "yT%d" % i) for i in range(2)]
    junk = c.sbuf([128, D], BF16, "junk"); stat = c.sbuf([128, 2], F32, "stat")
    h32 = [c.sbuf([128, 8, 128], F32, "h32_%d" % i) for i in range(2)]
    lg = c.sbuf([128, 32], F32, "lg"); top8 = c.sbuf([128, 8], F32, "top8"); msk = c.sbuf([128, 32], F32, "msk"); ex = c.sbuf([128, 32], F32, "ex")
    sm = c.sbuf([128, 2], F32, "sm")
    for t in range(NT):
        ts_ = slice(t * 128, (t + 1) * 128)
        oa_ = oas[t % 2]; ob_ = obs[t % 2]; oc_ = ocs[t % 2]; cr_ = crs[t % 2]; y = ys[t % 2]; xt = xs[t % 2]
        c.dma("sp", oa_[:], oaT[ts_], writes=[oa_]); c.dma("sp", ob_[:], obT[ts_], writes=[ob_])
        c.dma("sp", oc_[:], ocT[ts_], writes=[oc_]); c.dma("sp", cr_[:], crT[ts_], writes=[cr_])
        c.dma("sp", y[:, 768:1024], odT[ts_], writes=[y]); c.dma("sp", xt[:], x[ts_], writes=[xt])
        c.op("dve", lambda e: e.reciprocal(rs8[:], oa_[:, :, 64]), reads=[oa_], writes=[rs8])
        for m in range(8):
            c.op("dve", lambda e, m=m: e.tensor_scalar(on[:, m, :], oa_[:, m, 0:64], rs8[:, m:m + 1], None, ALU.mult), reads=[oa_, rs8], writes=[on])
        on4 = on[:].rearrange("p (h i) d -> p h i d", i=2)
        od3 = od_[:].rearrange("p (h d) -> p h d", d=64)
        c.op("dve", lambda e: e.scalar_tensor_tensor(od3, on4[:, :, 1, :], nlam[:, 0:1], on4[:, :, 0, :], ALU.mult, ALU.add), reads=[on, nlam], writes=[od_])
        c.op("dve", lambda e: e.tensor_tensor(sq[:], od_[:], od_[:], ALU.mult), reads=[od_], writes=[sq])
        c.op("dve", lambda e: e.tensor_reduce(st4[:], sq[:].rearrange("p (h d) -> p h d", d=64), AX.X, ALU.add), reads=[sq], writes=[st4])
        c.op("act", lambda e: e.activation(st4[:], st4[:], AF.Sqrt, bias=epsT[:], scale=1.0 / 64), reads=[st4, epsT], writes=[st4])
        c.op("dve", lambda e: e.reciprocal(st4[:], st4[:]), reads=[st4], writes=[st4])
        for h in range(4):
            c.op("dve", lambda e, h=h: e.scalar_tensor_tensor(y[:, h * 64:(h + 1) * 64], od_[:, h * 64:(h + 1) * 64], st4[:, h:h + 1], aog[:], ALU.mult, ALU.mult),
                 reads=[od_, st4, aog], writes=[y])
        ob4 = ob_[:].rearrange("p (h i) d -> p h i d", i=2)
        c.op("dve", lambda e: e.tensor_tensor(osum[:], ob4[:, :, 0, :], ob4[:, :, 1, :], ALU.add), reads=[ob_], writes=[osum])
        c.op("dve", lambda e: e.reciprocal(rs8[:, 0:4], osum[:, :, 64]), reads=[osum], writes=[rs8])
        for h in range(4):
            c.op("dve", lambda e, h=h: e.tensor_scalar(y[:, 256 + h * 64:256 + (h + 1) * 64], osum[:, h, 0:64], rs8[:, h:h + 1], None, ALU.mult), reads=[osum, rs8], writes=[y])
        c.op("dve", lambda e: e.tensor_tensor(sq[:], oc_[:], oc_[:], ALU.mult), reads=[oc_], writes=[sq])
        c.op("dve", lambda e: e.tensor_reduce(st4[:], sq[:].rearrange("p (h d) -> p h d", d=64), AX.X, ALU.add), reads=[sq], writes=[st4])
        c.op("act", lambda e: e.activation(st4[:], st4[:], AF.Sqrt, bias=epsT[:], scale=1.0 / 64), reads=[st4, epsT], writes=[st4])
        c.op("dve", lambda e: e.reciprocal(st4[:], st4[:]), reads=[st4], writes=[st4])
        c.op("act", lambda e: e.activation(cr_[:], cr_[:], AF.Silu), reads=[cr_], writes=[cr_])
        for h in range(4):
            c.op("dve", lambda e, h=h: e.scalar_tensor_tensor(y[:, 512 + h * 64:512 + (h + 1) * 64], oc_[:, h * 64:(h + 1) * 64], st4[:, h:h + 1], cog[:], ALU.mult, ALU.mult),
                 reads=[oc_, st4, cog], writes=[y])
        c.op("dve", lambda e: e.tensor_tensor(y[:, 512:768], y[:, 512:768], cr_[:], ALU.mult), reads=[y, cr_], writes=[y])
        yt = yT[t % 2]
        for half in range(2):
            ps = PS[2 + half]
            for kk in range(4):
                k = half * 4 + kk
                c.op("pe", lambda e, k=k, kk=kk, ps=ps: e.transpose(ps[:, kk * 128:(kk + 1) * 128], y[:, k * 128:(k + 1) * 128], ident[:]), reads=[y, ident], writes=[ps], acc=True)
            c.op("act", lambda e, ps=ps, half=half: e.activation(yt[:, half * 4:(half + 1) * 4, :].rearrange("p k t -> p (k t)"), ps[:], AF.Copy), reads=[ps], writes=[yt])
        for hf in range(2):
            ps = PS[4 + hf]
            for k in range(8):
                c.op("pe", lambda e, k=k, ps=ps, hf=hf: e.matmul(ps[:], yt[:, k, :], wob[:, k, hf * 512:(hf + 1) * 512], start=(k == 0), stop=(k == 7)),
                     reads=[yt, wobk[k]], writes=[ps], acc=True)
            c.op("dve", lambda e, ps=ps, hf=hf: e.tensor_tensor(y[:, hf * 512:(hf + 1) * 512], ps[:], g1b[:, hf * 512:(hf + 1) * 512], ALU.mult), reads=[ps, g1b], writes=[y])
        c.op("dve", lambda e: e.tensor_tensor(xt[:], xt[:], y[:], ALU.add), reads=[xt, y], writes=[xt])
        c.dma("pool", out[ts_], xt[:], reads=[xt], writes=[outt[t]])
        c.op("act", lambda e: e.activation(junk[:], xt[:], AF.Square, accum_out=stat[:, 0:1]), reads=[xt], writes=[junk, stat])
        c.op("act", lambda e: e.activation(stat[:, 1:2], stat[:, 0:1], AF.Sqrt, bias=epsT[:], scale=1.0 / D), reads=[stat, epsT], writes=[stat])
        c.op("dve", lambda e: e.reciprocal(stat[:, 1:2], stat[:, 1:2]), reads=[stat], writes=[stat])
        c.op("dve", lambda e: e.tensor_scalar(y[:], xt[:], stat[:, 1:2], None, ALU.mult), reads=[xt, stat], writes=[y])
        hh = h32[t % 2]
        for half in range(2):
            ps = PS[6 + half]
            for kk in range(4):
                k = half * 4 + kk
                c.op("pe", lambda e, k=k, kk=kk, ps=ps: e.transpose(ps[:, kk * 128:(kk + 1) * 128], y[:, k * 128:(k + 1) * 128], ident[:]), reads=[y, ident], writes=[ps], acc=True)
            for kk in range(4):
                k = half * 4 + kk
                c.op("dve", lambda e, k=k, kk=kk, ps=ps: e.tensor_scalar(hh[:, k, :], ps[:, kk * 128:(kk + 1) * 128], a2[:, k:k + 1], mod2[:, k:k + 1], ALU.mult, ALU.add),
                     reads=[ps, a2, mod2], writes=[hh])
        c.op("act", lambda e: e.activation(h2T[:, :, ts_], hh[:], AF.Copy), reads=[hh], writes=[h2Tt[t]])
        psr = PS[0]
        for k in range(8):
            c.op("pe", lambda e, k=k: e.matmul(psr[:, 0:32], hh[:, k, :], rw[:, k, :], start=(k == 0), stop=(k == 7)), reads=[hh, rw], writes=[psr], acc=True)
        c.op("dve", lambda e: e.tensor_tensor(lg[:], psr[:, 0:32], rb[:], ALU.add), reads=[psr, rb], writes=[lg])
        c.op("dve", lambda e: e.max(top8[:], lg[:]), reads=[lg], writes=[top8])
        c.op("dve", lambda e: e.tensor_scalar(msk[:], lg[:], top8[:, 3:4], None, ALU.is_ge), reads=[lg, top8], writes=[msk])
        c.op("dve", lambda e: e.tensor_scalar(sm[:, 0:1], top8[:, 0:1], -1.0, None, ALU.mult), reads=[top8], writes=[sm])
        c.op("act", lambda e: e.activation(ex[:], lg[:], AF.Exp, bias=sm[:, 0:1], scale=1.0), reads=[lg, sm], writes=[ex])
        c.op("dve", lambda e: e.tensor_tensor(ex[:], ex[:], msk[:], ALU.mult), reads=[ex, msk], writes=[ex])
        c.op("dve", lambda e: e.tensor_reduce(sm[:, 1:2], ex[:], AX.X, ALU.add), reads=[ex], writes=[sm])
        c.op("dve", lambda e: e.reciprocal(sm[:, 1:2], sm[:, 1:2]), reads=[sm], writes=[sm])
        c.op("dve", lambda e: e.tensor_scalar(Gall[:, t, :], ex[:], sm[:, 1:2], None, ALU.mult), reads=[ex, sm], writes=[Gt[t]])
        psg = PS[1]
        c.op("pe", lambda e: e.transpose(psg[0:32, 0:128], Gall[:, t, :], ident[:]), reads=[Gt[t], ident], writes=[psg])
        c.op("act", lambda e: e.activation(GT[:, ts_], psg[0:32, 0:128], AF.Copy), reads=[psg], writes=[GTt[t]])
    c.pop()

    c.push()
    TP = 1024; NTP = TP // 128
    bup = c.sbuf([128, NE, 16], F32, "bup")
    c.dma("sp", bup[:], bupT[:], writes=[bup])
    acc = c.sbuf([128, NTP, D], F32, "acc"); acct = [Buf(None, "acct%d" % t) for t in range(NTP)]
    actT = c.sbuf([128, 8, TP], BF16, "actT"); actb = [[Buf(None, "act_%d_%d" % (j, tg)) for tg in range(TP // 512)] for j in range(8)]
    wus = [c.sbuf([128, 8, 256], F32, "wus%d" % i) for i in range(2)]; wub = [c.sbuf([128, 8, 256], BF16, "wub%d" % i) for i in range(3)]
    wds = [c.sbuf([128, 8, 512], F32, "wds%d" % i) for i in range(2)]; wdb = [c.sbuf([128, 8, 512], BF16, "wdb%d" % i) for i in range(2)]
    gs = [c.sbuf([128, 512], F32, "gs%d" % i) for i in range(2)]; sg = [c.sbuf([128, 512], F32, "sg%d" % i) for i in range(2)]
    ls = [c.sbuf([128, 512], F32, "ls%d" % i) for i in range(2)]
    bds = c.sbuf([32, D], F32, "bds")
    c.dma("sp", bds[:], bdn[:], writes=[bds])
    fin = [c.sbuf([128, D], F32, "fin%d" % i) for i in range(2)]; x1s = [c.sbuf([128, D], F32, "x1s%d" % i) for i in range(2)]
    wup_v = wup.t.rearrange("e (k p) n -> e p k n", p=128)
    wdn_v = wdn.t.rearrange("e (j p) n -> e p j n", p=128)
    iu = 0; idn = 0; ie = 0
    for tp in range(T // TP):
        t0 = tp * NTP
        for e_ in range(n_exp):
            for j in range(8):
                st = wus[iu % 2]; wb_ = wub[iu % 3]; iu += 1
                c.dma("sp", st[:, :, 0:128], wup_v[e_, :, :, j * 128:(j + 1) * 128], writes=[st])
                c.dma("sp", st[:, :, 128:256], wup_v[e_, :, :, D + j * 128:D + (j + 1) * 128], writes=[st])
                c.op("pool", lambda e, st=st, wb_=wb_: e.tensor_copy(wb_[:], st[:]), reads=[st], writes=[wb_])
                for tg in range(TP // 512):
                    tsl = slice(tp * TP + tg * 512, tp * TP + (tg + 1) * 512)
                    pg = PS[(ie * 2) % 4]; pl = PS[(ie * 2 + 1) % 4]; g_ = gs[ie % 2]; s_ = sg[ie % 2]; l_ = ls[ie % 2]; ie += 1
                    hrd = h2Tt[tsl.start // 128:tsl.stop // 128]
                    for k in range(8):
                        c.op("pe", lambda e, k=k, pg=pg, wb_=wb_: e.matmul(pg[:], wb_[:, k, 0:128], h2T[:, k, tsl], start=(k == 0), stop=(k == 7)), reads=[wb_] + hrd, writes=[pg], acc=True)
                    for k in range(8):
                        c.op("pe", lambda e, k=k, pl=pl, wb_=wb_: e.matmul(pl[:], wb_[:, k, 128:256], h2T[:, k, tsl], start=(k == 0), stop=(k == 7)), reads=[wb_] + hrd, writes=[pl], acc=True)
                    c.op("dve", lambda e: e.tensor_scalar(g_[:], pg[:], bup[:, e_, j:j + 1], 7.0, ALU.add, ALU.min), reads=[pg, bup], writes=[g_])
                    c.op("act", lambda e: e.activation(s_[:], g_[:], AF.Sigmoid, scale=1.702), reads=[g_], writes=[s_])
                    c.op("act", lambda e: e.activation(l_[:], pl[:], AF.Identity, bias=bup[:, e_, 8 + j:9 + j], scale=1.0), reads=[pl, bup], writes=[l_])
                    c.op("pool", lambda e: e.tensor_scalar(l_[:], l_[:], 7.0, -7.0, ALU.min, ALU.max), reads=[l_], writes=[l_])
                    c.op("pool", lambda e: e.tensor_tensor(g_[:], g_[:], s_[:], ALU.mult), reads=[g_, s_], writes=[g_])
                    c.op("dve", lambda e: e.scalar_tensor_tensor(actT[:, j, tg * 512:(tg + 1) * 512], l_[:], 1.0, g_[:], ALU.add, ALU.mult), reads=[l_, g_], writes=[actb[j][tg]])
            for c2 in range(2):
                st = wds[idn % 2]; wd_ = wdb[idn % 2]; idn += 1
                c.dma("sp", st[:], wdn_v[e_, :, :, c2 * 512:(c2 + 1) * 512], writes=[st])
                c.op("pool", lambda e, st=st, wd_=wd_: e.tensor_copy(wd_[:], st[:]), reads=[st], writes=[wd_])
                for tt in range(NTP):
                    po = PS[4 + (tt % 4)]
                    for j in range(8):
                        c.op("pe", lambda e, j=j, po=po, wd_=wd_: e.matmul(po[:], actT[:, j, tt * 128:(tt + 1) * 128], wd_[:, j, :], start=(j == 0), stop=(j == 7)),
                             reads=[actb[j][tt // 4], wd_], writes=[po], acc=True)
                    dst = acc[:, tt, c2 * 512:(c2 + 1) * 512]
                    gsc = Gall[:, t0 + tt, e_:e_ + 1]
                    if e_ == 0:
                        c.op("dve", lambda e, po=po, dst=dst, gsc=gsc: e.tensor_scalar(dst, po[:], gsc, None, ALU.mult), reads=[po, Gt[t0 + tt]], writes=[acct[tt]])
                    else:
                        c.op("dve", lambda e, po=po, dst=dst, gsc=gsc: e.scalar_tensor_tensor(dst, po[:], gsc, dst, ALU.mult, ALU.add), reads=[po, Gt[t0 + tt], acct[tt]], writes=[acct[tt]])
        for tt in range(NTP):
            t = t0 + tt; ts_ = slice(t * 128, (t + 1) * 128)
            f = fin[tt % 2]; x1 = x1s[tt % 2]
            c.dma("sp", x1[:], out[ts_], reads=[outt[t]], writes=[x1])
            for hf in range(2):
                pb = PS[hf]
                c.op("pe", lambda e, pb=pb, hf=hf: e.matmul(pb[:], GT[:, ts_], bds[:, hf * 512:(hf + 1) * 512], start=True, stop=True), reads=[GTt[t], bds], writes=[pb])
                c.op("dve", lambda e, pb=pb, hf=hf: e.tensor_tensor(f[:, hf * 512:(hf + 1) * 512], pb[:], acc[:, tt, hf * 512:(hf + 1) * 512], ALU.add), reads=[pb, acct[tt]], writes=[f])
            c.op("dve", lambda e: e.tensor_tensor(f[:], f[:], g2b[:], ALU.mult), reads=[f, g2b], writes=[f])
            c.op("dve", lambda e: e.tensor_tensor(f[:], f[:], x1[:], ALU.add), reads=[f, x1], writes=[f])
            c.dma("pool", out[ts_], f[:], reads=[f, outt[t]], writes=[outt[t]])
    c.pop()
    c.finish(outt, "pool")
    c.close()
    return c

def pp(v):
    v = np.asarray(v, np.float32).reshape(-1, 128)
    return np.ascontiguousarray(v.T)
def consts():
    ident = np.eye(128, dtype=np.float32)
    i = np.arange(128)
    bd32 = (i[:, None] // 32 == i[None, :] // 32).astype(np.float32)
    bd64 = (i[:, None] // 64 == i[None, :] // 64).astype(np.float32)
    return ident, bd32, bd64
def prepA(inputs, l):
    ident, bd32, bd64 = consts()
    gains = np.zeros((128, 8), np.float32)
    gains[:, 0] = np.tile(inputs["a_q_gain"][l], 4); gains[:, 1] = np.tile(inputs["a_k_gain"][l], 4)
    gains[:, 2] = np.tile(inputs["b_q_gain"][l], 2); gains[:, 3] = np.tile(inputs["b_k_gain"][l], 2)
    gains[:, 4] = inputs["c_b_g"][l]
    common = dict(cT=pp(inputs["c"][0]), adaw=np.ascontiguousarray(inputs["ada_w"][l][:, :2048]), adabT=pp(inputs["ada_b"][l][:2048]),
                  n1gT=pp(inputs["norm1_g"][l]), w_in=np.ascontiguousarray(inputs["w_in"][l]), gains=gains,
                  wg2=np.ascontiguousarray(inputs["c_w_g2"][l]), ident=ident, bd32=bd32, bd64=bd64)
    return common

BF = ml_dtypes.bfloat16
S = 16384
def constsB():
    k = np.arange(128)[:, None]; q = np.arange(512)[None, :]
    cmask = np.stack([(128 * d + k <= q) for d in range(4)], axis=1).astype(BF)
    Z = np.zeros((32, 32, 128), BF)
    for m in range(32): Z[m, m, :] = 1
    iota = np.tile(np.arange(64, dtype=np.float32)[None, :], (128, 1))
    ident = np.eye(128, dtype=np.float32)
    rmask = np.ones((32, 2048), np.float32); rmask[:, ::64] = 0
    j = np.arange(64)[:, None]; i = np.arange(64)[None, :]
    tri8 = np.tile((j <= i).astype(np.float32), (1, 8))
    return dict(cmask=cmask, Z=Z, iota=iota, ident=ident, rmask=rmask, tri8=tri8)
def vlay(v, ntile):
    v1 = np.concatenate([v, np.ones((v.shape[0], 1), v.dtype)], axis=1)
    return np.ascontiguousarray(v1.reshape(ntile, 128, 65).transpose(1, 0, 2))
def prepB(A, inputs, l):
    cst = constsB()
    maps = []
    for r in range(8):
        m = r; h = r // 2; hb = r // 2; p = r % 2; hc = r % 4; nb = r % 4
        d = dict(cst)
        d["dq"] = np.ascontiguousarray(A["aqT"][32 * m:32 * m + 32]); d["dk"] = np.ascontiguousarray(A["akT"][32 * m:32 * m + 32])
        d["dv"] = vlay(A["av"][:, 64 * h:64 * h + 64], 128)
        d["mq"] = np.ascontiguousarray(A["bqT"][64 * hb:64 * hb + 64])
        kk = A["bkT"][64 * hb:64 * hb + 64].reshape(64, 64, 256)[:, p::2, :]
        d["mk"] = np.ascontiguousarray(kk.reshape(64, S // 2))
        vv = A["bv"][:, 64 * hb:64 * hb + 64].reshape(64, 256, 64)[p::2].reshape(S // 2, 64)
        d["mv"] = vlay(vv, 64)
        d["mq32"] = np.ascontiguousarray(A["bqT32"][64 * hb:64 * hb + 64]); d["mkm"] = np.ascontiguousarray(A["bkm"][64 * hb:64 * hb + 64])
        d["par"] = np.tile(np.array([[1.0 - p, float(p)]], np.float32), (128, 1))
        k = np.arange(128)[:, None]; q = np.arange(512)[None, :]
        d["bmask"] = np.concatenate([(256 * p + 128 * hh + k <= q) for hh in range(2)], axis=1).astype(BF)
        d["gq"] = np.ascontiguousarray(A["cqT"][32 * hc:32 * hc + 32]); d["gk"] = np.ascontiguousarray(A["ckT"][32 * hc:32 * hc + 32])
        d["gg"] = np.ascontiguousarray(A["gT"][32 * hc:32 * hc + 32])
        d["gv"] = np.ascontiguousarray(A["cv"][:, 64 * hc:64 * hc + 64].reshape(256, 64, 64).transpose(1, 0, 2))
        rx = np.zeros((64, S + 3), np.float32); rx[:, 3:] = A["dxT"][64 * nb:64 * nb + 64]
        d["rx"] = rx; d["rg"] = np.ascontiguousarray(A["dgT"][64 * nb:64 * nb + 64])
        rw = np.zeros((64, 8), np.float32)
        sl = slice(64 * nb, 64 * nb + 64)
        rw[:, 0:4] = inputs["d_conv_w"][l][:, sl].T; rw[:, 4] = inputs["d_conv_b"][l][sl]; rw[:, 5] = inputs["d_b_a"][l][sl]
        rw[:, 6] = inputs["d_b_x"][l][sl]; rw[:, 7] = inputs["d_lambda"][l][sl]
        d["rw"] = rw; d["rwa"] = np.ascontiguousarray(inputs["d_w_a"][l][nb]); d["rwx"] = np.ascontiguousarray(inputs["d_w_x"][l][nb])
        maps.append(d)
    return maps

def prepC(Bres, A, inputs, l, x_full):
    S_ = S
    oaT = np.stack([Bres[m]["oa"].T for m in range(8)], axis=1)
    obT = np.stack([Bres[r]["ob"].T for r in range(8)], axis=1)
    ocT = np.concatenate([Bres[h]["oc"].reshape(S_, 64) for h in range(4)], axis=1)
    odT = np.concatenate([Bres[nb]["od"] for nb in range(4)], axis=0).T
    crT = A["cr"]
    c_ = inputs["c"][0]
    cTb = np.ascontiguousarray(np.broadcast_to(pp(c_)[:, :, None], (128, 8, 128)))
    ab = inputs["ada_b"][l]
    adabB = np.ascontiguousarray(np.broadcast_to(np.stack([ab[2048:3072], ab[5120:6144]])[None], (128, 2, 1024)))
    lamv = np.ascontiguousarray(np.broadcast_to(np.stack([inputs["a_lam_q1"][l], inputs["a_lam_k1"][l], inputs["a_lam_q2"][l], inputs["a_lam_k2"][l]])[None], (128, 4, 32)))
    lam_init = 0.8 - 0.6 * math.exp(-0.3 * l)
    lamc = np.tile(np.array([[lam_init, 1.0 - lam_init]], np.float32), (128, 1))
    common = dict(lamc=lamc, cTb=cTb, adaw=np.ascontiguousarray(inputs["ada_w"][l]), adabB=adabB, adabT2=pp(ab[3072:5120]), lamv=lamv,
                  aog=np.ascontiguousarray(np.broadcast_to(inputs["a_out_gain"][l][None], (128, 64))),
                  cog=np.ascontiguousarray(np.broadcast_to(inputs["c_out_gain"][l][None], (128, 64))),
                  n2gT=pp(inputs["norm2_g"][l]), w_out=np.ascontiguousarray(inputs["w_out"][l]),
                  rwT=np.ascontiguousarray(inputs["router_w"][l].reshape(8, 128, 32).transpose(1, 0, 2)),
                  rbB=np.ascontiguousarray(np.broadcast_to(inputs["router_b"][l][None], (128, 32))), ident=np.eye(128, dtype=np.float32),
                  wup=np.ascontiguousarray(inputs["exp_w_up"][l]),
                  bupT=np.ascontiguousarray(inputs["exp_b_up"][l].reshape(32, 16, 128).transpose(2, 0, 1)),
                  wdn=np.ascontiguousarray(inputs["exp_w_down"][l]), bdn=np.ascontiguousarray(inputs["exp_b_down"][l]))
    maps = []
    for r in range(8):
        sl = slice(r * 2048, (r + 1) * 2048)
        maps.append(dict(common, x=np.ascontiguousarray(x_full[sl]), oaT=np.ascontiguousarray(oaT[sl]), obT=np.ascontiguousarray(obT[sl]),
                         ocT=np.ascontiguousarray(ocT[sl]), crT=np.ascontiguousarray(crT[sl]), odT=np.ascontiguousarray(odT[sl])))
    return maps


def _cat(R, k, axis):
    return np.concatenate([np.asarray(r[k]) for r in R], axis=axis)


def kernel(**inputs):
    inputs = {k: np.asarray(v) for k, v in inputs.items()}
    cores = list(range(8))
    x = np.ascontiguousarray(inputs["x"][0])
    for l in range(2):
        cA = build_A()
        common = prepA(inputs, l)
        mapsA = [dict(common, x=np.ascontiguousarray(x[i * 2048:(i + 1) * 2048])) for i in range(8)]
        RA = run_bass_kernel_spmd(cA.nc, mapsA, core_ids=cores).results
        A = {k: _cat(RA, k, 0 if k in ("av", "bv", "cv", "cr") else 1) for k in RA[0]}
        del RA, mapsA
        cB = build_B()
        mapsB = prepB(A, inputs, l)
        RB = run_bass_kernel_spmd(cB.nc, mapsB, core_ids=cores).results
        Bres = [{k: np.asarray(r[k]) for k in ("oa", "ob", "oc", "od")} for r in RB]
        del RB, mapsB
        cC = build_C()
        mapsC = prepC(Bres, A, inputs, l, x)
        RC = run_bass_kernel_spmd(cC.nc, mapsC, core_ids=cores).results
        x = _cat(RC, "out", 0)
        del RC, mapsC, A, Bres
    return np.ascontiguousarray(x[None]).astype(np.float32)
```

```python
import contextlib, math
import numpy as np
import ml_dtypes
import concourse.bass as bass
import concourse.mybir as mybir
from concourse.bass_utils import run_bass_kernel_spmd


F32 = mybir.dt.float32
BF16 = mybir.dt.bfloat16
I32 = mybir.dt.int32
AF = mybir.ActivationFunctionType
ALU = mybir.AluOpType
AX = mybir.AxisListType


class Buf:
    def __init__(self, t, name):
        self.t = t
        self.name = name
        self.w = None
        self.r = {}

    def __getitem__(self, idx):
        return self.t[idx]


class Ctx:
    NDS = 8

    def __init__(self):
        self.nc = bass.Bass("TRN2", target_bir_lowering=False)
        nc = self.nc
        self.es = contextlib.ExitStack()
        self.E = {"pe": nc.tensor, "act": nc.scalar, "dve": nc.vector, "pool": nc.gpsimd, "sp": nc.sync}
        self.sems = {}
        self.cnt = {}
        for e in ("pe", "act", "dve", "pool"):
            self.sems[e] = self.es.enter_context(nc.semaphore("s_" + e))
            self.cnt[e] = 0
        self.dq = {}
        for q in ("sp", "pool", "act"):
            ss = []
            for i in range(self.NDS):
                k = "d_%s%d" % (q, i)
                self.sems[k] = self.es.enter_context(nc.semaphore(k))
                ss.append(k)
            self.dq[q] = [ss, 0]
        self.seen = {e: {} for e in self.E}
        self.nbuf = 0
        self.ninstr = 0

    def sbuf(self, shape, dt, name=None):
        self.nbuf += 1
        name = "sb_" + (name or "%d" % self.nbuf)
        t = self.es.enter_context(self.nc.sbuf_tensor(name, list(shape), dt))
        return Buf(t, name)

    def psum(self, shape, dt, name=None):
        self.nbuf += 1
        name = name or "ps%d" % self.nbuf
        t = self.es.enter_context(self.nc.psum_tensor(name, list(shape), dt))
        return Buf(t, name)

    def dram(self, name, shape, dt, kind):
        t = self.nc.dram_tensor(name, list(shape), dt, kind=kind).ap()
        return Buf(t, name)

    def _wait(self, eng, tok):
        if tok is None:
            return
        k, v = tok
        if self.seen[eng].get(k, 0) >= v:
            return
        self.E[eng].wait_ge(self.sems[k], v)
        self.seen[eng][k] = v
        self.ninstr += 1

    def _deps(self, eng, reads, writes, acc=False):
        for b in reads:
            self._wait(eng, b.w)
        for b in writes:
            if not (acc and b.w is not None and b.w[0] == eng == "pe"):
                self._wait(eng, b.w)
            for k, v in b.r.items():
                self._wait(eng, (k, v))

    def _mark(self, tok, reads, writes):
        k, v = tok
        for b in reads:
            if b.r.get(k, 0) < v:
                b.r[k] = v
        for b in writes:
            b.w = tok
            b.r = {}

    def op(self, eng, fn, reads=(), writes=(), acc=False):
        self._deps(eng, reads, writes, acc)
        ins = fn(self.E[eng])
        self.cnt[eng] += 1
        ins.then_inc(self.sems[eng], 1)
        self.ninstr += 1
        self._mark((eng, self.cnt[eng]), reads, writes)
        return ins

    def dma(self, q, out, in_, reads=(), writes=(), **kw):
        ss, j = self.dq[q]
        k = ss[j % self.NDS]
        rnd = j // self.NDS
        if rnd > 0:
            self._wait(q, (k, 16 * rnd))
        self._deps(q, reads, writes)
        ins = self.E[q].dma_start(out=out, in_=in_, **kw)
        ins.then_inc(self.sems[k], 16)
        self.dq[q][1] = j + 1
        self.ninstr += 1
        self._mark((k, 16 * (rnd + 1)), reads, writes)
        return ins

    def finish(self, bufs, eng="sp"):
        for b in bufs:
            self._wait(eng, b.w)

    def close(self):
        self.es.close()


def _dq_tokens(self):
    toks = [(e, self.cnt[e]) for e in ("pe", "act", "dve", "pool")]
    for q, (ss, j) in self.dq.items():
        for i, k in enumerate(ss):
            n = (j - i + self.NDS - 1) // self.NDS if j > i else 0
            if n > 0:
                toks.append((k, 16 * n))
    return toks


def _barrier(self):
    toks = _dq_tokens(self)
    for e in ("pe", "act", "dve", "pool", "sp"):
        for tk in toks:
            self._wait(e, tk)


def _push(self):
    self._outer = getattr(self, "_outer", [])
    self._outer.append(self.es)
    self.es = contextlib.ExitStack()


def _pop(self):
    _barrier(self)
    self.es.close()
    self.es = self._outer.pop()


Ctx.barrier = _barrier
Ctx.push = _push
Ctx.pop = _pop


T = 2048
NT = T // 128
D = 1024
DIN = 2832
EPS = 1e-6

def build_A():
    c = Ctx(); nc = c.nc
    inp = lambda n, s, d=F32: c.dram(n, s, d, "ExternalInput")
    outp = lambda n, s, d=F32: c.dram(n, s, d, "ExternalOutput")
    x = inp("x", [T, D]); cT = inp("cT", [128, 8]); adaw = inp("adaw", [D, 2048])
    adabT = inp("adabT", [128, 16]); n1gT = inp("n1gT", [128, 8]); w_in = inp("w_in", [D, DIN])
    gains = inp("gains", [128, 8])
    wg2 = inp("wg2", [16, 128]); ident_d = inp("ident", [128, 128]); bd32_d = inp("bd32", [128, 128]); bd64_d = inp("bd64", [128, 128])
    aqT = outp("aqT", [256, T], BF16); akT = outp("akT", [256, T], BF16)
    bqT = outp("bqT", [256, T], BF16); bkT = outp("bkT", [256, T], BF16)
    bqT32 = outp("bqT32", [256, T]); bkm = outp("bkm", [256, T // 256])
    cqT = outp("cqT", [128, T]); ckT = outp("ckT", [128, T]); gT = outp("gT", [128, T])
    dxT = outp("dxT", [256, T]); dgT = outp("dgT", [256, T])
    av = outp("av", [T, 256], BF16); bv = outp("bv", [T, 256], BF16)
    cv = outp("cv", [T, 256]); cr = outp("cr", [T, 256])

    ident = c.sbuf([128, 128], F32); bd32 = c.sbuf([128, 128], F32); bd64 = c.sbuf([128, 128], F32)
    gn = c.sbuf([128, 8], F32); cond = c.sbuf([128, 8], F32); adab = c.sbuf([128, 16], F32); n1g = c.sbuf([128, 8], F32)
    wg2s = c.sbuf([16, 128], F32)
    epsT = c.sbuf([128, 1], F32); oneT = c.sbuf([128, 1], F32)
    for sb, dr in ((ident, ident_d), (bd32, bd32_d), (bd64, bd64_d), (gn, gains), (cond, cT), (adab, adabT), (n1g, n1gT), (wg2s, wg2)):
        c.dma("sp", sb[:], dr[:], writes=[sb])
    c.op("dve", lambda e: e.memset(epsT[:], EPS), writes=[epsT])
    c.op("dve", lambda e: e.memset(oneT[:], 1.0), writes=[oneT])
    c.op("act", lambda e: e.activation(cond[:], cond[:], AF.Silu), reads=[cond], writes=[cond])

    PS = [c.psum([128, 512], F32, "psb%d" % i) for i in range(8)]

    adaw_v = adaw.t.rearrange("(k p) n -> p k n", p=128)
    wst = [c.sbuf([128, 8, 512], F32, "adst%d" % i) for i in range(2)]
    modps = PS[0]
    for jj in range(4):
        st = wst[jj % 2]
        c.dma("sp", st[:], adaw_v[:, :, jj * 512:(jj + 1) * 512], writes=[st])
        for j4 in range(4):
            j = jj * 4 + j4
            for k in range(8):
                c.op("pe", lambda e, k=k, j=j, j4=j4, st=st: e.matmul(modps[:, j:j + 1], st[:, k, j4 * 128:(j4 + 1) * 128], cond[:, k:k + 1],
                                                        start=(k == 0), stop=(k == 7)), reads=[st, cond], writes=[modps], acc=True)
    mod = c.sbuf([128, 16], F32)
    c.op("dve", lambda e: e.tensor_tensor(mod[:], modps[:, 0:16], adab[:], ALU.add), reads=[modps, adab], writes=[mod])
    a1 = c.sbuf([128, 8], F32)
    c.op("dve", lambda e: e.scalar_tensor_tensor(a1[:], mod[:, 8:16], 1.0, n1g[:], ALU.add, ALU.mult), reads=[mod, n1g], writes=[a1])

    wb = c.sbuf([128, 8, DIN], BF16, "wb")
    wbk = [Buf(None, "wbk%d" % k) for k in range(8)]
    win_v = w_in.t.rearrange("(k p) n -> p k n", p=128)
    wstage = [c.sbuf([128, DIN], F32, "wstage%d" % i) for i in range(2)]
    for k in range(8):
        st = wstage[k % 2]
        c.dma("sp", st[:], win_v[:, k, :], writes=[st])
        eng = "dve" if k % 2 == 0 else "pool"
        c.op(eng, lambda e, k=k, st=st: e.tensor_copy(wb[:, k, :], st[:]), reads=[st], writes=[wbk[k]])

    hT = c.sbuf([128, 8, T], BF16, "hT")
    hTt = [Buf(None, "hTt%d" % t) for t in range(NT)]
    xts = [c.sbuf([128, D], F32, "xt%d" % i) for i in range(2)]
    junk = c.sbuf([128, D], BF16, "junk")
    stat = [c.sbuf([128, 2], F32, "stat%d" % i) for i in range(2)]
    for t in range(NT):
        xt = xts[t % 2]; stt = stat[t % 2]
        c.dma("sp", xt[:], x[t * 128:(t + 1) * 128, :], writes=[xt])
        c.op("act", lambda e: e.activation(junk[:], xt[:], AF.Square, accum_out=stt[:, 0:1]), reads=[xt], writes=[junk, stt])
        c.op("act", lambda e: e.activation(stt[:, 1:2], stt[:, 0:1], AF.Sqrt, bias=epsT[:], scale=1.0 / D), reads=[stt, epsT], writes=[stt])
        c.op("dve", lambda e: e.reciprocal(stt[:, 1:2], stt[:, 1:2]), reads=[stt], writes=[stt])
        c.op("dve", lambda e: e.tensor_scalar(xt[:], xt[:], stt[:, 1:2], None, ALU.mult), reads=[xt, stt], writes=[xt])
        for half in range(2):
            ps = PS[1 + half]
            for kk in range(4):
                k = half * 4 + kk
                c.op("pe", lambda e, k=k, kk=kk, ps=ps: e.transpose(ps[:, kk * 128:(kk + 1) * 128], xt[:, k * 128:(k + 1) * 128], ident[:]),
                     reads=[xt, ident], writes=[ps], acc=True)
            for kk in range(4):
                k = half * 4 + kk
                if kk % 2 == 0:
                    c.op("dve", lambda e, k=k, kk=kk, ps=ps: e.tensor_scalar(hT[:, k, t * 128:(t + 1) * 128], ps[:, kk * 128:(kk + 1) * 128],
                                                                 a1[:, k:k + 1], mod[:, k:k + 1], ALU.mult, ALU.add),
                         reads=[ps, a1, mod], writes=[hTt[t]])
                else:
                    c.op("act", lambda e, k=k, kk=kk, ps=ps: e.activation(hT[:, k, t * 128:(t + 1) * 128], ps[:, kk * 128:(kk + 1) * 128],
                                                              AF.Identity, bias=mod[:, k:k + 1], scale=a1[:, k:k + 1]),
                         reads=[ps, a1, mod], writes=[hTt[t]])

    pi = [0]
    def nextps():
        pi[0] += 1
        return PS[3 + pi[0] % 5]
    wball = wbk
    def proj_fm(col0, ncols, g):
        ps = nextps()
        for k in range(8):
            c.op("pe", lambda e, k=k: e.matmul(ps[0:ncols, :], wb[:, k, col0:col0 + ncols], hT[:, k, g * 512:(g + 1) * 512],
                                               start=(k == 0), stop=(k == 7)),
                 reads=[wball[k]] + hTt[g * 4:(g + 1) * 4], writes=[ps], acc=True)
        return ps

    sqb = [c.sbuf([128, 512], F32, "sq%d" % i) for i in range(2)]
    rsb = [c.sbuf([128, 512], F32, "rs%d" % i) for i in range(2)]
    ob16 = [c.sbuf([128, 512], BF16, "ob16_%d" % i) for i in range(3)]
    ob32 = [c.sbuf([128, 512], F32, "ob32_%d" % i) for i in range(3)]
    kms = c.sbuf([128, 2, NT // 2], F32, "kms")
    kmsb = [Buf(None, "kmsb%d" % i) for i in range(2)]
    ctr = [0]
    def normed(ps, bd, inv_d, gcol, dst16, dst32=None, kmchunk=None, g=None, rows=None):
        i = ctr[0]; ctr[0] += 1
        sq = sqb[i % 2]; rs = rsb[i % 2]; o16 = ob16[i % 3]
        c.op("act", lambda e: e.activation(sq[:], ps[:], AF.Square), reads=[ps], writes=[sq])
        ps2 = nextps()
        c.op("pe", lambda e: e.matmul(ps2[:], bd[:], sq[:], start=True, stop=True), reads=[bd, sq], writes=[ps2])
        c.op("act", lambda e: e.activation(rs[:], ps2[:], AF.Sqrt, bias=epsT[:], scale=inv_d), reads=[ps2, epsT], writes=[rs])
        c.op("dve", lambda e: e.reciprocal(rs[:], rs[:]), reads=[rs], writes=[rs])
        c.op("dve", lambda e: e.scalar_tensor_tensor(o16[:], ps[:], gn[:, gcol:gcol + 1], rs[:], ALU.mult, ALU.mult), reads=[ps, gn, rs], writes=[o16])
        c.dma("pool", dst16[rows, g * 512:(g + 1) * 512], o16[:], reads=[o16], writes=[dst16])
        if dst32 is not None or kmchunk is not None:
            o32 = ob32[i % 3]
            c.op("dve", lambda e: e.scalar_tensor_tensor(o32[:], ps[:], gn[:, gcol:gcol + 1], rs[:], ALU.mult, ALU.mult), reads=[ps, gn, rs], writes=[o32])
            if dst32 is not None:
                c.dma("pool", dst32[rows, g * 512:(g + 1) * 512], o32[:], reads=[o32], writes=[dst32])
            if kmchunk is not None:
                c.op("dve", lambda e: e.tensor_reduce(kms[:, kmchunk, g * 2:(g + 1) * 2], o32[:].rearrange("p (b t) -> p b t", t=256), AX.X, ALU.add),
                     reads=[o32], writes=[kmsb[kmchunk]])

    def raw32(ps, nrows, dst, rows, g):
        i = ctr[0]; ctr[0] += 1
        o32 = ob32[i % 3]
        c.op("act" if i % 2 else "dve", (lambda e: e.activation(o32[0:nrows, :], ps[0:nrows, :], AF.Copy)) if i % 2 else
             (lambda e: e.tensor_copy(o32[0:nrows, :], ps[0:nrows, :])), reads=[ps], writes=[o32])
        c.dma("pool", dst[rows, g * 512:(g + 1) * 512], o32[0:nrows, :], reads=[o32], writes=[dst])
        return o32

    lt = [c.sbuf([128, 512], F32, "lt%d" % i) for i in range(3)]
    tm16 = [c.sbuf([128, 512], BF16, "tm16_%d" % i) for i in range(2)]
    tm32 = [c.sbuf([128, 512], F32, "tm32_%d" % i) for i in range(2)]
    cgs = c.sbuf([16, 512], F32, "cgs")
    for g in range(T // 512):
        for ch in range(2):
            rows = slice(ch * 128, (ch + 1) * 128)
            normed(proj_fm(0 + ch * 128, 128, g), bd32, 1.0 / 32, 0, aqT, g=g, rows=rows)
            normed(proj_fm(256 + ch * 128, 128, g), bd32, 1.0 / 32, 1, akT, g=g, rows=rows)
            normed(proj_fm(768 + ch * 128, 128, g), bd64, 1.0 / 64, 2, bqT, dst32=bqT32, g=g, rows=rows)
            normed(proj_fm(1024 + ch * 128, 128, g), bd64, 1.0 / 64, 3, bkT, kmchunk=ch, g=g, rows=rows)
            raw32(proj_fm(2320 + ch * 128, 128, g), 128, dxT, rows, g)
            raw32(proj_fm(2576 + ch * 128, 128, g), 128, dgT, rows, g)
        raw32(proj_fm(1536, 128, g), 128, cqT, slice(0, 128), g)
        raw32(proj_fm(1664, 128, g), 128, ckT, slice(0, 128), g)
        psg = proj_fm(2048, 16, g)
        c.op("dve", lambda e: e.tensor_copy(cgs[:], psg[0:16, :]), reads=[psg], writes=[cgs])
        psz = nextps()
        c.op("pe", lambda e: e.matmul(psz[:], wg2s[:], cgs[:], start=True, stop=True), reads=[wg2s, cgs], writes=[psz])
        z, az, m = lt
        c.op("dve", lambda e: e.tensor_scalar(z[:], psz[:], gn[:, 4:5], None, ALU.add), reads=[psz, gn], writes=[z])
        c.op("act", lambda e: e.activation(az[:], z[:], AF.Abs), reads=[z], writes=[az])
        c.op("act", lambda e: e.activation(az[:], az[:], AF.Exp, scale=-1.0), reads=[az], writes=[az])
        c.op("act", lambda e: e.activation(az[:], az[:], AF.Ln, bias=oneT[:], scale=1.0), reads=[az, oneT], writes=[az])
        c.op("dve", lambda e: e.tensor_scalar(m[:], z[:], 0.0, None, ALU.min), reads=[z], writes=[m])
        c.op("dve", lambda e: e.tensor_tensor(m[:], m[:], az[:], ALU.subtract), reads=[m, az], writes=[m])
        c.op("dve", lambda e: e.tensor_scalar(m[:], m[:], 1.0 / 16, None, ALU.mult), reads=[m], writes=[m])
        c.dma("pool", gT[:, g * 512:(g + 1) * 512], m[:], reads=[m], writes=[gT])
        for tt in range(4):
            t = g * 4 + tt
            ps = nextps()
            for (o, col0) in ((0, 512), (256, 1280)):
                for k in range(8):
                    c.op("pe", lambda e, k=k, o=o, col0=col0: e.matmul(ps[:, o:o + 256], hT[:, k, t * 128:(t + 1) * 128], wb[:, k, col0:col0 + 256],
                                                                    start=(k == 0), stop=(k == 7)), reads=[wball[k], hTt[t]], writes=[ps], acc=True)
            o16 = tm16[t % 2]
            c.op("act", lambda e: e.activation(o16[:], ps[:], AF.Copy), reads=[ps], writes=[o16])
            c.dma("pool", av[t * 128:(t + 1) * 128, :], o16[:, 0:256], reads=[o16], writes=[av])
            c.dma("pool", bv[t * 128:(t + 1) * 128, :], o16[:, 256:512], reads=[o16], writes=[bv])
            ps = nextps()
            for (o, col0) in ((0, 1792), (256, 2064)):
                for k in range(8):
                    c.op("pe", lambda e, k=k, o=o, col0=col0: e.matmul(ps[:, o:o + 256], hT[:, k, t * 128:(t + 1) * 128], wb[:, k, col0:col0 + 256],
                                                                    start=(k == 0), stop=(k == 7)), reads=[wball[k], hTt[t]], writes=[ps], acc=True)
            o32 = tm32[t % 2]
            c.op("dve", lambda e: e.tensor_copy(o32[:], ps[:]), reads=[ps], writes=[o32])
            c.dma("pool", cv[t * 128:(t + 1) * 128, :], o32[:, 0:256], reads=[o32], writes=[cv])
            c.dma("pool", cr[t * 128:(t + 1) * 128, :], o32[:, 256:512], reads=[o32], writes=[cr])
    for ch in range(2):
        c.dma("pool", bkm[ch * 128:(ch + 1) * 128, :], kms[:, ch, :], reads=[kmsb[ch]], writes=[bkm])
    outs = [aqT, akT, bqT, bkT, bqT32, bkm, cqT, ckT, gT, dxT, dgT, av, bv, cv, cr]
    c.finish(outs, "pool")
    c.close()
    return c


S = 16384
BIG = 1.0e9
BIGB = 30000.0

def build_B(do_rg=True, do_gla=True, do_diff=True, do_moba=True, NG=32):
    c = Ctx(); nc = c.nc
    inp = lambda n, s, d=F32: c.dram(n, s, d, "ExternalInput")
    outp = lambda n, s, d=F32: c.dram(n, s, d, "ExternalOutput")
    dq = inp("dq", [32, S], BF16); dk = inp("dk", [32, S], BF16); dv = inp("dv", [128, 128, 65], BF16)
    mq = inp("mq", [64, S], BF16); mk = inp("mk", [64, S // 2], BF16); mv = inp("mv", [128, 64, 65], BF16)
    mq32 = inp("mq32", [64, S]); mkm = inp("mkm", [64, 64]); par_d = inp("par", [128, 2]); bmask_d = inp("bmask", [128, 1024], BF16)
    cmask_d = inp("cmask", [128, 4, 512], BF16); Z_d = inp("Z", [32, 32, 128], BF16); iota_d = inp("iota", [128, 64]); ident_d = inp("ident", [128, 128])
    gq = inp("gq", [32, S]); gk = inp("gk", [32, S]); gg = inp("gg", [32, S]); gv = inp("gv", [64, 256, 64])
    rmask_d = inp("rmask", [32, 2048]); tri8_d = inp("tri8", [64, 512])
    rx = inp("rx", [64, S + 3]); rg = inp("rg", [64, S]); rw_d = inp("rw", [64, 8]); rwa_d = inp("rwa", [64, 64]); rwx_d = inp("rwx", [64, 64])
    oa = outp("oa", [65, S]); ob = outp("ob", [65, S]); oc = outp("oc", [256, 64, 64]); od = outp("od", [64, S])

    PS1 = [c.psum([128, 512], F32, "ps1_%d" % i) for i in range(4)]
    PS2 = [c.psum([128, 1024], F32, "ps2_%d" % i) for i in range(2)]
    ident = c.sbuf([128, 128], F32, "ident_sb")
    c.dma("sp", ident[:], ident_d[:], writes=[ident])
    oneT = c.sbuf([128, 1], F32, "oneT")
    c.op("dve", lambda e: e.memset(oneT[:], 1.0), writes=[oneT])

    if do_rg:
        c.push()
        PW = 2048
        rw = c.sbuf([64, 8], F32, "rw"); rwa = c.sbuf([64, 64], F32, "rwa"); rwx = c.sbuf([64, 64], F32, "rwx")
        for sb, dr in ((rw, rw_d), (rwa, rwa_d), (rwx, rwx_d)):
            c.dma("sp", sb[:], dr[:], writes=[sb])
        cl = c.sbuf([64, 2], F32, "cl")
        c.op("act", lambda e: e.activation(cl[:, 0:1], rw[:, 7:8], AF.Exp, scale=-1.0), reads=[rw], writes=[cl])
        c.op("act", lambda e: e.activation(cl[:, 0:1], cl[:, 0:1], AF.Ln, bias=oneT[0:64, :], scale=1.0), reads=[cl, oneT], writes=[cl])
        c.op("dve", lambda e: e.tensor_scalar(cl[:, 1:2], cl[:, 0:1], -8.0, None, ALU.mult), reads=[cl], writes=[cl])
        xin = [c.sbuf([64, PW + 3], F32, "xin%d" % i) for i in range(2)]
        gin = [c.sbuf([64, PW], F32, "gin%d" % i) for i in range(2)]
        xc = c.sbuf([64, PW], F32, "xc"); rgt = c.sbuf([64, PW], F32, "rgt"); igt = c.sbuf([64, PW], F32, "igt")
        aa = c.sbuf([64, PW], F32, "aa"); bt = c.sbuf([64, PW], F32, "bt")
        hh = [c.sbuf([64, PW], F32, "hh%d" % i) for i in range(2)]
        uu = c.sbuf([64, PW], F32, "uu")
        for pc in range(S // PW):
            lo = pc * PW
            xi = xin[pc % 2]; gi = gin[pc % 2]; h = hh[pc % 2]; hp = hh[(pc + 1) % 2]
            c.dma("sp", xi[:], rx[:, lo:lo + PW + 3], writes=[xi])
            c.dma("sp", gi[:], rg[:, lo:lo + PW], writes=[gi])
            c.op("dve", lambda e: e.tensor_scalar(xc[:], xi[:, 3:PW + 3], rw[:, 3:4], rw[:, 4:5], ALU.mult, ALU.add), reads=[xi, rw], writes=[xc])
            for j in range(3):
                c.op("dve", lambda e, j=j: e.scalar_tensor_tensor(xc[:], xi[:, j:PW + j], rw[:, j:j + 1], xc[:], ALU.mult, ALU.add), reads=[xi, rw, xc], writes=[xc])
            for grp in range(PW // 512):
                sl = slice(grp * 512, (grp + 1) * 512)
                p1 = PS1[0]; p2 = PS1[1]
                c.op("pe", lambda e: e.matmul(p1[0:64, :], rwa[:], xc[:, sl], start=True, stop=True), reads=[rwa, xc], writes=[p1])
                c.op("pe", lambda e: e.matmul(p2[0:64, :], rwx[:], xc[:, sl], start=True, stop=True), reads=[rwx, xc], writes=[p2])
                c.op("act", lambda e: e.activation(rgt[:, sl], p1[0:64, :], AF.Sigmoid, bias=rw[:, 5:6], scale=1.0), reads=[p1, rw], writes=[rgt])
                c.op("act", lambda e: e.activation(igt[:, sl], p2[0:64, :], AF.Sigmoid, bias=rw[:, 6:7], scale=1.0), reads=[p2, rw], writes=[igt])
            c.op("act", lambda e: e.activation(aa[:], rgt[:], AF.Exp, scale=cl[:, 1:2]), reads=[rgt, cl], writes=[aa])
            c.op("act", lambda e: e.activation(bt[:], aa[:], AF.Square), reads=[aa], writes=[bt])
            c.op("dve", lambda e: e.tensor_scalar(bt[:], bt[:], -1.0, 1.0, ALU.mult, ALU.add), reads=[bt], writes=[bt])
            c.op("act", lambda e: e.activation(bt[:], bt[:], AF.Sqrt), reads=[bt], writes=[bt])
            c.op("dve", lambda e: e.tensor_tensor(bt[:], bt[:], igt[:], ALU.mult), reads=[bt, igt], writes=[bt])
            c.op("dve", lambda e: e.tensor_tensor(bt[:], bt[:], xc[:], ALU.mult), reads=[bt, xc], writes=[bt])
            if pc == 0:
                c.op("dve", lambda e: e.tensor_tensor_scan(h[:], aa[:], bt[:], 0.0, ALU.mult, ALU.add), reads=[aa, bt], writes=[h])
            else:
                c.op("dve", lambda e: e.tensor_tensor_scan(h[:], aa[:], bt[:], hp[:, PW - 1:PW], ALU.mult, ALU.add), reads=[aa, bt, hp], writes=[h])
            c.op("dve", lambda e: e.tensor_tensor(uu[:], gi[:], gi[:], ALU.mult), reads=[gi], writes=[uu])
            c.op("dve", lambda e: e.tensor_scalar(uu[:], uu[:], 0.044715, 1.0, ALU.mult, ALU.add), reads=[uu], writes=[uu])
            c.op("dve", lambda e: e.tensor_tensor(uu[:], uu[:], gi[:], ALU.mult), reads=[uu, gi], writes=[uu])
            c.op("act", lambda e: e.activation(uu[:], uu[:], AF.Sigmoid, scale=1.5957691216057308), reads=[uu], writes=[uu])
            c.op("dve", lambda e: e.tensor_tensor(uu[:], uu[:], gi[:], ALU.mult), reads=[uu, gi], writes=[uu])
            c.op("dve", lambda e: e.tensor_tensor(uu[:], uu[:], h[:], ALU.mult), reads=[uu, h], writes=[uu])
            c.dma("pool", od[:, lo:lo + PW], uu[:], reads=[uu], writes=[od])
        c.pop()

    if do_gla:
        c.push()
        PW = 2048; NCH = PW // 64
        rmask = c.sbuf([32, PW], F32, "rmask"); tri8 = c.sbuf([64, 512], F32, "tri8")
        c.dma("sp", rmask[:], rmask_d[:], writes=[rmask]); c.dma("sp", tri8[:], tri8_d[:], writes=[tri8])
        qs = [c.sbuf([32, PW], F32, "gq%d" % i) for i in range(2)]
        ks = [c.sbuf([32, PW], F32, "gk%d" % i) for i in range(2)]
        gs = [c.sbuf([32, PW], F32, "gg%d" % i) for i in range(2)]
        vs = [c.sbuf([64, NCH, 64], F32, "gv%d" % i) for i in range(2)]
        bcum = c.sbuf([32, PW], F32, "bcum"); eb = c.sbuf([32, PW], F32, "eb"); qe = c.sbuf([32, PW], F32, "qe")
        ke = c.sbuf([32, PW], F32, "ke"); kl = c.sbuf([32, PW], F32, "kl"); dec = c.sbuf([32, NCH], F32, "dec")
        attT = c.sbuf([64, NCH, 64], F32, "attT"); klT = c.sbuf([64, 256], F32, "klT")
        U = c.sbuf([32, NCH, 64], F32, "U"); Sall = c.sbuf([32, NCH + 1, 64], F32, "Sall")
        osb = [c.sbuf([64, 8, 64], F32, "gosb%d" % i) for i in range(2)]
        c.op("dve", lambda e: e.memset(Sall[:, 0, :], 0.0), writes=[Sall])
        for pc in range(S // PW):
            lo = pc * PW
            q = qs[pc % 2]; k = ks[pc % 2]; g = gs[pc % 2]; v = vs[pc % 2]
            c.dma("sp", q[:], gq[:, lo:lo + PW], writes=[q]); c.dma("sp", k[:], gk[:, lo:lo + PW], writes=[k])
            c.dma("sp", g[:], gg[:, lo:lo + PW], writes=[g]); c.dma("sp", v[:], gv[:, pc * NCH:(pc + 1) * NCH, :], writes=[v])
            c.op("dve", lambda e: e.tensor_tensor_scan(bcum[:], rmask[:], g[:], 0.0, ALU.mult, ALU.add), reads=[rmask, g], writes=[bcum])
            c.op("act", lambda e: e.activation(eb[:], bcum[:], AF.Exp), reads=[bcum], writes=[eb])
            c.op("dve", lambda e: e.scalar_tensor_tensor(qe[:], q[:], 32.0 ** -0.5, eb[:], ALU.mult, ALU.mult), reads=[q, eb], writes=[qe])
            c.op("act", lambda e: e.activation(eb[:], bcum[:], AF.Exp, scale=-1.0), reads=[bcum], writes=[eb])
            c.op("dve", lambda e: e.tensor_tensor(ke[:], k[:], eb[:], ALU.mult), reads=[k, eb], writes=[ke])
            bc3 = bcum[:].rearrange("p (c t) -> p c t", t=64)
            c.op("act", lambda e: e.activation(dec[:], bc3[:, :, 63], AF.Exp), reads=[bcum], writes=[dec])
            for cc in range(NCH):
                c.op("dve", lambda e, cc=cc: e.tensor_scalar(kl[:, cc * 64:(cc + 1) * 64], ke[:, cc * 64:(cc + 1) * 64], dec[:, cc:cc + 1], None, ALU.mult),
                     reads=[ke, dec], writes=[kl])
            for grp in range(NCH // 8):
                pA, pT_, pU = PS1[0], PS1[1], PS1[2]
                for cc in range(8):
                    ch = grp * 8 + cc; sl = slice(ch * 64, (ch + 1) * 64)
                    c.op("pe", lambda e, cc=cc, sl=sl: e.matmul(pA[0:64, cc * 64:(cc + 1) * 64], ke[:, sl], qe[:, sl], start=True, stop=True),
                         reads=[ke, qe], writes=[pA], acc=True)
                c.op("dve", lambda e: e.tensor_tensor(attT[:, grp * 8:(grp + 1) * 8, :].rearrange("p c t -> p (c t)"), pA[0:64, :], tri8[:], ALU.mult),
                     reads=[pA, tri8], writes=[attT])
                for cc in range(8):
                    ch = grp * 8 + cc; sl = slice(ch * 64, (ch + 1) * 64)
                    c.op("pe", lambda e, cc=cc, sl=sl: e.transpose(pT_[0:64, cc * 32:(cc + 1) * 32], kl[:, sl], ident[0:32, 0:32]),
                         reads=[kl, ident], writes=[pT_], acc=True)
                c.op("act", lambda e: e.activation(klT[:], pT_[0:64, 0:256], AF.Copy), reads=[pT_], writes=[klT])
                for cc in range(8):
                    ch = grp * 8 + cc
                    c.op("pe", lambda e, cc=cc, ch=ch: e.matmul(pU[0:32, cc * 64:(cc + 1) * 64], klT[:, cc * 32:(cc + 1) * 32], v[:, ch, :], start=True, stop=True),
                         reads=[klT, v], writes=[pU], acc=True)
                c.op("dve", lambda e: e.tensor_copy(U[:, grp * 8:(grp + 1) * 8, :].rearrange("p c t -> p (c t)"), pU[0:32, :]), reads=[pU], writes=[U])
            for e_ in range(64):
                c.op("dve", lambda e, e_=e_: e.tensor_tensor_scan(Sall[:, 1:NCH + 1, e_], dec[:], U[:, :, e_], Sall[:, 0, e_:e_ + 1], ALU.mult, ALU.add),
                     reads=[dec, U, Sall], writes=[Sall])
            for grp in range(NCH // 8):
                pO = PS1[3]
                for cc in range(8):
                    ch = grp * 8 + cc; sl = slice(ch * 64, (ch + 1) * 64)
                    c.op("pe", lambda e, cc=cc, ch=ch: e.matmul(pO[0:64, cc * 64:(cc + 1) * 64], attT[:, ch, :], v[:, ch, :], start=True, stop=False),
                         reads=[attT, v], writes=[pO], acc=True)
                    c.op("pe", lambda e, cc=cc, ch=ch, sl=sl: e.matmul(pO[0:64, cc * 64:(cc + 1) * 64], qe[:, sl], Sall[:, ch, :], start=False, stop=True),
                         reads=[qe, Sall], writes=[pO], acc=True)
                o = osb[grp % 2]
                c.op("act", lambda e: e.activation(o[:].rearrange("p c t -> p (c t)"), pO[0:64, :], AF.Copy), reads=[pO], writes=[o])
                c0 = pc * NCH + grp * 8
                c.dma("pool", oc[c0:c0 + 8, :, :].rearrange("c p e -> p c e"), o[:], reads=[o], writes=[oc])
            c.op("dve", lambda e: e.tensor_copy(Sall[:, 0, :], Sall[:, NCH, :]), reads=[Sall], writes=[Sall])
        c.pop()

    c.push()
    qT = c.sbuf([64, S], BF16, "qT"); kT = c.sbuf([64, S], BF16, "kT"); V = c.sbuf([128, 128, 65], BF16, "V")
    cmask = c.sbuf([128, 4, 512], BF16, "cmask"); bmask = c.sbuf([128, 1024], BF16, "bmask")
    pTs = [c.sbuf([128, 1024], BF16, "pT%d" % i) for i in range(3)]
    osbs = [c.sbuf([65, 512], F32, "osb%d" % i) for i in range(2)]
    c.dma("sp", cmask[:], cmask_d[:], writes=[cmask]); c.dma("sp", bmask[:], bmask_d[:], writes=[bmask])
    step = [0]

    def run_unit(groups, Kd, scale, out_d):
        steps = []
        for gi, G in enumerate(groups):
            n = len(G["pairs"])
            for pi_, pr in enumerate(G["pairs"]):
                steps.append((gi, pi_, n, pr))
        bufs = {}

        def emit_S(i):
            gi, pi_, n, (j0, j1) = steps[i]
            G = groups[gi]; g = G["g"]
            sps = PS2[i % 2]
            for hh, j in enumerate((j0, j1)):
                if G["extra"] is None:
                    c.op("pe", lambda e, hh=hh, j=j: e.matmul(sps[:, hh * 512:(hh + 1) * 512], kT[0:Kd, j * 128:(j + 1) * 128], qT[0:Kd, g * 512:(g + 1) * 512],
                                                             start=True, stop=True), reads=[kT, qT], writes=[sps], acc=True)
                else:
                    zl, biasT = G["extra"](pi_)
                    c.op("pe", lambda e, hh=hh, j=j: e.matmul(sps[:, hh * 512:(hh + 1) * 512], kT[0:Kd, j * 128:(j + 1) * 128], qT[0:Kd, g * 512:(g + 1) * 512],
                                                             start=True, stop=False), reads=[kT, qT], writes=[sps], acc=True)
                    c.op("pe", lambda e, hh=hh, zl=zl, biasT=biasT: e.matmul(sps[:, hh * 512:(hh + 1) * 512], zl, biasT[:], start=False, stop=True),
                         reads=[Zb, biasT], writes=[sps], acc=True)

        if groups[0].get("part1"):
            groups[0]["part1"](); groups[0]["part2"]()
        emit_S(0)
        for i, (gi, pi_, n, (j0, j1)) in enumerate(steps):
            G = groups[gi]; g = G["g"]
            sps = PS2[i % 2]; pT = pTs[i % 3]; pO = PS1[g % 2]
            if pi_ == 0 and gi + 1 < len(groups) and groups[gi + 1].get("part1"):
                groups[gi + 1]["part1"]()
            c.op("act", lambda e: e.activation(pT[:], sps[:], AF.Exp, scale=scale), reads=[sps], writes=[pT])
            if pi_ in G["masks"]:
                mk_ap, mk_buf = G["masks"][pi_]
                c.op("dve", lambda e: e.tensor_tensor(pT[:], pT[:], mk_ap, ALU.mult), reads=[pT, mk_buf], writes=[pT])
            if pi_ == n - 1 and gi + 1 < len(groups) and groups[gi + 1].get("part2"):
                groups[gi + 1]["part2"]()
            if i + 1 < len(steps):
                emit_S(i + 1)
            for hh, j in enumerate((j0, j1)):
                c.op("pe", lambda e, hh=hh, j=j: e.matmul(pO[0:65, :], V[:, j, :], pT[:, hh * 512:(hh + 1) * 512],
                                                         start=(pi_ == 0 and hh == 0), stop=(pi_ == n - 1 and hh == 1)),
                     reads=[V, pT], writes=[pO], acc=True)
            if pi_ == n - 1:
                o = osbs[g % 2]
                c.op("dve", lambda e: e.tensor_copy(o[:], pO[0:65, :]), reads=[pO], writes=[o])
                c.dma("pool", out_d[:, g * 512:(g + 1) * 512], o[:], reads=[o], writes=[out_d])

    if do_diff:
        c.dma("sp", qT[0:32, :], dq[:], writes=[qT]); c.dma("sp", kT[0:32, :], dk[:], writes=[kT]); c.dma("sp", V[:], dv[:], writes=[V])
        cm2 = cmask[:].rearrange("p d q -> p (d q)")
        groups = []
        for g in range(NG):
            npair = 2 * (g + 1)
            pairs = [(2 * i, 2 * i + 1) for i in range(npair)]
            masks = {npair - 2: (cm2[:, 0:1024], cmask), npair - 1: (cm2[:, 1024:2048], cmask)}
            groups.append(dict(g=g, pairs=pairs, masks=masks, extra=None))
        run_unit(groups, 32, 32.0 ** -0.5, oa)

    if do_moba:
        Zb = c.sbuf([32, 32, 128], BF16, "Zb"); iota = c.sbuf([128, 64], F32, "iota"); par = c.sbuf([128, 2], F32, "par")
        km = c.sbuf([64, 64], F32, "km")
        c.dma("sp", Zb[:], Z_d[:], writes=[Zb]); c.dma("sp", iota[:], iota_d[:], writes=[iota]); c.dma("sp", par[:], par_d[:], writes=[par])
        c.dma("sp", km[:], mkm[:], writes=[km])
        c.dma("sp", qT[0:64, :], mq[:], writes=[qT]); c.dma("sp", kT[0:64, 0:S // 2], mk[:], writes=[kT]); c.dma("sp", V[:, 0:64, :], mv[:], writes=[V])
        q32s = [c.sbuf([64, 512], F32, "q32_%d" % i) for i in range(2)]
        biasTs = [c.sbuf([32, 512], BF16, "biasT%d" % i) for i in range(2)]
        W = {n: [c.sbuf([128, 64], F32, "mw_%s%d" % (n, i)) for i in range(4)] for n in ("lt", "t1", "gm", "sel", "eq")}
        top8s = [c.sbuf([128, 8], F32, "top8_%d" % i) for i in range(4)]
        bps = [c.sbuf([128, 32], F32, "bp%d" % i) for i in range(4)]
        pG = PS1[3]; pB = PS1[2]

        def mk_part1(g):
            def part1():
                q32 = q32s[g % 2]
                c.dma("sp", q32[:], mq32[:, g * 512:(g + 1) * 512], writes=[q32])
                for qi in range(4):
                    c.op("pe", lambda e, qi=qi: e.matmul(pG[:, qi * 64:(qi + 1) * 64], q32[:, qi * 128:(qi + 1) * 128], km[:], start=True, stop=True),
                         reads=[q32, km], writes=[pG], acc=True)
                for qi in range(4):
                    qt = 4 * g + qi; own = float(qt // 2)
                    lt, t1, gm, sel, eq, top8, bp = W["lt"][qi], W["t1"][qi], W["gm"][qi], W["sel"][qi], W["eq"][qi], top8s[qi], bps[qi]
                    pGq = pG[:, qi * 64:(qi + 1) * 64]
                    c.op("dve", lambda e: e.tensor_single_scalar(lt[:], iota[:], own, ALU.is_lt), reads=[iota], writes=[lt])
                    c.op("dve", lambda e: e.tensor_scalar(t1[:], lt[:], -1.0, BIG, ALU.add, ALU.mult), reads=[lt], writes=[t1])
                    c.op("dve", lambda e: e.tensor_tensor(gm[:], pGq, lt[:], ALU.mult), reads=[pG, lt], writes=[gm])
                    c.op("dve", lambda e: e.tensor_tensor(gm[:], gm[:], t1[:], ALU.add), reads=[gm, t1], writes=[gm])
                    c.op("dve", lambda e: e.max(top8[:], gm[:]), reads=[gm], writes=[top8])
                    c.op("dve", lambda e: e.tensor_scalar(sel[:], gm[:], top8[:, 2:3], None, ALU.is_ge), reads=[gm, top8], writes=[sel])
                    c.op("dve", lambda e: e.tensor_tensor(sel[:], sel[:], lt[:], ALU.mult), reads=[sel, lt], writes=[sel])
                    c.op("dve", lambda e: e.tensor_single_scalar(eq[:], iota[:], own, ALU.is_equal), reads=[iota], writes=[eq])
                    c.op("dve", lambda e: e.tensor_tensor(sel[:], sel[:], eq[:], ALU.add), reads=[sel, eq], writes=[sel])
                    c.op("dve", lambda e: e.tensor_scalar(sel[:], sel[:], -1.0, BIGB, ALU.add, ALU.mult), reads=[sel], writes=[sel])
                    s3 = sel[:].rearrange("p (m two) -> p m two", two=2)
                    c.op("dve", lambda e: e.tensor_scalar(bp[:], s3[:, :, 0], par[:, 0:1], None, ALU.mult), reads=[sel, par], writes=[bp])
                    c.op("dve", lambda e: e.scalar_tensor_tensor(bp[:], s3[:, :, 1], par[:, 1:2], bp[:], ALU.mult, ALU.add), reads=[sel, par, bp], writes=[bp])
            return part1

        def mk_part2(g):
            def part2():
                biasT = biasTs[g % 2]
                for qi in range(4):
                    c.op("pe", lambda e, qi=qi: e.transpose(pB[0:32, qi * 128:(qi + 1) * 128], bps[qi][:], ident[:]), reads=[bps[qi], ident], writes=[pB], acc=True)
                c.op("act", lambda e: e.activation(biasT[:], pB[0:32, :], AF.Copy), reads=[pB], writes=[biasT])
            return part2

        groups = []
        for g in range(NG):
            pairs = [(2 * m, 2 * m + 1) for m in range(g + 1)]
            masks = {g: (bmask[:], bmask)}
            groups.append(dict(g=g, pairs=pairs, masks=masks, extra=(lambda m, g=g: (Zb[:, m, :], biasTs[g % 2])), part1=mk_part1(g), part2=mk_part2(g)))
        run_unit(groups, 64, 64.0 ** -0.5, ob)
    c.pop()
    c.finish([oa, ob, oc, od], "pool")
    c.close()
    return c


T = 2048
NT = T // 128
D = 1024
EPS = 1e-6
NE = 32

def build_C(n_exp=NE):
    c = Ctx(); nc = c.nc
    inp = lambda n, s, d=F32: c.dram(n, s, d, "ExternalInput")
    x = inp("x", [T, D]); oaT = inp("oaT", [T, 8, 65]); obT = inp("obT", [T, 8, 65]); ocT = inp("ocT", [T, 256]); crT = inp("crT", [T, 256]); odT = inp("odT", [T, 256])
    cTb = inp("cTb", [128, 8, 128]); adaw = inp("adaw", [D, 6144]); adabB = inp("adabB", [128, 2, 1024]); adabT2 = inp("adabT2", [128, 16])
    lamv = inp("lamv", [128, 4, 32]); lamc_d = inp("lamc", [128, 2]); aog_d = inp("aog", [128, 64]); cog_d = inp("cog", [128, 64]); n2gT = inp("n2gT", [128, 8])
    w_out = inp("w_out", [D, D]); rwT = inp("rwT", [128, 8, 32]); rbB = inp("rbB", [128, 32]); ident_d = inp("ident", [128, 128])
    wup = inp("wup", [NE, D, 2 * D]); bupT = inp("bupT", [128, NE, 16]); wdn = inp("wdn", [NE, D, D]); bdn = inp("bdn", [NE, D])
    out = c.dram("out", [T, D], F32, "ExternalOutput")
    outt = [Buf(out.t, "out%d" % t) for t in range(NT)]

    PS = [c.psum([128, 512], F32, "psb%d" % i) for i in range(8)]
    ident = c.sbuf([128, 128], F32, "ident"); epsT = c.sbuf([128, 1], F32, "epsT")
    c.dma("sp", ident[:], ident_d[:], writes=[ident])
    c.op("dve", lambda e: e.memset(epsT[:], EPS), writes=[epsT])
    g2b = c.sbuf([128, D], F32, "g2b")
    h2T = c.sbuf([128, 8, T], BF16, "h2T"); h2Tt = [Buf(None, "h2Tt%d" % t) for t in range(NT)]
    Gall = c.sbuf([128, NT, NE], F32, "Gall"); Gt = [Buf(None, "Gt%d" % t) for t in range(NT)]
    GT = c.sbuf([32, T], F32, "GT"); GTt = [Buf(None, "GTt%d" % t) for t in range(NT)]

    c.push()
    cb = c.sbuf([128, 8, 128], F32, "cb")
    c.dma("sp", cb[:], cTb[:], writes=[cb])
    c.op("act", lambda e: e.activation(cb[:], cb[:], AF.Silu), reads=[cb], writes=[cb])
    adaw_v = adaw.t.rearrange("(k p) n -> p k n", p=128)
    wst = [c.sbuf([128, 8, 512], F32, "adst%d" % i) for i in range(2)]
    g1b = c.sbuf([128, D], F32, "g1b"); abB = c.sbuf([128, 2, D], F32, "abB")
    c.dma("sp", abB[:], adabB[:], writes=[abB])
    si = 0
    for gi, (dst, col0) in enumerate(((g1b, 2048), (g2b, 5120))):
        for hf in range(2):
            st = wst[si % 2]; si += 1
            c.dma("sp", st[:], adaw_v[:, :, col0 + hf * 512:col0 + (hf + 1) * 512], writes=[st])
            ps = PS[hf]
            for k in range(8):
                c.op("pe", lambda e, k=k, st=st, ps=ps: e.matmul(ps[:], cb[:, k, :], st[:, k, :], start=(k == 0), stop=(k == 7)), reads=[cb, st], writes=[ps], acc=True)
            c.op("dve", lambda e, ps=ps, dst=dst, gi=gi, hf=hf: e.tensor_tensor(dst[:, hf * 512:(hf + 1) * 512], ps[:], abB[:, gi, hf * 512:(hf + 1) * 512], ALU.add),
                 reads=[ps, abB], writes=[dst])
    modps = PS[2]
    adab2 = c.sbuf([128, 16], F32, "adab2"); n2g = c.sbuf([128, 8], F32, "n2g")
    c.dma("sp", adab2[:], adabT2[:], writes=[adab2]); c.dma("sp", n2g[:], n2gT[:], writes=[n2g])
    for jj in range(4):
        st = wst[si % 2]; si += 1
        c.dma("sp", st[:], adaw_v[:, :, 3072 + jj * 512:3072 + (jj + 1) * 512], writes=[st])
        for j4 in range(4):
            j = jj * 4 + j4
            for k in range(8):
                c.op("pe", lambda e, k=k, j=j, j4=j4, st=st: e.matmul(modps[:, j:j + 1], st[:, k, j4 * 128:(j4 + 1) * 128], cb[:, k, 0:1],
                                                                  start=(k == 0), stop=(k == 7)), reads=[st, cb], writes=[modps], acc=True)
    mod2 = c.sbuf([128, 16], F32, "mod2"); a2 = c.sbuf([128, 8], F32, "a2")
    c.op("dve", lambda e: e.tensor_tensor(mod2[:], modps[:, 0:16], adab2[:], ALU.add), reads=[modps, adab2], writes=[mod2])
    c.op("dve", lambda e: e.scalar_tensor_tensor(a2[:], mod2[:, 8:16], 1.0, n2g[:], ALU.add, ALU.mult), reads=[mod2, n2g], writes=[a2])
    lv = c.sbuf([128, 4, 32], F32, "lv"); lsm = c.sbuf([128, 4], F32, "lsm"); nlam = c.sbuf([128, 1], F32, "nlam")
    c.dma("sp", lv[:], lamv[:], writes=[lv])
    c.op("dve", lambda e: e.tensor_tensor(lv[:, 0, :], lv[:, 0, :], lv[:, 1, :], ALU.mult), reads=[lv], writes=[lv])
    c.op("dve", lambda e: e.tensor_tensor(lv[:, 2, :], lv[:, 2, :], lv[:, 3, :], ALU.mult), reads=[lv], writes=[lv])
    c.op("dve", lambda e: e.tensor_reduce(lsm[:, 0:1], lv[:, 0, :], AX.X, ALU.add), reads=[lv], writes=[lsm])
    c.op("dve", lambda e: e.tensor_reduce(lsm[:, 1:2], lv[:, 2, :], AX.X, ALU.add), reads=[lv], writes=[lsm])
    c.op("act", lambda e: e.activation(lsm[:, 2:4], lsm[:, 0:2], AF.Exp), reads=[lsm], writes=[lsm])
    c.op("dve", lambda e: e.tensor_tensor(nlam[:], lsm[:, 3:4], lsm[:, 2:3], ALU.subtract), reads=[lsm], writes=[nlam])
    lamc = c.sbuf([128, 2], F32, "lamc")
    c.dma("sp", lamc[:], lamc_d[:], writes=[lamc])
    c.op("dve", lambda e: e.tensor_scalar(nlam[:], nlam[:], lamc[:, 0:1], None, ALU.subtract), reads=[nlam, lamc], writes=[nlam])
    aog = c.sbuf([128, 64], F32, "aog"); cog = c.sbuf([128, 64], F32, "cog")
    c.dma("sp", aog[:], aog_d[:], writes=[aog]); c.dma("sp", cog[:], cog_d[:], writes=[cog])
    c.op("dve", lambda e: e.tensor_scalar(aog[:], aog[:], lamc[:, 1:2], None, ALU.mult), reads=[aog, lamc], writes=[aog])
    wob = c.sbuf([128, 8, D], BF16, "wob"); wobk = [Buf(None, "wobk%d" % k) for k in range(8)]
    wo_v = w_out.t.rearrange("(k p) n -> p k n", p=128)
    wos = [c.sbuf([128, D], F32, "wos%d" % i) for i in range(2)]
    for k in range(8):
        st = wos[k % 2]
        c.dma("sp", st[:], wo_v[:, k, :], writes=[st])
        c.op("pool", lambda e, k=k, st=st: e.tensor_copy(wob[:, k, :], st[:]), reads=[st], writes=[wobk[k]])
    rw = c.sbuf([128, 8, 32], F32, "rw"); rb = c.sbuf([128, 32], F32, "rb")
    c.dma("sp", rw[:], rwT[:], writes=[rw]); c.dma("sp", rb[:], rbB[:], writes=[rb])

    oas = [c.sbuf([128, 8, 65], F32, "oas%d" % i) for i in range(2)]; obs = [c.sbuf([128, 8, 65], F32, "obs%d" % i) for i in range(2)]
    ocs = [c.sbuf([128, 256], F32, "ocs%d" % i) for i in range(2)]; crs = [c.sbuf([128, 256], F32, "crs%d" % i) for i in range(2)]
    ys = [c.sbuf([128, D], F32, "ys%d" % i) for i in range(2)]; xs = [c.sbuf([128, D], F32, "xs%d" % i) for i in range(2)]
    rs8 = c.sbuf([128, 8], F32, "rs8"); on = c.sbuf([128, 8, 64], F32, "on"); od_ = c.sbuf([128, 256], F32, "od_"); sq = c.sbuf([128, 256], F32, "sq")
    st4 = c.sbuf([128, 4], F32, "st4"); osum = c.sbuf([128, 4, 65], F32, "osum")
    yT = [c.sbuf([128, 8, 128], BF16, "yT%d" % i) for i in range(2)]
    junk = c.sbuf([128, D], BF16, "junk"); stat = c.sbuf([128, 2], F32, "stat")
    h32 = [c.sbuf([128, 8, 128], F32, "h32_%d" % i) for i in range(2)]
    lg = c.sbuf([128, 32], F32, "lg"); top8 = c.sbuf([128, 8], F32, "top8"); msk = c.sbuf([128, 32], F32, "msk"); ex = c.sbuf([128, 32], F32, "ex")
    sm = c.sbuf([128, 2], F32, "sm")
    for t in range(NT):
        ts_ = slice(t * 128, (t + 1) * 128)
        oa_ = oas[t % 2]; ob_ = obs[t % 2]; oc_ = ocs[t % 2]; cr_ = crs[t % 2]; y = ys[t % 2]; xt = xs[t % 2]
        c.dma("sp", oa_[:], oaT[ts_], writes=[oa_]); c.dma("sp", ob_[:], obT[ts_], writes=[ob_])
        c.dma("sp", oc_[:], ocT[ts_], writes=[oc_]); c.dma("sp", cr_[:], crT[ts_], writes=[cr_])
        c.dma("sp", y[:, 768:1024], odT[ts_], writes=[y]); c.dma("sp", xt[:], x[ts_], writes=[xt])
        c.op("dve", lambda e: e.reciprocal(rs8[:], oa_[:, :, 64]), reads=[oa_], writes=[rs8])
        for m in range(8):
            c.op("dve", lambda e, m=m: e.tensor_scalar(on[:, m, :], oa_[:, m, 0:64], rs8[:, m:m + 1], None, ALU.mult), reads=[oa_, rs8], writes=[on])
        on4 = on[:].rearrange("p (h i) d -> p h i d", i=2)
        od3 = od_[:].rearrange("p (h d) -> p h d", d=64)
        c.op("dve", lambda e: e.scalar_tensor_tensor(od3, on4[:, :, 1, :], nlam[:, 0:1], on4[:, :, 0, :], ALU.mult, ALU.add), reads=[on, nlam], writes=[od_])
        c.op("dve", lambda e: e.tensor_tensor(sq[:], od_[:], od_[:], ALU.mult), reads=[od_], writes=[sq])
        c.op("dve", lambda e: e.tensor_reduce(st4[:], sq[:].rearrange("p (h d) -> p h d", d=64), AX.X, ALU.add), reads=[sq], writes=[st4])
        c.op("act", lambda e: e.activation(st4[:], st4[:], AF.Sqrt, bias=epsT[:], scale=1.0 / 64), reads=[st4, epsT], writes=[st4])
        c.op("dve", lambda e: e.reciprocal(st4[:], st4[:]), reads=[st4], writes=[st4])
        for h in range(4):
            c.op("dve", lambda e, h=h: e.scalar_tensor_tensor(y[:, h * 64:(h + 1) * 64], od_[:, h * 64:(h + 1) * 64], st4[:, h:h + 1], aog[:], ALU.mult, ALU.mult),
                 reads=[od_, st4, aog], writes=[y])
        ob4 = ob_[:].rearrange("p (h i) d -> p h i d", i=2)
        c.op("dve", lambda e: e.tensor_tensor(osum[:], ob4[:, :, 0, :], ob4[:, :, 1, :], ALU.add), reads=[ob_], writes=[osum])
        c.op("dve", lambda e: e.reciprocal(rs8[:, 0:4], osum[:, :, 64]), reads=[osum], writes=[rs8])
        for h in range(4):
            c.op("dve", lambda e, h=h: e.tensor_scalar(y[:, 256 + h * 64:256 + (h + 1) * 64], osum[:, h, 0:64], rs8[:, h:h + 1], None, ALU.mult), reads=[osum, rs8], writes=[y])
        c.op("dve", lambda e: e.tensor_tensor(sq[:], oc_[:], oc_[:], ALU.mult), reads=[oc_], writes=[sq])
        c.op("dve", lambda e: e.tensor_reduce(st4[:], sq[:].rearrange("p (h d) -> p h d", d=64), AX.X, ALU.add), reads=[sq], writes=[st4])
        c.op("act", lambda e: e.activation(st4[:], st4[:], AF.Sqrt, bias=epsT[:], scale=1.0 / 64), reads=[st4, epsT], writes=[st4])
        c.op("dve", lambda e: e.reciprocal(st4[:], st4[:]), reads=[st4], writes=[st4])
        c.op("act", lambda e: e.activation(cr_[:], cr_[:], AF.Silu), reads=[cr_], writes=[cr_])
        for h in range(4):
            c.op("dve", lambda e, h=h: e.scalar_tensor_tensor(y[:, 512 + h * 64:512 + (h + 1) * 64], oc_[:, h * 64:(h + 1) * 64], st4[:, h:h + 1], cog[:], ALU.mult, ALU.mult),
                 reads=[oc_, st4, cog], writes=[y])
        c.op("dve", lambda e: e.tensor_tensor(y[:, 512:768], y[:, 512:768], cr_[:], ALU.mult), reads=[y, cr_], writes=[y])
        yt = yT[t % 2]
        for half in range(2):
            ps = PS[2 + half]
            for kk in range(4):
                k = half * 4 + kk
                c.op("pe", lambda e, k=k, kk=kk, ps=ps: e.transpose(ps[:, kk * 128:(kk + 1) * 128], y[:, k * 128:(k + 1) * 128], ident[:]), reads=[y, ident], writes=[ps], acc=True)
            c.op("act", lambda e, ps=ps, half=half: e.activation(yt[:, half * 4:(half + 1) * 4, :].rearrange("p k t -> p (k t)"), ps[:], AF.Copy), reads=[ps], writes=[yt])
        for hf in range(2):
            ps = PS[4 + hf]
            for k in range(8):
                c.op("pe", lambda e, k=k, ps=ps, hf=hf: e.matmul(ps[:], yt[:, k, :], wob[:, k, hf * 512:(hf + 1) * 512], start=(k == 0), stop=(k == 7)),
                     reads=[yt, wobk[k]], writes=[ps], acc=True)
            c.op("dve", lambda e, ps=ps, hf=hf: e.tensor_tensor(y[:, hf * 512:(hf + 1) * 512], ps[:], g1b[:, hf * 512:(hf + 1) * 512], ALU.mult), reads=[ps, g1b], writes=[y])
        c.op("dve", lambda e: e.tensor_tensor(xt[:], xt[:], y[:], ALU.add), reads=[xt, y], writes=[xt])
        c.dma("pool", out[ts_], xt[:], reads=[xt], writes=[outt[t]])
        c.op("act", lambda e: e.activation(junk[:], xt[:], AF.Square, accum_out=stat[:, 0:1]), reads=[xt], writes=[junk, stat])
        c.op("act", lambda e: e.activation(stat[:, 1:2], stat[:, 0:1], AF.Sqrt, bias=epsT[:], scale=1.0 / D), reads=[stat, epsT], writes=[stat])
        c.op("dve", lambda e: e.reciprocal(stat[:, 1:2], stat[:, 1:2]), reads=[stat], writes=[stat])
        c.op("dve", lambda e: e.tensor_scalar(y[:], xt[:], stat[:, 1:2], None, ALU.mult), reads=[xt, stat], writes=[y])
        hh = h32[t % 2]
        for half in range(2):
            ps = PS[6 + half]
            for kk in range(4):
                k = half * 4 + kk
                c.op("pe", lambda e, k=k, kk=kk, ps=ps: e.transpose(ps[:, kk * 128:(kk + 1) * 128], y[:, k * 128:(k + 1) * 128], ident[:]), reads=[y, ident], writes=[ps], acc=True)
            for kk in range(4):
                k = half * 4 + kk
                c.op("dve", lambda e, k=k, kk=kk, ps=ps: e.tensor_scalar(hh[:, k, :], ps[:, kk * 128:(kk + 1) * 128], a2[:, k:k + 1], mod2[:, k:k + 1], ALU.mult, ALU.add),
                     reads=[ps, a2, mod2], writes=[hh])
        c.op("act", lambda e: e.activation(h2T[:, :, ts_], hh[:], AF.Copy), reads=[hh], writes=[h2Tt[t]])
        psr = PS[0]
        for k in range(8):
            c.op("pe", lambda e, k=k: e.matmul(psr[:, 0:32], hh[:, k, :], rw[:, k, :], start=(k == 0), stop=(k == 7)), reads=[hh, rw], writes=[psr], acc=True)
        c.op("dve", lambda e: e.tensor_tensor(lg[:], psr[:, 0:32], rb[:], ALU.add), reads=[psr, rb], writes=[lg])
        c.op("dve", lambda e: e.max(top8[:], lg[:]), reads=[lg], writes=[top8])
        c.op("dve", lambda e: e.tensor_scalar(msk[:], lg[:], top8[:, 3:4], None, ALU.is_ge), reads=[lg, top8], writes=[msk])
        c.op("dve", lambda e: e.tensor_scalar(sm[:, 0:1], top8[:, 0:1], -1.0, None, ALU.mult), reads=[top8], writes=[sm])
        c.op("act", lambda e: e.activation(ex[:], lg[:], AF.Exp, bias=sm[:, 0:1], scale=1.0), reads=[lg, sm], writes=[ex])
        c.op("dve", lambda e: e.tensor_tensor(ex[:], ex[:], msk[:], ALU.mult), reads=[ex, msk], writes=[ex])
        c.op("dve", lambda e: e.tensor_reduce(sm[:, 1:2], ex[:], AX.X, ALU.add), reads=[ex], writes=[sm])
        c.op("dve", lambda e: e.reciprocal(sm[:, 1:2], sm[:, 1:2]), reads=[sm], writes=[sm])
        c.op("dve", lambda e: e.tensor_scalar(Gall[:, t, :], ex[:], sm[:, 1:2], None, ALU.mult), reads=[ex, sm], writes=[Gt[t]])
        psg = PS[1]
        c.op("pe", lambda e: e.transpose(psg[0:32, 0:128], Gall[:, t, :], ident[:]), reads=[Gt[t], ident], writes=[psg])
        c.op("act", lambda e: e.activation(GT[:, ts_], psg[0:32, 0:128], AF.Copy), reads=[psg], writes=[GTt[t]])
    c.pop()

    c.push()
    TP = 1024; NTP = TP // 128; NTG = TP // 512
    bup = c.sbuf([128, NE, 16], F32, "bup")
    c.dma("sp", bup[:], bupT[:], writes=[bup])
    Gsc = c.sbuf([128, NT, NE], F32, "Gsc")
    c.op("dve", lambda e: e.tensor_scalar(Gsc[:], Gall[:], 1.0 / 1.702, None, ALU.mult), reads=Gt, writes=[Gsc])
    acc = c.sbuf([128, NTP, D], F32, "acc"); acct = [Buf(None, "acct%d" % t) for t in range(NTP)]
    actT = c.sbuf([128, 8, TP], BF16, "actT"); actb = [[Buf(None, "act_%d_%d" % (j, tg)) for tg in range(NTG)] for j in range(8)]
    wus = [c.sbuf([128, 8, 256], F32, "wus%d" % i) for i in range(3)]; wub = [c.sbuf([128, 8, 256], BF16, "wub%d" % i) for i in range(4)]
    wds = [c.sbuf([128, 8, 512], F32, "wds%d" % i) for i in range(2)]; wdb = [c.sbuf([128, 8, 512], BF16, "wdb%d" % i) for i in range(2)]
    gs = [c.sbuf([128, 512], F32, "gs%d" % i) for i in range(2)]; sg = [c.sbuf([128, 512], F32, "sg%d" % i) for i in range(2)]
    ls = [c.sbuf([128, 512], F32, "ls%d" % i) for i in range(2)]
    bds = c.sbuf([32, D], F32, "bds")
    c.dma("sp", bds[:], bdn[:], writes=[bds])
    fin = c.sbuf([128, D], F32, "fin")
    wup_v = wup.t.rearrange("e (k p) n -> e p k n", p=128)
    wdn_v = wdn.t.rearrange("e (j p) n -> e p j n", p=128)
    slices = [(tp, e_, j) for tp in range(T // TP) for e_ in range(n_exp) for j in range(8)]

    def load_slice(i):
        tp, e_, j = slices[i]
        st = wus[i % 3]; wb_ = wub[i % 4]
        c.dma("sp", st[:, :, 0:128], wup_v[e_, :, :, j * 128:(j + 1) * 128], writes=[st])
        c.dma("sp", st[:, :, 128:256], wup_v[e_, :, :, D + j * 128:D + (j + 1) * 128], writes=[st])
        c.op("pool", lambda e: e.tensor_copy(wb_[:], st[:]), reads=[st], writes=[wb_])

    def load_down(e_):
        for c2 in range(2):
            st = wds[c2]; wd_ = wdb[c2]
            c.dma("sp", st[:], wdn_v[e_, :, :, c2 * 512:(c2 + 1) * 512], writes=[st])
            c.op("pool", lambda e, st=st, wd_=wd_: e.tensor_copy(wd_[:], st[:]), reads=[st], writes=[wd_])

    PF = 2
    for i in range(min(PF, len(slices))):
        load_slice(i)
    ie = 0
    for i, (tp, e_, j) in enumerate(slices):
        t0 = tp * NTP
        if i + PF < len(slices):
            load_slice(i + PF)
        if j == 1:
            load_down(e_)
        wb_ = wub[i % 4]
        for tg in range(NTG):
            tsl = slice(tp * TP + tg * 512, tp * TP + (tg + 1) * 512)
            pg = PS[(ie * 2) % 4]; pl = PS[(ie * 2 + 1) % 4]; g_ = gs[ie % 2]; s_ = sg[ie % 2]; l_ = ls[ie % 2]; ie += 1
            hrd = h2Tt[tsl.start // 128:tsl.stop // 128]
            for k in range(8):
                c.op("pe", lambda e, k=k: e.matmul(pg[:], wb_[:, k, 0:128], h2T[:, k, tsl], start=(k == 0), stop=(k == 7)), reads=[wb_] + hrd, writes=[pg], acc=True)
            for k in range(8):
                c.op("pe", lambda e, k=k: e.matmul(pl[:], wb_[:, k, 128:256], h2T[:, k, tsl], start=(k == 0), stop=(k == 7)), reads=[wb_] + hrd, writes=[pl], acc=True)
            c.op("dve", lambda e: e.tensor_scalar(g_[:], pg[:], bup[:, e_, j:j + 1], 7.0, ALU.add, ALU.min), reads=[pg, bup], writes=[g_])
            c.op("act", lambda e: e.activation(l_[:], pl[:], AF.Identity, bias=bup[:, e_, 8 + j:9 + j], scale=1.0), reads=[pl, bup], writes=[l_])
            c.op("act", lambda e: e.activation(s_[:], g_[:], AF.Silu, scale=1.702), reads=[g_], writes=[s_])
            c.op("pool", lambda e: e.tensor_scalar(l_[:], l_[:], 7.0, -7.0, ALU.min, ALU.max), reads=[l_], writes=[l_])
            c.op("dve", lambda e: e.scalar_tensor_tensor(actT[:, j, tg * 512:(tg + 1) * 512], l_[:], 1.0, s_[:], ALU.add, ALU.mult), reads=[l_, s_], writes=[actb[j][tg]])
        if j == 7:
            for c2 in range(2):
                wd_ = wdb[c2]
                for tt in range(NTP):
                    po = PS[4 + (tt % 4)]
                    for jj in range(8):
                        c.op("pe", lambda e, jj=jj: e.matmul(po[:], actT[:, jj, tt * 128:(tt + 1) * 128], wd_[:, jj, :], start=(jj == 0), stop=(jj == 7)),
                             reads=[actb[jj][tt // 4], wd_], writes=[po], acc=True)
                    dst = acc[:, tt, c2 * 512:(c2 + 1) * 512]
                    gsc = Gsc[:, t0 + tt, e_:e_ + 1]
                    if e_ == 0:
                        c.op("dve", lambda e: e.tensor_scalar(dst, po[:], gsc, None, ALU.mult), reads=[po, Gsc], writes=[acct[tt]])
                    else:
                        c.op("dve", lambda e: e.scalar_tensor_tensor(dst, po[:], gsc, dst, ALU.mult, ALU.add), reads=[po, Gsc, acct[tt]], writes=[acct[tt]])
            if e_ == n_exp - 1:
                for tt in range(NTP):
                    t = t0 + tt; ts_ = slice(t * 128, (t + 1) * 128)
                    f = fin
                    x1b = wds[0]; x1 = x1b[:].rearrange("p j c -> p (j c)")[:, 0:D]
                    c.dma("sp", x1, out[ts_], reads=[outt[t]], writes=[x1b])
                    for hf in range(2):
                        pb = PS[hf]
                        c.op("pe", lambda e, pb=pb, hf=hf: e.matmul(pb[:], GT[:, ts_], bds[:, hf * 512:(hf + 1) * 512], start=True, stop=True), reads=[GTt[t], bds], writes=[pb])
                        c.op("dve", lambda e, pb=pb, hf=hf: e.tensor_tensor(f[:, hf * 512:(hf + 1) * 512], pb[:], acc[:, tt, hf * 512:(hf + 1) * 512], ALU.add), reads=[pb, acct[tt]], writes=[f])
                    c.op("dve", lambda e: e.tensor_tensor(f[:], f[:], g2b[:], ALU.mult), reads=[f, g2b], writes=[f])
                    c.op("dve", lambda e: e.tensor_tensor(f[:], f[:], x1, ALU.add), reads=[f, x1b], writes=[f])
                    c.dma("pool", out[ts_], f[:], reads=[f, outt[t]], writes=[outt[t]])
    c.pop()
    c.finish(outt, "pool")
    c.close()
    return c

def pp(v):
    v = np.asarray(v, np.float32).reshape(-1, 128)
    return np.ascontiguousarray(v.T)
def consts():
    ident = np.eye(128, dtype=np.float32)
    i = np.arange(128)
    bd32 = (i[:, None] // 32 == i[None, :] // 32).astype(np.float32)
    bd64 = (i[:, None] // 64 == i[None, :] // 64).astype(np.float32)
    return ident, bd32, bd64
def prepA(inputs, l):
    ident, bd32, bd64 = consts()
    gains = np.zeros((128, 8), np.float32)
    gains[:, 0] = np.tile(inputs["a_q_gain"][l], 4); gains[:, 1] = np.tile(inputs["a_k_gain"][l], 4)
    gains[:, 2] = np.tile(inputs["b_q_gain"][l], 2); gains[:, 3] = np.tile(inputs["b_k_gain"][l], 2)
    gains[:, 4] = inputs["c_b_g"][l]
    common = dict(cT=pp(inputs["c"][0]), adaw=np.ascontiguousarray(inputs["ada_w"][l][:, :2048]), adabT=pp(inputs["ada_b"][l][:2048]),
                  n1gT=pp(inputs["norm1_g"][l]), w_in=np.ascontiguousarray(inputs["w_in"][l]), gains=gains,
                  wg2=np.ascontiguousarray(inputs["c_w_g2"][l]), ident=ident, bd32=bd32, bd64=bd64)
    return common

BF = ml_dtypes.bfloat16
S = 16384
def constsB():
    k = np.arange(128)[:, None]; q = np.arange(512)[None, :]
    cmask = np.stack([(128 * d + k <= q) for d in range(4)], axis=1).astype(BF)
    Z = np.zeros((32, 32, 128), BF)
    for m in range(32): Z[m, m, :] = 1
    iota = np.tile(np.arange(64, dtype=np.float32)[None, :], (128, 1))
    ident = np.eye(128, dtype=np.float32)
    rmask = np.ones((32, 2048), np.float32); rmask[:, ::64] = 0
    j = np.arange(64)[:, None]; i = np.arange(64)[None, :]
    tri8 = np.tile((j <= i).astype(np.float32), (1, 8))
    return dict(cmask=cmask, Z=Z, iota=iota, ident=ident, rmask=rmask, tri8=tri8)
def vlay(v, ntile):
    v1 = np.concatenate([v, np.ones((v.shape[0], 1), v.dtype)], axis=1)
    return np.ascontiguousarray(v1.reshape(ntile, 128, 65).transpose(1, 0, 2))
def prepB(A, inputs, l):
    cst = constsB()
    maps = []
    for r in range(8):
        m = r; h = r // 2; hb = r // 2; p = r % 2; hc = r % 4; nb = r % 4
        d = dict(cst)
        d["dq"] = np.ascontiguousarray(A["aqT"][32 * m:32 * m + 32]); d["dk"] = np.ascontiguousarray(A["akT"][32 * m:32 * m + 32])
        d["dv"] = vlay(A["av"][:, 64 * h:64 * h + 64], 128)
        d["mq"] = np.ascontiguousarray(A["bqT"][64 * hb:64 * hb + 64])
        kk = A["bkT"][64 * hb:64 * hb + 64].reshape(64, 64, 256)[:, p::2, :]
        d["mk"] = np.ascontiguousarray(kk.reshape(64, S // 2))
        vv = A["bv"][:, 64 * hb:64 * hb + 64].reshape(64, 256, 64)[p::2].reshape(S // 2, 64)
        d["mv"] = vlay(vv, 64)
        d["mq32"] = np.ascontiguousarray(A["bqT32"][64 * hb:64 * hb + 64]); d["mkm"] = np.ascontiguousarray(A["bkm"][64 * hb:64 * hb + 64])
        d["par"] = np.tile(np.array([[1.0 - p, float(p)]], np.float32), (128, 1))
        k = np.arange(128)[:, None]; q = np.arange(512)[None, :]
        d["bmask"] = np.concatenate([(256 * p + 128 * hh + k <= q) for hh in range(2)], axis=1).astype(BF)
        d["gq"] = np.ascontiguousarray(A["cqT"][32 * hc:32 * hc + 32]); d["gk"] = np.ascontiguousarray(A["ckT"][32 * hc:32 * hc + 32])
        d["gg"] = np.ascontiguousarray(A["gT"][32 * hc:32 * hc + 32])
        d["gv"] = np.ascontiguousarray(A["cv"][:, 64 * hc:64 * hc + 64].reshape(256, 64, 64).transpose(1, 0, 2))
        rx = np.zeros((64, S + 3), np.float32); rx[:, 3:] = A["dxT"][64 * nb:64 * nb + 64]
        d["rx"] = rx; d["rg"] = np.ascontiguousarray(A["dgT"][64 * nb:64 * nb + 64])
        rw = np.zeros((64, 8), np.float32)
        sl = slice(64 * nb, 64 * nb + 64)
        rw[:, 0:4] = inputs["d_conv_w"][l][:, sl].T; rw[:, 4] = inputs["d_conv_b"][l][sl]; rw[:, 5] = inputs["d_b_a"][l][sl]
        rw[:, 6] = inputs["d_b_x"][l][sl]; rw[:, 7] = inputs["d_lambda"][l][sl]
        d["rw"] = rw; d["rwa"] = np.ascontiguousarray(inputs["d_w_a"][l][nb]); d["rwx"] = np.ascontiguousarray(inputs["d_w_x"][l][nb])
        maps.append(d)
    return maps

def prepC(Bres, A, inputs, l, x_full):
    S_ = S
    oaT = np.stack([Bres[m]["oa"].T for m in range(8)], axis=1)
    obT = np.stack([Bres[r]["ob"].T for r in range(8)], axis=1)
    ocT = np.concatenate([Bres[h]["oc"].reshape(S_, 64) for h in range(4)], axis=1)
    odT = np.concatenate([Bres[nb]["od"] for nb in range(4)], axis=0).T
    crT = A["cr"]
    c_ = inputs["c"][0]
    cTb = np.ascontiguousarray(np.broadcast_to(pp(c_)[:, :, None], (128, 8, 128)))
    ab = inputs["ada_b"][l]
    adabB = np.ascontiguousarray(np.broadcast_to(np.stack([ab[2048:3072], ab[5120:6144]])[None], (128, 2, 1024)))
    lamv = np.ascontiguousarray(np.broadcast_to(np.stack([inputs["a_lam_q1"][l], inputs["a_lam_k1"][l], inputs["a_lam_q2"][l], inputs["a_lam_k2"][l]])[None], (128, 4, 32)))
    lam_init = 0.8 - 0.6 * math.exp(-0.3 * l)
    lamc = np.tile(np.array([[lam_init, 1.0 - lam_init]], np.float32), (128, 1))
    common = dict(lamc=lamc, cTb=cTb, adaw=np.ascontiguousarray(inputs["ada_w"][l]), adabB=adabB, adabT2=pp(ab[3072:5120]), lamv=lamv,
                  aog=np.ascontiguousarray(np.broadcast_to(inputs["a_out_gain"][l][None], (128, 64))),
                  cog=np.ascontiguousarray(np.broadcast_to(inputs["c_out_gain"][l][None], (128, 64))),
                  n2gT=pp(inputs["norm2_g"][l]), w_out=np.ascontiguousarray(inputs["w_out"][l]),
                  rwT=np.ascontiguousarray(inputs["router_w"][l].reshape(8, 128, 32).transpose(1, 0, 2)),
                  rbB=np.ascontiguousarray(np.broadcast_to(inputs["router_b"][l][None], (128, 32))), ident=np.eye(128, dtype=np.float32),
                  wup=np.ascontiguousarray(inputs["exp_w_up"][l]),
                  bupT=np.ascontiguousarray(inputs["exp_b_up"][l].reshape(32, 16, 128).transpose(2, 0, 1)),
                  wdn=np.ascontiguousarray(inputs["exp_w_down"][l]), bdn=np.ascontiguousarray(inputs["exp_b_down"][l]))
    maps = []
    for r in range(8):
        sl = slice(r * 2048, (r + 1) * 2048)
        maps.append(dict(common, x=np.ascontiguousarray(x_full[sl]), oaT=np.ascontiguousarray(oaT[sl]), obT=np.ascontiguousarray(obT[sl]),
                         ocT=np.ascontiguousarray(ocT[sl]), crT=np.ascontiguousarray(crT[sl]), odT=np.ascontiguousarray(odT[sl])))
    return maps


def _cat(R, k, axis):
    return np.concatenate([np.asarray(r[k]) for r in R], axis=axis)


def kernel(**inputs):
    inputs = {k: np.asarray(v) for k, v in inputs.items()}
    cores = list(range(8))
    x = np.ascontiguousarray(inputs["x"][0])
    for l in range(2):
        cA = build_A()
        common = prepA(inputs, l)
        mapsA = [dict(common, x=np.ascontiguousarray(x[i * 2048:(i + 1) * 2048])) for i in range(8)]
        RA = run_bass_kernel_spmd(cA.nc, mapsA, core_ids=cores).results
        A = {k: _cat(RA, k, 0 if k in ("av", "bv", "cv", "cr") else 1) for k in RA[0]}
        del RA, mapsA
        cB = build_B()
        mapsB = prepB(A, inputs, l)
        RB = run_bass_kernel_spmd(cB.nc, mapsB, core_ids=cores).results
        Bres = [{k: np.asarray(r[k]) for k in ("oa", "ob", "oc", "od")} for r in RB]
        del RB, mapsB
        cC = build_C()
        mapsC = prepC(Bres, A, inputs, l, x)
        RC = run_bass_kernel_spmd(cC.nc, mapsC, core_ids=cores).results
        x = _cat(RC, "out", 0)
        del RC, mapsC, A, Bres
    return np.ascontiguousarray(x[None]).astype(np.float32)
```

```python
import contextlib, math
import numpy as np
import ml_dtypes
import concourse.bass as bass
import concourse.mybir as mybir
from concourse.bass_utils import run_bass_kernel_spmd


F32 = mybir.dt.float32
BF16 = mybir.dt.bfloat16
I32 = mybir.dt.int32
AF = mybir.ActivationFunctionType
ALU = mybir.AluOpType
AX = mybir.AxisListType


class Buf:
    def __init__(self, t, name):
        self.t = t
        self.name = name
        self.w = {}
        self.r = {}

    def __getitem__(self, idx):
        return self.t[idx]


class Ctx:
    NDS = 8

    def __init__(self):
        self.nc = bass.Bass("TRN2", target_bir_lowering=False)
        nc = self.nc
        self.es = contextlib.ExitStack()
        self.es_root = self.es
        self.E = {"pe": nc.tensor, "act": nc.scalar, "dve": nc.vector, "pool": nc.gpsimd, "sp": nc.sync}
        self.sems = {}
        self.cnt = {}
        for e in ("pe", "act", "dve", "pool"):
            self.sems[e] = self.es.enter_context(nc.semaphore("s_" + e))
            self.cnt[e] = 0
        self.dq = {}
        for q in ("sp", "pool", "act"):
            ss = []
            for i in range(self.NDS):
                k = "d_%s%d" % (q, i)
                self.sems[k] = self.es.enter_context(nc.semaphore(k))
                ss.append(k)
            self.dq[q] = [ss, 0]
        self.seen = {e: {} for e in self.E}
        self.ekey = {}
        self.eno = {}
        self.nbuf = 0
        self.ninstr = 0

    def sbuf(self, shape, dt, name=None):
        self.nbuf += 1
        name = "sb%d_%s" % (self.nbuf, name or "x")
        t = self.es.enter_context(self.nc.sbuf_tensor(name, list(shape), dt))
        return Buf(t, name)

    def psum(self, shape, dt, name=None):
        self.nbuf += 1
        name = "ps%d_%s" % (self.nbuf, name or "x")
        t = self.es.enter_context(self.nc.psum_tensor(name, list(shape), dt))
        return Buf(t, name)

    def dram(self, name, shape, dt, kind):
        t = self.nc.dram_tensor(name, list(shape), dt, kind=kind).ap()
        return Buf(t, name)

    def _wait(self, eng, tok):
        if tok is None:
            return
        k, v = tok
        if self.seen[eng].get(k, 0) >= v:
            return
        self.E[eng].wait_ge(self.sems[k], v)
        self.seen[eng][k] = v
        self.ninstr += 1

    def _deps(self, eng, reads, writes, acc=False, nowaw=False):
        for b in reads:
            for k, v in b.w.items():
                self._wait(eng, (k, v))
        for b in writes:
            if not (nowaw or (acc and eng == "pe" and all(k.split("#")[0] == "pe" for k in b.w))):
                for k, v in b.w.items():
                    self._wait(eng, (k, v))
            for k, v in b.r.items():
                self._wait(eng, (k, v))

    def _mark(self, tok, reads, writes, nowaw=False):
        k, v = tok
        for b in reads:
            if b.r.get(k, 0) < v:
                b.r[k] = v
        for b in writes:
            if nowaw:
                b.w[k] = max(b.w.get(k, 0), v)
            else:
                b.w = {k: v}
                b.r = {}

    EPOCH = 24000

    def op(self, eng, fn, reads=(), writes=(), acc=False):
        key = self.ekey.get(eng, eng)
        if self.cnt[key] >= self.EPOCH:
            n = self.eno.get(eng, 0) + 1
            self.eno[eng] = n
            key = "%s#%d" % (eng, n)
            self.sems[key] = self.es_root.enter_context(self.nc.semaphore("s_%s_%d" % (eng, n)))
            self.cnt[key] = 0
            self.ekey[eng] = key
        self._deps(eng, reads, writes, acc)
        ins = fn(self.E[eng])
        self.cnt[key] += 1
        ins.then_inc(self.sems[key], 1)
        self.ninstr += 1
        self._mark((key, self.cnt[key]), reads, writes)
        return ins

    def dma(self, q, out, in_, reads=(), writes=(), nowaw=False, **kw):
        ss, j = self.dq[q]
        k = ss[j % self.NDS]
        rnd = j // self.NDS
        if rnd > 0:
            self._wait(q, (k, 16 * rnd))
        self._deps(q, reads, writes, nowaw=nowaw)
        if callable(in_):
            in_ = in_()
        if callable(out):
            out = out()
        ins = self.E[q].dma_start(out=out, in_=in_, **kw)
        ins.then_inc(self.sems[k], 16)
        self.dq[q][1] = j + 1
        self.ninstr += 1
        self._mark((k, 16 * (rnd + 1)), reads, writes, nowaw=nowaw)
        return ins

    def collective(self, kind, send_t, recv_t, reads=(), writes=()):
        if "cc" not in self.sems:
            self.sems["cc"] = self.es_root.enter_context(self.nc.semaphore("s_cc"))
            self.cnt["cc"] = 0
        self._deps("pool", reads, writes)
        ins = self.nc.gpsimd.collective_compute(kind, ALU.bypass, replica_groups=[list(range(8))],
                                                ins=[send_t.ap().opt()], outs=[recv_t.ap().opt()])
        self.cnt["cc"] += 1
        ins.then_inc(self.sems["cc"])
        self.ninstr += 1
        self._mark(("cc", self.cnt["cc"]), reads, writes)
        return ins

    def finish(self, bufs, eng="sp"):
        for b in bufs:
            for k, v in b.w.items():
                self._wait(eng, (k, v))

    def close(self):
        self.es.close()


def _dq_tokens(self):
    toks = [(k, v) for k, v in self.cnt.items() if v > 0]
    for q, (ss, j) in self.dq.items():
        for i, k in enumerate(ss):
            n = (j - i + self.NDS - 1) // self.NDS if j > i else 0
            if n > 0:
                toks.append((k, 16 * n))
    return toks


def _barrier(self):
    toks = _dq_tokens(self)
    for e in ("pe", "act", "dve", "pool", "sp"):
        for tk in toks:
            self._wait(e, tk)


def _push(self):
    self._outer = getattr(self, "_outer", [])
    self._outer.append(self.es)
    self.es = contextlib.ExitStack()


def _pop(self):
    _barrier(self)
    self.es.close()
    self.es = self._outer.pop()


Ctx.barrier = _barrier
Ctx.push = _push
Ctx.pop = _pop


T = 2048
NT = T // 128
D = 1024
DIN = 2832
EPS = 1e-6
NE = 32
S = 16384
BIG = 1.0e9
BIGB = 30000.0
DEPTH = 2


class XBig:
    def __init__(self, c, name, R, W, dt):
        self.c = c; self.R = R; self.W = W
        self.st = c.nc.dram_tensor(name + "_s", [8 * R, W], dt)
        self.rt = c.nc.dram_tensor(name + "_r", [64 * R, W], dt)
        self.mt = c.nc.dram_tensor(name + "_m", [8 * R, W], dt)
        self.sb = Buf(self.st, name + "_s"); self.rb = Buf(self.rt, name + "_r"); self.mb = Buf(self.mt, name + "_m")

    def blk_m(self, src):
        return self.mt[src * self.R:(src + 1) * self.R, :]

    def localize(self, q, pid):
        v = self.rt.ap().rearrange("(s d r) w -> s d r w", s=8, d=8)
        for src in range(8):
            self.c.dma(q, self.blk_m(src), (lambda src=src: v[src, bass.ds(pid, 1), :, :].rearrange("o r w -> (o r) w")),
                       reads=[self.rb], writes=[self.mb], nowaw=(src > 0))

    def blk_s(self, dest):
        return self.st[dest * self.R:(dest + 1) * self.R, :]

    def blk_r(self, src):
        v = self.rt.ap().rearrange("(s d r) w -> s d r w", s=8, d=8)
        return v[src, bass.ds(self.c.pid, 1), :, :].rearrange("o r w -> (o r) w")

    def gather(self):
        self.c.collective("AllGather", self.st, self.rt, reads=[self.sb], writes=[self.rb])


class XB:
    def __init__(self, big, off, nrows, mode, p):
        self.big = big; self.off = off; self.nrows = nrows; self.mode = mode; self.p = p
        self.sb = big.sb; self.rb = big.mb

    def _view(self, blk):
        x = blk[self.off:self.off + self.nrows, :]
        if self.mode == "wide":
            x = x.rearrange("(a x) w -> a (x w)", x=self.p)
        elif self.mode == "narrow":
            x = x.rearrange("r (y e) -> (r y) e", e=self.p)
        elif self.mode == "km":
            x = x[:, 0:512].rearrange("o (a e) -> (o a) e", e=8)
        return x

    def s(self, dest, r0, r1):
        return self._view(self.big.blk_s(dest))[r0:r1, :]

    def r(self, src, r0, r1):
        return self._view(self.big.blk_m(src))[r0:r1, :]


def build_F(n_exp=NE, depth=DEPTH):
    c = Ctx(); nc = c.nc
    pid_sp = nc.sync.partition_id(); pid_pool = nc.gpsimd.partition_id()
    inp = lambda n, s, d=F32: c.dram(n, s, d, "ExternalInput")
    x_in = inp("x", [T, D]); cT_d = inp("cT", [128, 8]); cTb_d = inp("cTb", [128, 8, 128])
    ident_d = inp("ident", [128, 128]); bd32_d = inp("bd32", [128, 128]); bd64_d = inp("bd64", [128, 128])
    par_d = inp("par", [128, 2]); bmask_d = inp("bmask", [128, 1024], BF16)
    cmask_d = inp("cmask", [128, 4, 512], BF16); Z_d = inp("Z", [32, 32, 128], BF16); iota_d = inp("iota", [128, 64])
    rmask_d = inp("rmask", [32, 2048]); tri8_d = inp("tri8", [64, 512])
    L = []
    for l in range(depth):
        sfx = str(l)
        L.append(dict(
            adaw=inp("adaw" + sfx, [D, 6144]), adabT=inp("adabT" + sfx, [128, 16]), n1gT=inp("n1gT" + sfx, [128, 8]), w_in=inp("w_in" + sfx, [D, DIN]),
            gains=inp("gains" + sfx, [128, 8]), wg2=inp("wg2" + sfx, [16, 128]),
            rw=inp("rw" + sfx, [64, 8]), rwa=inp("rwa" + sfx, [64, 64]), rwx=inp("rwx" + sfx, [64, 64]),
            adabB=inp("adabB" + sfx, [128, 2, 1024]), adabT2=inp("adabT2" + sfx, [128, 16]), lamv=inp("lamv" + sfx, [128, 4, 32]), lamc=inp("lamc" + sfx, [128, 2]),
            aog=inp("aog" + sfx, [128, 64]), cog=inp("cog" + sfx, [128, 64]), n2gT=inp("n2gT" + sfx, [128, 8]), w_out=inp("w_out" + sfx, [D, D]),
            rwT=inp("rwT" + sfx, [128, 8, 32]), rbB=inp("rbB" + sfx, [128, 32]),
            wup=inp("wup" + sfx, [NE, D, 2 * D]), bupT=inp("bupT" + sfx, [128, NE, 16]), wdn=inp("wdn" + sfx, [NE, D, D]), bdn=inp("bdn" + sfx, [NE, D])))
    out = c.dram("out", [T, D], F32, "ExternalOutput")
    outt = [Buf(out.t, "out%d" % t) for t in range(NT)]
    xbuf_t = nc.dram_tensor("xbuf", [T, D], F32)
    xbt = [Buf(xbuf_t, "xb%d" % t) for t in range(NT)]
    xint = [Buf(x_in.t, "xin%d" % t) for t in range(NT)]
    crb_t = nc.dram_tensor("crb", [T, 256], F32)
    crbt = [Buf(crb_t, "crb%d" % t) for t in range(NT)]
    XA16 = XBig(c, "xa16", 256, 2048, BF16)
    XA32 = XBig(c, "xa32", 705, 1024, F32)
    XB32 = XBig(c, "xb32", 768, 1024, F32)
    XQK = XB(XA16, 0, 64, "native", None)
    XMQ = XB(XA16, 64, 64, "native", None)
    XMK = XB(XA16, 128, 32, "narrow", 1024)
    XDV = XB(XA16, 160, 64, "narrow", 64)
    XMV = XB(XA16, 224, 32, "narrow", 64)
    XMQ32 = XB(XA32, 0, 128, "wide", 2)
    XG = XB(XA32, 128, 192, "wide", 2)
    XGV = XB(XA32, 320, 128, "narrow", 64)
    XRG = XB(XA32, 448, 256, "wide", 2)
    XKM = XB(XA32, 704, 1, "km", None)
    XOA = XB(XB32, 0, 256, "narrow", 128); XOB = XB(XB32, 256, 256, "narrow", 128)
    XOC = XB(XB32, 512, 128, "narrow", 64); XOD = XB(XB32, 640, 128, "narrow", 64)
    XA = [XA16, XA32]
    XBs = [XB32]

    ident = c.sbuf([128, 128], F32, "ident"); epsT = c.sbuf([128, 1], F32, "epsT"); oneT = c.sbuf([128, 1], F32, "oneT")
    c.dma("sp", ident[:], ident_d[:], writes=[ident])
    c.op("dve", lambda e: e.memset(epsT[:], EPS), writes=[epsT])
    c.op("dve", lambda e: e.memset(oneT[:], 1.0), writes=[oneT])
    sq_ = [0]

    def stq():
        sq_[0] += 1
        return "pool" if sq_[0] % 2 else "sp"

    def partA(l, xsrc_t, xsrc_b):
        P = L[l]
        c.push()
        PS = [c.psum([128, 512], F32, "apsb%d_%d" % (l, i)) for i in range(8)]
        bd32 = c.sbuf([128, 128], F32, "bd32"); bd64 = c.sbuf([128, 128], F32, "bd64")
        gn = c.sbuf([128, 8], F32, "gn"); cond = c.sbuf([128, 8], F32, "cond"); adab = c.sbuf([128, 16], F32, "adab"); n1g = c.sbuf([128, 8], F32, "n1g")
        wg2s = c.sbuf([16, 128], F32, "wg2s")
        for sb, dr in ((bd32, bd32_d), (bd64, bd64_d), (gn, P["gains"]), (cond, cT_d), (adab, P["adabT"]), (n1g, P["n1gT"]), (wg2s, P["wg2"])):
            c.dma("sp", sb[:], dr[:], writes=[sb])
        c.op("act", lambda e: e.activation(cond[:], cond[:], AF.Silu), reads=[cond], writes=[cond])
        adaw_v = P["adaw"].t.rearrange("(k p) n -> p k n", p=128)
        wst = [c.sbuf([128, 8, 512], F32, "adst%d" % i) for i in range(2)]
        modps = PS[0]
        for jj in range(4):
            st = wst[jj % 2]
            c.dma("sp", st[:], adaw_v[:, :, jj * 512:(jj + 1) * 512], writes=[st])
            for j4 in range(4):
                j = jj * 4 + j4
                for k in range(8):
                    c.op("pe", lambda e, k=k, j=j, j4=j4, st=st: e.matmul(modps[:, j:j + 1], st[:, k, j4 * 128:(j4 + 1) * 128], cond[:, k:k + 1],
                                                                      start=(k == 0), stop=(k == 7)), reads=[st, cond], writes=[modps], acc=True)
        mod = c.sbuf([128, 16], F32, "mod")
        c.op("dve", lambda e: e.tensor_tensor(mod[:], modps[:, 0:16], adab[:], ALU.add), reads=[modps, adab], writes=[mod])
        a1 = c.sbuf([128, 8], F32, "a1")
        c.op("dve", lambda e: e.scalar_tensor_tensor(a1[:], mod[:, 8:16], 1.0, n1g[:], ALU.add, ALU.mult), reads=[mod, n1g], writes=[a1])
        wb = c.sbuf([128, 8, DIN], BF16, "wb")
        wbk = [Buf(None, "wbk%d" % k) for k in range(8)]
        win_v = P["w_in"].t.rearrange("(k p) n -> p k n", p=128)
        wstage = [c.sbuf([128, DIN], F32, "wstage%d" % i) for i in range(2)]
        for k in range(8):
            st = wstage[k % 2]
            c.dma("sp", st[:], win_v[:, k, :], writes=[st])
            eng = "dve" if k % 2 == 0 else "pool"
            c.op(eng, lambda e, k=k, st=st: e.tensor_copy(wb[:, k, :], st[:]), reads=[st], writes=[wbk[k]])
        hT = c.sbuf([128, 8, T], BF16, "hT")
        hTt = [Buf(None, "hTt%d" % t) for t in range(NT)]
        xts = [c.sbuf([128, D], F32, "xt%d" % i) for i in range(2)]
        junk = c.sbuf([128, D], BF16, "junk")
        stat = [c.sbuf([128, 2], F32, "stat%d" % i) for i in range(2)]
        for t in range(NT):
            xt = xts[t % 2]; stt = stat[t % 2]
            c.dma("sp", xt[:], xsrc_t[t * 128:(t + 1) * 128, :], reads=[xsrc_b[t]], writes=[xt])
            c.op("act", lambda e: e.activation(junk[:], xt[:], AF.Square, accum_out=stt[:, 0:1]), reads=[xt], writes=[junk, stt])
            c.op("act", lambda e: e.activation(stt[:, 1:2], stt[:, 0:1], AF.Sqrt, bias=epsT[:], scale=1.0 / D), reads=[stt, epsT], writes=[stt])
            c.op("dve", lambda e: e.reciprocal(stt[:, 1:2], stt[:, 1:2]), reads=[stt], writes=[stt])
            c.op("dve", lambda e: e.tensor_scalar(xt[:], xt[:], stt[:, 1:2], None, ALU.mult), reads=[xt, stt], writes=[xt])
            for half in range(2):
                ps = PS[1 + half]
                for kk in range(4):
                    k = half * 4 + kk
                    c.op("pe", lambda e, k=k, kk=kk, ps=ps: e.transpose(ps[:, kk * 128:(kk + 1) * 128], xt[:, k * 128:(k + 1) * 128], ident[:]),
                         reads=[xt, ident], writes=[ps], acc=True)
                for kk in range(4):
                    k = half * 4 + kk
                    if kk % 2 == 0:
                        c.op("dve", lambda e, k=k, kk=kk, ps=ps: e.tensor_scalar(hT[:, k, t * 128:(t + 1) * 128], ps[:, kk * 128:(kk + 1) * 128],
                                                                     a1[:, k:k + 1], mod[:, k:k + 1], ALU.mult, ALU.add),
                             reads=[ps, a1, mod], writes=[hTt[t]])
                    else:
                        c.op("act", lambda e, k=k, kk=kk, ps=ps: e.activation(hT[:, k, t * 128:(t + 1) * 128], ps[:, kk * 128:(kk + 1) * 128],
                                                                  AF.Identity, bias=mod[:, k:k + 1], scale=a1[:, k:k + 1]),
                             reads=[ps, a1, mod], writes=[hTt[t]])
        pi = [0]

        def nextps():
            pi[0] += 1
            return PS[3 + pi[0] % 5]

        def proj_fm(col0, ncols, g):
            ps = nextps()
            for k in range(8):
                c.op("pe", lambda e, k=k: e.matmul(ps[0:ncols, :], wb[:, k, col0:col0 + ncols], hT[:, k, g * 512:(g + 1) * 512],
                                                   start=(k == 0), stop=(k == 7)),
                     reads=[wbk[k]] + hTt[g * 4:(g + 1) * 4], writes=[ps], acc=True)
            return ps

        sqb = [c.sbuf([128, 512], F32, "sq%d" % i) for i in range(2)]
        rsb = [c.sbuf([128, 512], F32, "rs%d" % i) for i in range(2)]
        ob16 = [c.sbuf([128, 512], BF16, "ob16_%d" % i) for i in range(3)]
        ob32 = [c.sbuf([128, 512], F32, "ob32_%d" % i) for i in range(3)]
        kms = c.sbuf([128, 2, NT // 2], F32, "kms")
        kmsb = [Buf(None, "kmsb%d" % i) for i in range(2)]
        ctr = [0]

        def send(xb, dest, r0, r1, cols, src_ap, src_buf):
            c.dma(stq(), xb.s(dest, r0, r1)[:, cols], src_ap, reads=[src_buf], writes=[xb.sb], nowaw=True)

        def normed(ps, bd, inv_d, gcol, want32):
            i = ctr[0]; ctr[0] += 1
            sq = sqb[i % 2]; rs = rsb[i % 2]; o16 = ob16[i % 3]
            c.op("act", lambda e: e.activation(sq[:], ps[:], AF.Square), reads=[ps], writes=[sq])
            ps2 = nextps()
            c.op("pe", lambda e: e.matmul(ps2[:], bd[:], sq[:], start=True, stop=True), reads=[bd, sq], writes=[ps2])
            c.op("act", lambda e: e.activation(rs[:], ps2[:], AF.Sqrt, bias=epsT[:], scale=inv_d), reads=[ps2, epsT], writes=[rs])
            c.op("dve", lambda e: e.reciprocal(rs[:], rs[:]), reads=[rs], writes=[rs])
            c.op("dve", lambda e: e.scalar_tensor_tensor(o16[:], ps[:], gn[:, gcol:gcol + 1], rs[:], ALU.mult, ALU.mult), reads=[ps, gn, rs], writes=[o16])
            o32 = None
            if want32:
                o32 = ob32[i % 3]
                c.op("dve", lambda e: e.scalar_tensor_tensor(o32[:], ps[:], gn[:, gcol:gcol + 1], rs[:], ALU.mult, ALU.mult), reads=[ps, gn, rs], writes=[o32])
            return o16, o32

        def raw32(ps, nrows):
            i = ctr[0]; ctr[0] += 1
            o32 = ob32[i % 3]
            if i % 2:
                c.op("act", lambda e: e.activation(o32[0:nrows, :], ps[0:nrows, :], AF.Copy), reads=[ps], writes=[o32])
            else:
                c.op("dve", lambda e: e.tensor_copy(o32[0:nrows, :], ps[0:nrows, :]), reads=[ps], writes=[o32])
            return o32

        lt = [c.sbuf([128, 512], F32, "lt%d" % i) for i in range(3)]
        tm16 = [c.sbuf([128, 512], BF16, "tm16_%d" % i) for i in range(2)]
        tm32 = [c.sbuf([128, 512], F32, "tm32_%d" % i) for i in range(2)]
        cgs = c.sbuf([16, 512], F32, "cgs")
        for g in range(T // 512):
            gc = slice(g * 512, (g + 1) * 512)
            for ch in range(2):
                o16, _ = normed(proj_fm(0 + ch * 128, 128, g), bd32, 1.0 / 32, 0, False)
                for m_ in range(4):
                    send(XQK, 4 * ch + m_, 0, 32, gc, o16[32 * m_:32 * m_ + 32, :], o16)
                o16, _ = normed(proj_fm(256 + ch * 128, 128, g), bd32, 1.0 / 32, 1, False)
                for m_ in range(4):
                    send(XQK, 4 * ch + m_, 32, 64, gc, o16[32 * m_:32 * m_ + 32, :], o16)
                o16, o32 = normed(proj_fm(768 + ch * 128, 128, g), bd64, 1.0 / 64, 2, True)
                for hh in range(2):
                    h = 2 * ch + hh
                    for p_ in range(2):
                        send(XMQ, 2 * h + p_, 0, 64, gc, o16[64 * hh:64 * hh + 64, :], o16)
                        send(XMQ32, 2 * h + p_, 0, 64, gc, o32[64 * hh:64 * hh + 64, :], o32)
                o16, o32 = normed(proj_fm(1024 + ch * 128, 128, g), bd64, 1.0 / 64, 3, True)
                c.op("dve", lambda e, ch=ch, o32=o32: e.tensor_reduce(kms[:, ch, g * 2:(g + 1) * 2], o32[:].rearrange("p (b t) -> p b t", t=256), AX.X, ALU.add),
                     reads=[o32], writes=[kmsb[ch]])
                for hh in range(2):
                    h = 2 * ch + hh
                    for p_ in range(2):
                        send(XMK, 2 * h + p_, 0, 64, slice(g * 256, (g + 1) * 256), o16[64 * hh:64 * hh + 64, p_ * 256:(p_ + 1) * 256], o16)
                o32 = raw32(proj_fm(2320 + ch * 128, 128, g), 128)
                for hh in range(2):
                    send(XRG, 2 * ch + hh, 0, 64, gc, o32[64 * hh:64 * hh + 64, :], o32)
                o32 = raw32(proj_fm(2576 + ch * 128, 128, g), 128)
                for hh in range(2):
                    send(XRG, 2 * ch + hh, 64, 128, gc, o32[64 * hh:64 * hh + 64, :], o32)
            o32 = raw32(proj_fm(1536, 128, g), 128)
            for hc in range(4):
                send(XG, hc, 0, 32, gc, o32[32 * hc:32 * hc + 32, :], o32)
            o32 = raw32(proj_fm(1664, 128, g), 128)
            for hc in range(4):
                send(XG, hc, 32, 64, gc, o32[32 * hc:32 * hc + 32, :], o32)
            psg = proj_fm(2048, 16, g)
            c.op("dve", lambda e: e.tensor_copy(cgs[:], psg[0:16, :]), reads=[psg], writes=[cgs])
            psz = nextps()
            c.op("pe", lambda e: e.matmul(psz[:], wg2s[:], cgs[:], start=True, stop=True), reads=[wg2s, cgs], writes=[psz])
            z, az, m = lt
            c.op("dve", lambda e: e.tensor_scalar(z[:], psz[:], gn[:, 4:5], None, ALU.add), reads=[psz, gn], writes=[z])
            c.op("act", lambda e: e.activation(az[:], z[:], AF.Abs), reads=[z], writes=[az])
            c.op("act", lambda e: e.activation(az[:], az[:], AF.Exp, scale=-1.0), reads=[az], writes=[az])
            c.op("act", lambda e: e.activation(az[:], az[:], AF.Ln, bias=oneT[:], scale=1.0), reads=[az, oneT], writes=[az])
            c.op("dve", lambda e: e.tensor_scalar(m[:], z[:], 0.0, None, ALU.min), reads=[z], writes=[m])
            c.op("dve", lambda e: e.tensor_tensor(m[:], m[:], az[:], ALU.subtract), reads=[m, az], writes=[m])
            c.op("dve", lambda e: e.tensor_scalar(m[:], m[:], 1.0 / 16, None, ALU.mult), reads=[m], writes=[m])
            for hc in range(4):
                send(XG, hc, 64, 96, gc, m[32 * hc:32 * hc + 32, :], m)
            for tt in range(4):
                t = g * 4 + tt
                ps = nextps()
                for (o, col0) in ((0, 512), (256, 1280)):
                    for k in range(8):
                        c.op("pe", lambda e, k=k, o=o, col0=col0: e.matmul(ps[:, o:o + 256], hT[:, k, t * 128:(t + 1) * 128], wb[:, k, col0:col0 + 256],
                                                                        start=(k == 0), stop=(k == 7)), reads=[wbk[k], hTt[t]], writes=[ps], acc=True)
                o16 = tm16[t % 2]
                c.op("act", lambda e: e.activation(o16[:], ps[:], AF.Copy), reads=[ps], writes=[o16])
                par_ = (t // 2) % 2; row = ((t // 2) // 2) * 256 + (t % 2) * 128
                for h in range(4):
                    for i2 in range(2):
                        send(XDV, 2 * h + i2, t * 128, (t + 1) * 128, slice(0, 64), o16[:, 64 * h:64 * h + 64], o16)
                    send(XMV, 2 * h + par_, row, row + 128, slice(0, 64), o16[:, 256 + 64 * h:256 + 64 * h + 64], o16)
                ps = nextps()
                for (o, col0) in ((0, 1792), (256, 2064)):
                    for k in range(8):
                        c.op("pe", lambda e, k=k, o=o, col0=col0: e.matmul(ps[:, o:o + 256], hT[:, k, t * 128:(t + 1) * 128], wb[:, k, col0:col0 + 256],
                                                                        start=(k == 0), stop=(k == 7)), reads=[wbk[k], hTt[t]], writes=[ps], acc=True)
                o32 = tm32[t % 2]
                c.op("dve", lambda e: e.tensor_copy(o32[:], ps[:]), reads=[ps], writes=[o32])
                for hc in range(4):
                    send(XGV, hc, t * 128, (t + 1) * 128, slice(0, 64), o32[:, 64 * hc:64 * hc + 64], o32)
                c.dma(stq(), crb_t[t * 128:(t + 1) * 128, :], o32[:, 256:512], reads=[o32], writes=[crbt[t]])
        for ch in range(2):
            for hh in range(2):
                h = 2 * ch + hh
                for p_ in range(2):
                    send(XKM, 2 * h + p_, 0, 64, slice(0, 8), kms[64 * hh:64 * hh + 64, ch, :], kmsb[ch])
        c.pop()

    def partB(l):
        P = L[l]
        c.push()
        PS1 = [c.psum([128, 512], F32, "bps1_%d_%d" % (l, i)) for i in range(4)]
        PS2 = [c.psum([128, 1024], F32, "bps2_%d_%d" % (l, i)) for i in range(2)]
        c.push()
        PW = 2048
        rw = c.sbuf([64, 8], F32, "rw"); rwa = c.sbuf([64, 64], F32, "rwa"); rwx = c.sbuf([64, 64], F32, "rwx")
        for sb, dr in ((rw, P["rw"]), (rwa, P["rwa"]), (rwx, P["rwx"])):
            c.dma("sp", sb[:], dr[:], writes=[sb])
        cl = c.sbuf([64, 2], F32, "cl")
        c.op("act", lambda e: e.activation(cl[:, 0:1], rw[:, 7:8], AF.Exp, scale=-1.0), reads=[rw], writes=[cl])
        c.op("act", lambda e: e.activation(cl[:, 0:1], cl[:, 0:1], AF.Ln, bias=oneT[0:64, :], scale=1.0), reads=[cl, oneT], writes=[cl])
        c.op("dve", lambda e: e.tensor_scalar(cl[:, 1:2], cl[:, 0:1], -8.0, None, ALU.mult), reads=[cl], writes=[cl])
        xin = [c.sbuf([64, PW + 3], F32, "xin%d" % i) for i in range(2)]
        gin = [c.sbuf([64, PW], F32, "gin%d" % i) for i in range(2)]
        xc = c.sbuf([64, PW], F32, "xc"); rgt = c.sbuf([64, PW], F32, "rgt"); igt = c.sbuf([64, PW], F32, "igt")
        aa = c.sbuf([64, PW], F32, "aa"); bt = c.sbuf([64, PW], F32, "bt")
        hh_ = [c.sbuf([64, PW], F32, "hh%d" % i) for i in range(2)]
        uu = c.sbuf([64, PW], F32, "uu"); odT = c.sbuf([128, 16, 64], F32, "odT")
        for pc in range(S // PW):
            xi = xin[pc % 2]; gi = gin[pc % 2]; h = hh_[pc % 2]; hp = hh_[(pc + 1) % 2]
            c.dma("sp", xi[:, 3:PW + 3], (lambda: XRG.r(pc, 0, 64)), reads=[XRG.rb], writes=[xi])
            if pc == 0:
                c.op("dve", lambda e: e.memset(xi[:, 0:3], 0.0), writes=[xi])
            else:
                c.dma("sp", xi[:, 0:3], (lambda: XRG.r(pc - 1, 0, 64)[:, PW - 3:PW]), reads=[XRG.rb], writes=[xi], nowaw=True)
            c.dma("sp", gi[:], (lambda: XRG.r(pc, 64, 128)), reads=[XRG.rb], writes=[gi])
            c.op("dve", lambda e: e.tensor_scalar(xc[:], xi[:, 3:PW + 3], rw[:, 3:4], rw[:, 4:5], ALU.mult, ALU.add), reads=[xi, rw], writes=[xc])
            for j in range(3):
                c.op("dve", lambda e, j=j: e.scalar_tensor_tensor(xc[:], xi[:, j:PW + j], rw[:, j:j + 1], xc[:], ALU.mult, ALU.add), reads=[xi, rw, xc], writes=[xc])
            for grp in range(PW // 512):
                sl = slice(grp * 512, (grp + 1) * 512)
                p1 = PS1[0]; p2 = PS1[1]
                c.op("pe", lambda e: e.matmul(p1[0:64, :], rwa[:], xc[:, sl], start=True, stop=True), reads=[rwa, xc], writes=[p1])
                c.op("pe", lambda e: e.matmul(p2[0:64, :], rwx[:], xc[:, sl], start=True, stop=True), reads=[rwx, xc], writes=[p2])
                c.op("act", lambda e: e.activation(rgt[:, sl], p1[0:64, :], AF.Sigmoid, bias=rw[:, 5:6], scale=1.0), reads=[p1, rw], writes=[rgt])
                c.op("act", lambda e: e.activation(igt[:, sl], p2[0:64, :], AF.Sigmoid, bias=rw[:, 6:7], scale=1.0), reads=[p2, rw], writes=[igt])
            c.op("act", lambda e: e.activation(aa[:], rgt[:], AF.Exp, scale=cl[:, 1:2]), reads=[rgt, cl], writes=[aa])
            c.op("act", lambda e: e.activation(bt[:], aa[:], AF.Square), reads=[aa], writes=[bt])
            c.op("dve", lambda e: e.tensor_scalar(bt[:], bt[:], -1.0, 1.0, ALU.mult, ALU.add), reads=[bt], writes=[bt])
            c.op("act", lambda e: e.activation(bt[:], bt[:], AF.Sqrt), reads=[bt], writes=[bt])
            c.op("dve", lambda e: e.tensor_tensor(bt[:], bt[:], igt[:], ALU.mult), reads=[bt, igt], writes=[bt])
            c.op("dve", lambda e: e.tensor_tensor(bt[:], bt[:], xc[:], ALU.mult), reads=[bt, xc], writes=[bt])
            if pc == 0:
                c.op("dve", lambda e: e.tensor_tensor_scan(h[:], aa[:], bt[:], 0.0, ALU.mult, ALU.add), reads=[aa, bt], writes=[h])
            else:
                c.op("dve", lambda e: e.tensor_tensor_scan(h[:], aa[:], bt[:], hp[:, PW - 1:PW], ALU.mult, ALU.add), reads=[aa, bt, hp], writes=[h])
            c.op("dve", lambda e: e.tensor_tensor(uu[:], gi[:], gi[:], ALU.mult), reads=[gi], writes=[uu])
            c.op("dve", lambda e: e.tensor_scalar(uu[:], uu[:], 0.044715, 1.0, ALU.mult, ALU.add), reads=[uu], writes=[uu])
            c.op("dve", lambda e: e.tensor_tensor(uu[:], uu[:], gi[:], ALU.mult), reads=[uu, gi], writes=[uu])
            c.op("act", lambda e: e.activation(uu[:], uu[:], AF.Sigmoid, scale=1.5957691216057308), reads=[uu], writes=[uu])
            c.op("dve", lambda e: e.tensor_tensor(uu[:], uu[:], gi[:], ALU.mult), reads=[uu, gi], writes=[uu])
            c.op("dve", lambda e: e.tensor_tensor(uu[:], uu[:], h[:], ALU.mult), reads=[uu, h], writes=[uu])
            for half in range(2):
                pt = PS1[2 + half]
                for k8 in range(8):
                    k = half * 8 + k8
                    c.op("pe", lambda e, k=k, k8=k8, pt=pt: e.transpose(pt[:, k8 * 64:(k8 + 1) * 64], uu[:, k * 128:(k + 1) * 128], ident[0:64, 0:64]),
                         reads=[uu, ident], writes=[pt], acc=True)
                c.op("act", lambda e, pt=pt, half=half: e.activation(odT[:, half * 8:(half + 1) * 8, :].rearrange("p k e -> p (k e)"), pt[:], AF.Copy), reads=[pt], writes=[odT])
            c.dma("pool", XOD.s(pc, 0, 2048).rearrange("(t p) e -> p t e", p=128), odT[:], reads=[odT], writes=[XOD.sb], nowaw=True)
        c.pop()
        c.push()
        NCH = PW // 64
        rmask = c.sbuf([32, PW], F32, "rmask"); tri8 = c.sbuf([64, 512], F32, "tri8")
        c.dma("sp", rmask[:], rmask_d[:], writes=[rmask]); c.dma("sp", tri8[:], tri8_d[:], writes=[tri8])
        qs = [c.sbuf([32, PW], F32, "gq%d" % i) for i in range(2)]
        ks = [c.sbuf([32, PW], F32, "gk%d" % i) for i in range(2)]
        gs_ = [c.sbuf([32, PW], F32, "gg%d" % i) for i in range(2)]
        vs = [c.sbuf([64, NCH, 64], F32, "gv%d" % i) for i in range(2)]
        bcum = c.sbuf([32, PW], F32, "bcum"); eb = c.sbuf([32, PW], F32, "eb"); qe = c.sbuf([32, PW], F32, "qe")
        ke = c.sbuf([32, PW], F32, "ke"); kl = c.sbuf([32, PW], F32, "kl"); dec = c.sbuf([32, NCH], F32, "dec")
        attT = c.sbuf([64, NCH, 64], F32, "attT"); klT = c.sbuf([64, 256], F32, "klT")
        U = c.sbuf([32, NCH, 64], F32, "U"); Sall = c.sbuf([32, NCH + 1, 64], F32, "Sall")
        osb = [c.sbuf([64, 8, 64], F32, "gosb%d" % i) for i in range(2)]
        c.op("dve", lambda e: e.memset(Sall[:, 0, :], 0.0), writes=[Sall])
        for pc in range(S // PW):
            q = qs[pc % 2]; k = ks[pc % 2]; g = gs_[pc % 2]; v = vs[pc % 2]
            c.dma("sp", q[:], (lambda: XG.r(pc, 0, 32)), reads=[XG.rb], writes=[q]); c.dma("sp", k[:], (lambda: XG.r(pc, 32, 64)), reads=[XG.rb], writes=[k])
            c.dma("sp", g[:], (lambda: XG.r(pc, 64, 96)), reads=[XG.rb], writes=[g])
            c.dma("sp", v[:], (lambda: XGV.r(pc, 0, 2048).rearrange("(c p) e -> p c e", p=64)), reads=[XGV.rb], writes=[v])
            c.op("dve", lambda e: e.tensor_tensor_scan(bcum[:], rmask[:], g[:], 0.0, ALU.mult, ALU.add), reads=[rmask, g], writes=[bcum])
            c.op("act", lambda e: e.activation(eb[:], bcum[:], AF.Exp), reads=[bcum], writes=[eb])
            c.op("dve", lambda e: e.scalar_tensor_tensor(qe[:], q[:], 32.0 ** -0.5, eb[:], ALU.mult, ALU.mult), reads=[q, eb], writes=[qe])
            c.op("act", lambda e: e.activation(eb[:], bcum[:], AF.Exp, scale=-1.0), reads=[bcum], writes=[eb])
            c.op("dve", lambda e: e.tensor_tensor(ke[:], k[:], eb[:], ALU.mult), reads=[k, eb], writes=[ke])
            bc3 = bcum[:].rearrange("p (c t) -> p c t", t=64)
            c.op("act", lambda e: e.activation(dec[:], bc3[:, :, 63], AF.Exp), reads=[bcum], writes=[dec])
            for cc in range(NCH):
                c.op("dve", lambda e, cc=cc: e.tensor_scalar(kl[:, cc * 64:(cc + 1) * 64], ke[:, cc * 64:(cc + 1) * 64], dec[:, cc:cc + 1], None, ALU.mult),
                     reads=[ke, dec], writes=[kl])
            for grp in range(NCH // 8):
                pA, pT_, pU = PS1[0], PS1[1], PS1[2]
                for cc in range(8):
                    ch = grp * 8 + cc; sl = slice(ch * 64, (ch + 1) * 64)
                    c.op("pe", lambda e, cc=cc, sl=sl: e.matmul(pA[0:64, cc * 64:(cc + 1) * 64], ke[:, sl], qe[:, sl], start=True, stop=True),
                         reads=[ke, qe], writes=[pA], acc=True)
                c.op("dve", lambda e: e.tensor_tensor(attT[:, grp * 8:(grp + 1) * 8, :].rearrange("p c t -> p (c t)"), pA[0:64, :], tri8[:], ALU.mult),
                     reads=[pA, tri8], writes=[attT])
                for cc in range(8):
                    ch = grp * 8 + cc; sl = slice(ch * 64, (ch + 1) * 64)
                    c.op("pe", lambda e, cc=cc, sl=sl: e.transpose(pT_[0:64, cc * 32:(cc + 1) * 32], kl[:, sl], ident[0:32, 0:32]),
                         reads=[kl, ident], writes=[pT_], acc=True)
                c.op("act", lambda e: e.activation(klT[:], pT_[0:64, 0:256], AF.Copy), reads=[pT_], writes=[klT])
                for cc in range(8):
                    ch = grp * 8 + cc
                    c.op("pe", lambda e, cc=cc, ch=ch: e.matmul(pU[0:32, cc * 64:(cc + 1) * 64], klT[:, cc * 32:(cc + 1) * 32], v[:, ch, :], start=True, stop=True),
                         reads=[klT, v], writes=[pU], acc=True)
                c.op("dve", lambda e: e.tensor_copy(U[:, grp * 8:(grp + 1) * 8, :].rearrange("p c t -> p (c t)"), pU[0:32, :]), reads=[pU], writes=[U])
            for e_ in range(64):
                c.op("dve", lambda e, e_=e_: e.tensor_tensor_scan(Sall[:, 1:NCH + 1, e_], dec[:], U[:, :, e_], Sall[:, 0, e_:e_ + 1], ALU.mult, ALU.add),
                     reads=[dec, U, Sall], writes=[Sall])
            for grp in range(NCH // 8):
                pO = PS1[3]
                for cc in range(8):
                    ch = grp * 8 + cc; sl = slice(ch * 64, (ch + 1) * 64)
                    c.op("pe", lambda e, cc=cc, ch=ch: e.matmul(pO[0:64, cc * 64:(cc + 1) * 64], attT[:, ch, :], v[:, ch, :], start=True, stop=False),
                         reads=[attT, v], writes=[pO], acc=True)
                    c.op("pe", lambda e, cc=cc, ch=ch, sl=sl: e.matmul(pO[0:64, cc * 64:(cc + 1) * 64], qe[:, sl], Sall[:, ch, :], start=False, stop=True),
                         reads=[qe, Sall], writes=[pO], acc=True)
                o = osb[grp % 2]
                c.op("act", lambda e: e.activation(o[:].rearrange("p c t -> p (c t)"), pO[0:64, :], AF.Copy), reads=[pO], writes=[o])
                c.dma("pool", XOC.s(pc, grp * 512, (grp + 1) * 512).rearrange("(c p) e -> p c e", p=64), o[:], reads=[o], writes=[XOC.sb], nowaw=True)
            c.op("dve", lambda e: e.tensor_copy(Sall[:, 0, :], Sall[:, NCH, :]), reads=[Sall], writes=[Sall])
        c.pop()
        c.push()
        qT = c.sbuf([64, S], BF16, "qT"); kT = c.sbuf([64, S], BF16, "kT"); V = c.sbuf([128, 128, 65], BF16, "V")
        cmask = c.sbuf([128, 4, 512], BF16, "cmask"); bmask = c.sbuf([128, 1024], BF16, "bmask")
        pTs = [c.sbuf([128, 1024], BF16, "pT%d" % i) for i in range(3)]
        osbs = [c.sbuf([65, 512], F32, "osb%d" % i) for i in range(2)]
        oTs = [c.sbuf([128, 4, 65], F32, "oT%d" % i) for i in range(2)]
        c.dma("sp", cmask[:], cmask_d[:], writes=[cmask]); c.dma("sp", bmask[:], bmask_d[:], writes=[bmask])
        c.op("dve", lambda e: e.memset(V[:, :, 64:65], 1.0), writes=[V])

        def run_unit(groups, Kd, scale, XO):
            steps = []
            for gi, G in enumerate(groups):
                n = len(G["pairs"])
                for pi_, pr in enumerate(G["pairs"]):
                    steps.append((gi, pi_, n, pr))

            def emit_S(i):
                gi, pi_, n, (j0, j1) = steps[i]
                G = groups[gi]; g = G["g"]
                sps = PS2[i % 2]
                for hh, j in enumerate((j0, j1)):
                    if G["extra"] is None:
                        c.op("pe", lambda e, hh=hh, j=j: e.matmul(sps[:, hh * 512:(hh + 1) * 512], kT[0:Kd, j * 128:(j + 1) * 128], qT[0:Kd, g * 512:(g + 1) * 512],
                                                                 start=True, stop=True), reads=[kT, qT], writes=[sps], acc=True)
                    else:
                        zl, biasT, Zb = G["extra"](pi_)
                        c.op("pe", lambda e, hh=hh, j=j: e.matmul(sps[:, hh * 512:(hh + 1) * 512], kT[0:Kd, j * 128:(j + 1) * 128], qT[0:Kd, g * 512:(g + 1) * 512],
                                                                 start=True, stop=False), reads=[kT, qT], writes=[sps], acc=True)
                        c.op("pe", lambda e, hh=hh, zl=zl, biasT=biasT: e.matmul(sps[:, hh * 512:(hh + 1) * 512], zl, biasT[:], start=False, stop=True),
                             reads=[Zb, biasT], writes=[sps], acc=True)

            if groups[0].get("part1"):
                groups[0]["part1"](); groups[0]["part2"]()
            emit_S(0)
            for i, (gi, pi_, n, (j0, j1)) in enumerate(steps):
                G = groups[gi]; g = G["g"]
                sps = PS2[i % 2]; pT = pTs[i % 3]; pO = PS1[g % 2]
                if pi_ == 0 and gi + 1 < len(groups) and groups[gi + 1].get("part1"):
                    groups[gi + 1]["part1"]()
                c.op("act", lambda e: e.activation(pT[:], sps[:], AF.Exp, scale=scale), reads=[sps], writes=[pT])
                if pi_ in G["masks"]:
                    mk_ap, mk_buf = G["masks"][pi_]
                    c.op("dve", lambda e: e.tensor_tensor(pT[:], pT[:], mk_ap, ALU.mult), reads=[pT, mk_buf], writes=[pT])
                if pi_ == n - 1 and gi + 1 < len(groups) and groups[gi + 1].get("part2"):
                    groups[gi + 1]["part2"]()
                if i + 1 < len(steps):
                    emit_S(i + 1)
                for hh, j in enumerate((j0, j1)):
                    c.op("pe", lambda e, hh=hh, j=j: e.matmul(pO[0:65, :], V[:, j, :], pT[:, hh * 512:(hh + 1) * 512],
                                                             start=(pi_ == 0 and hh == 0), stop=(pi_ == n - 1 and hh == 1)),
                         reads=[V, pT], writes=[pO], acc=True)
                if pi_ == n - 1:
                    o = osbs[g % 2]; oT = oTs[g % 2]
                    c.op("dve", lambda e: e.tensor_copy(o[:], pO[0:65, :]), reads=[pO], writes=[o])
                    ptq = PS1[3] if G["extra"] is None else PS1[0 if g % 2 else 1]
                    ptq = PS1[3] if G["extra"] is None else PS1[3]
                    for qi in range(4):
                        c.op("pe", lambda e, qi=qi: e.transpose(ptq[:, qi * 65:(qi + 1) * 65], o[:, qi * 128:(qi + 1) * 128], ident[0:65, 0:65]),
                             reads=[o, ident], writes=[ptq], acc=True)
                    c.op("dve", lambda e: e.tensor_copy(oT[:].rearrange("p q e -> p (q e)"), ptq[:, 0:260]), reads=[ptq], writes=[oT])
                    c.dma("pool", XO.s(g // 4, (g % 4) * 512, (g % 4 + 1) * 512)[:, 0:65].rearrange("(q p) e -> p q e", p=128), oT[:],
                          reads=[oT], writes=[XO.sb], nowaw=True)

        for i in range(8):
            c.dma("sp", qT[0:32, i * 2048:(i + 1) * 2048], (lambda: XQK.r(i, 0, 32)), reads=[XQK.rb], writes=[qT], nowaw=True)
            c.dma("sp", kT[0:32, i * 2048:(i + 1) * 2048], (lambda: XQK.r(i, 32, 64)), reads=[XQK.rb], writes=[kT], nowaw=True)
            c.dma("sp", V[:, i * 16:(i + 1) * 16, 0:64], (lambda: XDV.r(i, 0, 2048).rearrange("(t p) e -> p t e", p=128)), reads=[XDV.rb], writes=[V], nowaw=True)
        cm2 = cmask[:].rearrange("p d q -> p (d q)")
        groups = []
        for g in range(32):
            npair = 2 * (g + 1)
            pairs = [(2 * i, 2 * i + 1) for i in range(npair)]
            masks = {npair - 2: (cm2[:, 0:1024], cmask), npair - 1: (cm2[:, 1024:2048], cmask)}
            groups.append(dict(g=g, pairs=pairs, masks=masks, extra=None))
        run_unit(groups, 32, 32.0 ** -0.5, XOA)
        Zb = c.sbuf([32, 32, 128], BF16, "Zb"); iota = c.sbuf([128, 64], F32, "iota"); par = c.sbuf([128, 2], F32, "par")
        km = c.sbuf([64, 64], F32, "km")
        c.dma("sp", Zb[:], Z_d[:], writes=[Zb]); c.dma("sp", iota[:], iota_d[:], writes=[iota]); c.dma("sp", par[:], par_d[:], writes=[par])
        for i in range(8):
            c.dma("sp", km[:, 8 * i:8 * i + 8], (lambda: XKM.r(i, 0, 64)), reads=[XKM.rb], writes=[km], nowaw=True)
            c.dma("sp", qT[0:64, i * 2048:(i + 1) * 2048], (lambda: XMQ.r(i, 0, 64)), reads=[XMQ.rb], writes=[qT], nowaw=(i > 0))
            c.dma("sp", kT[0:64, i * 1024:(i + 1) * 1024], (lambda: XMK.r(i, 0, 64)), reads=[XMK.rb], writes=[kT], nowaw=(i > 0))
            c.dma("sp", V[:, i * 8:(i + 1) * 8, 0:64], (lambda: XMV.r(i, 0, 1024).rearrange("(t p) e -> p t e", p=128)), reads=[XMV.rb], writes=[V], nowaw=(i > 0))
        q32s = [c.sbuf([64, 512], F32, "q32_%d" % i) for i in range(2)]
        biasTs = [c.sbuf([32, 512], BF16, "biasT%d" % i) for i in range(2)]
        W = {n: [c.sbuf([128, 64], F32, "mw_%s%d" % (n, i)) for i in range(4)] for n in ("lt", "t1", "gm", "sel", "eq")}
        top8s = [c.sbuf([128, 8], F32, "top8_%d" % i) for i in range(4)]
        bps = [c.sbuf([128, 32], F32, "bp%d" % i) for i in range(4)]
        pG = PS1[3]; pB = PS1[2]

        def mk_part1(g):
            def part1():
                q32 = q32s[g % 2]
                c.dma("sp", q32[:], (lambda: XMQ32.r(g // 4, 0, 64)[:, (g % 4) * 512:(g % 4 + 1) * 512]), reads=[XMQ32.rb], writes=[q32])
                for qi in range(4):
                    c.op("pe", lambda e, qi=qi: e.matmul(pG[:, qi * 64:(qi + 1) * 64], q32[:, qi * 128:(qi + 1) * 128], km[:], start=True, stop=True),
                         reads=[q32, km], writes=[pG], acc=True)
                for qi in range(4):
                    qt = 4 * g + qi; own = float(qt // 2)
                    lt, t1, gm, sel, eq, top8, bp = W["lt"][qi], W["t1"][qi], W["gm"][qi], W["sel"][qi], W["eq"][qi], top8s[qi], bps[qi]
                    pGq = pG[:, qi * 64:(qi + 1) * 64]
                    c.op("dve", lambda e: e.tensor_single_scalar(lt[:], iota[:], own, ALU.is_lt), reads=[iota], writes=[lt])
                    c.op("dve", lambda e: e.tensor_scalar(t1[:], lt[:], -1.0, BIG, ALU.add, ALU.mult), reads=[lt], writes=[t1])
                    c.op("dve", lambda e: e.tensor_tensor(gm[:], pGq, lt[:], ALU.mult), reads=[pG, lt], writes=[gm])
                    c.op("dve", lambda e: e.tensor_tensor(gm[:], gm[:], t1[:], ALU.add), reads=[gm, t1], writes=[gm])
                    c.op("dve", lambda e: e.max(top8[:], gm[:]), reads=[gm], writes=[top8])
                    c.op("dve", lambda e: e.tensor_scalar(sel[:], gm[:], top8[:, 2:3], None, ALU.is_ge), reads=[gm, top8], writes=[sel])
                    c.op("dve", lambda e: e.tensor_tensor(sel[:], sel[:], lt[:], ALU.mult), reads=[sel, lt], writes=[sel])
                    c.op("dve", lambda e: e.tensor_single_scalar(eq[:], iota[:], own, ALU.is_equal), reads=[iota], writes=[eq])
                    c.op("dve", lambda e: e.tensor_tensor(sel[:], sel[:], eq[:], ALU.add), reads=[sel, eq], writes=[sel])
                    c.op("dve", lambda e: e.tensor_scalar(sel[:], sel[:], -1.0, BIGB, ALU.add, ALU.mult), reads=[sel], writes=[sel])
                    s3 = sel[:].rearrange("p (m two) -> p m two", two=2)
                    c.op("dve", lambda e: e.tensor_scalar(bp[:], s3[:, :, 0], par[:, 0:1], None, ALU.mult), reads=[sel, par], writes=[bp])
                    c.op("dve", lambda e: e.scalar_tensor_tensor(bp[:], s3[:, :, 1], par[:, 1:2], bp[:], ALU.mult, ALU.add), reads=[sel, par, bp], writes=[bp])
            return part1

        def mk_part2(g):
            def part2():
                biasT = biasTs[g % 2]
                for qi in range(4):
                    c.op("pe", lambda e, qi=qi: e.transpose(pB[0:32, qi * 128:(qi + 1) * 128], bps[qi][:], ident[:]), reads=[bps[qi], ident], writes=[pB], acc=True)
                c.op("act", lambda e: e.activation(biasT[:], pB[0:32, :], AF.Copy), reads=[pB], writes=[biasT])
            return part2

        groups = []
        for g in range(32):
            pairs = [(2 * m, 2 * m + 1) for m in range(g + 1)]
            masks = {g: (bmask[:], bmask)}
            groups.append(dict(g=g, pairs=pairs, masks=masks, extra=(lambda m, g=g: (Zb[:, m, :], biasTs[g % 2], Zb)), part1=mk_part1(g), part2=mk_part2(g)))
        run_unit(groups, 64, 64.0 ** -0.5, XOB)
        c.pop()
        c.pop()

    def partC(l, xsrc_t, xsrc_b, dst_t, dst_b):
        P = L[l]
        c.push()
        PS = [c.psum([128, 512], F32, "cpsb%d_%d" % (l, i)) for i in range(8)]
        g2b = c.sbuf([128, D], F32, "g2b")
        h2T = c.sbuf([128, 8, T], BF16, "h2T"); h2Tt = [Buf(None, "h2Tt%d" % t) for t in range(NT)]
        Gall = c.sbuf([128, NT, NE], F32, "Gall"); Gt = [Buf(None, "Gt%d" % t) for t in range(NT)]
        GT = c.sbuf([32, T], F32, "GT"); GTt = [Buf(None, "GTt%d" % t) for t in range(NT)]
        c.push()
        cb = c.sbuf([128, 8, 128], F32, "cb")
        c.dma("sp", cb[:], cTb_d[:], writes=[cb])
        c.op("act", lambda e: e.activation(cb[:], cb[:], AF.Silu), reads=[cb], writes=[cb])
        adaw_v = P["adaw"].t.rearrange("(k p) n -> p k n", p=128)
        wst = [c.sbuf([128, 8, 512], F32, "adst%d" % i) for i in range(2)]
        g1b = c.sbuf([128, D], F32, "g1b"); abB = c.sbuf([128, 2, D], F32, "abB")
        c.dma("sp", abB[:], P["adabB"][:], writes=[abB])
        si = 0
        for gi, (dst, col0) in enumerate(((g1b, 2048), (g2b, 5120))):
            for hf in range(2):
                st = wst[si % 2]; si += 1
                c.dma("sp", st[:], adaw_v[:, :, col0 + hf * 512:col0 + (hf + 1) * 512], writes=[st])
                ps = PS[hf]
                for k in range(8):
                    c.op("pe", lambda e, k=k, st=st, ps=ps: e.matmul(ps[:], cb[:, k, :], st[:, k, :], start=(k == 0), stop=(k == 7)), reads=[cb, st], writes=[ps], acc=True)
                c.op("dve", lambda e, ps=ps, dst=dst, gi=gi, hf=hf: e.tensor_tensor(dst[:, hf * 512:(hf + 1) * 512], ps[:], abB[:, gi, hf * 512:(hf + 1) * 512], ALU.add),
                     reads=[ps, abB], writes=[dst])
        modps = PS[2]
        adab2 = c.sbuf([128, 16], F32, "adab2"); n2g = c.sbuf([128, 8], F32, "n2g")
        c.dma("sp", adab2[:], P["adabT2"][:], writes=[adab2]); c.dma("sp", n2g[:], P["n2gT"][:], writes=[n2g])
        for jj in range(4):
            st = wst[si % 2]; si += 1
            c.dma("sp", st[:], adaw_v[:, :, 3072 + jj * 512:3072 + (jj + 1) * 512], writes=[st])
            for j4 in range(4):
                j = jj * 4 + j4
                for k in range(8):
                    c.op("pe", lambda e, k=k, j=j, j4=j4, st=st: e.matmul(modps[:, j:j + 1], st[:, k, j4 * 128:(j4 + 1) * 128], cb[:, k, 0:1],
                                                                      start=(k == 0), stop=(k == 7)), reads=[st, cb], writes=[modps], acc=True)
        mod2 = c.sbuf([128, 16], F32, "mod2"); a2 = c.sbuf([128, 8], F32, "a2")
        c.op("dve", lambda e: e.tensor_tensor(mod2[:], modps[:, 0:16], adab2[:], ALU.add), reads=[modps, adab2], writes=[mod2])
        c.op("dve", lambda e: e.scalar_tensor_tensor(a2[:], mod2[:, 8:16], 1.0, n2g[:], ALU.add, ALU.mult), reads=[mod2, n2g], writes=[a2])
        lv = c.sbuf([128, 4, 32], F32, "lv"); lsm = c.sbuf([128, 4], F32, "lsm"); nlam = c.sbuf([128, 1], F32, "nlam")
        c.dma("sp", lv[:], P["lamv"][:], writes=[lv])
        c.op("dve", lambda e: e.tensor_tensor(lv[:, 0, :], lv[:, 0, :], lv[:, 1, :], ALU.mult), reads=[lv], writes=[lv])
        c.op("dve", lambda e: e.tensor_tensor(lv[:, 2, :], lv[:, 2, :], lv[:, 3, :], ALU.mult), reads=[lv], writes=[lv])
        c.op("dve", lambda e: e.tensor_reduce(lsm[:, 0:1], lv[:, 0, :], AX.X, ALU.add), reads=[lv], writes=[lsm])
        c.op("dve", lambda e: e.tensor_reduce(lsm[:, 1:2], lv[:, 2, :], AX.X, ALU.add), reads=[lv], writes=[lsm])
        c.op("act", lambda e: e.activation(lsm[:, 2:4], lsm[:, 0:2], AF.Exp), reads=[lsm], writes=[lsm])
        c.op("dve", lambda e: e.tensor_tensor(nlam[:], lsm[:, 3:4], lsm[:, 2:3], ALU.subtract), reads=[lsm], writes=[nlam])
        lamc = c.sbuf([128, 2], F32, "lamc")
        c.dma("sp", lamc[:], P["lamc"][:], writes=[lamc])
        c.op("dve", lambda e: e.tensor_scalar(nlam[:], nlam[:], lamc[:, 0:1], None, ALU.subtract), reads=[nlam, lamc], writes=[nlam])
        aog = c.sbuf([128, 64], F32, "aog"); cog = c.sbuf([128, 64], F32, "cog")
        c.dma("sp", aog[:], P["aog"][:], writes=[aog]); c.dma("sp", cog[:], P["cog"][:], writes=[cog])
        c.op("dve", lambda e: e.tensor_scalar(aog[:], aog[:], lamc[:, 1:2], None, ALU.mult), reads=[aog, lamc], writes=[aog])
        wob = c.sbuf([128, 8, D], BF16, "wob"); wobk = [Buf(None, "wobk%d" % k) for k in range(8)]
        wo_v = P["w_out"].t.rearrange("(k p) n -> p k n", p=128)
        wos = [c.sbuf([128, D], F32, "wos%d" % i) for i in range(2)]
        for k in range(8):
            st = wos[k % 2]
            c.dma("sp", st[:], wo_v[:, k, :], writes=[st])
            c.op("pool", lambda e, k=k, st=st: e.tensor_copy(wob[:, k, :], st[:]), reads=[st], writes=[wobk[k]])
        rw = c.sbuf([128, 8, 32], F32, "rwr"); rb = c.sbuf([128, 32], F32, "rb")
        c.dma("sp", rw[:], P["rwT"][:], writes=[rw]); c.dma("sp", rb[:], P["rbB"][:], writes=[rb])
        oas = [c.sbuf([128, 8, 65], F32, "oas%d" % i) for i in range(2)]; obs = [c.sbuf([128, 8, 65], F32, "obs%d" % i) for i in range(2)]
        ocs = [c.sbuf([128, 256], F32, "ocs%d" % i) for i in range(2)]; crs = [c.sbuf([128, 256], F32, "crs%d" % i) for i in range(2)]
        ys = [c.sbuf([128, D], F32, "ys%d" % i) for i in range(2)]; xs = [c.sbuf([128, D], F32, "xs%d" % i) for i in range(2)]
        rs8 = c.sbuf([128, 8], F32, "rs8"); on = c.sbuf([128, 8, 64], F32, "on"); od_ = c.sbuf([128, 256], F32, "od_"); sq = c.sbuf([128, 256], F32, "sq")
        st4 = c.sbuf([128, 4], F32, "st4"); osum = c.sbuf([128, 4, 65], F32, "osum")
        yT = [c.sbuf([128, 8, 128], BF16, "yT%d" % i) for i in range(2)]
        junk = c.sbuf([128, D], BF16, "junk"); stat = c.sbuf([128, 2], F32, "stat")
        h32 = [c.sbuf([128, 8, 128], F32, "h32_%d" % i) for i in range(2)]
        lg = c.sbuf([128, 32], F32, "lg"); top8 = c.sbuf([128, 8], F32, "top8"); msk = c.sbuf([128, 32], F32, "msk"); ex = c.sbuf([128, 32], F32, "ex")
        sm = c.sbuf([128, 2], F32, "sm")
        for t in range(NT):
            ts_ = slice(t * 128, (t + 1) * 128)
            oa_ = oas[t % 2]; ob_ = obs[t % 2]; oc_ = ocs[t % 2]; cr_ = crs[t % 2]; y = ys[t % 2]; xt = xs[t % 2]
            for m_ in range(8):
                c.dma("sp", oa_[:, m_, :], (lambda: XOA.r(m_, t * 128, (t + 1) * 128)[:, 0:65]), reads=[XOA.rb], writes=[oa_], nowaw=(m_ > 0))
                c.dma("sp", ob_[:, m_, :], (lambda: XOB.r(m_, t * 128, (t + 1) * 128)[:, 0:65]), reads=[XOB.rb], writes=[ob_], nowaw=(m_ > 0))
            for hc in range(4):
                c.dma("sp", oc_[:, hc * 64:(hc + 1) * 64], (lambda: XOC.r(hc, t * 128, (t + 1) * 128)), reads=[XOC.rb], writes=[oc_], nowaw=(hc > 0))
                c.dma("sp", y[:, 768 + hc * 64:768 + (hc + 1) * 64], (lambda: XOD.r(hc, t * 128, (t + 1) * 128)), reads=[XOD.rb], writes=[y], nowaw=(hc > 0))
            c.dma("sp", cr_[:], crb_t[ts_, :], reads=[crbt[t]], writes=[cr_])
            c.dma("sp", xt[:], xsrc_t[ts_, :], reads=[xsrc_b[t]], writes=[xt])
            c.op("dve", lambda e: e.reciprocal(rs8[:], oa_[:, :, 64]), reads=[oa_], writes=[rs8])
            for m in range(8):
                c.op("dve", lambda e, m=m: e.tensor_scalar(on[:, m, :], oa_[:, m, 0:64], rs8[:, m:m + 1], None, ALU.mult), reads=[oa_, rs8], writes=[on])
            on4 = on[:].rearrange("p (h i) d -> p h i d", i=2)
            od3 = od_[:].rearrange("p (h d) -> p h d", d=64)
            c.op("dve", lambda e: e.scalar_tensor_tensor(od3, on4[:, :, 1, :], nlam[:, 0:1], on4[:, :, 0, :], ALU.mult, ALU.add), reads=[on, nlam], writes=[od_])
            c.op("dve", lambda e: e.tensor_tensor(sq[:], od_[:], od_[:], ALU.mult), reads=[od_], writes=[sq])
            c.op("dve", lambda e: e.tensor_reduce(st4[:], sq[:].rearrange("p (h d) -> p h d", d=64), AX.X, ALU.add), reads=[sq], writes=[st4])
            c.op("act", lambda e: e.activation(st4[:], st4[:], AF.Sqrt, bias=epsT[:], scale=1.0 / 64), reads=[st4, epsT], writes=[st4])
            c.op("dve", lambda e: e.reciprocal(st4[:], st4[:]), reads=[st4], writes=[st4])
            for h in range(4):
                c.op("dve", lambda e, h=h: e.scalar_tensor_tensor(y[:, h * 64:(h + 1) * 64], od_[:, h * 64:(h + 1) * 64], st4[:, h:h + 1], aog[:], ALU.mult, ALU.mult),
                     reads=[od_, st4, aog], writes=[y])
            ob4 = ob_[:].rearrange("p (h i) d -> p h i d", i=2)
            c.op("dve", lambda e: e.tensor_tensor(osum[:], ob4[:, :, 0, :], ob4[:, :, 1, :], ALU.add), reads=[ob_], writes=[osum])
            c.op("dve", lambda e: e.reciprocal(rs8[:, 0:4], osum[:, :, 64]), reads=[osum], writes=[rs8])
            for h in range(4):
                c.op("dve", lambda e, h=h: e.tensor_scalar(y[:, 256 + h * 64:256 + (h + 1) * 64], osum[:, h, 0:64], rs8[:, h:h + 1], None, ALU.mult), reads=[osum, rs8], writes=[y])
            c.op("dve", lambda e: e.tensor_tensor(sq[:], oc_[:], oc_[:], ALU.mult), reads=[oc_], writes=[sq])
            c.op("dve", lambda e: e.tensor_reduce(st4[:], sq[:].rearrange("p (h d) -> p h d", d=64), AX.X, ALU.add), reads=[sq], writes=[st4])
            c.op("act", lambda e: e.activation(st4[:], st4[:], AF.Sqrt, bias=epsT[:], scale=1.0 / 64), reads=[st4, epsT], writes=[st4])
            c.op("dve", lambda e: e.reciprocal(st4[:], st4[:]), reads=[st4], writes=[st4])
            c.op("act", lambda e: e.activation(cr_[:], cr_[:], AF.Silu), reads=[cr_], writes=[cr_])
            for h in range(4):
                c.op("dve", lambda e, h=h: e.scalar_tensor_tensor(y[:, 512 + h * 64:512 + (h + 1) * 64], oc_[:, h * 64:(h + 1) * 64], st4[:, h:h + 1], cog[:], ALU.mult, ALU.mult),
                     reads=[oc_, st4, cog], writes=[y])
            c.op("dve", lambda e: e.tensor_tensor(y[:, 512:768], y[:, 512:768], cr_[:], ALU.mult), reads=[y, cr_], writes=[y])
            yt = yT[t % 2]
            for half in range(2):
                ps = PS[2 + half]
                for kk in range(4):
                    k = half * 4 + kk
                    c.op("pe", lambda e, k=k, kk=kk, ps=ps: e.transpose(ps[:, kk * 128:(kk + 1) * 128], y[:, k * 128:(k + 1) * 128], ident[:]), reads=[y, ident], writes=[ps], acc=True)
                c.op("act", lambda e, ps=ps, half=half: e.activation(yt[:, half * 4:(half + 1) * 4, :].rearrange("p k t -> p (k t)"), ps[:], AF.Copy), reads=[ps], writes=[yt])
            for hf in range(2):
                ps = PS[4 + hf]
                for k in range(8):
                    c.op("pe", lambda e, k=k, ps=ps, hf=hf: e.matmul(ps[:], yt[:, k, :], wob[:, k, hf * 512:(hf + 1) * 512], start=(k == 0), stop=(k == 7)),
                         reads=[yt, wobk[k]], writes=[ps], acc=True)
                c.op("dve", lambda e, ps=ps, hf=hf: e.tensor_tensor(y[:, hf * 512:(hf + 1) * 512], ps[:], g1b[:, hf * 512:(hf + 1) * 512], ALU.mult), reads=[ps, g1b], writes=[y])
            c.op("dve", lambda e: e.tensor_tensor(xt[:], xt[:], y[:], ALU.add), reads=[xt, y], writes=[xt])
            c.dma("pool", xbuf_t[ts_, :], xt[:], reads=[xt], writes=[xbt[t]])
            c.op("act", lambda e: e.activation(junk[:], xt[:], AF.Square, accum_out=stat[:, 0:1]), reads=[xt], writes=[junk, stat])
            c.op("act", lambda e: e.activation(stat[:, 1:2], stat[:, 0:1], AF.Sqrt, bias=epsT[:], scale=1.0 / D), reads=[stat, epsT], writes=[stat])
            c.op("dve", lambda e: e.reciprocal(stat[:, 1:2], stat[:, 1:2]), reads=[stat], writes=[stat])
            c.op("dve", lambda e: e.tensor_scalar(y[:], xt[:], stat[:, 1:2], None, ALU.mult), reads=[xt, stat], writes=[y])
            hh = h32[t % 2]
            for half in range(2):
                ps = PS[6 + half]
                for kk in range(4):
                    k = half * 4 + kk
                    c.op("pe", lambda e, k=k, kk=kk, ps=ps: e.transpose(ps[:, kk * 128:(kk + 1) * 128], y[:, k * 128:(k + 1) * 128], ident[:]), reads=[y, ident], writes=[ps], acc=True)
                for kk in range(4):
                    k = half * 4 + kk
                    c.op("dve", lambda e, k=k, kk=kk, ps=ps: e.tensor_scalar(hh[:, k, :], ps[:, kk * 128:(kk + 1) * 128], a2[:, k:k + 1], mod2[:, k:k + 1], ALU.mult, ALU.add),
                         reads=[ps, a2, mod2], writes=[hh])
            c.op("act", lambda e: e.activation(h2T[:, :, ts_], hh[:], AF.Copy), reads=[hh], writes=[h2Tt[t]])
            psr = PS[0]
            for k in range(8):
                c.op("pe", lambda e, k=k: e.matmul(psr[:, 0:32], hh[:, k, :], rw[:, k, :], start=(k == 0), stop=(k == 7)), reads=[hh, rw], writes=[psr], acc=True)
            c.op("dve", lambda e: e.tensor_tensor(lg[:], psr[:, 0:32], rb[:], ALU.add), reads=[psr, rb], writes=[lg])
            c.op("dve", lambda e: e.max(top8[:], lg[:]), reads=[lg], writes=[top8])
            c.op("dve", lambda e: e.tensor_scalar(msk[:], lg[:], top8[:, 3:4], None, ALU.is_ge), reads=[lg, top8], writes=[msk])
            c.op("dve", lambda e: e.tensor_scalar(sm[:, 0:1], top8[:, 0:1], -1.0, None, ALU.mult), reads=[top8], writes=[sm])
            c.op("act", lambda e: e.activation(ex[:], lg[:], AF.Exp, bias=sm[:, 0:1], scale=1.0), reads=[lg, sm], writes=[ex])
            c.op("dve", lambda e: e.tensor_tensor(ex[:], ex[:], msk[:], ALU.mult), reads=[ex, msk], writes=[ex])
            c.op("dve", lambda e: e.tensor_reduce(sm[:, 1:2], ex[:], AX.X, ALU.add), reads=[ex], writes=[sm])
            c.op("dve", lambda e: e.reciprocal(sm[:, 1:2], sm[:, 1:2]), reads=[sm], writes=[sm])
            c.op("dve", lambda e: e.tensor_scalar(Gall[:, t, :], ex[:], sm[:, 1:2], None, ALU.mult), reads=[ex, sm], writes=[Gt[t]])
            psg = PS[1]
            c.op("pe", lambda e: e.transpose(psg[0:32, 0:128], Gall[:, t, :], ident[:]), reads=[Gt[t], ident], writes=[psg])
            c.op("act", lambda e: e.activation(GT[:, ts_], psg[0:32, 0:128], AF.Copy), reads=[psg], writes=[GTt[t]])
        c.pop()
        c.push()
        TP = 1024; NTP = TP // 128; NTG = TP // 512
        bup = c.sbuf([128, NE, 16], F32, "bup")
        c.dma("sp", bup[:], P["bupT"][:], writes=[bup])
        Gsc = c.sbuf([128, NT, NE], F32, "Gsc")
        c.op("dve", lambda e: e.tensor_scalar(Gsc[:], Gall[:], 1.0 / 1.702, None, ALU.mult), reads=Gt, writes=[Gsc])
        acc = c.sbuf([128, NTP, D], F32, "acc"); acct = [Buf(None, "acct%d" % t) for t in range(NTP)]
        actT = c.sbuf([128, 8, TP], BF16, "actT"); actb = [[Buf(None, "act_%d_%d" % (j, tg)) for tg in range(NTG)] for j in range(8)]
        wus = [c.sbuf([128, 8, 256], F32, "wus%d" % i) for i in range(3)]; wub = [c.sbuf([128, 8, 256], BF16, "wub%d" % i) for i in range(4)]
        wds = [c.sbuf([128, 8, 512], F32, "wds%d" % i) for i in range(2)]; wdb = [c.sbuf([128, 8, 512], BF16, "wdb%d" % i) for i in range(2)]
        gs = [c.sbuf([128, 512], F32, "gs%d" % i) for i in range(2)]; sg = [c.sbuf([128, 512], F32, "sg%d" % i) for i in range(2)]
        ls = [c.sbuf([128, 512], F32, "ls%d" % i) for i in range(2)]
        bds = c.sbuf([32, D], F32, "bds")
        c.dma("sp", bds[:], P["bdn"][:], writes=[bds])
        fin = c.sbuf([128, D], F32, "fin")
        wup_v = P["wup"].t.rearrange("e (k p) n -> e p k n", p=128)
        wdn_v = P["wdn"].t.rearrange("e (j p) n -> e p j n", p=128)
        slices = [(tp, e_, j) for tp in range(T // TP) for e_ in range(n_exp) for j in range(8)]

        def load_slice(i):
            tp, e_, j = slices[i]
            st = wus[i % 3]; wb_ = wub[i % 4]
            c.dma("sp", st[:, :, 0:128], wup_v[e_, :, :, j * 128:(j + 1) * 128], writes=[st])
            c.dma("sp", st[:, :, 128:256], wup_v[e_, :, :, D + j * 128:D + (j + 1) * 128], writes=[st], nowaw=True)
            c.op("pool", lambda e: e.tensor_copy(wb_[:], st[:]), reads=[st], writes=[wb_])

        def load_down(e_):
            for c2 in range(2):
                st = wds[c2]; wd_ = wdb[c2]
                c.dma("sp", st[:], wdn_v[e_, :, :, c2 * 512:(c2 + 1) * 512], writes=[st])
                c.op("pool", lambda e, st=st, wd_=wd_: e.tensor_copy(wd_[:], st[:]), reads=[st], writes=[wd_])

        PF = 2
        for i in range(min(PF, len(slices))):
            load_slice(i)
        ie = 0
        for i, (tp, e_, j) in enumerate(slices):
            t0 = tp * NTP
            if i + PF < len(slices):
                load_slice(i + PF)
            if j == 1:
                load_down(e_)
            wb_ = wub[i % 4]
            for tg in range(NTG):
                tsl = slice(tp * TP + tg * 512, tp * TP + (tg + 1) * 512)
                pg = PS[(ie * 2) % 4]; pl = PS[(ie * 2 + 1) % 4]; g_ = gs[ie % 2]; s_ = sg[ie % 2]; l_ = ls[ie % 2]; ie += 1
                hrd = h2Tt[tsl.start // 128:tsl.stop // 128]
                for k in range(8):
                    c.op("pe", lambda e, k=k: e.matmul(pg[:], wb_[:, k, 0:128], h2T[:, k, tsl], start=(k == 0), stop=(k == 7)), reads=[wb_] + hrd, writes=[pg], acc=True)
                for k in range(8):
                    c.op("pe", lambda e, k=k: e.matmul(pl[:], wb_[:, k, 128:256], h2T[:, k, tsl], start=(k == 0), stop=(k == 7)), reads=[wb_] + hrd, writes=[pl], acc=True)
                c.op("dve", lambda e: e.tensor_scalar(g_[:], pg[:], bup[:, e_, j:j + 1], 7.0, ALU.add, ALU.min), reads=[pg, bup], writes=[g_])
                c.op("act", lambda e: e.activation(l_[:], pl[:], AF.Identity, bias=bup[:, e_, 8 + j:9 + j], scale=1.0), reads=[pl, bup], writes=[l_])
                c.op("act", lambda e: e.activation(s_[:], g_[:], AF.Silu, scale=1.702), reads=[g_], writes=[s_])
                c.op("pool", lambda e: e.tensor_scalar(l_[:], l_[:], 7.0, -7.0, ALU.min, ALU.max), reads=[l_], writes=[l_])
                c.op("dve", lambda e: e.scalar_tensor_tensor(actT[:, j, tg * 512:(tg + 1) * 512], l_[:], 1.0, s_[:], ALU.add, ALU.mult), reads=[l_, s_], writes=[actb[j][tg]])
            if j == 7:
                for c2 in range(2):
                    wd_ = wdb[c2]
                    for tt in range(NTP):
                        po = PS[4 + (tt % 4)]
                        for jj in range(8):
                            c.op("pe", lambda e, jj=jj: e.matmul(po[:], actT[:, jj, tt * 128:(tt + 1) * 128], wd_[:, jj, :], start=(jj == 0), stop=(jj == 7)),
                                 reads=[actb[jj][tt // 4], wd_], writes=[po], acc=True)
                        dst = acc[:, tt, c2 * 512:(c2 + 1) * 512]
                        gsc = Gsc[:, t0 + tt, e_:e_ + 1]
                        if e_ == 0:
                            c.op("dve", lambda e: e.tensor_scalar(dst, po[:], gsc, None, ALU.mult), reads=[po, Gsc], writes=[acct[tt]])
                        else:
                            c.op("dve", lambda e: e.scalar_tensor_tensor(dst, po[:], gsc, dst, ALU.mult, ALU.add), reads=[po, Gsc, acct[tt]], writes=[acct[tt]])
                if e_ == n_exp - 1:
                    for tt in range(NTP):
                        t = t0 + tt; ts_ = slice(t * 128, (t + 1) * 128)
                        f = fin
                        x1b = wds[0]; x1 = x1b[:].rearrange("p j c -> p (j c)")[:, 0:D]
                        c.dma("sp", x1, xbuf_t[ts_, :], reads=[xbt[t]], writes=[x1b])
                        for hf in range(2):
                            pb = PS[hf]
                            c.op("pe", lambda e, pb=pb, hf=hf: e.matmul(pb[:], GT[:, ts_], bds[:, hf * 512:(hf + 1) * 512], start=True, stop=True), reads=[GTt[t], bds], writes=[pb])
                            c.op("dve", lambda e, pb=pb, hf=hf: e.tensor_tensor(f[:, hf * 512:(hf + 1) * 512], pb[:], acc[:, tt, hf * 512:(hf + 1) * 512], ALU.add), reads=[pb, acct[tt]], writes=[f])
                        c.op("dve", lambda e: e.tensor_tensor(f[:], f[:], g2b[:], ALU.mult), reads=[f, g2b], writes=[f])
                        c.op("dve", lambda e: e.tensor_tensor(f[:], f[:], x1, ALU.add), reads=[f, x1b], writes=[f])
                        c.dma("pool", dst_t[ts_, :], f[:], reads=[f], writes=[dst_b[t]])
        c.pop()
        c.pop()

    for l in range(depth):
        if l == 0:
            xs_t, xs_b = x_in.t, xint
        else:
            xs_t, xs_b = xbuf_t, xbt
        partA(l, xs_t, xs_b)
        for X in XA:
            X.gather()
        for X in XA:
            X.localize("sp", pid_sp)
        partB(l)
        for X in XBs:
            X.gather()
        for X in XBs:
            X.localize("pool", pid_pool)
        last = (l == depth - 1)
        partC(l, xs_t, xs_b, out.t if last else xbuf_t, outt if last else xbt)
    c.finish(outt, "pool")
    c.finish(outt, "sp")
    c.close()
    return c

BF = ml_dtypes.bfloat16


def pp(v):
    v = np.asarray(v, np.float32).reshape(-1, 128)
    return np.ascontiguousarray(v.T)


def rep128(v):
    v = np.asarray(v, np.float32)
    return np.ascontiguousarray(np.broadcast_to(v[None], (128,) + v.shape))


def prepF(inputs):
    i128 = np.arange(128)
    glob = dict(
        cT=pp(inputs["c"][0]), cTb=np.ascontiguousarray(np.broadcast_to(pp(inputs["c"][0])[:, :, None], (128, 8, 128))),
        ident=np.eye(128, dtype=np.float32),
        bd32=(i128[:, None] // 32 == i128[None, :] // 32).astype(np.float32),
        bd64=(i128[:, None] // 64 == i128[None, :] // 64).astype(np.float32))
    k = np.arange(128)[:, None]; q = np.arange(512)[None, :]
    glob["cmask"] = np.stack([(128 * d + k <= q) for d in range(4)], axis=1).astype(BF)
    Z = np.zeros((32, 32, 128), BF)
    for m in range(32):
        Z[m, m, :] = 1
    glob["Z"] = Z
    glob["iota"] = np.tile(np.arange(64, dtype=np.float32)[None, :], (128, 1))
    rmask = np.ones((32, 2048), np.float32); rmask[:, ::64] = 0
    glob["rmask"] = rmask
    j = np.arange(64)[:, None]; i = np.arange(64)[None, :]
    glob["tri8"] = np.tile((j <= i).astype(np.float32), (1, 8))
    per_layer = []
    for l in range(2):
        sfx = str(l)
        gains = np.zeros((128, 8), np.float32)
        gains[:, 0] = np.tile(inputs["a_q_gain"][l], 4); gains[:, 1] = np.tile(inputs["a_k_gain"][l], 4)
        gains[:, 2] = np.tile(inputs["b_q_gain"][l], 2); gains[:, 3] = np.tile(inputs["b_k_gain"][l], 2)
        gains[:, 4] = inputs["c_b_g"][l]
        ab = inputs["ada_b"][l]
        lam_init = 0.8 - 0.6 * math.exp(-0.3 * l)
        d = {
            "adaw" + sfx: np.ascontiguousarray(inputs["ada_w"][l]), "adabT" + sfx: pp(ab[:2048]), "n1gT" + sfx: pp(inputs["norm1_g"][l]),
            "w_in" + sfx: np.ascontiguousarray(inputs["w_in"][l]), "gains" + sfx: gains, "wg2" + sfx: np.ascontiguousarray(inputs["c_w_g2"][l]),
            "adabB" + sfx: rep128(np.stack([ab[2048:3072], ab[5120:6144]])), "adabT2" + sfx: pp(ab[3072:5120]),
            "lamv" + sfx: rep128(np.stack([inputs["a_lam_q1"][l], inputs["a_lam_k1"][l], inputs["a_lam_q2"][l], inputs["a_lam_k2"][l]])),
            "lamc" + sfx: np.tile(np.array([[lam_init, 1.0 - lam_init]], np.float32), (128, 1)),
            "aog" + sfx: rep128(inputs["a_out_gain"][l]), "cog" + sfx: rep128(inputs["c_out_gain"][l]), "n2gT" + sfx: pp(inputs["norm2_g"][l]),
            "w_out" + sfx: np.ascontiguousarray(inputs["w_out"][l]),
            "rwT" + sfx: np.ascontiguousarray(inputs["router_w"][l].reshape(8, 128, 32).transpose(1, 0, 2)), "rbB" + sfx: rep128(inputs["router_b"][l]),
            "wup" + sfx: np.ascontiguousarray(inputs["exp_w_up"][l]),
            "bupT" + sfx: np.ascontiguousarray(inputs["exp_b_up"][l].reshape(32, 16, 128).transpose(2, 0, 1)),
            "wdn" + sfx: np.ascontiguousarray(inputs["exp_w_down"][l]), "bdn" + sfx: np.ascontiguousarray(inputs["exp_b_down"][l])}
        per_layer.append(d)
    maps = []
    x = inputs["x"][0]
    for r in range(8):
        p = r % 2; nb = r % 4
        m = dict(glob)
        for d in per_layer:
            m.update(d)
        m["x"] = np.ascontiguousarray(x[r * 2048:(r + 1) * 2048])
        m["par"] = np.tile(np.array([[1.0 - p, float(p)]], np.float32), (128, 1))
        m["bmask"] = np.concatenate([(256 * p + 128 * hh + k <= q) for hh in range(2)], axis=1).astype(BF)
        for l in range(2):
            sfx = str(l)
            rw = np.zeros((64, 8), np.float32)
            sl = slice(64 * nb, 64 * nb + 64)
            rw[:, 0:4] = inputs["d_conv_w"][l][:, sl].T; rw[:, 4] = inputs["d_conv_b"][l][sl]; rw[:, 5] = inputs["d_b_a"][l][sl]
            rw[:, 6] = inputs["d_b_x"][l][sl]; rw[:, 7] = inputs["d_lambda"][l][sl]
            m["rw" + sfx] = rw
            m["rwa" + sfx] = np.ascontiguousarray(inputs["d_w_a"][l][nb]); m["rwx" + sfx] = np.ascontiguousarray(inputs["d_w_x"][l][nb])
        maps.append(m)
    return maps


def kernel(**inputs):
    inputs = {k: np.asarray(v) for k, v in inputs.items()}
    cF = build_F()
    maps = prepF(inputs)
    R = run_bass_kernel_spmd(cF.nc, maps, core_ids=list(range(8))).results
    x = np.concatenate([np.asarray(r["out"]) for r in R], axis=0)
    return np.ascontiguousarray(x[None]).astype(np.float32)
```

```python
import contextlib, math
import numpy as np
import ml_dtypes
import concourse.bass as bass
import concourse.mybir as mybir
from concourse.bass_utils import run_bass_kernel_spmd


F32 = mybir.dt.float32
BF16 = mybir.dt.bfloat16
I32 = mybir.dt.int32
AF = mybir.ActivationFunctionType
ALU = mybir.AluOpType
AX = mybir.AxisListType


class Buf:
    def __init__(self, t, name):
        self.t = t
        self.name = name
        self.w = {}
        self.r = {}

    def __getitem__(self, idx):
        return self.t[idx]


class Ctx:
    NDS = 8

    def __init__(self):
        self.nc = bass.Bass("TRN2", target_bir_lowering=False)
        nc = self.nc
        self.es = contextlib.ExitStack()
        self.es_root = self.es
        self.E = {"pe": nc.tensor, "act": nc.scalar, "dve": nc.vector, "pool": nc.gpsimd, "sp": nc.sync}
        self.sems = {}
        self.cnt = {}
        for e in ("pe", "act", "dve", "pool"):
            self.sems[e] = self.es.enter_context(nc.semaphore("s_" + e))
            self.cnt[e] = 0
        self.dq = {}
        for q in ("sp", "pool", "act"):
            ss = []
            for i in range(self.NDS):
                k = "d_%s%d" % (q, i)
                self.sems[k] = self.es.enter_context(nc.semaphore(k))
                ss.append(k)
            self.dq[q] = [ss, 0]
        self.seen = {e: {} for e in self.E}
        self.ekey = {}
        self.eno = {}
        self.nbuf = 0
        self.ninstr = 0

    def sbuf(self, shape, dt, name=None):
        self.nbuf += 1
        name = "sb%d_%s" % (self.nbuf, name or "x")
        t = self.es.enter_context(self.nc.sbuf_tensor(name, list(shape), dt))
        return Buf(t, name)

    def psum(self, shape, dt, name=None):
        self.nbuf += 1
        name = "ps%d_%s" % (self.nbuf, name or "x")
        t = self.es.enter_context(self.nc.psum_tensor(name, list(shape), dt))
        return Buf(t, name)

    def dram(self, name, shape, dt, kind):
        t = self.nc.dram_tensor(name, list(shape), dt, kind=kind).ap()
        return Buf(t, name)

    def _wait(self, eng, tok):
        if tok is None:
            return
        k, v = tok
        if self.seen[eng].get(k, 0) >= v:
            return
        self.E[eng].wait_ge(self.sems[k], v)
        self.seen[eng][k] = v
        self.ninstr += 1

    def _deps(self, eng, reads, writes, acc=False, nowaw=False):
        for b in reads:
            for k, v in b.w.items():
                self._wait(eng, (k, v))
        for b in writes:
            if not (nowaw or (acc and eng == "pe" and all(k.split("#")[0] == "pe" for k in b.w))):
                for k, v in b.w.items():
                    self._wait(eng, (k, v))
            for k, v in b.r.items():
                self._wait(eng, (k, v))

    def _mark(self, tok, reads, writes, nowaw=False):
        k, v = tok
        for b in reads:
            if b.r.get(k, 0) < v:
                b.r[k] = v
        for b in writes:
            if nowaw:
                b.w[k] = max(b.w.get(k, 0), v)
            else:
                b.w = {k: v}
                b.r = {}

    EPOCH = 24000

    def op(self, eng, fn, reads=(), writes=(), acc=False):
        key = self.ekey.get(eng, eng)
        if self.cnt[key] >= self.EPOCH:
            n = self.eno.get(eng, 0) + 1
            self.eno[eng] = n
            key = "%s#%d" % (eng, n)
            self.sems[key] = self.es_root.enter_context(self.nc.semaphore("s_%s_%d" % (eng, n)))
            self.cnt[key] = 0
            self.ekey[eng] = key
        self._deps(eng, reads, writes, acc)
        ins = fn(self.E[eng])
        self.cnt[key] += 1
        ins.then_inc(self.sems[key], 1)
        self.ninstr += 1
        self._mark((key, self.cnt[key]), reads, writes)
        return ins

    def dma(self, q, out, in_, reads=(), writes=(), nowaw=False, **kw):
        ss, j = self.dq[q]
        k = ss[j % self.NDS]
        rnd = j // self.NDS
        if rnd > 0:
            self._wait(q, (k, 16 * rnd))
        self._deps(q, reads, writes, nowaw=nowaw)
        if callable(in_):
            in_ = in_()
        if callable(out):
            out = out()
        ins = self.E[q].dma_start(out=out, in_=in_, **kw)
        ins.then_inc(self.sems[k], 16)
        self.dq[q][1] = j + 1
        self.ninstr += 1
        self._mark((k, 16 * (rnd + 1)), reads, writes, nowaw=nowaw)
        return ins

    def collective(self, kind, send_t, recv_t, reads=(), writes=()):
        if "cc" not in self.sems:
            self.sems["cc"] = self.es_root.enter_context(self.nc.semaphore("s_cc"))
            self.cnt["cc"] = 0
        self._deps("pool", reads, writes)
        ins = self.nc.gpsimd.collective_compute(kind, ALU.bypass, replica_groups=[list(range(8))],
                                                ins=[send_t.ap().opt()], outs=[recv_t.ap().opt()])
        self.cnt["cc"] += 1
        ins.then_inc(self.sems["cc"])
        self.ninstr += 1
        self._mark(("cc", self.cnt["cc"]), reads, writes)
        return ins

    def finish(self, bufs, eng="sp"):
        for b in bufs:
            for k, v in b.w.items():
                self._wait(eng, (k, v))

    def close(self):
        self.es.close()


def _dq_tokens(self):
    toks = [(k, v) for k, v in self.cnt.items() if v > 0]
    for q, (ss, j) in self.dq.items():
        for i, k in enumerate(ss):
            n = (j - i + self.NDS - 1) // self.NDS if j > i else 0
            if n > 0:
                toks.append((k, 16 * n))
    return toks


def _barrier(self):
    toks = _dq_tokens(self)
    for e in ("pe", "act", "dve", "pool", "sp"):
        for tk in toks:
            self._wait(e, tk)


def _push(self):
    self._outer = getattr(self, "_outer", [])
    self._outer.append(self.es)
    self.es = contextlib.ExitStack()


def _pop(self):
    _barrier(self)
    self.es.close()
    self.es = self._outer.pop()


Ctx.barrier = _barrier
Ctx.push = _push
Ctx.pop = _pop


T = 2048
NT = T // 128
D = 1024
DIN = 2832
EPS = 1e-6
NE = 32
S = 16384
BIG = 1.0e9
BIGB = 30000.0
DEPTH = 2


class XBig:
    def __init__(self, c, name, R, W, dt):
        self.c = c; self.R = R; self.W = W
        self.st = c.nc.dram_tensor(name + "_s", [8 * R, W], dt)
        self.rt = c.nc.dram_tensor(name + "_r", [64 * R, W], dt)
        self.mt = c.nc.dram_tensor(name + "_m", [8 * R, W], dt)
        self.sb = Buf(self.st, name + "_s"); self.rb = Buf(self.rt, name + "_r"); self.mb = Buf(self.mt, name + "_m")

    def blk_m(self, src):
        return self.mt[src * self.R:(src + 1) * self.R, :]

    def localize(self, q, pid):
        v = self.rt.ap().rearrange("(s d r) w -> s d r w", s=8, d=8)
        for src in range(8):
            self.c.dma(q, self.blk_m(src), (lambda src=src: v[src, bass.ds(pid, 1), :, :].rearrange("o r w -> (o r) w")),
                       reads=[self.rb], writes=[self.mb], nowaw=(src > 0))

    def blk_s(self, dest):
        return self.st[dest * self.R:(dest + 1) * self.R, :]

    def blk_r(self, src):
        v = self.rt.ap().rearrange("(s d r) w -> s d r w", s=8, d=8)
        return v[src, bass.ds(self.c.pid, 1), :, :].rearrange("o r w -> (o r) w")

    def gather(self):
        self.c.collective("AllGather", self.st, self.rt, reads=[self.sb], writes=[self.rb])


class XB:
    def __init__(self, big, off, nrows, mode, p):
        self.big = big; self.off = off; self.nrows = nrows; self.mode = mode; self.p = p
        self.sb = big.sb; self.rb = big.mb

    def _view(self, blk):
        x = blk[self.off:self.off + self.nrows, :]
        if self.mode == "wide":
            x = x.rearrange("(a x) w -> a (x w)", x=self.p)
        elif self.mode == "narrow":
            x = x.rearrange("r (y e) -> (r y) e", e=self.p)
        elif self.mode == "km":
            x = x[:, 0:512].rearrange("o (a e) -> (o a) e", e=8)
        return x

    def s(self, dest, r0, r1):
        return self._view(self.big.blk_s(dest))[r0:r1, :]

    def r(self, src, r0, r1):
        return self._view(self.big.blk_m(src))[r0:r1, :]


def build_F(n_exp=NE, depth=DEPTH):
    c = Ctx(); nc = c.nc
    pid_sp = nc.sync.partition_id(); pid_pool = nc.gpsimd.partition_id()
    inp = lambda n, s, d=F32: c.dram(n, s, d, "ExternalInput")
    x_in = inp("x", [T, D]); cT_d = inp("cT", [128, 8]); cTb_d = inp("cTb", [128, 8, 128])
    ident_d = inp("ident", [128, 128]); bd32_d = inp("bd32", [128, 128]); bd64_d = inp("bd64", [128, 128])
    par_d = inp("par", [128, 2]); bmask_d = inp("bmask", [128, 1024], BF16)
    cmask_d = inp("cmask", [128, 4, 512], BF16); Z_d = inp("Z", [32, 32, 128], BF16); iota_d = inp("iota", [128, 64])
    rmask_d = inp("rmask", [32, 2048]); tri8_d = inp("tri8", [64, 512])
    L = []
    for l in range(depth):
        sfx = str(l)
        L.append(dict(
            adaw=inp("adaw" + sfx, [D, 6144]), adabT=inp("adabT" + sfx, [128, 16]), n1gT=inp("n1gT" + sfx, [128, 8]), w_in=inp("w_in" + sfx, [D, DIN]),
            gains=inp("gains" + sfx, [128, 8]), wg2=inp("wg2" + sfx, [16, 128]),
            rw=inp("rw" + sfx, [64, 8]), rwa=inp("rwa" + sfx, [64, 64]), rwx=inp("rwx" + sfx, [64, 64]),
            adabB=inp("adabB" + sfx, [128, 2, 1024]), adabT2=inp("adabT2" + sfx, [128, 16]), lamv=inp("lamv" + sfx, [128, 4, 32]), lamc=inp("lamc" + sfx, [128, 2]),
            aog=inp("aog" + sfx, [128, 64]), cog=inp("cog" + sfx, [128, 64]), n2gT=inp("n2gT" + sfx, [128, 8]), w_out=inp("w_out" + sfx, [D, D]),
            rwT=inp("rwT" + sfx, [128, 8, 32]), rbB=inp("rbB" + sfx, [128, 32]),
            wup=inp("wup" + sfx, [NE, D, 2 * D]), bupT=inp("bupT" + sfx, [128, NE, 16]), wdn=inp("wdn" + sfx, [NE, D, D]), bdn=inp("bdn" + sfx, [NE, D])))
    out = c.dram("out", [T, D], F32, "ExternalOutput")
    outt = [Buf(out.t, "out%d" % t) for t in range(NT)]
    xbuf_t = nc.dram_tensor("xbuf", [T, D], F32)
    xbt = [Buf(xbuf_t, "xb%d" % t) for t in range(NT)]
    xint = [Buf(x_in.t, "xin%d" % t) for t in range(NT)]
    crb_t = nc.dram_tensor("crb", [T, 256], F32)
    crbt = [Buf(crb_t, "crb%d" % t) for t in range(NT)]
    XA16 = XBig(c, "xa16", 256, 2048, BF16)
    XA32 = XBig(c, "xa32", 705, 1024, F32)
    XBA = XBig(c, "xba", 512, 1024, F32)
    XBO = XBig(c, "xbo", 256, 1024, F32)
    XQK = XB(XA16, 0, 64, "native", None)
    XMQ = XB(XA16, 64, 64, "native", None)
    XMK = XB(XA16, 128, 32, "narrow", 1024)
    XDV = XB(XA16, 160, 64, "narrow", 64)
    XMV = XB(XA16, 224, 32, "narrow", 64)
    XMQ32 = XB(XA32, 0, 128, "wide", 2)
    XG = XB(XA32, 128, 192, "wide", 2)
    XGV = XB(XA32, 320, 128, "narrow", 64)
    XRG = XB(XA32, 448, 256, "wide", 2)
    XKM = XB(XA32, 704, 1, "km", None)
    XOA = XB(XBA, 0, 256, "narrow", 128); XOB = XB(XBA, 256, 256, "narrow", 128)
    XOC = XB(XBO, 0, 128, "narrow", 64); XOD = XB(XBO, 128, 128, "narrow", 64)

    ident = c.sbuf([128, 128], F32, "ident"); epsT = c.sbuf([128, 1], F32, "epsT"); oneT = c.sbuf([128, 1], F32, "oneT")
    c.dma("sp", ident[:], ident_d[:], writes=[ident])
    c.op("dve", lambda e: e.memset(epsT[:], EPS), writes=[epsT])
    c.op("dve", lambda e: e.memset(oneT[:], 1.0), writes=[oneT])
    sq_ = [0]

    def stq():
        sq_[0] += 1
        return "pool" if sq_[0] % 2 else "sp"

    def partA(l, xsrc_t, xsrc_b):
        P = L[l]
        c.push()
        PS = [c.psum([128, 512], F32, "apsb%d_%d" % (l, i)) for i in range(8)]
        bd32 = c.sbuf([128, 128], F32, "bd32"); bd64 = c.sbuf([128, 128], F32, "bd64")
        gn = c.sbuf([128, 8], F32, "gn"); cond = c.sbuf([128, 8], F32, "cond"); adab = c.sbuf([128, 16], F32, "adab"); n1g = c.sbuf([128, 8], F32, "n1g")
        wg2s = c.sbuf([16, 128], F32, "wg2s")
        for sb, dr in ((bd32, bd32_d), (bd64, bd64_d), (gn, P["gains"]), (cond, cT_d), (adab, P["adabT"]), (n1g, P["n1gT"]), (wg2s, P["wg2"])):
            c.dma("sp", sb[:], dr[:], writes=[sb])
        c.op("act", lambda e: e.activation(cond[:], cond[:], AF.Silu), reads=[cond], writes=[cond])
        adaw_v = P["adaw"].t.rearrange("(k p) n -> p k n", p=128)
        wst = [c.sbuf([128, 8, 512], F32, "adst%d" % i) for i in range(2)]
        modps = PS[0]
        for jj in range(4):
            st = wst[jj % 2]
            c.dma("sp", st[:], adaw_v[:, :, jj * 512:(jj + 1) * 512], writes=[st])
            for j4 in range(4):
                j = jj * 4 + j4
                for k in range(8):
                    c.op("pe", lambda e, k=k, j=j, j4=j4, st=st: e.matmul(modps[:, j:j + 1], st[:, k, j4 * 128:(j4 + 1) * 128], cond[:, k:k + 1],
                                                                      start=(k == 0), stop=(k == 7)), reads=[st, cond], writes=[modps], acc=True)
        mod = c.sbuf([128, 16], F32, "mod")
        c.op("dve", lambda e: e.tensor_tensor(mod[:], modps[:, 0:16], adab[:], ALU.add), reads=[modps, adab], writes=[mod])
        a1 = c.sbuf([128, 8], F32, "a1")
        c.op("dve", lambda e: e.scalar_tensor_tensor(a1[:], mod[:, 8:16], 1.0, n1g[:], ALU.add, ALU.mult), reads=[mod, n1g], writes=[a1])
        wb = c.sbuf([128, 8, DIN], BF16, "wb")
        wbk = [Buf(None, "wbk%d" % k) for k in range(8)]
        win_v = P["w_in"].t.rearrange("(k p) n -> p k n", p=128)
        wstage = [c.sbuf([128, DIN], F32, "wstage%d" % i) for i in range(2)]
        for k in range(8):
            st = wstage[k % 2]
            c.dma("sp", st[:], win_v[:, k, :], writes=[st])
            eng = "dve" if k % 2 == 0 else "pool"
            c.op(eng, lambda e, k=k, st=st: e.tensor_copy(wb[:, k, :], st[:]), reads=[st], writes=[wbk[k]])
        hT = c.sbuf([128, 8, T], BF16, "hT")
        hTt = [Buf(None, "hTt%d" % t) for t in range(NT)]
        xts = [c.sbuf([128, D], F32, "xt%d" % i) for i in range(2)]
        junk = c.sbuf([128, D], BF16, "junk")
        stat = [c.sbuf([128, 2], F32, "stat%d" % i) for i in range(2)]
        for t in range(NT):
            xt = xts[t % 2]; stt = stat[t % 2]
            c.dma("sp", xt[:], xsrc_t[t * 128:(t + 1) * 128, :], reads=[xsrc_b[t]], writes=[xt])
            c.op("act", lambda e: e.activation(junk[:], xt[:], AF.Square, accum_out=stt[:, 0:1]), reads=[xt], writes=[junk, stt])
            c.op("act", lambda e: e.activation(stt[:, 1:2], stt[:, 0:1], AF.Sqrt, bias=epsT[:], scale=1.0 / D), reads=[stt, epsT], writes=[stt])
            c.op("dve", lambda e: e.reciprocal(stt[:, 1:2], stt[:, 1:2]), reads=[stt], writes=[stt])
            c.op("dve", lambda e: e.tensor_scalar(xt[:], xt[:], stt[:, 1:2], None, ALU.mult), reads=[xt, stt], writes=[xt])
            for half in range(2):
                ps = PS[1 + half]
                for kk in range(4):
                    k = half * 4 + kk
                    c.op("pe", lambda e, k=k, kk=kk, ps=ps: e.transpose(ps[:, kk * 128:(kk + 1) * 128], xt[:, k * 128:(k + 1) * 128], ident[:]),
                         reads=[xt, ident], writes=[ps], acc=True)
                for kk in range(4):
                    k = half * 4 + kk
                    if kk % 2 == 0:
                        c.op("dve", lambda e, k=k, kk=kk, ps=ps: e.tensor_scalar(hT[:, k, t * 128:(t + 1) * 128], ps[:, kk * 128:(kk + 1) * 128],
                                                                     a1[:, k:k + 1], mod[:, k:k + 1], ALU.mult, ALU.add),
                             reads=[ps, a1, mod], writes=[hTt[t]])
                    else:
                        c.op("act", lambda e, k=k, kk=kk, ps=ps: e.activation(hT[:, k, t * 128:(t + 1) * 128], ps[:, kk * 128:(kk + 1) * 128],
                                                                  AF.Identity, bias=mod[:, k:k + 1], scale=a1[:, k:k + 1]),
                             reads=[ps, a1, mod], writes=[hTt[t]])
        pi = [0]

        def nextps():
            pi[0] += 1
            return PS[3 + pi[0] % 5]

        def proj_fm(col0, ncols, g):
            ps = nextps()
            for k in range(8):
                c.op("pe", lambda e, k=k: e.matmul(ps[0:ncols, :], wb[:, k, col0:col0 + ncols], hT[:, k, g * 512:(g + 1) * 512],
                                                   start=(k == 0), stop=(k == 7)),
                     reads=[wbk[k]] + hTt[g * 4:(g + 1) * 4], writes=[ps], acc=True)
            return ps

        sqb = [c.sbuf([128, 512], F32, "sq%d" % i) for i in range(2)]
        rsb = [c.sbuf([128, 512], F32, "rs%d" % i) for i in range(2)]
        ob16 = [c.sbuf([128, 512], BF16, "ob16_%d" % i) for i in range(3)]
        ob32 = [c.sbuf([128, 512], F32, "ob32_%d" % i) for i in range(3)]
        kms = c.sbuf([128, 2, NT // 2], F32, "kms")
        kmsb = [Buf(None, "kmsb%d" % i) for i in range(2)]
        ctr = [0]

        def send(xb, dest, r0, r1, cols, src_ap, src_buf):
            c.dma(stq(), xb.s(dest, r0, r1)[:, cols], src_ap, reads=[src_buf], writes=[xb.sb], nowaw=True)

        def normed(ps, bd, inv_d, gcol, want32):
            i = ctr[0]; ctr[0] += 1
            sq = sqb[i % 2]; rs = rsb[i % 2]; o16 = ob16[i % 3]
            c.op("act", lambda e: e.activation(sq[:], ps[:], AF.Square), reads=[ps], writes=[sq])
            ps2 = nextps()
            c.op("pe", lambda e: e.matmul(ps2[:], bd[:], sq[:], start=True, stop=True), reads=[bd, sq], writes=[ps2])
            c.op("act", lambda e: e.activation(rs[:], ps2[:], AF.Sqrt, bias=epsT[:], scale=inv_d), reads=[ps2, epsT], writes=[rs])
            c.op("dve", lambda e: e.reciprocal(rs[:], rs[:]), reads=[rs], writes=[rs])
            c.op("dve", lambda e: e.scalar_tensor_tensor(o16[:], ps[:], gn[:, gcol:gcol + 1], rs[:], ALU.mult, ALU.mult), reads=[ps, gn, rs], writes=[o16])
            o32 = None
            if want32:
                o32 = ob32[i % 3]
                c.op("dve", lambda e: e.scalar_tensor_tensor(o32[:], ps[:], gn[:, gcol:gcol + 1], rs[:], ALU.mult, ALU.mult), reads=[ps, gn, rs], writes=[o32])
            return o16, o32

        def raw32(ps, nrows):
            i = ctr[0]; ctr[0] += 1
            o32 = ob32[i % 3]
            if i % 2:
                c.op("act", lambda e: e.activation(o32[0:nrows, :], ps[0:nrows, :], AF.Copy), reads=[ps], writes=[o32])
            else:
                c.op("dve", lambda e: e.tensor_copy(o32[0:nrows, :], ps[0:nrows, :]), reads=[ps], writes=[o32])
            return o32

        lt = [c.sbuf([128, 512], F32, "lt%d" % i) for i in range(3)]
        tm16 = [c.sbuf([128, 512], BF16, "tm16_%d" % i) for i in range(2)]
        tm32 = [c.sbuf([128, 512], F32, "tm32_%d" % i) for i in range(2)]
        cgs = c.sbuf([16, 512], F32, "cgs")
        for g in range(T // 512):
            gc = slice(g * 512, (g + 1) * 512)
            for ch in range(2):
                o16, _ = normed(proj_fm(0 + ch * 128, 128, g), bd32, 1.0 / 32, 0, False)
                for m_ in range(4):
                    send(XQK, 4 * ch + m_, 0, 32, gc, o16[32 * m_:32 * m_ + 32, :], o16)
                o16, _ = normed(proj_fm(256 + ch * 128, 128, g), bd32, 1.0 / 32, 1, False)
                for m_ in range(4):
                    send(XQK, 4 * ch + m_, 32, 64, gc, o16[32 * m_:32 * m_ + 32, :], o16)
                o16, o32 = normed(proj_fm(768 + ch * 128, 128, g), bd64, 1.0 / 64, 2, True)
                for hh in range(2):
                    h = 2 * ch + hh
                    for p_ in range(2):
                        send(XMQ, 2 * h + p_, 0, 64, gc, o16[64 * hh:64 * hh + 64, :], o16)
                        send(XMQ32, 2 * h + p_, 0, 64, gc, o32[64 * hh:64 * hh + 64, :], o32)
                o16, o32 = normed(proj_fm(1024 + ch * 128, 128, g), bd64, 1.0 / 64, 3, True)
                c.op("dve", lambda e, ch=ch, o32=o32: e.tensor_reduce(kms[:, ch, g * 2:(g + 1) * 2], o32[:].rearrange("p (b t) -> p b t", t=256), AX.X, ALU.add),
                     reads=[o32], writes=[kmsb[ch]])
                for hh in range(2):
                    h = 2 * ch + hh
                    for p_ in range(2):
                        send(XMK, 2 * h + p_, 0, 64, slice(g * 256, (g + 1) * 256), o16[64 * hh:64 * hh + 64, p_ * 256:(p_ + 1) * 256], o16)
                o32 = raw32(proj_fm(2320 + ch * 128, 128, g), 128)
                for hh in range(2):
                    send(XRG, 2 * ch + hh, 0, 64, gc, o32[64 * hh:64 * hh + 64, :], o32)
                o32 = raw32(proj_fm(2576 + ch * 128, 128, g), 128)
                for hh in range(2):
                    send(XRG, 2 * ch + hh, 64, 128, gc, o32[64 * hh:64 * hh + 64, :], o32)
            o32 = raw32(proj_fm(1536, 128, g), 128)
            for hc in range(4):
                send(XG, hc, 0, 32, gc, o32[32 * hc:32 * hc + 32, :], o32)
            o32 = raw32(proj_fm(1664, 128, g), 128)
            for hc in range(4):
                send(XG, hc, 32, 64, gc, o32[32 * hc:32 * hc + 32, :], o32)
            psg = proj_fm(2048, 16, g)
            c.op("dve", lambda e: e.tensor_copy(cgs[:], psg[0:16, :]), reads=[psg], writes=[cgs])
            psz = nextps()
            c.op("pe", lambda e: e.matmul(psz[:], wg2s[:], cgs[:], start=True, stop=True), reads=[wg2s, cgs], writes=[psz])
            z, az, m = lt
            c.op("dve", lambda e: e.tensor_scalar(z[:], psz[:], gn[:, 4:5], None, ALU.add), reads=[psz, gn], writes=[z])
            c.op("act", lambda e: e.activation(az[:], z[:], AF.Abs), reads=[z], writes=[az])
            c.op("act", lambda e: e.activation(az[:], az[:], AF.Exp, scale=-1.0), reads=[az], writes=[az])
            c.op("act", lambda e: e.activation(az[:], az[:], AF.Ln, bias=oneT[:], scale=1.0), reads=[az, oneT], writes=[az])
            c.op("dve", lambda e: e.tensor_scalar(m[:], z[:], 0.0, None, ALU.min), reads=[z], writes=[m])
            c.op("dve", lambda e: e.tensor_tensor(m[:], m[:], az[:], ALU.subtract), reads=[m, az], writes=[m])
            c.op("dve", lambda e: e.tensor_scalar(m[:], m[:], 1.0 / 16, None, ALU.mult), reads=[m], writes=[m])
            for hc in range(4):
                send(XG, hc, 64, 96, gc, m[32 * hc:32 * hc + 32, :], m)
            for tt in range(4):
                t = g * 4 + tt
                ps = nextps()
                for (o, col0) in ((0, 512), (256, 1280)):
                    for k in range(8):
                        c.op("pe", lambda e, k=k, o=o, col0=col0: e.matmul(ps[:, o:o + 256], hT[:, k, t * 128:(t + 1) * 128], wb[:, k, col0:col0 + 256],
                                                                        start=(k == 0), stop=(k == 7)), reads=[wbk[k], hTt[t]], writes=[ps], acc=True)
                o16 = tm16[t % 2]
                c.op("act", lambda e: e.activation(o16[:], ps[:], AF.Copy), reads=[ps], writes=[o16])
                par_ = (t // 2) % 2; row = ((t // 2) // 2) * 256 + (t % 2) * 128
                for h in range(4):
                    for i2 in range(2):
                        send(XDV, 2 * h + i2, t * 128, (t + 1) * 128, slice(0, 64), o16[:, 64 * h:64 * h + 64], o16)
                    send(XMV, 2 * h + par_, row, row + 128, slice(0, 64), o16[:, 256 + 64 * h:256 + 64 * h + 64], o16)
                ps = nextps()
                for (o, col0) in ((0, 1792), (256, 2064)):
                    for k in range(8):
                        c.op("pe", lambda e, k=k, o=o, col0=col0: e.matmul(ps[:, o:o + 256], hT[:, k, t * 128:(t + 1) * 128], wb[:, k, col0:col0 + 256],
                                                                        start=(k == 0), stop=(k == 7)), reads=[wbk[k], hTt[t]], writes=[ps], acc=True)
                o32 = tm32[t % 2]
                c.op("dve", lambda e: e.tensor_copy(o32[:], ps[:]), reads=[ps], writes=[o32])
                for hc in range(4):
                    send(XGV, hc, t * 128, (t + 1) * 128, slice(0, 64), o32[:, 64 * hc:64 * hc + 64], o32)
                c.dma(stq(), crb_t[t * 128:(t + 1) * 128, :], o32[:, 256:512], reads=[o32], writes=[crbt[t]])
        for ch in range(2):
            for hh in range(2):
                h = 2 * ch + hh
                for p_ in range(2):
                    send(XKM, 2 * h + p_, 0, 64, slice(0, 8), kms[64 * hh:64 * hh + 64, ch, :], kmsb[ch])
        c.pop()

    def partB(l, after_gla, before_attn):
        P = L[l]
        c.push()
        PS1 = [c.psum([128, 512], F32, "bps1_%d_%d" % (l, i)) for i in range(4)]
        PS2 = [c.psum([128, 1024], F32, "bps2_%d_%d" % (l, i)) for i in range(2)]
        c.push()
        PW = 2048
        rw = c.sbuf([64, 8], F32, "rw"); rwa = c.sbuf([64, 64], F32, "rwa"); rwx = c.sbuf([64, 64], F32, "rwx")
        for sb, dr in ((rw, P["rw"]), (rwa, P["rwa"]), (rwx, P["rwx"])):
            c.dma("sp", sb[:], dr[:], writes=[sb])
        cl = c.sbuf([64, 2], F32, "cl")
        c.op("act", lambda e: e.activation(cl[:, 0:1], rw[:, 7:8], AF.Exp, scale=-1.0), reads=[rw], writes=[cl])
        c.op("act", lambda e: e.activation(cl[:, 0:1], cl[:, 0:1], AF.Ln, bias=oneT[0:64, :], scale=1.0), reads=[cl, oneT], writes=[cl])
        c.op("dve", lambda e: e.tensor_scalar(cl[:, 1:2], cl[:, 0:1], -8.0, None, ALU.mult), reads=[cl], writes=[cl])
        xin = [c.sbuf([64, PW + 3], F32, "xin%d" % i) for i in range(2)]
        gin = [c.sbuf([64, PW], F32, "gin%d" % i) for i in range(2)]
        xc = c.sbuf([64, PW], F32, "xc"); rgt = c.sbuf([64, PW], F32, "rgt"); igt = c.sbuf([64, PW], F32, "igt")
        aa = c.sbuf([64, PW], F32, "aa"); bt = c.sbuf([64, PW], F32, "bt")
        hh_ = [c.sbuf([64, PW], F32, "hh%d" % i) for i in range(2)]
        uu = c.sbuf([64, PW], F32, "uu"); odT = c.sbuf([128, 16, 64], F32, "odT")
        for pc in range(S // PW):
            xi = xin[pc % 2]; gi = gin[pc % 2]; h = hh_[pc % 2]; hp = hh_[(pc + 1) % 2]
            c.dma("sp", xi[:, 3:PW + 3], (lambda: XRG.r(pc, 0, 64)), reads=[XRG.rb], writes=[xi])
            if pc == 0:
                c.op("dve", lambda e: e.memset(xi[:, 0:3], 0.0), writes=[xi])
            else:
                c.dma("sp", xi[:, 0:3], (lambda: XRG.r(pc - 1, 0, 64)[:, PW - 3:PW]), reads=[XRG.rb], writes=[xi], nowaw=True)
            c.dma("sp", gi[:], (lambda: XRG.r(pc, 64, 128)), reads=[XRG.rb], writes=[gi])
            c.op("dve", lambda e: e.tensor_scalar(xc[:], xi[:, 3:PW + 3], rw[:, 3:4], rw[:, 4:5], ALU.mult, ALU.add), reads=[xi, rw], writes=[xc])
            for j in range(3):
                c.op("dve", lambda e, j=j: e.scalar_tensor_tensor(xc[:], xi[:, j:PW + j], rw[:, j:j + 1], xc[:], ALU.mult, ALU.add), reads=[xi, rw, xc], writes=[xc])
            for grp in range(PW // 512):
                sl = slice(grp * 512, (grp + 1) * 512)
                p1 = PS1[0]; p2 = PS1[1]
                c.op("pe", lambda e: e.matmul(p1[0:64, :], rwa[:], xc[:, sl], start=True, stop=True), reads=[rwa, xc], writes=[p1])
                c.op("pe", lambda e: e.matmul(p2[0:64, :], rwx[:], xc[:, sl], start=True, stop=True), reads=[rwx, xc], writes=[p2])
                c.op("act", lambda e: e.activation(rgt[:, sl], p1[0:64, :], AF.Sigmoid, bias=rw[:, 5:6], scale=1.0), reads=[p1, rw], writes=[rgt])
                c.op("act", lambda e: e.activation(igt[:, sl], p2[0:64, :], AF.Sigmoid, bias=rw[:, 6:7], scale=1.0), reads=[p2, rw], writes=[igt])
            c.op("act", lambda e: e.activation(aa[:], rgt[:], AF.Exp, scale=cl[:, 1:2]), reads=[rgt, cl], writes=[aa])
            c.op("act", lambda e: e.activation(bt[:], aa[:], AF.Square), reads=[aa], writes=[bt])
            c.op("dve", lambda e: e.tensor_scalar(bt[:], bt[:], -1.0, 1.0, ALU.mult, ALU.add), reads=[bt], writes=[bt])
            c.op("act", lambda e: e.activation(bt[:], bt[:], AF.Sqrt), reads=[bt], writes=[bt])
            c.op("dve", lambda e: e.tensor_tensor(bt[:], bt[:], igt[:], ALU.mult), reads=[bt, igt], writes=[bt])
            c.op("dve", lambda e: e.tensor_tensor(bt[:], bt[:], xc[:], ALU.mult), reads=[bt, xc], writes=[bt])
            if pc == 0:
                c.op("dve", lambda e: e.tensor_tensor_scan(h[:], aa[:], bt[:], 0.0, ALU.mult, ALU.add), reads=[aa, bt], writes=[h])
            else:
                c.op("dve", lambda e: e.tensor_tensor_scan(h[:], aa[:], bt[:], hp[:, PW - 1:PW], ALU.mult, ALU.add), reads=[aa, bt, hp], writes=[h])
            c.op("dve", lambda e: e.tensor_tensor(uu[:], gi[:], gi[:], ALU.mult), reads=[gi], writes=[uu])
            c.op("dve", lambda e: e.tensor_scalar(uu[:], uu[:], 0.044715, 1.0, ALU.mult, ALU.add), reads=[uu], writes=[uu])
            c.op("dve", lambda e: e.tensor_tensor(uu[:], uu[:], gi[:], ALU.mult), reads=[uu, gi], writes=[uu])
            c.op("act", lambda e: e.activation(uu[:], uu[:], AF.Sigmoid, scale=1.5957691216057308), reads=[uu], writes=[uu])
            c.op("dve", lambda e: e.tensor_tensor(uu[:], uu[:], gi[:], ALU.mult), reads=[uu, gi], writes=[uu])
            c.op("dve", lambda e: e.tensor_tensor(uu[:], uu[:], h[:], ALU.mult), reads=[uu, h], writes=[uu])
            for half in range(2):
                pt = PS1[2 + half]
                for k8 in range(8):
                    k = half * 8 + k8
                    c.op("pe", lambda e, k=k, k8=k8, pt=pt: e.transpose(pt[:, k8 * 64:(k8 + 1) * 64], uu[:, k * 128:(k + 1) * 128], ident[0:64, 0:64]),
                         reads=[uu, ident], writes=[pt], acc=True)
                c.op("act", lambda e, pt=pt, half=half: e.activation(odT[:, half * 8:(half + 1) * 8, :].rearrange("p k e -> p (k e)"), pt[:], AF.Copy), reads=[pt], writes=[odT])
            c.dma("pool", XOD.s(pc, 0, 2048).rearrange("(t p) e -> p t e", p=128), odT[:], reads=[odT], writes=[XOD.sb], nowaw=True)
        c.pop()
        c.push()
        NCH = PW // 64
        rmask = c.sbuf([32, PW], F32, "rmask"); tri8 = c.sbuf([64, 512], F32, "tri8")
        c.dma("sp", rmask[:], rmask_d[:], writes=[rmask]); c.dma("sp", tri8[:], tri8_d[:], writes=[tri8])
        qs = [c.sbuf([32, PW], F32, "gq%d" % i) for i in range(2)]
        ks = [c.sbuf([32, PW], F32, "gk%d" % i) for i in range(2)]
        gs_ = [c.sbuf([32, PW], F32, "gg%d" % i) for i in range(2)]
        vs = [c.sbuf([64, NCH, 64], F32, "gv%d" % i) for i in range(2)]
        bcum = c.sbuf([32, PW], F32, "bcum"); eb = c.sbuf([32, PW], F32, "eb"); qe = c.sbuf([32, PW], F32, "qe")
        ke = c.sbuf([32, PW], F32, "ke"); kl = c.sbuf([32, PW], F32, "kl"); dec = c.sbuf([32, NCH], F32, "dec")
        attT = c.sbuf([64, NCH, 64], F32, "attT"); klT = c.sbuf([64, 256], F32, "klT")
        U = c.sbuf([32, NCH, 64], F32, "U"); Sall = c.sbuf([32, NCH + 1, 64], F32, "Sall")
        osb = [c.sbuf([64, 8, 64], F32, "gosb%d" % i) for i in range(2)]
        c.op("dve", lambda e: e.memset(Sall[:, 0, :], 0.0), writes=[Sall])
        for pc in range(S // PW):
            q = qs[pc % 2]; k = ks[pc % 2]; g = gs_[pc % 2]; v = vs[pc % 2]
            c.dma("sp", q[:], (lambda: XG.r(pc, 0, 32)), reads=[XG.rb], writes=[q]); c.dma("sp", k[:], (lambda: XG.r(pc, 32, 64)), reads=[XG.rb], writes=[k])
            c.dma("sp", g[:], (lambda: XG.r(pc, 64, 96)), reads=[XG.rb], writes=[g])
            c.dma("sp", v[:], (lambda: XGV.r(pc, 0, 2048).rearrange("(c p) e -> p c e", p=64)), reads=[XGV.rb], writes=[v])
            c.op("dve", lambda e: e.tensor_tensor_scan(bcum[:], rmask[:], g[:], 0.0, ALU.mult, ALU.add), reads=[rmask, g], writes=[bcum])
            c.op("act", lambda e: e.activation(eb[:], bcum[:], AF.Exp), reads=[bcum], writes=[eb])
            c.op("dve", lambda e: e.scalar_tensor_tensor(qe[:], q[:], 32.0 ** -0.5, eb[:], ALU.mult, ALU.mult), reads=[q, eb], writes=[qe])
            c.op("act", lambda e: e.activation(eb[:], bcum[:], AF.Exp, scale=-1.0), reads=[bcum], writes=[eb])
            c.op("dve", lambda e: e.tensor_tensor(ke[:], k[:], eb[:], ALU.mult), reads=[k, eb], writes=[ke])
            bc3 = bcum[:].rearrange("p (c t) -> p c t", t=64)
            c.op("act", lambda e: e.activation(dec[:], bc3[:, :, 63], AF.Exp), reads=[bcum], writes=[dec])
            for cc in range(NCH):
                c.op("dve", lambda e, cc=cc: e.tensor_scalar(kl[:, cc * 64:(cc + 1) * 64], ke[:, cc * 64:(cc + 1) * 64], dec[:, cc:cc + 1], None, ALU.mult),
                     reads=[ke, dec], writes=[kl])
            for grp in range(NCH // 8):
                pA, pT_, pU = PS1[0], PS1[1], PS1[2]
                for cc in range(8):
                    ch = grp * 8 + cc; sl = slice(ch * 64, (ch + 1) * 64)
                    c.op("pe", lambda e, cc=cc, sl=sl: e.matmul(pA[0:64, cc * 64:(cc + 1) * 64], ke[:, sl], qe[:, sl], start=True, stop=True),
                         reads=[ke, qe], writes=[pA], acc=True)
                c.op("dve", lambda e: e.tensor_tensor(attT[:, grp * 8:(grp + 1) * 8, :].rearrange("p c t -> p (c t)"), pA[0:64, :], tri8[:], ALU.mult),
                     reads=[pA, tri8], writes=[attT])
                for cc in range(8):
                    ch = grp * 8 + cc; sl = slice(ch * 64, (ch + 1) * 64)
                    c.op("pe", lambda e, cc=cc, sl=sl: e.transpose(pT_[0:64, cc * 32:(cc + 1) * 32], kl[:, sl], ident[0:32, 0:32]),
                         reads=[kl, ident], writes=[pT_], acc=True)
                c.op("act", lambda e: e.activation(klT[:], pT_[0:64, 0:256], AF.Copy), reads=[pT_], writes=[klT])
                for cc in range(8):
                    ch = grp * 8 + cc
                    c.op("pe", lambda e, cc=cc, ch=ch: e.matmul(pU[0:32, cc * 64:(cc + 1) * 64], klT[:, cc * 32:(cc + 1) * 32], v[:, ch, :], start=True, stop=True),
                         reads=[klT, v], writes=[pU], acc=True)
                c.op("dve", lambda e: e.tensor_copy(U[:, grp * 8:(grp + 1) * 8, :].rearrange("p c t -> p (c t)"), pU[0:32, :]), reads=[pU], writes=[U])
            for e_ in range(64):
                c.op("dve", lambda e, e_=e_: e.tensor_tensor_scan(Sall[:, 1:NCH + 1, e_], dec[:], U[:, :, e_], Sall[:, 0, e_:e_ + 1], ALU.mult, ALU.add),
                     reads=[dec, U, Sall], writes=[Sall])
            for grp in range(NCH // 8):
                pO = PS1[3]
                for cc in range(8):
                    ch = grp * 8 + cc; sl = slice(ch * 64, (ch + 1) * 64)
                    c.op("pe", lambda e, cc=cc, ch=ch: e.matmul(pO[0:64, cc * 64:(cc + 1) * 64], attT[:, ch, :], v[:, ch, :], start=True, stop=False),
                         reads=[attT, v], writes=[pO], acc=True)
                    c.op("pe", lambda e, cc=cc, ch=ch, sl=sl: e.matmul(pO[0:64, cc * 64:(cc + 1) * 64], qe[:, sl], Sall[:, ch, :], start=False, stop=True),
                         reads=[qe, Sall], writes=[pO], acc=True)
                o = osb[grp % 2]
                c.op("act", lambda e: e.activation(o[:].rearrange("p c t -> p (c t)"), pO[0:64, :], AF.Copy), reads=[pO], writes=[o])
                c.dma("pool", XOC.s(pc, grp * 512, (grp + 1) * 512).rearrange("(c p) e -> p c e", p=64), o[:], reads=[o], writes=[XOC.sb], nowaw=True)
            c.op("dve", lambda e: e.tensor_copy(Sall[:, 0, :], Sall[:, NCH, :]), reads=[Sall], writes=[Sall])
        c.pop()
        after_gla()
        c.push()
        before_attn()
        qT = c.sbuf([64, S], BF16, "qT"); kT = c.sbuf([64, S], BF16, "kT"); V = c.sbuf([128, 128, 65], BF16, "V")
        cmask = c.sbuf([128, 4, 512], BF16, "cmask"); bmask = c.sbuf([128, 1024], BF16, "bmask")
        pTs = [c.sbuf([128, 1024], BF16, "pT%d" % i) for i in range(3)]
        osbs = [c.sbuf([65, 512], F32, "osb%d" % i) for i in range(2)]
        oTs = [c.sbuf([128, 4, 65], F32, "oT%d" % i) for i in range(2)]
        c.dma("sp", cmask[:], cmask_d[:], writes=[cmask]); c.dma("sp", bmask[:], bmask_d[:], writes=[bmask])
        c.op("dve", lambda e: e.memset(V[:, :, 64:65], 1.0), writes=[V])

        def run_unit(groups, Kd, scale, XO):
            steps = []
            for gi, G in enumerate(groups):
                n = len(G["pairs"])
                for pi_, pr in enumerate(G["pairs"]):
                    steps.append((gi, pi_, n, pr))

            def emit_S(i):
                gi, pi_, n, (j0, j1) = steps[i]
                G = groups[gi]; g = G["g"]
                sps = PS2[i % 2]
                for hh, j in enumerate((j0, j1)):
                    if G["extra"] is None:
                        c.op("pe", lambda e, hh=hh, j=j: e.matmul(sps[:, hh * 512:(hh + 1) * 512], kT[0:Kd, j * 128:(j + 1) * 128], qT[0:Kd, g * 512:(g + 1) * 512],
                                                                 start=True, stop=True), reads=[kT, qT], writes=[sps], acc=True)
                    else:
                        zl, biasT, Zb = G["extra"](pi_)
                        c.op("pe", lambda e, hh=hh, j=j: e.matmul(sps[:, hh * 512:(hh + 1) * 512], kT[0:Kd, j * 128:(j + 1) * 128], qT[0:Kd, g * 512:(g + 1) * 512],
                                                                 start=True, stop=False), reads=[kT, qT], writes=[sps], acc=True)
                        c.op("pe", lambda e, hh=hh, zl=zl, biasT=biasT: e.matmul(sps[:, hh * 512:(hh + 1) * 512], zl, biasT[:], start=False, stop=True),
                             reads=[Zb, biasT], writes=[sps], acc=True)

            if groups[0].get("part1"):
                groups[0]["part1"](); groups[0]["part2"]()
            emit_S(0)
            for i, (gi, pi_, n, (j0, j1)) in enumerate(steps):
                G = groups[gi]; g = G["g"]
                sps = PS2[i % 2]; pT = pTs[i % 3]; pO = PS1[g % 2]
                if pi_ == 0 and gi + 1 < len(groups) and groups[gi + 1].get("part1"):
                    groups[gi + 1]["part1"]()
                c.op("act", lambda e: e.activation(pT[:], sps[:], AF.Exp, scale=scale), reads=[sps], writes=[pT])
                if pi_ in G["masks"]:
                    mk_ap, mk_buf = G["masks"][pi_]
                    c.op("dve", lambda e: e.tensor_tensor(pT[:], pT[:], mk_ap, ALU.mult), reads=[pT, mk_buf], writes=[pT])
                if pi_ == n - 1 and gi + 1 < len(groups) and groups[gi + 1].get("part2"):
                    groups[gi + 1]["part2"]()
                if i + 1 < len(steps):
                    emit_S(i + 1)
                for hh, j in enumerate((j0, j1)):
                    c.op("pe", lambda e, hh=hh, j=j: e.matmul(pO[0:65, :], V[:, j, :], pT[:, hh * 512:(hh + 1) * 512],
                                                             start=(pi_ == 0 and hh == 0), stop=(pi_ == n - 1 and hh == 1)),
                         reads=[V, pT], writes=[pO], acc=True)
                if pi_ == n - 1:
                    o = osbs[g % 2]; oT = oTs[g % 2]
                    c.op("dve", lambda e: e.tensor_copy(o[:], pO[0:65, :]), reads=[pO], writes=[o])
                    ptq = PS1[3] if G["extra"] is None else PS1[0 if g % 2 else 1]
                    ptq = PS1[3] if G["extra"] is None else PS1[3]
                    for qi in range(4):
                        c.op("pe", lambda e, qi=qi: e.transpose(ptq[:, qi * 65:(qi + 1) * 65], o[:, qi * 128:(qi + 1) * 128], ident[0:65, 0:65]),
                             reads=[o, ident], writes=[ptq], acc=True)
                    c.op("dve", lambda e: e.tensor_copy(oT[:].rearrange("p q e -> p (q e)"), ptq[:, 0:260]), reads=[ptq], writes=[oT])
                    c.dma("sp", XO.s(g // 4, (g % 4) * 512, (g % 4 + 1) * 512)[:, 0:65].rearrange("(q p) e -> p q e", p=128), oT[:],
                          reads=[oT], writes=[XO.sb], nowaw=True)

        for i in range(8):
            c.dma("sp", qT[0:32, i * 2048:(i + 1) * 2048], (lambda: XQK.r(i, 0, 32)), reads=[XQK.rb], writes=[qT], nowaw=True)
            c.dma("sp", kT[0:32, i * 2048:(i + 1) * 2048], (lambda: XQK.r(i, 32, 64)), reads=[XQK.rb], writes=[kT], nowaw=True)
            c.dma("sp", V[:, i * 16:(i + 1) * 16, 0:64], (lambda: XDV.r(i, 0, 2048).rearrange("(t p) e -> p t e", p=128)), reads=[XDV.rb], writes=[V], nowaw=True)
        cm2 = cmask[:].rearrange("p d q -> p (d q)")
        groups = []
        for g in range(32):
            npair = 2 * (g + 1)
            pairs = [(2 * i, 2 * i + 1) for i in range(npair)]
            masks = {npair - 2: (cm2[:, 0:1024], cmask), npair - 1: (cm2[:, 1024:2048], cmask)}
            groups.append(dict(g=g, pairs=pairs, masks=masks, extra=None))
        run_unit(groups, 32, 32.0 ** -0.5, XOA)
        Zb = c.sbuf([32, 32, 128], BF16, "Zb"); iota = c.sbuf([128, 64], F32, "iota"); par = c.sbuf([128, 2], F32, "par")
        km = c.sbuf([64, 64], F32, "km")
        c.dma("sp", Zb[:], Z_d[:], writes=[Zb]); c.dma("sp", iota[:], iota_d[:], writes=[iota]); c.dma("sp", par[:], par_d[:], writes=[par])
        for i in range(8):
            c.dma("sp", km[:, 8 * i:8 * i + 8], (lambda: XKM.r(i, 0, 64)), reads=[XKM.rb], writes=[km], nowaw=True)
            c.dma("sp", qT[0:64, i * 2048:(i + 1) * 2048], (lambda: XMQ.r(i, 0, 64)), reads=[XMQ.rb], writes=[qT], nowaw=(i > 0))
            c.dma("sp", kT[0:64, i * 1024:(i + 1) * 1024], (lambda: XMK.r(i, 0, 64)), reads=[XMK.rb], writes=[kT], nowaw=(i > 0))
            c.dma("sp", V[:, i * 8:(i + 1) * 8, 0:64], (lambda: XMV.r(i, 0, 1024).rearrange("(t p) e -> p t e", p=128)), reads=[XMV.rb], writes=[V], nowaw=(i > 0))
        q32s = [c.sbuf([64, 512], F32, "q32_%d" % i) for i in range(2)]
        biasTs = [c.sbuf([32, 512], BF16, "biasT%d" % i) for i in range(2)]
        W = {n: [c.sbuf([128, 64], F32, "mw_%s%d" % (n, i)) for i in range(4)] for n in ("lt", "t1", "gm", "sel", "eq")}
        top8s = [c.sbuf([128, 8], F32, "top8_%d" % i) for i in range(4)]
        bps = [c.sbuf([128, 32], F32, "bp%d" % i) for i in range(4)]
        pG = PS1[3]; pB = PS1[2]

        def mk_part1(g):
            def part1():
                q32 = q32s[g % 2]
                c.dma("sp", q32[:], (lambda: XMQ32.r(g // 4, 0, 64)[:, (g % 4) * 512:(g % 4 + 1) * 512]), reads=[XMQ32.rb], writes=[q32])
                for qi in range(4):
                    c.op("pe", lambda e, qi=qi: e.matmul(pG[:, qi * 64:(qi + 1) * 64], q32[:, qi * 128:(qi + 1) * 128], km[:], start=True, stop=True),
                         reads=[q32, km], writes=[pG], acc=True)
                for qi in range(4):
                    qt = 4 * g + qi; own = float(qt // 2)
                    lt, t1, gm, sel, eq, top8, bp = W["lt"][qi], W["t1"][qi], W["gm"][qi], W["sel"][qi], W["eq"][qi], top8s[qi], bps[qi]
                    pGq = pG[:, qi * 64:(qi + 1) * 64]
                    c.op("dve", lambda e: e.tensor_single_scalar(lt[:], iota[:], own, ALU.is_lt), reads=[iota], writes=[lt])
                    c.op("dve", lambda e: e.tensor_scalar(t1[:], lt[:], -1.0, BIG, ALU.add, ALU.mult), reads=[lt], writes=[t1])
                    c.op("dve", lambda e: e.tensor_tensor(gm[:], pGq, lt[:], ALU.mult), reads=[pG, lt], writes=[gm])
                    c.op("dve", lambda e: e.tensor_tensor(gm[:], gm[:], t1[:], ALU.add), reads=[gm, t1], writes=[gm])
                    c.op("dve", lambda e: e.max(top8[:], gm[:]), reads=[gm], writes=[top8])
                    c.op("dve", lambda e: e.tensor_scalar(sel[:], gm[:], top8[:, 2:3], None, ALU.is_ge), reads=[gm, top8], writes=[sel])
                    c.op("dve", lambda e: e.tensor_tensor(sel[:], sel[:], lt[:], ALU.mult), reads=[sel, lt], writes=[sel])
                    c.op("dve", lambda e: e.tensor_single_scalar(eq[:], iota[:], own, ALU.is_equal), reads=[iota], writes=[eq])
                    c.op("dve", lambda e: e.tensor_tensor(sel[:], sel[:], eq[:], ALU.add), reads=[sel, eq], writes=[sel])
                    c.op("dve", lambda e: e.tensor_scalar(sel[:], sel[:], -1.0, BIGB, ALU.add, ALU.mult), reads=[sel], writes=[sel])
                    s3 = sel[:].rearrange("p (m two) -> p m two", two=2)
                    c.op("dve", lambda e: e.tensor_scalar(bp[:], s3[:, :, 0], par[:, 0:1], None, ALU.mult), reads=[sel, par], writes=[bp])
                    c.op("dve", lambda e: e.scalar_tensor_tensor(bp[:], s3[:, :, 1], par[:, 1:2], bp[:], ALU.mult, ALU.add), reads=[sel, par, bp], writes=[bp])
            return part1

        def mk_part2(g):
            def part2():
                biasT = biasTs[g % 2]
                for qi in range(4):
                    c.op("pe", lambda e, qi=qi: e.transpose(pB[0:32, qi * 128:(qi + 1) * 128], bps[qi][:], ident[:]), reads=[bps[qi], ident], writes=[pB], acc=True)
                c.op("act", lambda e: e.activation(biasT[:], pB[0:32, :], AF.Copy), reads=[pB], writes=[biasT])
            return part2

        groups = []
        for g in range(32):
            pairs = [(2 * m, 2 * m + 1) for m in range(g + 1)]
            masks = {g: (bmask[:], bmask)}
            groups.append(dict(g=g, pairs=pairs, masks=masks, extra=(lambda m, g=g: (Zb[:, m, :], biasTs[g % 2], Zb)), part1=mk_part1(g), part2=mk_part2(g)))
        run_unit(groups, 64, 64.0 ** -0.5, XOB)
        c.pop()
        c.pop()

    def partC(l, xsrc_t, xsrc_b, dst_t, dst_b):
        P = L[l]
        c.push()
        PS = [c.psum([128, 512], F32, "cpsb%d_%d" % (l, i)) for i in range(8)]
        g2b = c.sbuf([128, D], F32, "g2b")
        h2T = c.sbuf([128, 8, T], BF16, "h2T"); h2Tt = [Buf(None, "h2Tt%d" % t) for t in range(NT)]
        Gall = c.sbuf([128, NT, NE], F32, "Gall"); Gt = [Buf(None, "Gt%d" % t) for t in range(NT)]
        GT = c.sbuf([32, T], F32, "GT"); GTt = [Buf(None, "GTt%d" % t) for t in range(NT)]
        c.push()
        cb = c.sbuf([128, 8, 128], F32, "cb")
        c.dma("sp", cb[:], cTb_d[:], writes=[cb])
        c.op("act", lambda e: e.activation(cb[:], cb[:], AF.Silu), reads=[cb], writes=[cb])
        adaw_v = P["adaw"].t.rearrange("(k p) n -> p k n", p=128)
        wst = [c.sbuf([128, 8, 512], F32, "adst%d" % i) for i in range(2)]
        g1b = c.sbuf([128, D], F32, "g1b"); abB = c.sbuf([128, 2, D], F32, "abB")
        c.dma("sp", abB[:], P["adabB"][:], writes=[abB])
        si = 0
        for gi, (dst, col0) in enumerate(((g1b, 2048), (g2b, 5120))):
            for hf in range(2):
                st = wst[si % 2]; si += 1
                c.dma("sp", st[:], adaw_v[:, :, col0 + hf * 512:col0 + (hf + 1) * 512], writes=[st])
                ps = PS[hf]
                for k in range(8):
                    c.op("pe", lambda e, k=k, st=st, ps=ps: e.matmul(ps[:], cb[:, k, :], st[:, k, :], start=(k == 0), stop=(k == 7)), reads=[cb, st], writes=[ps], acc=True)
                c.op("dve", lambda e, ps=ps, dst=dst, gi=gi, hf=hf: e.tensor_tensor(dst[:, hf * 512:(hf + 1) * 512], ps[:], abB[:, gi, hf * 512:(hf + 1) * 512], ALU.add),
                     reads=[ps, abB], writes=[dst])
        modps = PS[2]
        adab2 = c.sbuf([128, 16], F32, "adab2"); n2g = c.sbuf([128, 8], F32, "n2g")
        c.dma("sp", adab2[:], P["adabT2"][:], writes=[adab2]); c.dma("sp", n2g[:], P["n2gT"][:], writes=[n2g])
        for jj in range(4):
            st = wst[si % 2]; si += 1
            c.dma("sp", st[:], adaw_v[:, :, 3072 + jj * 512:3072 + (jj + 1) * 512], writes=[st])
            for j4 in range(4):
                j = jj * 4 + j4
                for k in range(8):
                    c.op("pe", lambda e, k=k, j=j, j4=j4, st=st: e.matmul(modps[:, j:j + 1], st[:, k, j4 * 128:(j4 + 1) * 128], cb[:, k, 0:1],
                                                                      start=(k == 0), stop=(k == 7)), reads=[st, cb], writes=[modps], acc=True)
        mod2 = c.sbuf([128, 16], F32, "mod2"); a2 = c.sbuf([128, 8], F32, "a2")
        c.op("dve", lambda e: e.tensor_tensor(mod2[:], modps[:, 0:16], adab2[:], ALU.add), reads=[modps, adab2], writes=[mod2])
        c.op("dve", lambda e: e.scalar_tensor_tensor(a2[:], mod2[:, 8:16], 1.0, n2g[:], ALU.add, ALU.mult), reads=[mod2, n2g], writes=[a2])
        lv = c.sbuf([128, 4, 32], F32, "lv"); lsm = c.sbuf([128, 4], F32, "lsm"); nlam = c.sbuf([128, 1], F32, "nlam")
        c.dma("sp", lv[:], P["lamv"][:], writes=[lv])
        c.op("dve", lambda e: e.tensor_tensor(lv[:, 0, :], lv[:, 0, :], lv[:, 1, :], ALU.mult), reads=[lv], writes=[lv])
        c.op("dve", lambda e: e.tensor_tensor(lv[:, 2, :], lv[:, 2, :], lv[:, 3, :], ALU.mult), reads=[lv], writes=[lv])
        c.op("dve", lambda e: e.tensor_reduce(lsm[:, 0:1], lv[:, 0, :], AX.X, ALU.add), reads=[lv], writes=[lsm])
        c.op("dve", lambda e: e.tensor_reduce(lsm[:, 1:2], lv[:, 2, :], AX.X, ALU.add), reads=[lv], writes=[lsm])
        c.op("act", lambda e: e.activation(lsm[:, 2:4], lsm[:, 0:2], AF.Exp), reads=[lsm], writes=[lsm])
        c.op("dve", lambda e: e.tensor_tensor(nlam[:], lsm[:, 3:4], lsm[:, 2:3], ALU.subtract), reads=[lsm], writes=[nlam])
        lamc = c.sbuf([128, 2], F32, "lamc")
        c.dma("sp", lamc[:], P["lamc"][:], writes=[lamc])
        c.op("dve", lambda e: e.tensor_scalar(nlam[:], nlam[:], lamc[:, 0:1], None, ALU.subtract), reads=[nlam, lamc], writes=[nlam])
        aog = c.sbuf([128, 64], F32, "aog"); cog = c.sbuf([128, 64], F32, "cog")
        c.dma("sp", aog[:], P["aog"][:], writes=[aog]); c.dma("sp", cog[:], P["cog"][:], writes=[cog])
        c.op("dve", lambda e: e.tensor_scalar(aog[:], aog[:], lamc[:, 1:2], None, ALU.mult), reads=[aog, lamc], writes=[aog])
        wob = c.sbuf([128, 8, D], BF16, "wob"); wobk = [Buf(None, "wobk%d" % k) for k in range(8)]
        wo_v = P["w_out"].t.rearrange("(k p) n -> p k n", p=128)
        wos = [c.sbuf([128, D], F32, "wos%d" % i) for i in range(2)]
        for k in range(8):
            st = wos[k % 2]
            c.dma("sp", st[:], wo_v[:, k, :], writes=[st])
            c.op("pool", lambda e, k=k, st=st: e.tensor_copy(wob[:, k, :], st[:]), reads=[st], writes=[wobk[k]])
        rw = c.sbuf([128, 8, 32], F32, "rwr"); rb = c.sbuf([128, 32], F32, "rb")
        c.dma("sp", rw[:], P["rwT"][:], writes=[rw]); c.dma("sp", rb[:], P["rbB"][:], writes=[rb])
        oas = [c.sbuf([128, 8, 65], F32, "oas%d" % i) for i in range(2)]; obs = [c.sbuf([128, 8, 65], F32, "obs%d" % i) for i in range(2)]
        ocs = [c.sbuf([128, 256], F32, "ocs%d" % i) for i in range(2)]; crs = [c.sbuf([128, 256], F32, "crs%d" % i) for i in range(2)]
        ys = [c.sbuf([128, D], F32, "ys%d" % i) for i in range(2)]; xs = [c.sbuf([128, D], F32, "xs%d" % i) for i in range(2)]
        rs8 = c.sbuf([128, 8], F32, "rs8"); on = c.sbuf([128, 8, 64], F32, "on"); od_ = c.sbuf([128, 256], F32, "od_"); sq = c.sbuf([128, 256], F32, "sq")
        st4 = c.sbuf([128, 4], F32, "st4"); osum = c.sbuf([128, 4, 65], F32, "osum")
        yT = [c.sbuf([128, 8, 128], BF16, "yT%d" % i) for i in range(2)]
        junk = c.sbuf([128, D], BF16, "junk"); stat = c.sbuf([128, 2], F32, "stat")
        h32 = [c.sbuf([128, 8, 128], F32, "h32_%d" % i) for i in range(2)]
        lg = c.sbuf([128, 32], F32, "lg"); top8 = c.sbuf([128, 8], F32, "top8"); msk = c.sbuf([128, 32], F32, "msk"); ex = c.sbuf([128, 32], F32, "ex")
        sm = c.sbuf([128, 2], F32, "sm")
        for t in range(NT):
            ts_ = slice(t * 128, (t + 1) * 128)
            oa_ = oas[t % 2]; ob_ = obs[t % 2]; oc_ = ocs[t % 2]; cr_ = crs[t % 2]; y = ys[t % 2]; xt = xs[t % 2]
            for m_ in range(8):
                c.dma("sp", oa_[:, m_, :], (lambda: XOA.r(m_, t * 128, (t + 1) * 128)[:, 0:65]), reads=[XOA.rb], writes=[oa_], nowaw=(m_ > 0))
                c.dma("sp", ob_[:, m_, :], (lambda: XOB.r(m_, t * 128, (t + 1) * 128)[:, 0:65]), reads=[XOB.rb], writes=[ob_], nowaw=(m_ > 0))
            for hc in range(4):
                c.dma("sp", oc_[:, hc * 64:(hc + 1) * 64], (lambda: XOC.r(hc, t * 128, (t + 1) * 128)), reads=[XOC.rb], writes=[oc_], nowaw=(hc > 0))
                c.dma("sp", y[:, 768 + hc * 64:768 + (hc + 1) * 64], (lambda: XOD.r(hc, t * 128, (t + 1) * 128)), reads=[XOD.rb], writes=[y], nowaw=(hc > 0))
            c.dma("sp", cr_[:], crb_t[ts_, :], reads=[crbt[t]], writes=[cr_])
            c.dma("sp", xt[:], xsrc_t[ts_, :], reads=[xsrc_b[t]], writes=[xt])
            c.op("dve", lambda e: e.reciprocal(rs8[:], oa_[:, :, 64]), reads=[oa_], writes=[rs8])
            for m in range(8):
                c.op("dve", lambda e, m=m: e.tensor_scalar(on[:, m, :], oa_[:, m, 0:64], rs8[:, m:m + 1], None, ALU.mult), reads=[oa_, rs8], writes=[on])
            on4 = on[:].rearrange("p (h i) d -> p h i d", i=2)
            od3 = od_[:].rearrange("p (h d) -> p h d", d=64)
            c.op("dve", lambda e: e.scalar_tensor_tensor(od3, on4[:, :, 1, :], nlam[:, 0:1], on4[:, :, 0, :], ALU.mult, ALU.add), reads=[on, nlam], writes=[od_])
            c.op("dve", lambda e: e.tensor_tensor(sq[:], od_[:], od_[:], ALU.mult), reads=[od_], writes=[sq])
            c.op("dve", lambda e: e.tensor_reduce(st4[:], sq[:].rearrange("p (h d) -> p h d", d=64), AX.X, ALU.add), reads=[sq], writes=[st4])
            c.op("act", lambda e: e.activation(st4[:], st4[:], AF.Sqrt, bias=epsT[:], scale=1.0 / 64), reads=[st4, epsT], writes=[st4])
            c.op("dve", lambda e: e.reciprocal(st4[:], st4[:]), reads=[st4], writes=[st4])
            for h in range(4):
                c.op("dve", lambda e, h=h: e.scalar_tensor_tensor(y[:, h * 64:(h + 1) * 64], od_[:, h * 64:(h + 1) * 64], st4[:, h:h + 1], aog[:], ALU.mult, ALU.mult),
                     reads=[od_, st4, aog], writes=[y])
            ob4 = ob_[:].rearrange("p (h i) d -> p h i d", i=2)
            c.op("dve", lambda e: e.tensor_tensor(osum[:], ob4[:, :, 0, :], ob4[:, :, 1, :], ALU.add), reads=[ob_], writes=[osum])
            c.op("dve", lambda e: e.reciprocal(rs8[:, 0:4], osum[:, :, 64]), reads=[osum], writes=[rs8])
            for h in range(4):
                c.op("dve", lambda e, h=h: e.tensor_scalar(y[:, 256 + h * 64:256 + (h + 1) * 64], osum[:, h, 0:64], rs8[:, h:h + 1], None, ALU.mult), reads=[osum, rs8], writes=[y])
            c.op("dve", lambda e: e.tensor_tensor(sq[:], oc_[:], oc_[:], ALU.mult), reads=[oc_], writes=[sq])
            c.op("dve", lambda e: e.tensor_reduce(st4[:], sq[:].rearrange("p (h d) -> p h d", d=64), AX.X, ALU.add), reads=[sq], writes=[st4])
            c.op("act", lambda e: e.activation(st4[:], st4[:], AF.Sqrt, bias=epsT[:], scale=1.0 / 64), reads=[st4, epsT], writes=[st4])
            c.op("dve", lambda e: e.reciprocal(st4[:], st4[:]), reads=[st4], writes=[st4])
            c.op("act", lambda e: e.activation(cr_[:], cr_[:], AF.Silu), reads=[cr_], writes=[cr_])
            for h in range(4):
                c.op("dve", lambda e, h=h: e.scalar_tensor_tensor(y[:, 512 + h * 64:512 + (h + 1) * 64], oc_[:, h * 64:(h + 1) * 64], st4[:, h:h + 1], cog[:], ALU.mult, ALU.mult),
                     reads=[oc_, st4, cog], writes=[y])
            c.op("dve", lambda e: e.tensor_tensor(y[:, 512:768], y[:, 512:768], cr_[:], ALU.mult), reads=[y, cr_], writes=[y])
            yt = yT[t % 2]
            for half in range(2):
                ps = PS[2 + half]
                for kk in range(4):
                    k = half * 4 + kk
                    c.op("pe", lambda e, k=k, kk=kk, ps=ps: e.transpose(ps[:, kk * 128:(kk + 1) * 128], y[:, k * 128:(k + 1) * 128], ident[:]), reads=[y, ident], writes=[ps], acc=True)
                c.op("act", lambda e, ps=ps, half=half: e.activation(yt[:, half * 4:(half + 1) * 4, :].rearrange("p k t -> p (k t)"), ps[:], AF.Copy), reads=[ps], writes=[yt])
            for hf in range(2):
                ps = PS[4 + hf]
                for k in range(8):
                    c.op("pe", lambda e, k=k, ps=ps, hf=hf: e.matmul(ps[:], yt[:, k, :], wob[:, k, hf * 512:(hf + 1) * 512], start=(k == 0), stop=(k == 7)),
                         reads=[yt, wobk[k]], writes=[ps], acc=True)
                c.op("dve", lambda e, ps=ps, hf=hf: e.tensor_tensor(y[:, hf * 512:(hf + 1) * 512], ps[:], g1b[:, hf * 512:(hf + 1) * 512], ALU.mult), reads=[ps, g1b], writes=[y])
            c.op("dve", lambda e: e.tensor_tensor(xt[:], xt[:], y[:], ALU.add), reads=[xt, y], writes=[xt])
            c.dma("pool", xbuf_t[ts_, :], xt[:], reads=[xt], writes=[xbt[t]])
            c.op("act", lambda e: e.activation(junk[:], xt[:], AF.Square, accum_out=stat[:, 0:1]), reads=[xt], writes=[junk, stat])
            c.op("act", lambda e: e.activation(stat[:, 1:2], stat[:, 0:1], AF.Sqrt, bias=epsT[:], scale=1.0 / D), reads=[stat, epsT], writes=[stat])
            c.op("dve", lambda e: e.reciprocal(stat[:, 1:2], stat[:, 1:2]), reads=[stat], writes=[stat])
            c.op("dve", lambda e: e.tensor_scalar(y[:], xt[:], stat[:, 1:2], None, ALU.mult), reads=[xt, stat], writes=[y])
            hh = h32[t % 2]
            for half in range(2):
                ps = PS[6 + half]
                for kk in range(4):
                    k = half * 4 + kk
                    c.op("pe", lambda e, k=k, kk=kk, ps=ps: e.transpose(ps[:, kk * 128:(kk + 1) * 128], y[:, k * 128:(k + 1) * 128], ident[:]), reads=[y, ident], writes=[ps], acc=True)
                for kk in range(4):
                    k = half * 4 + kk
                    c.op("dve", lambda e, k=k, kk=kk, ps=ps: e.tensor_scalar(hh[:, k, :], ps[:, kk * 128:(kk + 1) * 128], a2[:, k:k + 1], mod2[:, k:k + 1], ALU.mult, ALU.add),
                         reads=[ps, a2, mod2], writes=[hh])
            c.op("act", lambda e: e.activation(h2T[:, :, ts_], hh[:], AF.Copy), reads=[hh], writes=[h2Tt[t]])
            psr = PS[0]
            for k in range(8):
                c.op("pe", lambda e, k=k: e.matmul(psr[:, 0:32], hh[:, k, :], rw[:, k, :], start=(k == 0), stop=(k == 7)), reads=[hh, rw], writes=[psr], acc=True)
            c.op("dve", lambda e: e.tensor_tensor(lg[:], psr[:, 0:32], rb[:], ALU.add), reads=[psr, rb], writes=[lg])
            c.op("dve", lambda e: e.max(top8[:], lg[:]), reads=[lg], writes=[top8])
            c.op("dve", lambda e: e.tensor_scalar(msk[:], lg[:], top8[:, 3:4], None, ALU.is_ge), reads=[lg, top8], writes=[msk])
            c.op("dve", lambda e: e.tensor_scalar(sm[:, 0:1], top8[:, 0:1], -1.0, None, ALU.mult), reads=[top8], writes=[sm])
            c.op("act", lambda e: e.activation(ex[:], lg[:], AF.Exp, bias=sm[:, 0:1], scale=1.0), reads=[lg, sm], writes=[ex])
            c.op("dve", lambda e: e.tensor_tensor(ex[:], ex[:], msk[:], ALU.mult), reads=[ex, msk], writes=[ex])
            c.op("dve", lambda e: e.tensor_reduce(sm[:, 1:2], ex[:], AX.X, ALU.add), reads=[ex], writes=[sm])
            c.op("dve", lambda e: e.reciprocal(sm[:, 1:2], sm[:, 1:2]), reads=[sm], writes=[sm])
            c.op("dve", lambda e: e.tensor_scalar(Gall[:, t, :], ex[:], sm[:, 1:2], None, ALU.mult), reads=[ex, sm], writes=[Gt[t]])
            psg = PS[1]
            c.op("pe", lambda e: e.transpose(psg[0:32, 0:128], Gall[:, t, :], ident[:]), reads=[Gt[t], ident], writes=[psg])
            c.op("act", lambda e: e.activation(GT[:, ts_], psg[0:32, 0:128], AF.Copy), reads=[psg], writes=[GTt[t]])
        c.pop()
        c.push()
        TP = 1024; NTP = TP // 128; NTG = TP // 512
        bup = c.sbuf([128, NE, 16], F32, "bup")
        c.dma("sp", bup[:], P["bupT"][:], writes=[bup])
        Gsc = c.sbuf([128, NT, NE], F32, "Gsc")
        c.op("dve", lambda e: e.tensor_scalar(Gsc[:], Gall[:], 1.0 / 1.702, None, ALU.mult), reads=Gt, writes=[Gsc])
        acc = c.sbuf([128, NTP, D], F32, "acc"); acct = [Buf(None, "acct%d" % t) for t in range(NTP)]
        actT = c.sbuf([128, 8, TP], BF16, "actT"); actb = [[Buf(None, "act_%d_%d" % (j, tg)) for tg in range(NTG)] for j in range(8)]
        wus = [c.sbuf([128, 8, 256], F32, "wus%d" % i) for i in range(3)]; wub = [c.sbuf([128, 8, 256], BF16, "wub%d" % i) for i in range(4)]
        wds = [c.sbuf([128, 8, 512], F32, "wds%d" % i) for i in range(2)]; wdb = [c.sbuf([128, 8, 512], BF16, "wdb%d" % i) for i in range(2)]
        gs = [c.sbuf([128, 512], F32, "gs%d" % i) for i in range(2)]; sg = [c.sbuf([128, 512], F32, "sg%d" % i) for i in range(2)]
        ls = [c.sbuf([128, 512], F32, "ls%d" % i) for i in range(2)]
        bds = c.sbuf([32, D], F32, "bds")
        c.dma("sp", bds[:], P["bdn"][:], writes=[bds])
        fin = c.sbuf([128, D], F32, "fin")
        wup_v = P["wup"].t.rearrange("e (k p) n -> e p k n", p=128)
        wdn_v = P["wdn"].t.rearrange("e (j p) n -> e p j n", p=128)
        slices = [(tp, e_, j) for tp in range(T // TP) for e_ in range(n_exp) for j in range(8)]

        def load_slice(i):
            tp, e_, j = slices[i]
            st = wus[i % 3]; wb_ = wub[i % 4]
            c.dma("sp", st[:, :, 0:128], wup_v[e_, :, :, j * 128:(j + 1) * 128], writes=[st])
            c.dma("sp", st[:, :, 128:256], wup_v[e_, :, :, D + j * 128:D + (j + 1) * 128], writes=[st], nowaw=True)
            c.op("act", lambda e: e.activation(wb_[:].rearrange("p k c -> p (k c)"), st[:].rearrange("p k c -> p (k c)"), AF.Copy), reads=[st], writes=[wb_])

        def load_down(e_):
            for c2 in range(2):
                st = wds[c2]; wd_ = wdb[c2]
                c.dma("sp", st[:], wdn_v[e_, :, :, c2 * 512:(c2 + 1) * 512], writes=[st])
                c.op("act", lambda e, st=st, wd_=wd_: e.activation(wd_[:].rearrange("p j c -> p (j c)"), st[:].rearrange("p j c -> p (j c)"), AF.Copy), reads=[st], writes=[wd_])

        PF = 2
        for i in range(min(PF, len(slices))):
            load_slice(i)
        ie = 0
        for i, (tp, e_, j) in enumerate(slices):
            t0 = tp * NTP
            if i + PF < len(slices):
                load_slice(i + PF)
            if j == 1:
                load_down(e_)
            wb_ = wub[i % 4]
            for tg in range(NTG):
                tsl = slice(tp * TP + tg * 512, tp * TP + (tg + 1) * 512)
                pg = PS[(ie * 2) % 4]; pl = PS[(ie * 2 + 1) % 4]; g_ = gs[ie % 2]; s_ = sg[ie % 2]; l_ = ls[ie % 2]; ie += 1
                hrd = h2Tt[tsl.start // 128:tsl.stop // 128]
                for k in range(8):
                    c.op("pe", lambda e, k=k: e.matmul(pg[:], wb_[:, k, 0:128], h2T[:, k, tsl], start=(k == 0), stop=(k == 7)), reads=[wb_] + hrd, writes=[pg], acc=True)
                for k in range(8):
                    c.op("pe", lambda e, k=k: e.matmul(pl[:], wb_[:, k, 128:256], h2T[:, k, tsl], start=(k == 0), stop=(k == 7)), reads=[wb_] + hrd, writes=[pl], acc=True)
                c.op("dve", lambda e: e.tensor_scalar(g_[:], pg[:], bup[:, e_, j:j + 1], 7.0, ALU.add, ALU.min), reads=[pg, bup], writes=[g_])
                c.op("act", lambda e: e.activation(l_[:], pl[:], AF.Identity, bias=bup[:, e_, 8 + j:9 + j], scale=1.0), reads=[pl, bup], writes=[l_])
                c.op("act", lambda e: e.activation(s_[:], g_[:], AF.Silu, scale=1.702), reads=[g_], writes=[s_])
                c.op("pool", lambda e: e.tensor_scalar(l_[:], l_[:], 7.0, -7.0, ALU.min, ALU.max), reads=[l_], writes=[l_])
                c.op("dve", lambda e: e.scalar_tensor_tensor(actT[:, j, tg * 512:(tg + 1) * 512], l_[:], 1.0, s_[:], ALU.add, ALU.mult), reads=[l_, s_], writes=[actb[j][tg]])
            if j == 7:
                for c2 in range(2):
                    wd_ = wdb[c2]
                    for tt in range(NTP):
                        po = PS[4 + (tt % 4)]
                        for jj in range(8):
                            c.op("pe", lambda e, jj=jj: e.matmul(po[:], actT[:, jj, tt * 128:(tt + 1) * 128], wd_[:, jj, :], start=(jj == 0), stop=(jj == 7)),
                                 reads=[actb[jj][tt // 4], wd_], writes=[po], acc=True)
                        dst = acc[:, tt, c2 * 512:(c2 + 1) * 512]
                        gsc = Gsc[:, t0 + tt, e_:e_ + 1]
                        if e_ == 0:
                            c.op("dve", lambda e: e.tensor_scalar(dst, po[:], gsc, None, ALU.mult), reads=[po, Gsc], writes=[acct[tt]])
                        else:
                            c.op("dve", lambda e: e.scalar_tensor_tensor(dst, po[:], gsc, dst, ALU.mult, ALU.add), reads=[po, Gsc, acct[tt]], writes=[acct[tt]])
                if e_ == n_exp - 1:
                    for tt in range(NTP):
                        t = t0 + tt; ts_ = slice(t * 128, (t + 1) * 128)
                        f = fin
                        x1b = wds[0]; x1 = x1b[:].rearrange("p j c -> p (j c)")[:, 0:D]
                        c.dma("sp", x1, xbuf_t[ts_, :], reads=[xbt[t]], writes=[x1b])
                        for hf in range(2):
                            pb = PS[hf]
                            c.op("pe", lambda e, pb=pb, hf=hf: e.matmul(pb[:], GT[:, ts_], bds[:, hf * 512:(hf + 1) * 512], start=True, stop=True), reads=[GTt[t], bds], writes=[pb])
                            c.op("dve", lambda e, pb=pb, hf=hf: e.tensor_tensor(f[:, hf * 512:(hf + 1) * 512], pb[:], acc[:, tt, hf * 512:(hf + 1) * 512], ALU.add), reads=[pb, acct[tt]], writes=[f])
                        c.op("dve", lambda e: e.tensor_tensor(f[:], f[:], g2b[:], ALU.mult), reads=[f, g2b], writes=[f])
                        c.op("dve", lambda e: e.tensor_tensor(f[:], f[:], x1, ALU.add), reads=[f, x1b], writes=[f])
                        c.dma("pool", dst_t[ts_, :], f[:], reads=[f], writes=[dst_b[t]])
        c.pop()
        c.pop()

    for l in range(depth):
        if l == 0:
            xs_t, xs_b = x_in.t, xint
        else:
            xs_t, xs_b = xbuf_t, xbt
        partA(l, xs_t, xs_b)
        XA32.gather(); XA16.gather()
        XA32.localize("sp", pid_sp)

        def after_gla():
            XBO.gather(); XBO.localize("pool", pid_pool)

        partB(l, after_gla, lambda: XA16.localize("sp", pid_sp))
        XBA.gather(); XBA.localize("pool", pid_pool)
        last = (l == depth - 1)
        partC(l, xs_t, xs_b, out.t if last else xbuf_t, outt if last else xbt)
    c.finish(outt, "pool")
    c.finish(outt, "sp")
    c.close()
    return c

BF = ml_dtypes.bfloat16


def pp(v):
    v = np.asarray(v, np.float32).reshape(-1, 128)
    return np.ascontiguousarray(v.T)


def rep128(v):
    v = np.asarray(v, np.float32)
    return np.ascontiguousarray(np.broadcast_to(v[None], (128,) + v.shape))


def prepF(inputs):
    i128 = np.arange(128)
    glob = dict(
        cT=pp(inputs["c"][0]), cTb=np.ascontiguousarray(np.broadcast_to(pp(inputs["c"][0])[:, :, None], (128, 8, 128))),
        ident=np.eye(128, dtype=np.float32),
        bd32=(i128[:, None] // 32 == i128[None, :] // 32).astype(np.float32),
        bd64=(i128[:, None] // 64 == i128[None, :] // 64).astype(np.float32))
    k = np.arange(128)[:, None]; q = np.arange(512)[None, :]
    glob["cmask"] = np.stack([(128 * d + k <= q) for d in range(4)], axis=1).astype(BF)
    Z = np.zeros((32, 32, 128), BF)
    for m in range(32):
        Z[m, m, :] = 1
    glob["Z"] = Z
    glob["iota"] = np.tile(np.arange(64, dtype=np.float32)[None, :], (128, 1))
    rmask = np.ones((32, 2048), np.float32); rmask[:, ::64] = 0
    glob["rmask"] = rmask
    j = np.arange(64)[:, None]; i = np.arange(64)[None, :]
    glob["tri8"] = np.tile((j <= i).astype(np.float32), (1, 8))
    per_layer = []
    for l in range(2):
        sfx = str(l)
        gains = np.zeros((128, 8), np.float32)
        gains[:, 0] = np.tile(inputs["a_q_gain"][l], 4); gains[:, 1] = np.tile(inputs["a_k_gain"][l], 4)
        gains[:, 2] = np.tile(inputs["b_q_gain"][l], 2); gains[:, 3] = np.tile(inputs["b_k_gain"][l], 2)
        gains[:, 4] = inputs["c_b_g"][l]
        ab = inputs["ada_b"][l]
        lam_init = 0.8 - 0.6 * math.exp(-0.3 * l)
        d = {
            "adaw" + sfx: np.ascontiguousarray(inputs["ada_w"][l]), "adabT" + sfx: pp(ab[:2048]), "n1gT" + sfx: pp(inputs["norm1_g"][l]),
            "w_in" + sfx: np.ascontiguousarray(inputs["w_in"][l]), "gains" + sfx: gains, "wg2" + sfx: np.ascontiguousarray(inputs["c_w_g2"][l]),
            "adabB" + sfx: rep128(np.stack([ab[2048:3072], ab[5120:6144]])), "adabT2" + sfx: pp(ab[3072:5120]),
            "lamv" + sfx: rep128(np.stack([inputs["a_lam_q1"][l], inputs["a_lam_k1"][l], inputs["a_lam_q2"][l], inputs["a_lam_k2"][l]])),
            "lamc" + sfx: np.tile(np.array([[lam_init, 1.0 - lam_init]], np.float32), (128, 1)),
            "aog" + sfx: rep128(inputs["a_out_gain"][l]), "cog" + sfx: rep128(inputs["c_out_gain"][l]), "n2gT" + sfx: pp(inputs["norm2_g"][l]),
            "w_out" + sfx: np.ascontiguousarray(inputs["w_out"][l]),
            "rwT" + sfx: np.ascontiguousarray(inputs["router_w"][l].reshape(8, 128, 32).transpose(1, 0, 2)), "rbB" + sfx: rep128(inputs["router_b"][l]),
            "wup" + sfx: np.ascontiguousarray(inputs["exp_w_up"][l]),
            "bupT" + sfx: np.ascontiguousarray(inputs["exp_b_up"][l].reshape(32, 16, 128).transpose(2, 0, 1)),
            "wdn" + sfx: np.ascontiguousarray(inputs["exp_w_down"][l]), "bdn" + sfx: np.ascontiguousarray(inputs["exp_b_down"][l])}
        per_layer.append(d)
    maps = []
    x = inputs["x"][0]
    for r in range(8):
        p = r % 2; nb = r % 4
        m = dict(glob)
        for d in per_layer:
            m.update(d)
        m["x"] = np.ascontiguousarray(x[r * 2048:(r + 1) * 2048])
        m["par"] = np.tile(np.array([[1.0 - p, float(p)]], np.float32), (128, 1))
        m["bmask"] = np.concatenate([(256 * p + 128 * hh + k <= q) for hh in range(2)], axis=1).astype(BF)
        for l in range(2):
            sfx = str(l)
            rw = np.zeros((64, 8), np.float32)
            sl = slice(64 * nb, 64 * nb + 64)
            rw[:, 0:4] = inputs["d_conv_w"][l][:, sl].T; rw[:, 4] = inputs["d_conv_b"][l][sl]; rw[:, 5] = inputs["d_b_a"][l][sl]
            rw[:, 6] = inputs["d_b_x"][l][sl]; rw[:, 7] = inputs["d_lambda"][l][sl]
            m["rw" + sfx] = rw
            m["rwa" + sfx] = np.ascontiguousarray(inputs["d_w_a"][l][nb]); m["rwx" + sfx] = np.ascontiguousarray(inputs["d_w_x"][l][nb])
        maps.append(m)
    return maps


def kernel(**inputs):
    inputs = {k: np.asarray(v) for k, v in inputs.items()}
    cF = build_F()
    maps = prepF(inputs)
    R = run_bass_kernel_spmd(cF.nc, maps, core_ids=list(range(8))).results
    x = np.concatenate([np.asarray(r["out"]) for r in R], axis=0)
    return np.ascontiguousarray(x[None]).astype(np.float32)
```

```python
import contextlib, math
import numpy as np
import ml_dtypes
import concourse.bass as bass
import concourse.mybir as mybir
from concourse.bass_utils import run_bass_kernel_spmd


F32 = mybir.dt.float32
BF16 = mybir.dt.bfloat16
I32 = mybir.dt.int32
AF = mybir.ActivationFunctionType
ALU = mybir.AluOpType
AX = mybir.AxisListType


class Buf:
    def __init__(self, t, name):
        self.t = t
        self.name = name
        self.w = {}
        self.r = {}

    def __getitem__(self, idx):
        return self.t[idx]


class Ctx:
    NDS = 8

    def __init__(self):
        self.nc = bass.Bass("TRN2", target_bir_lowering=False)
        nc = self.nc
        self.es = contextlib.ExitStack()
        self.es_root = self.es
        self.E = {"pe": nc.tensor, "act": nc.scalar, "dve": nc.vector, "pool": nc.gpsimd, "sp": nc.sync}
        self.sems = {}
        self.cnt = {}
        for e in ("pe", "act", "dve", "pool"):
            self.sems[e] = self.es.enter_context(nc.semaphore("s_" + e))
            self.cnt[e] = 0
        self.dq = {}
        for q in ("sp", "pool", "act"):
            ss = []
            for i in range(self.NDS):
                k = "d_%s%d" % (q, i)
                self.sems[k] = self.es.enter_context(nc.semaphore(k))
                ss.append(k)
            self.dq[q] = [ss, 0]
        self.seen = {e: {} for e in self.E}
        self.ekey = {}
        self.eno = {}
        self.nbuf = 0
        self.ninstr = 0

    def sbuf(self, shape, dt, name=None):
        self.nbuf += 1
        name = "sb%d_%s" % (self.nbuf, name or "x")
        t = self.es.enter_context(self.nc.sbuf_tensor(name, list(shape), dt))
        return Buf(t, name)

    def psum(self, shape, dt, name=None):
        self.nbuf += 1
        name = "ps%d_%s" % (self.nbuf, name or "x")
        t = self.es.enter_context(self.nc.psum_tensor(name, list(shape), dt))
        return Buf(t, name)

    def dram(self, name, shape, dt, kind):
        t = self.nc.dram_tensor(name, list(shape), dt, kind=kind).ap()
        return Buf(t, name)

    def _wait(self, eng, tok):
        if tok is None:
            return
        k, v = tok
        if self.seen[eng].get(k, 0) >= v:
            return
        self.E[eng].wait_ge(self.sems[k], v)
        self.seen[eng][k] = v
        self.ninstr += 1

    def _deps(self, eng, reads, writes, acc=False, nowaw=False):
        for b in reads:
            for k, v in b.w.items():
                self._wait(eng, (k, v))
        for b in writes:
            if not (nowaw or (acc and eng == "pe" and all(k.split("#")[0] == "pe" for k in b.w))):
                for k, v in b.w.items():
                    self._wait(eng, (k, v))
            for k, v in b.r.items():
                self._wait(eng, (k, v))

    def _mark(self, tok, reads, writes, nowaw=False):
        k, v = tok
        for b in reads:
            if b.r.get(k, 0) < v:
                b.r[k] = v
        for b in writes:
            if nowaw:
                b.w[k] = max(b.w.get(k, 0), v)
            else:
                b.w = {k: v}
                b.r = {}

    EPOCH = 24000

    def op(self, eng, fn, reads=(), writes=(), acc=False):
        key = self.ekey.get(eng, eng)
        if self.cnt[key] >= self.EPOCH:
            n = self.eno.get(eng, 0) + 1
            self.eno[eng] = n
            key = "%s#%d" % (eng, n)
            self.sems[key] = self.es_root.enter_context(self.nc.semaphore("s_%s_%d" % (eng, n)))
            self.cnt[key] = 0
            self.ekey[eng] = key
        self._deps(eng, reads, writes, acc)
        ins = fn(self.E[eng])
        self.cnt[key] += 1
        ins.then_inc(self.sems[key], 1)
        self.ninstr += 1
        self._mark((key, self.cnt[key]), reads, writes)
        return ins

    def dma(self, q, out, in_, reads=(), writes=(), nowaw=False, **kw):
        ss, j = self.dq[q]
        k = ss[j % self.NDS]
        rnd = j // self.NDS
        if rnd > 0:
            self._wait(q, (k, 16 * rnd))
        self._deps(q, reads, writes, nowaw=nowaw)
        if callable(in_):
            in_ = in_()
        if callable(out):
            out = out()
        ins = self.E[q].dma_start(out=out, in_=in_, **kw)
        ins.then_inc(self.sems[k], 16)
        self.dq[q][1] = j + 1
        self.ninstr += 1
        self._mark((k, 16 * (rnd + 1)), reads, writes, nowaw=nowaw)
        return ins

    def collective(self, kind, send_t, recv_t, reads=(), writes=()):
        if "cc" not in self.sems:
            self.sems["cc"] = self.es_root.enter_context(self.nc.semaphore("s_cc"))
            self.cnt["cc"] = 0
        self._deps("pool", reads, writes)
        ins = self.nc.gpsimd.collective_compute(kind, ALU.bypass, replica_groups=[list(range(8))],
                                                ins=[send_t.ap().opt()], outs=[recv_t.ap().opt()])
        self.cnt["cc"] += 1
        ins.then_inc(self.sems["cc"])
        self.ninstr += 1
        self._mark(("cc", self.cnt["cc"]), reads, writes)
        return ins

    def finish(self, bufs, eng="sp"):
        for b in bufs:
            for k, v in b.w.items():
                self._wait(eng, (k, v))

    def close(self):
        self.es.close()


def _dq_tokens(self):
    toks = [(k, v) for k, v in self.cnt.items() if v > 0]
    for q, (ss, j) in self.dq.items():
        for i, k in enumerate(ss):
            n = (j - i + self.NDS - 1) // self.NDS if j > i else 0
            if n > 0:
                toks.append((k, 16 * n))
    return toks


def _barrier(self):
    toks = _dq_tokens(self)
    for e in ("pe", "act", "dve", "pool", "sp"):
        for tk in toks:
            self._wait(e, tk)


def _push(self):
    self._outer = getattr(self, "_outer", [])
    self._outer.append(self.es)
    self.es = contextlib.ExitStack()


def _pop(self):
    _barrier(self)
    self.es.close()
    self.es = self._outer.pop()


Ctx.barrier = _barrier
Ctx.push = _push
Ctx.pop = _pop


T = 2048
NT = T // 128
D = 1024
DIN = 2832
EPS = 1e-6
NE = 32
S = 16384
BIG = 1.0e9
BIGB = 30000.0
DEPTH = 2


class XBig:
    def __init__(self, c, name, R, W, dt):
        self.c = c; self.R = R; self.W = W
        self.st = c.nc.dram_tensor(name + "_s", [8 * R, W], dt)
        self.rt = c.nc.dram_tensor(name + "_r", [64 * R, W], dt)
        self.mt = c.nc.dram_tensor(name + "_m", [8 * R, W], dt)
        self.sb = Buf(self.st, name + "_s"); self.rb = Buf(self.rt, name + "_r"); self.mb = Buf(self.mt, name + "_m")

    def blk_m(self, src):
        return self.mt[src * self.R:(src + 1) * self.R, :]

    def localize(self, q, pid):
        v = self.rt.ap().rearrange("(s d r) w -> s d r w", s=8, d=8)
        for src in range(8):
            self.c.dma(q, self.blk_m(src), (lambda src=src: v[src, bass.ds(pid, 1), :, :].rearrange("o r w -> (o r) w")),
                       reads=[self.rb], writes=[self.mb], nowaw=(src > 0))

    def blk_s(self, dest):
        return self.st[dest * self.R:(dest + 1) * self.R, :]

    def blk_r(self, src):
        v = self.rt.ap().rearrange("(s d r) w -> s d r w", s=8, d=8)
        return v[src, bass.ds(self.c.pid, 1), :, :].rearrange("o r w -> (o r) w")

    def gather(self):
        self.c.collective("AllGather", self.st, self.rt, reads=[self.sb], writes=[self.rb])


class XB:
    def __init__(self, big, off, nrows, mode, p):
        self.big = big; self.off = off; self.nrows = nrows; self.mode = mode; self.p = p
        self.sb = big.sb; self.rb = big.mb

    def _view(self, blk):
        x = blk[self.off:self.off + self.nrows, :]
        if self.mode == "wide":
            x = x.rearrange("(a x) w -> a (x w)", x=self.p)
        elif self.mode == "narrow":
            x = x.rearrange("r (y e) -> (r y) e", e=self.p)
        elif self.mode == "km":
            x = x[:, 0:512].rearrange("o (a e) -> (o a) e", e=8)
        return x

    def s(self, dest, r0, r1):
        return self._view(self.big.blk_s(dest))[r0:r1, :]

    def r(self, src, r0, r1):
        return self._view(self.big.blk_m(src))[r0:r1, :]


def build_F(n_exp=NE, depth=DEPTH):
    c = Ctx(); nc = c.nc
    pid_sp = nc.sync.partition_id(); pid_pool = nc.gpsimd.partition_id()
    inp = lambda n, s, d=F32: c.dram(n, s, d, "ExternalInput")
    x_in = inp("x", [T, D]); cT_d = inp("cT", [128, 8]); cTb_d = inp("cTb", [128, 8, 128])
    ident_d = inp("ident", [128, 128]); bd32_d = inp("bd32", [128, 128]); bd64_d = inp("bd64", [128, 128])
    par_d = inp("par", [128, 2]); bmask_d = inp("bmask", [128, 1024], BF16)
    cmask_d = inp("cmask", [128, 4, 512], BF16); Z_d = inp("Z", [32, 32, 128], BF16); iota_d = inp("iota", [128, 64])
    rmask_d = inp("rmask", [32, 2048]); tri8_d = inp("tri8", [64, 512])
    L = []
    for l in range(depth):
        sfx = str(l)
        L.append(dict(
            adaw=inp("adaw" + sfx, [D, 6144]), adabT=inp("adabT" + sfx, [128, 16]), n1gT=inp("n1gT" + sfx, [128, 8]), w_in=inp("w_in" + sfx, [D, DIN]),
            gains=inp("gains" + sfx, [128, 8]), wg2=inp("wg2" + sfx, [16, 128]),
            rw=inp("rw" + sfx, [64, 8]), rwa=inp("rwa" + sfx, [64, 64]), rwx=inp("rwx" + sfx, [64, 64]),
            adabB=inp("adabB" + sfx, [128, 2, 1024]), adabT2=inp("adabT2" + sfx, [128, 16]), lamv=inp("lamv" + sfx, [128, 4, 32]), lamc=inp("lamc" + sfx, [128, 2]),
            aog=inp("aog" + sfx, [128, 64]), cog=inp("cog" + sfx, [128, 64]), n2gT=inp("n2gT" + sfx, [128, 8]), w_out=inp("w_out" + sfx, [D, D]),
            rwT=inp("rwT" + sfx, [128, 8, 32]), rbB=inp("rbB" + sfx, [128, 32]),
            wup=inp("wup" + sfx, [NE, D, 2 * D]), bupT=inp("bupT" + sfx, [128, NE, 16]), wdn=inp("wdn" + sfx, [NE, D, D]), bdn=inp("bdn" + sfx, [NE, D])))
    out = c.dram("out", [T, D], F32, "ExternalOutput")
    outt = [Buf(out.t, "out%d" % t) for t in range(NT)]
    xbuf_t = nc.dram_tensor("xbuf", [T, D], F32)
    xbt = [Buf(xbuf_t, "xb%d" % t) for t in range(NT)]
    xint = [Buf(x_in.t, "xin%d" % t) for t in range(NT)]
    crb_t = nc.dram_tensor("crb", [T, 256], F32)
    crbt = [Buf(crb_t, "crb%d" % t) for t in range(NT)]
    XA16 = XBig(c, "xa16", 256, 2048, BF16)
    XA32 = XBig(c, "xa32", 705, 1024, F32)
    XBA = XBig(c, "xba", 512, 1024, F32)
    XBO = XBig(c, "xbo", 256, 1024, F32)
    XQK = XB(XA16, 0, 64, "native", None)
    XMQ = XB(XA16, 64, 64, "native", None)
    XMK = XB(XA16, 128, 32, "narrow", 1024)
    XDV = XB(XA16, 160, 64, "narrow", 64)
    XMV = XB(XA16, 224, 32, "narrow", 64)
    XMQ32 = XB(XA32, 0, 128, "wide", 2)
    XG = XB(XA32, 128, 192, "wide", 2)
    XGV = XB(XA32, 320, 128, "narrow", 64)
    XRG = XB(XA32, 448, 256, "wide", 2)
    XKM = XB(XA32, 704, 1, "km", None)
    XOA = XB(XBA, 0, 256, "narrow", 128); XOB = XB(XBA, 256, 256, "narrow", 128)
    XOC = XB(XBO, 0, 128, "narrow", 64); XOD = XB(XBO, 128, 128, "narrow", 64)

    ident = c.sbuf([128, 128], F32, "ident"); epsT = c.sbuf([128, 1], F32, "epsT"); oneT = c.sbuf([128, 1], F32, "oneT")
    c.dma("sp", ident[:], ident_d[:], writes=[ident])
    c.op("dve", lambda e: e.memset(epsT[:], EPS), writes=[epsT])
    c.op("dve", lambda e: e.memset(oneT[:], 1.0), writes=[oneT])
    sq_ = [0]

    def stq():
        sq_[0] += 1
        return "pool" if sq_[0] % 2 else "sp"

    def partA(l, xsrc_t, xsrc_b):
        P = L[l]
        c.push()
        PS = [c.psum([128, 512], F32, "apsb%d_%d" % (l, i)) for i in range(8)]
        bd32 = c.sbuf([128, 128], F32, "bd32"); bd64 = c.sbuf([128, 128], F32, "bd64")
        gn = c.sbuf([128, 8], F32, "gn"); cond = c.sbuf([128, 8], F32, "cond"); adab = c.sbuf([128, 16], F32, "adab"); n1g = c.sbuf([128, 8], F32, "n1g")
        wg2s = c.sbuf([16, 128], F32, "wg2s")
        for sb, dr in ((bd32, bd32_d), (bd64, bd64_d), (gn, P["gains"]), (cond, cT_d), (adab, P["adabT"]), (n1g, P["n1gT"]), (wg2s, P["wg2"])):
            c.dma("sp", sb[:], dr[:], writes=[sb])
        c.op("act", lambda e: e.activation(cond[:], cond[:], AF.Silu), reads=[cond], writes=[cond])
        adaw_v = P["adaw"].t.rearrange("(k p) n -> p k n", p=128)
        wst = [c.sbuf([128, 8, 512], F32, "adst%d" % i) for i in range(2)]
        modps = PS[0]
        for jj in range(4):
            st = wst[jj % 2]
            c.dma("sp", st[:], adaw_v[:, :, jj * 512:(jj + 1) * 512], writes=[st])
            for j4 in range(4):
                j = jj * 4 + j4
                for k in range(8):
                    c.op("pe", lambda e, k=k, j=j, j4=j4, st=st: e.matmul(modps[:, j:j + 1], st[:, k, j4 * 128:(j4 + 1) * 128], cond[:, k:k + 1],
                                                                      start=(k == 0), stop=(k == 7)), reads=[st, cond], writes=[modps], acc=True)
        mod = c.sbuf([128, 16], F32, "mod")
        c.op("dve", lambda e: e.tensor_tensor(mod[:], modps[:, 0:16], adab[:], ALU.add), reads=[modps, adab], writes=[mod])
        a1 = c.sbuf([128, 8], F32, "a1")
        c.op("dve", lambda e: e.scalar_tensor_tensor(a1[:], mod[:, 8:16], 1.0, n1g[:], ALU.add, ALU.mult), reads=[mod, n1g], writes=[a1])
        wb = c.sbuf([128, 8, DIN], BF16, "wb")
        wbk = [Buf(None, "wbk%d" % k) for k in range(8)]
        win_v = P["w_in"].t.rearrange("(k p) n -> p k n", p=128)
        wstage = [c.sbuf([128, DIN], F32, "wstage%d" % i) for i in range(2)]
        for k in range(8):
            st = wstage[k % 2]
            c.dma("sp", st[:], win_v[:, k, :], writes=[st])
            eng = "dve" if k % 2 == 0 else "pool"
            c.op(eng, lambda e, k=k, st=st: e.tensor_copy(wb[:, k, :], st[:]), reads=[st], writes=[wbk[k]])
        hT = c.sbuf([128, 8, T], BF16, "hT")
        hTt = [Buf(None, "hTt%d" % t) for t in range(NT)]
        xts = [c.sbuf([128, D], F32, "xt%d" % i) for i in range(2)]
        junk = c.sbuf([128, D], BF16, "junk")
        stat = [c.sbuf([128, 2], F32, "stat%d" % i) for i in range(2)]
        for t in range(NT):
            xt = xts[t % 2]; stt = stat[t % 2]
            c.dma("sp", xt[:], xsrc_t[t * 128:(t + 1) * 128, :], reads=[xsrc_b[t]], writes=[xt])
            c.op("act", lambda e: e.activation(junk[:], xt[:], AF.Square, accum_out=stt[:, 0:1]), reads=[xt], writes=[junk, stt])
            c.op("act", lambda e: e.activation(stt[:, 1:2], stt[:, 0:1], AF.Sqrt, bias=epsT[:], scale=1.0 / D), reads=[stt, epsT], writes=[stt])
            c.op("dve", lambda e: e.reciprocal(stt[:, 1:2], stt[:, 1:2]), reads=[stt], writes=[stt])
            c.op("dve", lambda e: e.tensor_scalar(xt[:], xt[:], stt[:, 1:2], None, ALU.mult), reads=[xt, stt], writes=[xt])
            for half in range(2):
                ps = PS[1 + half]
                for kk in range(4):
                    k = half * 4 + kk
                    c.op("pe", lambda e, k=k, kk=kk, ps=ps: e.transpose(ps[:, kk * 128:(kk + 1) * 128], xt[:, k * 128:(k + 1) * 128], ident[:]),
                         reads=[xt, ident], writes=[ps], acc=True)
                for kk in range(4):
                    k = half * 4 + kk
                    if kk % 2 == 0:
                        c.op("dve", lambda e, k=k, kk=kk, ps=ps: e.tensor_scalar(hT[:, k, t * 128:(t + 1) * 128], ps[:, kk * 128:(kk + 1) * 128],
                                                                     a1[:, k:k + 1], mod[:, k:k + 1], ALU.mult, ALU.add),
                             reads=[ps, a1, mod], writes=[hTt[t]])
                    else:
                        c.op("act", lambda e, k=k, kk=kk, ps=ps: e.activation(hT[:, k, t * 128:(t + 1) * 128], ps[:, kk * 128:(kk + 1) * 128],
                                                                  AF.Identity, bias=mod[:, k:k + 1], scale=a1[:, k:k + 1]),
                             reads=[ps, a1, mod], writes=[hTt[t]])
        pi = [0]

        def nextps():
            pi[0] += 1
            return PS[3 + pi[0] % 5]

        def proj_fm(col0, ncols, g):
            ps = nextps()
            for k in range(8):
                c.op("pe", lambda e, k=k: e.matmul(ps[0:ncols, :], wb[:, k, col0:col0 + ncols], hT[:, k, g * 512:(g + 1) * 512],
                                                   start=(k == 0), stop=(k == 7)),
                     reads=[wbk[k]] + hTt[g * 4:(g + 1) * 4], writes=[ps], acc=True)
            return ps

        sqb = [c.sbuf([128, 512], F32, "sq%d" % i) for i in range(2)]
        rsb = [c.sbuf([128, 512], F32, "rs%d" % i) for i in range(2)]
        ob16 = [c.sbuf([128, 512], BF16, "ob16_%d" % i) for i in range(3)]
        ob32 = [c.sbuf([128, 512], F32, "ob32_%d" % i) for i in range(3)]
        kms = c.sbuf([128, 2, NT // 2], F32, "kms")
        kmsb = [Buf(None, "kmsb%d" % i) for i in range(2)]
        ctr = [0]

        def send(xb, dest, r0, r1, cols, src_ap, src_buf):
            c.dma(stq(), xb.s(dest, r0, r1)[:, cols], src_ap, reads=[src_buf], writes=[xb.sb], nowaw=True)

        def normed(ps, bd, inv_d, gcol, want32):
            i = ctr[0]; ctr[0] += 1
            sq = sqb[i % 2]; rs = rsb[i % 2]; o16 = ob16[i % 3]
            c.op("act", lambda e: e.activation(sq[:], ps[:], AF.Square), reads=[ps], writes=[sq])
            ps2 = nextps()
            c.op("pe", lambda e: e.matmul(ps2[:], bd[:], sq[:], start=True, stop=True), reads=[bd, sq], writes=[ps2])
            c.op("act", lambda e: e.activation(rs[:], ps2[:], AF.Sqrt, bias=epsT[:], scale=inv_d), reads=[ps2, epsT], writes=[rs])
            c.op("dve", lambda e: e.reciprocal(rs[:], rs[:]), reads=[rs], writes=[rs])
            c.op("dve", lambda e: e.scalar_tensor_tensor(o16[:], ps[:], gn[:, gcol:gcol + 1], rs[:], ALU.mult, ALU.mult), reads=[ps, gn, rs], writes=[o16])
            o32 = None
            if want32:
                o32 = ob32[i % 3]
                c.op("dve", lambda e: e.scalar_tensor_tensor(o32[:], ps[:], gn[:, gcol:gcol + 1], rs[:], ALU.mult, ALU.mult), reads=[ps, gn, rs], writes=[o32])
            return o16, o32

        def raw32(ps, nrows):
            i = ctr[0]; ctr[0] += 1
            o32 = ob32[i % 3]
            if i % 2:
                c.op("act", lambda e: e.activation(o32[0:nrows, :], ps[0:nrows, :], AF.Copy), reads=[ps], writes=[o32])
            else:
                c.op("dve", lambda e: e.tensor_copy(o32[0:nrows, :], ps[0:nrows, :]), reads=[ps], writes=[o32])
            return o32

        lt = [c.sbuf([128, 512], F32, "lt%d" % i) for i in range(3)]
        tm16 = [c.sbuf([128, 512], BF16, "tm16_%d" % i) for i in range(2)]
        tm32 = [c.sbuf([128, 512], F32, "tm32_%d" % i) for i in range(2)]
        cgs = c.sbuf([16, 512], F32, "cgs")
        for g in range(T // 512):
            gc = slice(g * 512, (g + 1) * 512)
            for ch in range(2):
                o16, _ = normed(proj_fm(0 + ch * 128, 128, g), bd32, 1.0 / 32, 0, False)
                for m_ in range(4):
                    send(XQK, 4 * ch + m_, 0, 32, gc, o16[32 * m_:32 * m_ + 32, :], o16)
                o16, _ = normed(proj_fm(256 + ch * 128, 128, g), bd32, 1.0 / 32, 1, False)
                for m_ in range(4):
                    send(XQK, 4 * ch + m_, 32, 64, gc, o16[32 * m_:32 * m_ + 32, :], o16)
                o16, o32 = normed(proj_fm(768 + ch * 128, 128, g), bd64, 1.0 / 64, 2, True)
                for hh in range(2):
                    h = 2 * ch + hh
                    for p_ in range(2):
                        send(XMQ, 2 * h + p_, 0, 64, gc, o16[64 * hh:64 * hh + 64, :], o16)
                        send(XMQ32, 2 * h + p_, 0, 64, gc, o32[64 * hh:64 * hh + 64, :], o32)
                o16, o32 = normed(proj_fm(1024 + ch * 128, 128, g), bd64, 1.0 / 64, 3, True)
                c.op("dve", lambda e, ch=ch, o32=o32: e.tensor_reduce(kms[:, ch, g * 2:(g + 1) * 2], o32[:].rearrange("p (b t) -> p b t", t=256), AX.X, ALU.add),
                     reads=[o32], writes=[kmsb[ch]])
                for hh in range(2):
                    h = 2 * ch + hh
                    for p_ in range(2):
                        send(XMK, 2 * h + p_, 0, 64, slice(g * 256, (g + 1) * 256), o16[64 * hh:64 * hh + 64, p_ * 256:(p_ + 1) * 256], o16)
                o32 = raw32(proj_fm(2320 + ch * 128, 128, g), 128)
                for hh in range(2):
                    send(XRG, 2 * ch + hh, 0, 64, gc, o32[64 * hh:64 * hh + 64, :], o32)
                o32 = raw32(proj_fm(2576 + ch * 128, 128, g), 128)
                for hh in range(2):
                    send(XRG, 2 * ch + hh, 64, 128, gc, o32[64 * hh:64 * hh + 64, :], o32)
            o32 = raw32(proj_fm(1536, 128, g), 128)
            for hc in range(4):
                send(XG, hc, 0, 32, gc, o32[32 * hc:32 * hc + 32, :], o32)
            o32 = raw32(proj_fm(1664, 128, g), 128)
            for hc in range(4):
                send(XG, hc, 32, 64, gc, o32[32 * hc:32 * hc + 32, :], o32)
            psg = proj_fm(2048, 16, g)
            c.op("dve", lambda e: e.tensor_copy(cgs[:], psg[0:16, :]), reads=[psg], writes=[cgs])
            psz = nextps()
            c.op("pe", lambda e: e.matmul(psz[:], wg2s[:], cgs[:], start=True, stop=True), reads=[wg2s, cgs], writes=[psz])
            z, az, m = lt
            c.op("dve", lambda e: e.tensor_scalar(z[:], psz[:], gn[:, 4:5], None, ALU.add), reads=[psz, gn], writes=[z])
            c.op("act", lambda e: e.activation(az[:], z[:], AF.Abs), reads=[z], writes=[az])
            c.op("act", lambda e: e.activation(az[:], az[:], AF.Exp, scale=-1.0), reads=[az], writes=[az])
            c.op("act", lambda e: e.activation(az[:], az[:], AF.Ln, bias=oneT[:], scale=1.0), reads=[az, oneT], writes=[az])
            c.op("dve", lambda e: e.tensor_scalar(m[:], z[:], 0.0, None, ALU.min), reads=[z], writes=[m])
            c.op("dve", lambda e: e.tensor_tensor(m[:], m[:], az[:], ALU.subtract), reads=[m, az], writes=[m])
            c.op("dve", lambda e: e.tensor_scalar(m[:], m[:], 1.0 / 16, None, ALU.mult), reads=[m], writes=[m])
            for hc in range(4):
                send(XG, hc, 64, 96, gc, m[32 * hc:32 * hc + 32, :], m)
            for tt in range(4):
                t = g * 4 + tt
                ps = nextps()
                for (o, col0) in ((0, 512), (256, 1280)):
                    for k in range(8):
                        c.op("pe", lambda e, k=k, o=o, col0=col0: e.matmul(ps[:, o:o + 256], hT[:, k, t * 128:(t + 1) * 128], wb[:, k, col0:col0 + 256],
                                                                        start=(k == 0), stop=(k == 7)), reads=[wbk[k], hTt[t]], writes=[ps], acc=True)
                o16 = tm16[t % 2]
                c.op("act", lambda e: e.activation(o16[:], ps[:], AF.Copy), reads=[ps], writes=[o16])
                par_ = (t // 2) % 2; row = ((t // 2) // 2) * 256 + (t % 2) * 128
                for h in range(4):
                    for i2 in range(2):
                        send(XDV, 2 * h + i2, t * 128, (t + 1) * 128, slice(0, 64), o16[:, 64 * h:64 * h + 64], o16)
                    send(XMV, 2 * h + par_, row, row + 128, slice(0, 64), o16[:, 256 + 64 * h:256 + 64 * h + 64], o16)
                ps = nextps()
                for (o, col0) in ((0, 1792), (256, 2064)):
                    for k in range(8):
                        c.op("pe", lambda e, k=k, o=o, col0=col0: e.matmul(ps[:, o:o + 256], hT[:, k, t * 128:(t + 1) * 128], wb[:, k, col0:col0 + 256],
                                                                        start=(k == 0), stop=(k == 7)), reads=[wbk[k], hTt[t]], writes=[ps], acc=True)
                o32 = tm32[t % 2]
                c.op("dve", lambda e: e.tensor_copy(o32[:], ps[:]), reads=[ps], writes=[o32])
                for hc in range(4):
                    send(XGV, hc, t * 128, (t + 1) * 128, slice(0, 64), o32[:, 64 * hc:64 * hc + 64], o32)
                c.dma(stq(), crb_t[t * 128:(t + 1) * 128, :], o32[:, 256:512], reads=[o32], writes=[crbt[t]])
        for ch in range(2):
            for hh in range(2):
                h = 2 * ch + hh
                for p_ in range(2):
                    send(XKM, 2 * h + p_, 0, 64, slice(0, 8), kms[64 * hh:64 * hh + 64, ch, :], kmsb[ch])
        c.pop()

    def partB(l, after_gla, before_attn):
        P = L[l]
        c.push()
        PS1 = [c.psum([128, 512], F32, "bps1_%d_%d" % (l, i)) for i in range(4)]
        PS2 = [c.psum([128, 1024], F32, "bps2_%d_%d" % (l, i)) for i in range(2)]
        c.push()
        PW = 2048
        rw = c.sbuf([64, 8], F32, "rw"); rwa = c.sbuf([64, 64], F32, "rwa"); rwx = c.sbuf([64, 64], F32, "rwx")
        for sb, dr in ((rw, P["rw"]), (rwa, P["rwa"]), (rwx, P["rwx"])):
            c.dma("sp", sb[:], dr[:], writes=[sb])
        cl = c.sbuf([64, 2], F32, "cl")
        c.op("act", lambda e: e.activation(cl[:, 0:1], rw[:, 7:8], AF.Exp, scale=-1.0), reads=[rw], writes=[cl])
        c.op("act", lambda e: e.activation(cl[:, 0:1], cl[:, 0:1], AF.Ln, bias=oneT[0:64, :], scale=1.0), reads=[cl, oneT], writes=[cl])
        c.op("dve", lambda e: e.tensor_scalar(cl[:, 1:2], cl[:, 0:1], -8.0, None, ALU.mult), reads=[cl], writes=[cl])
        xin = [c.sbuf([64, PW + 3], F32, "xin%d" % i) for i in range(2)]
        gin = [c.sbuf([64, PW], F32, "gin%d" % i) for i in range(2)]
        xc = c.sbuf([64, PW], F32, "xc"); rgt = c.sbuf([64, PW], F32, "rgt"); igt = c.sbuf([64, PW], F32, "igt")
        aa = c.sbuf([64, PW], F32, "aa"); bt = c.sbuf([64, PW], F32, "bt")
        hh_ = [c.sbuf([64, PW], F32, "hh%d" % i) for i in range(2)]
        uu = c.sbuf([64, PW], F32, "uu"); odT = c.sbuf([128, 16, 64], F32, "odT")
        for pc in range(S // PW):
            xi = xin[pc % 2]; gi = gin[pc % 2]; h = hh_[pc % 2]; hp = hh_[(pc + 1) % 2]
            c.dma("sp", xi[:, 3:PW + 3], (lambda: XRG.r(pc, 0, 64)), reads=[XRG.rb], writes=[xi])
            if pc == 0:
                c.op("dve", lambda e: e.memset(xi[:, 0:3], 0.0), writes=[xi])
            else:
                c.dma("sp", xi[:, 0:3], (lambda: XRG.r(pc - 1, 0, 64)[:, PW - 3:PW]), reads=[XRG.rb], writes=[xi], nowaw=True)
            c.dma("sp", gi[:], (lambda: XRG.r(pc, 64, 128)), reads=[XRG.rb], writes=[gi])
            c.op("dve", lambda e: e.tensor_scalar(xc[:], xi[:, 3:PW + 3], rw[:, 3:4], rw[:, 4:5], ALU.mult, ALU.add), reads=[xi, rw], writes=[xc])
            for j in range(3):
                c.op("dve", lambda e, j=j: e.scalar_tensor_tensor(xc[:], xi[:, j:PW + j], rw[:, j:j + 1], xc[:], ALU.mult, ALU.add), reads=[xi, rw, xc], writes=[xc])
            for grp in range(PW // 512):
                sl = slice(grp * 512, (grp + 1) * 512)
                p1 = PS1[0]; p2 = PS1[1]
                c.op("pe", lambda e: e.matmul(p1[0:64, :], rwa[:], xc[:, sl], start=True, stop=True), reads=[rwa, xc], writes=[p1])
                c.op("pe", lambda e: e.matmul(p2[0:64, :], rwx[:], xc[:, sl], start=True, stop=True), reads=[rwx, xc], writes=[p2])
                c.op("act", lambda e: e.activation(rgt[:, sl], p1[0:64, :], AF.Sigmoid, bias=rw[:, 5:6], scale=1.0), reads=[p1, rw], writes=[rgt])
                c.op("act", lambda e: e.activation(igt[:, sl], p2[0:64, :], AF.Sigmoid, bias=rw[:, 6:7], scale=1.0), reads=[p2, rw], writes=[igt])
            c.op("act", lambda e: e.activation(aa[:], rgt[:], AF.Exp, scale=cl[:, 1:2]), reads=[rgt, cl], writes=[aa])
            c.op("act", lambda e: e.activation(bt[:], aa[:], AF.Square), reads=[aa], writes=[bt])
            c.op("dve", lambda e: e.tensor_scalar(bt[:], bt[:], -1.0, 1.0, ALU.mult, ALU.add), reads=[bt], writes=[bt])
            c.op("act", lambda e: e.activation(bt[:], bt[:], AF.Sqrt), reads=[bt], writes=[bt])
            c.op("dve", lambda e: e.tensor_tensor(bt[:], bt[:], igt[:], ALU.mult), reads=[bt, igt], writes=[bt])
            c.op("dve", lambda e: e.tensor_tensor(bt[:], bt[:], xc[:], ALU.mult), reads=[bt, xc], writes=[bt])
            if pc == 0:
                c.op("dve", lambda e: e.tensor_tensor_scan(h[:], aa[:], bt[:], 0.0, ALU.mult, ALU.add), reads=[aa, bt], writes=[h])
            else:
                c.op("dve", lambda e: e.tensor_tensor_scan(h[:], aa[:], bt[:], hp[:, PW - 1:PW], ALU.mult, ALU.add), reads=[aa, bt, hp], writes=[h])
            c.op("dve", lambda e: e.tensor_tensor(uu[:], gi[:], gi[:], ALU.mult), reads=[gi], writes=[uu])
            c.op("dve", lambda e: e.tensor_scalar(uu[:], uu[:], 0.044715, 1.0, ALU.mult, ALU.add), reads=[uu], writes=[uu])
            c.op("dve", lambda e: e.tensor_tensor(uu[:], uu[:], gi[:], ALU.mult), reads=[uu, gi], writes=[uu])
            c.op("act", lambda e: e.activation(uu[:], uu[:], AF.Sigmoid, scale=1.5957691216057308), reads=[uu], writes=[uu])
            c.op("dve", lambda e: e.tensor_tensor(uu[:], uu[:], gi[:], ALU.mult), reads=[uu, gi], writes=[uu])
            c.op("dve", lambda e: e.tensor_tensor(uu[:], uu[:], h[:], ALU.mult), reads=[uu, h], writes=[uu])
            for half in range(2):
                pt = PS1[2 + half]
                for k8 in range(8):
                    k = half * 8 + k8
                    c.op("pe", lambda e, k=k, k8=k8, pt=pt: e.transpose(pt[:, k8 * 64:(k8 + 1) * 64], uu[:, k * 128:(k + 1) * 128], ident[0:64, 0:64]),
                         reads=[uu, ident], writes=[pt], acc=True)
                c.op("act", lambda e, pt=pt, half=half: e.activation(odT[:, half * 8:(half + 1) * 8, :].rearrange("p k e -> p (k e)"), pt[:], AF.Copy), reads=[pt], writes=[odT])
            c.dma("pool", XOD.s(pc, 0, 2048).rearrange("(t p) e -> p t e", p=128), odT[:], reads=[odT], writes=[XOD.sb], nowaw=True)
        c.pop()
        c.push()
        NCH = PW // 64
        rmask = c.sbuf([32, PW], F32, "rmask"); tri8 = c.sbuf([64, 512], F32, "tri8")
        c.dma("sp", rmask[:], rmask_d[:], writes=[rmask]); c.dma("sp", tri8[:], tri8_d[:], writes=[tri8])
        qs = [c.sbuf([32, PW], F32, "gq%d" % i) for i in range(2)]
        ks = [c.sbuf([32, PW], F32, "gk%d" % i) for i in range(2)]
        gs_ = [c.sbuf([32, PW], F32, "gg%d" % i) for i in range(2)]
        vs = [c.sbuf([64, NCH, 64], F32, "gv%d" % i) for i in range(2)]
        bcum = c.sbuf([32, PW], F32, "bcum"); eb = c.sbuf([32, PW], F32, "eb"); qe = c.sbuf([32, PW], F32, "qe")
        ke = c.sbuf([32, PW], F32, "ke"); kl = c.sbuf([32, PW], F32, "kl"); dec = c.sbuf([32, NCH], F32, "dec")
        attT = c.sbuf([64, NCH, 64], F32, "attT"); klT = c.sbuf([64, 256], F32, "klT")
        U = c.sbuf([32, NCH, 64], F32, "U"); Sall = c.sbuf([32, NCH + 1, 64], F32, "Sall")
        osb = [c.sbuf([64, 8, 64], F32, "gosb%d" % i) for i in range(2)]
        c.op("dve", lambda e: e.memset(Sall[:, 0, :], 0.0), writes=[Sall])
        for pc in range(S // PW):
            q = qs[pc % 2]; k = ks[pc % 2]; g = gs_[pc % 2]; v = vs[pc % 2]
            c.dma("sp", q[:], (lambda: XG.r(pc, 0, 32)), reads=[XG.rb], writes=[q]); c.dma("sp", k[:], (lambda: XG.r(pc, 32, 64)), reads=[XG.rb], writes=[k])
            c.dma("sp", g[:], (lambda: XG.r(pc, 64, 96)), reads=[XG.rb], writes=[g])
            c.dma("sp", v[:], (lambda: XGV.r(pc, 0, 2048).rearrange("(c p) e -> p c e", p=64)), reads=[XGV.rb], writes=[v])
            c.op("dve", lambda e: e.tensor_tensor_scan(bcum[:], rmask[:], g[:], 0.0, ALU.mult, ALU.add), reads=[rmask, g], writes=[bcum])
            c.op("act", lambda e: e.activation(eb[:], bcum[:], AF.Exp), reads=[bcum], writes=[eb])
            c.op("dve", lambda e: e.scalar_tensor_tensor(qe[:], q[:], 32.0 ** -0.5, eb[:], ALU.mult, ALU.mult), reads=[q, eb], writes=[qe])
            c.op("act", lambda e: e.activation(eb[:], bcum[:], AF.Exp, scale=-1.0), reads=[bcum], writes=[eb])
            c.op("dve", lambda e: e.tensor_tensor(ke[:], k[:], eb[:], ALU.mult), reads=[k, eb], writes=[ke])
            bc3 = bcum[:].rearrange("p (c t) -> p c t", t=64)
            c.op("act", lambda e: e.activation(dec[:], bc3[:, :, 63], AF.Exp), reads=[bcum], writes=[dec])
            for cc in range(NCH):
                c.op("dve", lambda e, cc=cc: e.tensor_scalar(kl[:, cc * 64:(cc + 1) * 64], ke[:, cc * 64:(cc + 1) * 64], dec[:, cc:cc + 1], None, ALU.mult),
                     reads=[ke, dec], writes=[kl])
            for grp in range(NCH // 8):
                pA, pT_, pU = PS1[0], PS1[1], PS1[2]
                for cc in range(8):
                    ch = grp * 8 + cc; sl = slice(ch * 64, (ch + 1) * 64)
                    c.op("pe", lambda e, cc=cc, sl=sl: e.matmul(pA[0:64, cc * 64:(cc + 1) * 64], ke[:, sl], qe[:, sl], start=True, stop=True),
                         reads=[ke, qe], writes=[pA], acc=True)
                c.op("dve", lambda e: e.tensor_tensor(attT[:, grp * 8:(grp + 1) * 8, :].rearrange("p c t -> p (c t)"), pA[0:64, :], tri8[:], ALU.mult),
                     reads=[pA, tri8], writes=[attT])
                for cc in range(8):
                    ch = grp * 8 + cc; sl = slice(ch * 64, (ch + 1) * 64)
                    c.op("pe", lambda e, cc=cc, sl=sl: e.transpose(pT_[0:64, cc * 32:(cc + 1) * 32], kl[:, sl], ident[0:32, 0:32]),
                         reads=[kl, ident], writes=[pT_], acc=True)
                c.op("act", lambda e: e.activation(klT[:], pT_[0:64, 0:256], AF.Copy), reads=[pT_], writes=[klT])
                for cc in range(8):
                    ch = grp * 8 + cc
                    c.op("pe", lambda e, cc=cc, ch=ch: e.matmul(pU[0:32, cc * 64:(cc + 1) * 64], klT[:, cc * 32:(cc + 1) * 32], v[:, ch, :], start=True, stop=True),
                         reads=[klT, v], writes=[pU], acc=True)
                c.op("dve", lambda e: e.tensor_copy(U[:, grp * 8:(grp + 1) * 8, :].rearrange("p c t -> p (c t)"), pU[0:32, :]), reads=[pU], writes=[U])
            for e_ in range(64):
                c.op("dve", lambda e, e_=e_: e.tensor_tensor_scan(Sall[:, 1:NCH + 1, e_], dec[:], U[:, :, e_], Sall[:, 0, e_:e_ + 1], ALU.mult, ALU.add),
                     reads=[dec, U, Sall], writes=[Sall])
            for grp in range(NCH // 8):
                pO = PS1[3]
                for cc in range(8):
                    ch = grp * 8 + cc; sl = slice(ch * 64, (ch + 1) * 64)
                    c.op("pe", lambda e, cc=cc, ch=ch: e.matmul(pO[0:64, cc * 64:(cc + 1) * 64], attT[:, ch, :], v[:, ch, :], start=True, stop=False),
                         reads=[attT, v], writes=[pO], acc=True)
                    c.op("pe", lambda e, cc=cc, ch=ch, sl=sl: e.matmul(pO[0:64, cc * 64:(cc + 1) * 64], qe[:, sl], Sall[:, ch, :], start=False, stop=True),
                         reads=[qe, Sall], writes=[pO], acc=True)
                o = osb[grp % 2]
                c.op("act", lambda e: e.activation(o[:].rearrange("p c t -> p (c t)"), pO[0:64, :], AF.Copy), reads=[pO], writes=[o])
                c.dma("pool", XOC.s(pc, grp * 512, (grp + 1) * 512).rearrange("(c p) e -> p c e", p=64), o[:], reads=[o], writes=[XOC.sb], nowaw=True)
            c.op("dve", lambda e: e.tensor_copy(Sall[:, 0, :], Sall[:, NCH, :]), reads=[Sall], writes=[Sall])
        c.pop()
        after_gla()
        c.push()
        before_attn()
        qT = c.sbuf([64, S], BF16, "qT"); kT = c.sbuf([64, S], BF16, "kT"); V = c.sbuf([128, 128, 65], BF16, "V")
        cmask = c.sbuf([128, 4, 512], BF16, "cmask"); bmask = c.sbuf([128, 1024], BF16, "bmask")
        pTs = [c.sbuf([128, 1024], BF16, "pT%d" % i) for i in range(3)]
        osbs = [c.sbuf([65, 512], F32, "osb%d" % i) for i in range(2)]
        oTs = [c.sbuf([128, 4, 65], F32, "oT%d" % i) for i in range(2)]
        c.dma("sp", cmask[:], cmask_d[:], writes=[cmask]); c.dma("sp", bmask[:], bmask_d[:], writes=[bmask])
        c.op("dve", lambda e: e.memset(V[:, :, 64:65], 1.0), writes=[V])

        def run_unit(groups, Kd, scale, XO):
            steps = []
            for gi, G in enumerate(groups):
                n = len(G["pairs"])
                for pi_, pr in enumerate(G["pairs"]):
                    steps.append((gi, pi_, n, pr))

            def emit_S(i):
                gi, pi_, n, (j0, j1) = steps[i]
                G = groups[gi]; g = G["g"]
                sps = PS2[i % 2]
                for hh, j in enumerate((j0, j1)):
                    if G["extra"] is None:
                        c.op("pe", lambda e, hh=hh, j=j: e.matmul(sps[:, hh * 512:(hh + 1) * 512], kT[0:Kd, j * 128:(j + 1) * 128], qT[0:Kd, g * 512:(g + 1) * 512],
                                                                 start=True, stop=True), reads=[kT, qT], writes=[sps], acc=True)
                    else:
                        zl, biasT, Zb = G["extra"](pi_)
                        c.op("pe", lambda e, hh=hh, j=j: e.matmul(sps[:, hh * 512:(hh + 1) * 512], kT[0:Kd, j * 128:(j + 1) * 128], qT[0:Kd, g * 512:(g + 1) * 512],
                                                                 start=True, stop=False), reads=[kT, qT], writes=[sps], acc=True)
                        c.op("pe", lambda e, hh=hh, zl=zl, biasT=biasT: e.matmul(sps[:, hh * 512:(hh + 1) * 512], zl, biasT[:], start=False, stop=True),
                             reads=[Zb, biasT], writes=[sps], acc=True)

            if groups[0].get("part1"):
                groups[0]["part1"](); groups[0]["part2"]()
            emit_S(0)
            for i, (gi, pi_, n, (j0, j1)) in enumerate(steps):
                G = groups[gi]; g = G["g"]
                sps = PS2[i % 2]; pT = pTs[i % 3]; pO = PS1[g % 2]
                if pi_ == 0 and gi + 1 < len(groups) and groups[gi + 1].get("part1"):
                    groups[gi + 1]["part1"]()
                c.op("act", lambda e: e.activation(pT[:], sps[:], AF.Exp, scale=scale), reads=[sps], writes=[pT])
                if pi_ in G["masks"]:
                    mk_ap, mk_buf = G["masks"][pi_]
                    c.op("dve", lambda e: e.tensor_tensor(pT[:], pT[:], mk_ap, ALU.mult), reads=[pT, mk_buf], writes=[pT])
                if pi_ == n - 1 and gi + 1 < len(groups) and groups[gi + 1].get("part2"):
                    groups[gi + 1]["part2"]()
                if i + 1 < len(steps):
                    emit_S(i + 1)
                for hh, j in enumerate((j0, j1)):
                    c.op("pe", lambda e, hh=hh, j=j: e.matmul(pO[0:65, :], V[:, j, :], pT[:, hh * 512:(hh + 1) * 512],
                                                             start=(pi_ == 0 and hh == 0), stop=(pi_ == n - 1 and hh == 1)),
                         reads=[V, pT], writes=[pO], acc=True)
                if pi_ == n - 1:
                    o = osbs[g % 2]; oT = oTs[g % 2]
                    c.op("dve", lambda e: e.tensor_copy(o[:], pO[0:65, :]), reads=[pO], writes=[o])
                    ptq = PS1[3] if G["extra"] is None else PS1[0 if g % 2 else 1]
                    ptq = PS1[3] if G["extra"] is None else PS1[3]
                    for qi in range(4):
                        c.op("pe", lambda e, qi=qi: e.transpose(ptq[:, qi * 65:(qi + 1) * 65], o[:, qi * 128:(qi + 1) * 128], ident[0:65, 0:65]),
                             reads=[o, ident], writes=[ptq], acc=True)
                    c.op("dve", lambda e: e.tensor_copy(oT[:].rearrange("p q e -> p (q e)"), ptq[:, 0:260]), reads=[ptq], writes=[oT])
                    c.dma("sp", XO.s(g // 4, (g % 4) * 512, (g % 4 + 1) * 512)[:, 0:65].rearrange("(q p) e -> p q e", p=128), oT[:],
                          reads=[oT], writes=[XO.sb], nowaw=True)

        for i in range(8):
            c.dma("sp", qT[0:32, i * 2048:(i + 1) * 2048], (lambda: XQK.r(i, 0, 32)), reads=[XQK.rb], writes=[qT], nowaw=True)
            c.dma("sp", kT[0:32, i * 2048:(i + 1) * 2048], (lambda: XQK.r(i, 32, 64)), reads=[XQK.rb], writes=[kT], nowaw=True)
            c.dma("sp", V[:, i * 16:(i + 1) * 16, 0:64], (lambda: XDV.r(i, 0, 2048).rearrange("(t p) e -> p t e", p=128)), reads=[XDV.rb], writes=[V], nowaw=True)
        cm2 = cmask[:].rearrange("p d q -> p (d q)")
        groups = []
        for g in range(32):
            npair = 2 * (g + 1)
            pairs = [(2 * i, 2 * i + 1) for i in range(npair)]
            masks = {npair - 2: (cm2[:, 0:1024], cmask), npair - 1: (cm2[:, 1024:2048], cmask)}
            groups.append(dict(g=g, pairs=pairs, masks=masks, extra=None))
        run_unit(groups, 32, 32.0 ** -0.5, XOA)
        Zb = c.sbuf([32, 32, 128], BF16, "Zb"); iota = c.sbuf([128, 64], F32, "iota"); par = c.sbuf([128, 2], F32, "par")
        km = c.sbuf([64, 64], F32, "km")
        c.dma("sp", Zb[:], Z_d[:], writes=[Zb]); c.dma("sp", iota[:], iota_d[:], writes=[iota]); c.dma("sp", par[:], par_d[:], writes=[par])
        for i in range(8):
            c.dma("sp", km[:, 8 * i:8 * i + 8], (lambda: XKM.r(i, 0, 64)), reads=[XKM.rb], writes=[km], nowaw=True)
            c.dma("sp", qT[0:64, i * 2048:(i + 1) * 2048], (lambda: XMQ.r(i, 0, 64)), reads=[XMQ.rb], writes=[qT], nowaw=(i > 0))
            c.dma("sp", kT[0:64, i * 1024:(i + 1) * 1024], (lambda: XMK.r(i, 0, 64)), reads=[XMK.rb], writes=[kT], nowaw=(i > 0))
            c.dma("sp", V[:, i * 8:(i + 1) * 8, 0:64], (lambda: XMV.r(i, 0, 1024).rearrange("(t p) e -> p t e", p=128)), reads=[XMV.rb], writes=[V], nowaw=(i > 0))
        q32s = [c.sbuf([64, 512], F32, "q32_%d" % i) for i in range(2)]
        biasTs = [c.sbuf([32, 512], BF16, "biasT%d" % i) for i in range(2)]
        W = {n: [c.sbuf([128, 64], F32, "mw_%s%d" % (n, i)) for i in range(4)] for n in ("lt", "t1", "gm", "sel", "eq")}
        top8s = [c.sbuf([128, 8], F32, "top8_%d" % i) for i in range(4)]
        bps = [c.sbuf([128, 32], F32, "bp%d" % i) for i in range(4)]
        pG = PS1[3]; pB = PS1[2]

        def mk_part1(g):
            def part1():
                q32 = q32s[g % 2]
                c.dma("sp", q32[:], (lambda: XMQ32.r(g // 4, 0, 64)[:, (g % 4) * 512:(g % 4 + 1) * 512]), reads=[XMQ32.rb], writes=[q32])
                for qi in range(4):
                    c.op("pe", lambda e, qi=qi: e.matmul(pG[:, qi * 64:(qi + 1) * 64], q32[:, qi * 128:(qi + 1) * 128], km[:], start=True, stop=True),
                         reads=[q32, km], writes=[pG], acc=True)
                for qi in range(4):
                    qt = 4 * g + qi; own = float(qt // 2)
                    lt, t1, gm, sel, eq, top8, bp = W["lt"][qi], W["t1"][qi], W["gm"][qi], W["sel"][qi], W["eq"][qi], top8s[qi], bps[qi]
                    pGq = pG[:, qi * 64:(qi + 1) * 64]
                    c.op("dve", lambda e: e.tensor_single_scalar(lt[:], iota[:], own, ALU.is_lt), reads=[iota], writes=[lt])
                    c.op("dve", lambda e: e.tensor_scalar(t1[:], lt[:], -1.0, BIG, ALU.add, ALU.mult), reads=[lt], writes=[t1])
                    c.op("dve", lambda e: e.tensor_tensor(gm[:], pGq, lt[:], ALU.mult), reads=[pG, lt], writes=[gm])
                    c.op("dve", lambda e: e.tensor_tensor(gm[:], gm[:], t1[:], ALU.add), reads=[gm, t1], writes=[gm])
                    c.op("dve", lambda e: e.max(top8[:], gm[:]), reads=[gm], writes=[top8])
                    c.op("dve", lambda e: e.tensor_scalar(sel[:], gm[:], top8[:, 2:3], None, ALU.is_ge), reads=[gm, top8], writes=[sel])
                    c.op("dve", lambda e: e.tensor_tensor(sel[:], sel[:], lt[:], ALU.mult), reads=[sel, lt], writes=[sel])
                    c.op("dve", lambda e: e.tensor_single_scalar(eq[:], iota[:], own, ALU.is_equal), reads=[iota], writes=[eq])
                    c.op("dve", lambda e: e.tensor_tensor(sel[:], sel[:], eq[:], ALU.add), reads=[sel, eq], writes=[sel])
                    c.op("dve", lambda e: e.tensor_scalar(sel[:], sel[:], -1.0, BIGB, ALU.add, ALU.mult), reads=[sel], writes=[sel])
                    s3 = sel[:].rearrange("p (m two) -> p m two", two=2)
                    c.op("dve", lambda e: e.tensor_scalar(bp[:], s3[:, :, 0], par[:, 0:1], None, ALU.mult), reads=[sel, par], writes=[bp])
                    c.op("dve", lambda e: e.scalar_tensor_tensor(bp[:], s3[:, :, 1], par[:, 1:2], bp[:], ALU.mult, ALU.add), reads=[sel, par, bp], writes=[bp])
            return part1

        def mk_part2(g):
            def part2():
                biasT = biasTs[g % 2]
                for qi in range(4):
                    c.op("pe", lambda e, qi=qi: e.transpose(pB[0:32, qi * 128:(qi + 1) * 128], bps[qi][:], ident[:]), reads=[bps[qi], ident], writes=[pB], acc=True)
                c.op("act", lambda e: e.activation(biasT[:], pB[0:32, :], AF.Copy), reads=[pB], writes=[biasT])
            return part2

        groups = []
        for g in range(32):
            pairs = [(2 * m, 2 * m + 1) for m in range(g + 1)]
            masks = {g: (bmask[:], bmask)}
            groups.append(dict(g=g, pairs=pairs, masks=masks, extra=(lambda m, g=g: (Zb[:, m, :], biasTs[g % 2], Zb)), part1=mk_part1(g), part2=mk_part2(g)))
        run_unit(groups, 64, 64.0 ** -0.5, XOB)
        c.pop()
        c.pop()

    def partC(l, xsrc_t, xsrc_b, dst_t, dst_b):
        P = L[l]
        c.push()
        PS = [c.psum([128, 512], F32, "cpsb%d_%d" % (l, i)) for i in range(8)]
        g2b = c.sbuf([128, D], F32, "g2b")
        h2T = c.sbuf([128, 8, T], BF16, "h2T"); h2Tt = [Buf(None, "h2Tt%d" % t) for t in range(NT)]
        Gall = c.sbuf([128, NT, NE], F32, "Gall"); Gt = [Buf(None, "Gt%d" % t) for t in range(NT)]
        GT = c.sbuf([32, T], F32, "GT"); GTt = [Buf(None, "GTt%d" % t) for t in range(NT)]
        c.push()
        cb = c.sbuf([128, 8, 128], F32, "cb")
        c.dma("sp", cb[:], cTb_d[:], writes=[cb])
        c.op("act", lambda e: e.activation(cb[:], cb[:], AF.Silu), reads=[cb], writes=[cb])
        adaw_v = P["adaw"].t.rearrange("(k p) n -> p k n", p=128)
        wst = [c.sbuf([128, 8, 512], F32, "adst%d" % i) for i in range(2)]
        g1b = c.sbuf([128, D], F32, "g1b"); abB = c.sbuf([128, 2, D], F32, "abB")
        c.dma("sp", abB[:], P["adabB"][:], writes=[abB])
        si = 0
        for gi, (dst, col0) in enumerate(((g1b, 2048), (g2b, 5120))):
            for hf in range(2):
                st = wst[si % 2]; si += 1
                c.dma("sp", st[:], adaw_v[:, :, col0 + hf * 512:col0 + (hf + 1) * 512], writes=[st])
                ps = PS[hf]
                for k in range(8):
                    c.op("pe", lambda e, k=k, st=st, ps=ps: e.matmul(ps[:], cb[:, k, :], st[:, k, :], start=(k == 0), stop=(k == 7)), reads=[cb, st], writes=[ps], acc=True)
                c.op("dve", lambda e, ps=ps, dst=dst, gi=gi, hf=hf: e.tensor_tensor(dst[:, hf * 512:(hf + 1) * 512], ps[:], abB[:, gi, hf * 512:(hf + 1) * 512], ALU.add),
                     reads=[ps, abB], writes=[dst])
        modps = PS[2]
        adab2 = c.sbuf([128, 16], F32, "adab2"); n2g = c.sbuf([128, 8], F32, "n2g")
        c.dma("sp", adab2[:], P["adabT2"][:], writes=[adab2]); c.dma("sp", n2g[:], P["n2gT"][:], writes=[n2g])
        for jj in range(4):
            st = wst[si % 2]; si += 1
            c.dma("sp", st[:], adaw_v[:, :, 3072 + jj * 512:3072 + (jj + 1) * 512], writes=[st])
            for j4 in range(4):
                j = jj * 4 + j4
                for k in range(8):
                    c.op("pe", lambda e, k=k, j=j, j4=j4, st=st: e.matmul(modps[:, j:j + 1], st[:, k, j4 * 128:(j4 + 1) * 128], cb[:, k, 0:1],
                                                                      start=(k == 0), stop=(k == 7)), reads=[st, cb], writes=[modps], acc=True)
        mod2 = c.sbuf([128, 16], F32, "mod2"); a2 = c.sbuf([128, 8], F32, "a2")
        c.op("dve", lambda e: e.tensor_tensor(mod2[:], modps[:, 0:16], adab2[:], ALU.add), reads=[modps, adab2], writes=[mod2])
        c.op("dve", lambda e: e.scalar_tensor_tensor(a2[:], mod2[:, 8:16], 1.0, n2g[:], ALU.add, ALU.mult), reads=[mod2, n2g], writes=[a2])
        lv = c.sbuf([128, 4, 32], F32, "lv"); lsm = c.sbuf([128, 4], F32, "lsm"); nlam = c.sbuf([128, 1], F32, "nlam")
        c.dma("sp", lv[:], P["lamv"][:], writes=[lv])
        c.op("dve", lambda e: e.tensor_tensor(lv[:, 0, :], lv[:, 0, :], lv[:, 1, :], ALU.mult), reads=[lv], writes=[lv])
        c.op("dve", lambda e: e.tensor_tensor(lv[:, 2, :], lv[:, 2, :], lv[:, 3, :], ALU.mult), reads=[lv], writes=[lv])
        c.op("dve", lambda e: e.tensor_reduce(lsm[:, 0:1], lv[:, 0, :], AX.X, ALU.add), reads=[lv], writes=[lsm])
        c.op("dve", lambda e: e.tensor_reduce(lsm[:, 1:2], lv[:, 2, :], AX.X, ALU.add), reads=[lv], writes=[lsm])
        c.op("act", lambda e: e.activation(lsm[:, 2:4], lsm[:, 0:2], AF.Exp), reads=[lsm], writes=[lsm])
        c.op("dve", lambda e: e.tensor_tensor(nlam[:], lsm[:, 3:4], lsm[:, 2:3], ALU.subtract), reads=[lsm], writes=[nlam])
        lamc = c.sbuf([128, 2], F32, "lamc")
        c.dma("sp", lamc[:], P["lamc"][:], writes=[lamc])
        c.op("dve", lambda e: e.tensor_scalar(nlam[:], nlam[:], lamc[:, 0:1], None, ALU.subtract), reads=[nlam, lamc], writes=[nlam])
        aog = c.sbuf([128, 64], F32, "aog"); cog = c.sbuf([128, 64], F32, "cog")
        c.dma("sp", aog[:], P["aog"][:], writes=[aog]); c.dma("sp", cog[:], P["cog"][:], writes=[cog])
        c.op("dve", lambda e: e.tensor_scalar(aog[:], aog[:], lamc[:, 1:2], None, ALU.mult), reads=[aog, lamc], writes=[aog])
        wob = c.sbuf([128, 8, D], BF16, "wob"); wobk = [Buf(None, "wobk%d" % k) for k in range(8)]
        wo_v = P["w_out"].t.rearrange("(k p) n -> p k n", p=128)
        wos = [c.sbuf([128, D], F32, "wos%d" % i) for i in range(2)]
        for k in range(8):
            st = wos[k % 2]
            c.dma("sp", st[:], wo_v[:, k, :], writes=[st])
            c.op("pool", lambda e, k=k, st=st: e.tensor_copy(wob[:, k, :], st[:]), reads=[st], writes=[wobk[k]])
        rw = c.sbuf([128, 8, 32], F32, "rwr"); rb = c.sbuf([128, 32], F32, "rb")
        c.dma("sp", rw[:], P["rwT"][:], writes=[rw]); c.dma("sp", rb[:], P["rbB"][:], writes=[rb])
        oas = [c.sbuf([128, 8, 65], F32, "oas%d" % i) for i in range(2)]; obs = [c.sbuf([128, 8, 65], F32, "obs%d" % i) for i in range(2)]
        ocs = [c.sbuf([128, 256], F32, "ocs%d" % i) for i in range(2)]; crs = [c.sbuf([128, 256], F32, "crs%d" % i) for i in range(2)]
        ys = [c.sbuf([128, D], F32, "ys%d" % i) for i in range(2)]; xs = [c.sbuf([128, D], F32, "xs%d" % i) for i in range(2)]
        rs8 = c.sbuf([128, 8], F32, "rs8"); on = c.sbuf([128, 8, 64], F32, "on"); od_ = c.sbuf([128, 256], F32, "od_"); sq = c.sbuf([128, 256], F32, "sq")
        st4 = c.sbuf([128, 4], F32, "st4"); osum = c.sbuf([128, 4, 65], F32, "osum")
        yT = [c.sbuf([128, 8, 128], BF16, "yT%d" % i) for i in range(2)]
        junk = c.sbuf([128, D], BF16, "junk"); stat = c.sbuf([128, 2], F32, "stat")
        h32 = [c.sbuf([128, 8, 128], F32, "h32_%d" % i) for i in range(2)]
        lg = c.sbuf([128, 32], F32, "lg"); top8 = c.sbuf([128, 8], F32, "top8"); msk = c.sbuf([128, 32], F32, "msk"); ex = c.sbuf([128, 32], F32, "ex")
        sm = c.sbuf([128, 2], F32, "sm")
        for t in range(NT):
            ts_ = slice(t * 128, (t + 1) * 128)
            oa_ = oas[t % 2]; ob_ = obs[t % 2]; oc_ = ocs[t % 2]; cr_ = crs[t % 2]; y = ys[t % 2]; xt = xs[t % 2]
            for m_ in range(8):
                c.dma("sp", oa_[:, m_, :], (lambda: XOA.r(m_, t * 128, (t + 1) * 128)[:, 0:65]), reads=[XOA.rb], writes=[oa_], nowaw=(m_ > 0))
                c.dma("sp", ob_[:, m_, :], (lambda: XOB.r(m_, t * 128, (t + 1) * 128)[:, 0:65]), reads=[XOB.rb], writes=[ob_], nowaw=(m_ > 0))
            for hc in range(4):
                c.dma("sp", oc_[:, hc * 64:(hc + 1) * 64], (lambda: XOC.r(hc, t * 128, (t + 1) * 128)), reads=[XOC.rb], writes=[oc_], nowaw=(hc > 0))
                c.dma("sp", y[:, 768 + hc * 64:768 + (hc + 1) * 64], (lambda: XOD.r(hc, t * 128, (t + 1) * 128)), reads=[XOD.rb], writes=[y], nowaw=(hc > 0))
            c.dma("sp", cr_[:], crb_t[ts_, :], reads=[crbt[t]], writes=[cr_])
            c.dma("sp", xt[:], xsrc_t[ts_, :], reads=[xsrc_b[t]], writes=[xt])
            c.op("dve", lambda e: e.reciprocal(rs8[:], oa_[:, :, 64]), reads=[oa_], writes=[rs8])
            for m in range(8):
                c.op("dve", lambda e, m=m: e.tensor_scalar(on[:, m, :], oa_[:, m, 0:64], rs8[:, m:m + 1], None, ALU.mult), reads=[oa_, rs8], writes=[on])
            on4 = on[:].rearrange("p (h i) d -> p h i d", i=2)
            od3 = od_[:].rearrange("p (h d) -> p h d", d=64)
            c.op("dve", lambda e: e.scalar_tensor_tensor(od3, on4[:, :, 1, :], nlam[:, 0:1], on4[:, :, 0, :], ALU.mult, ALU.add), reads=[on, nlam], writes=[od_])
            c.op("dve", lambda e: e.tensor_tensor(sq[:], od_[:], od_[:], ALU.mult), reads=[od_], writes=[sq])
            c.op("dve", lambda e: e.tensor_reduce(st4[:], sq[:].rearrange("p (h d) -> p h d", d=64), AX.X, ALU.add), reads=[sq], writes=[st4])
            c.op("act", lambda e: e.activation(st4[:], st4[:], AF.Sqrt, bias=epsT[:], scale=1.0 / 64), reads=[st4, epsT], writes=[st4])
            c.op("dve", lambda e: e.reciprocal(st4[:], st4[:]), reads=[st4], writes=[st4])
            for h in range(4):
                c.op("dve", lambda e, h=h: e.scalar_tensor_tensor(y[:, h * 64:(h + 1) * 64], od_[:, h * 64:(h + 1) * 64], st4[:, h:h + 1], aog[:], ALU.mult, ALU.mult),
                     reads=[od_, st4, aog], writes=[y])
            ob4 = ob_[:].rearrange("p (h i) d -> p h i d", i=2)
            c.op("dve", lambda e: e.tensor_tensor(osum[:], ob4[:, :, 0, :], ob4[:, :, 1, :], ALU.add), reads=[ob_], writes=[osum])
            c.op("dve", lambda e: e.reciprocal(rs8[:, 0:4], osum[:, :, 64]), reads=[osum], writes=[rs8])
            for h in range(4):
                c.op("dve", lambda e, h=h: e.tensor_scalar(y[:, 256 + h * 64:256 + (h + 1) * 64], osum[:, h, 0:64], rs8[:, h:h + 1], None, ALU.mult), reads=[osum, rs8], writes=[y])
            c.op("dve", lambda e: e.tensor_tensor(sq[:], oc_[:], oc_[:], ALU.mult), reads=[oc_], writes=[sq])
            c.op("dve", lambda e: e.tensor_reduce(st4[:], sq[:].rearrange("p (h d) -> p h d", d=64), AX.X, ALU.add), reads=[sq], writes=[st4])
            c.op("act", lambda e: e.activation(st4[:], st4[:], AF.Sqrt, bias=epsT[:], scale=1.0 / 64), reads=[st4, epsT], writes=[st4])
            c.op("dve", lambda e: e.reciprocal(st4[:], st4[:]), reads=[st4], writes=[st4])
            c.op("act", lambda e: e.activation(cr_[:], cr_[:], AF.Silu), reads=[cr_], writes=[cr_])
            for h in range(4):
                c.op("dve", lambda e, h=h: e.scalar_tensor_tensor(y[:, 512 + h * 64:512 + (h + 1) * 64], oc_[:, h * 64:(h + 1) * 64], st4[:, h:h + 1], cog[:], ALU.mult, ALU.mult),
                     reads=[oc_, st4, cog], writes=[y])
            c.op("dve", lambda e: e.tensor_tensor(y[:, 512:768], y[:, 512:768], cr_[:], ALU.mult), reads=[y, cr_], writes=[y])
            yt = yT[t % 2]
            for half in range(2):
                ps = PS[2 + half]
                for kk in range(4):
                    k = half * 4 + kk
                    c.op("pe", lambda e, k=k, kk=kk, ps=ps: e.transpose(ps[:, kk * 128:(kk + 1) * 128], y[:, k * 128:(k + 1) * 128], ident[:]), reads=[y, ident], writes=[ps], acc=True)
                c.op("act", lambda e, ps=ps, half=half: e.activation(yt[:, half * 4:(half + 1) * 4, :].rearrange("p k t -> p (k t)"), ps[:], AF.Copy), reads=[ps], writes=[yt])
            for hf in range(2):
                ps = PS[4 + hf]
                for k in range(8):
                    c.op("pe", lambda e, k=k, ps=ps, hf=hf: e.matmul(ps[:], yt[:, k, :], wob[:, k, hf * 512:(hf + 1) * 512], start=(k == 0), stop=(k == 7)),
                         reads=[yt, wobk[k]], writes=[ps], acc=True)
                c.op("dve", lambda e, ps=ps, hf=hf: e.tensor_tensor(y[:, hf * 512:(hf + 1) * 512], ps[:], g1b[:, hf * 512:(hf + 1) * 512], ALU.mult), reads=[ps, g1b], writes=[y])
            c.op("dve", lambda e: e.tensor_tensor(xt[:], xt[:], y[:], ALU.add), reads=[xt, y], writes=[xt])
            c.dma("pool", xbuf_t[ts_, :], xt[:], reads=[xt], writes=[xbt[t]])
            c.op("act", lambda e: e.activation(junk[:], xt[:], AF.Square, accum_out=stat[:, 0:1]), reads=[xt], writes=[junk, stat])
            c.op("act", lambda e: e.activation(stat[:, 1:2], stat[:, 0:1], AF.Sqrt, bias=epsT[:], scale=1.0 / D), reads=[stat, epsT], writes=[stat])
            c.op("dve", lambda e: e.reciprocal(stat[:, 1:2], stat[:, 1:2]), reads=[stat], writes=[stat])
            c.op("dve", lambda e: e.tensor_scalar(y[:], xt[:], stat[:, 1:2], None, ALU.mult), reads=[xt, stat], writes=[y])
            hh = h32[t % 2]
            for half in range(2):
                ps = PS[6 + half]
                for kk in range(4):
                    k = half * 4 + kk
                    c.op("pe", lambda e, k=k, kk=kk, ps=ps: e.transpose(ps[:, kk * 128:(kk + 1) * 128], y[:, k * 128:(k + 1) * 128], ident[:]), reads=[y, ident], writes=[ps], acc=True)
                for kk in range(4):
                    k = half * 4 + kk
                    c.op("dve", lambda e, k=k, kk=kk, ps=ps: e.tensor_scalar(hh[:, k, :], ps[:, kk * 128:(kk + 1) * 128], a2[:, k:k + 1], mod2[:, k:k + 1], ALU.mult, ALU.add),
                         reads=[ps, a2, mod2], writes=[hh])
            c.op("act", lambda e: e.activation(h2T[:, :, ts_], hh[:], AF.Copy), reads=[hh], writes=[h2Tt[t]])
            psr = PS[0]
            for k in range(8):
                c.op("pe", lambda e, k=k: e.matmul(psr[:, 0:32], hh[:, k, :], rw[:, k, :], start=(k == 0), stop=(k == 7)), reads=[hh, rw], writes=[psr], acc=True)
            c.op("dve", lambda e: e.tensor_tensor(lg[:], psr[:, 0:32], rb[:], ALU.add), reads=[psr, rb], writes=[lg])
            c.op("dve", lambda e: e.max(top8[:], lg[:]), reads=[lg], writes=[top8])
            c.op("dve", lambda e: e.tensor_scalar(msk[:], lg[:], top8[:, 3:4], None, ALU.is_ge), reads=[lg, top8], writes=[msk])
            c.op("dve", lambda e: e.tensor_scalar(sm[:, 0:1], top8[:, 0:1], -1.0, None, ALU.mult), reads=[top8], writes=[sm])
            c.op("act", lambda e: e.activation(ex[:], lg[:], AF.Exp, bias=sm[:, 0:1], scale=1.0), reads=[lg, sm], writes=[ex])
            c.op("dve", lambda e: e.tensor_tensor(ex[:], ex[:], msk[:], ALU.mult), reads=[ex, msk], writes=[ex])
            c.op("dve", lambda e: e.tensor_reduce(sm[:, 1:2], ex[:], AX.X, ALU.add), reads=[ex], writes=[sm])
            c.op("dve", lambda e: e.reciprocal(sm[:, 1:2], sm[:, 1:2]), reads=[sm], writes=[sm])
            c.op("dve", lambda e: e.tensor_scalar(Gall[:, t, :], ex[:], sm[:, 1:2], None, ALU.mult), reads=[ex, sm], writes=[Gt[t]])
            psg = PS[1]
            c.op("pe", lambda e: e.transpose(psg[0:32, 0:128], Gall[:, t, :], ident[:]), reads=[Gt[t], ident], writes=[psg])
            c.op("act", lambda e: e.activation(GT[:, ts_], psg[0:32, 0:128], AF.Copy), reads=[psg], writes=[GTt[t]])
        c.pop()
        c.push()
        TP = 1024; NTP = TP // 128; NTG = TP // 512
        bup = c.sbuf([128, NE, 16], F32, "bup")
        c.dma("sp", bup[:], P["bupT"][:], writes=[bup])
        bup1 = c.sbuf([128, NE, 8], F32, "bup1")
        c.op("dve", lambda e: e.tensor_scalar(bup1[:], bup[:, :, 8:16], 1.0, None, ALU.add), reads=[bup], writes=[bup1])
        Gsc = c.sbuf([128, NT, NE], F32, "Gsc")
        c.op("dve", lambda e: e.tensor_scalar(Gsc[:], Gall[:], 1.0 / 1.702, None, ALU.mult), reads=Gt, writes=[Gsc])
        acc = c.sbuf([128, NTP, D], F32, "acc"); acct = [Buf(None, "acct%d" % t) for t in range(NTP)]
        actT = c.sbuf([128, 8, TP], BF16, "actT"); actb = [[Buf(None, "act_%d_%d" % (j, tg)) for tg in range(NTG)] for j in range(8)]
        wus = [c.sbuf([128, 8, 256], F32, "wus%d" % i) for i in range(3)]; wub = [c.sbuf([128, 8, 256], BF16, "wub%d" % i) for i in range(4)]
        wds = [c.sbuf([128, 8, 512], F32, "wds%d" % i) for i in range(2)]; wdb = [c.sbuf([128, 8, 512], BF16, "wdb%d" % i) for i in range(2)]
        gs = [c.sbuf([128, 512], F32, "gs%d" % i) for i in range(2)]; sg = [c.sbuf([128, 512], F32, "sg%d" % i) for i in range(2)]
        ls = [c.sbuf([128, 512], F32, "ls%d" % i) for i in range(2)]
        bds = c.sbuf([32, D], F32, "bds")
        c.dma("sp", bds[:], P["bdn"][:], writes=[bds])
        fin = c.sbuf([128, D], F32, "fin")
        wup_v = P["wup"].t.rearrange("e (k p) n -> e p k n", p=128)
        wdn_v = P["wdn"].t.rearrange("e (j p) n -> e p j n", p=128)
        slices = [(tp, e_, j) for tp in range(T // TP) for e_ in range(n_exp) for j in range(8)]

        def load_slice(i):
            tp, e_, j = slices[i]
            st = wus[i % 3]; wb_ = wub[i % 4]
            c.dma("sp", st[:, :, 0:128], wup_v[e_, :, :, j * 128:(j + 1) * 128], writes=[st])
            c.dma("sp", st[:, :, 128:256], wup_v[e_, :, :, D + j * 128:D + (j + 1) * 128], writes=[st], nowaw=True)
            c.op("act", lambda e: e.activation(wb_[:].rearrange("p k c -> p (k c)"), st[:].rearrange("p k c -> p (k c)"), AF.Copy), reads=[st], writes=[wb_])

        def load_down(e_):
            for c2 in range(2):
                st = wds[c2]; wd_ = wdb[c2]
                c.dma("sp", st[:], wdn_v[e_, :, :, c2 * 512:(c2 + 1) * 512], writes=[st])
                c.op("act", lambda e, st=st, wd_=wd_: e.activation(wd_[:].rearrange("p j c -> p (j c)"), st[:].rearrange("p j c -> p (j c)"), AF.Copy), reads=[st], writes=[wd_])

        PF = 2
        for i in range(min(PF, len(slices))):
            load_slice(i)
        ie = 0
        for i, (tp, e_, j) in enumerate(slices):
            t0 = tp * NTP
            if i + PF < len(slices):
                load_slice(i + PF)
            if j == 1:
                load_down(e_)
            wb_ = wub[i % 4]
            for tg in range(NTG):
                tsl = slice(tp * TP + tg * 512, tp * TP + (tg + 1) * 512)
                pg = PS[(ie * 2) % 4]; pl = PS[(ie * 2 + 1) % 4]; g_ = gs[ie % 2]; s_ = sg[ie % 2]; l_ = ls[ie % 2]; ie += 1
                hrd = h2Tt[tsl.start // 128:tsl.stop // 128]
                for k in range(8):
                    c.op("pe", lambda e, k=k: e.matmul(pg[:], wb_[:, k, 0:128], h2T[:, k, tsl], start=(k == 0), stop=(k == 7)), reads=[wb_] + hrd, writes=[pg], acc=True)
                for k in range(8):
                    c.op("pe", lambda e, k=k: e.matmul(pl[:], wb_[:, k, 128:256], h2T[:, k, tsl], start=(k == 0), stop=(k == 7)), reads=[wb_] + hrd, writes=[pl], acc=True)
                c.op("dve", lambda e: e.tensor_scalar(g_[:], pg[:], bup[:, e_, j:j + 1], 7.0, ALU.add, ALU.min), reads=[pg, bup], writes=[g_])
                c.op("act", lambda e: e.activation(s_[:], g_[:], AF.Silu, scale=1.702), reads=[g_], writes=[s_])
                c.op("dve", lambda e: e.tensor_scalar(l_[:], pl[:], bup1[:, e_, j:j + 1], -6.0, ALU.add, ALU.max), reads=[pl, bup1], writes=[l_])
                c.op("dve", lambda e: e.scalar_tensor_tensor(actT[:, j, tg * 512:(tg + 1) * 512], l_[:], 8.0, s_[:], ALU.min, ALU.mult), reads=[l_, s_], writes=[actb[j][tg]])
            if j == 7:
                for c2 in range(2):
                    wd_ = wdb[c2]
                    for tt in range(NTP):
                        po = PS[4 + (tt % 4)]
                        for jj in range(8):
                            c.op("pe", lambda e, jj=jj: e.matmul(po[:], actT[:, jj, tt * 128:(tt + 1) * 128], wd_[:, jj, :], start=(jj == 0), stop=(jj == 7)),
                                 reads=[actb[jj][tt // 4], wd_], writes=[po], acc=True)
                        dst = acc[:, tt, c2 * 512:(c2 + 1) * 512]
                        gsc = Gsc[:, t0 + tt, e_:e_ + 1]
                        if e_ == 0:
                            c.op("dve", lambda e: e.tensor_scalar(dst, po[:], gsc, None, ALU.mult), reads=[po, Gsc], writes=[acct[tt]])
                        else:
                            c.op("dve", lambda e: e.scalar_tensor_tensor(dst, po[:], gsc, dst, ALU.mult, ALU.add), reads=[po, Gsc, acct[tt]], writes=[acct[tt]])
                if e_ == n_exp - 1:
                    for tt in range(NTP):
                        t = t0 + tt; ts_ = slice(t * 128, (t + 1) * 128)
                        f = fin
                        x1b = wds[0]; x1 = x1b[:].rearrange("p j c -> p (j c)")[:, 0:D]
                        c.dma("sp", x1, xbuf_t[ts_, :], reads=[xbt[t]], writes=[x1b])
                        for hf in range(2):
                            pb = PS[hf]
                            c.op("pe", lambda e, pb=pb, hf=hf: e.matmul(pb[:], GT[:, ts_], bds[:, hf * 512:(hf + 1) * 512], start=True, stop=True), reads=[GTt[t], bds], writes=[pb])
                            c.op("dve", lambda e, pb=pb, hf=hf: e.tensor_tensor(f[:, hf * 512:(hf + 1) * 512], pb[:], acc[:, tt, hf * 512:(hf + 1) * 512], ALU.add), reads=[pb, acct[tt]], writes=[f])
                        c.op("dve", lambda e: e.tensor_tensor(f[:], f[:], g2b[:], ALU.mult), reads=[f, g2b], writes=[f])
                        c.op("dve", lambda e: e.tensor_tensor(f[:], f[:], x1, ALU.add), reads=[f, x1b], writes=[f])
                        c.dma("pool", dst_t[ts_, :], f[:], reads=[f], writes=[dst_b[t]])
        c.pop()
        c.pop()

    for l in range(depth):
        if l == 0:
            xs_t, xs_b = x_in.t, xint
        else:
            xs_t, xs_b = xbuf_t, xbt
        partA(l, xs_t, xs_b)
        XA32.gather(); XA16.gather()
        XA32.localize("sp", pid_sp)

        def after_gla():
            XBO.gather(); XBO.localize("pool", pid_pool)

        partB(l, after_gla, lambda: XA16.localize("sp", pid_sp))
        XBA.gather(); XBA.localize("pool", pid_pool)
        last = (l == depth - 1)
        partC(l, xs_t, xs_b, out.t if last else xbuf_t, outt if last else xbt)
    c.finish(outt, "pool")
    c.finish(outt, "sp")
    c.close()
    return c

BF = ml_dtypes.bfloat16


def pp(v):
    v = np.asarray(v, np.float32).reshape(-1, 128)
    return np.ascontiguousarray(v.T)


def rep128(v):
    v = np.asarray(v, np.float32)
    return np.ascontiguousarray(np.broadcast_to(v[None], (128,) + v.shape))


def prepF(inputs):
    i128 = np.arange(128)
    glob = dict(
        cT=pp(inputs["c"][0]), cTb=np.ascontiguousarray(np.broadcast_to(pp(inputs["c"][0])[:, :, None], (128, 8, 128))),
        ident=np.eye(128, dtype=np.float32),
        bd32=(i128[:, None] // 32 == i128[None, :] // 32).astype(np.float32),
        bd64=(i128[:, None] // 64 == i128[None, :] // 64).astype(np.float32))
    k = np.arange(128)[:, None]; q = np.arange(512)[None, :]
    glob["cmask"] = np.stack([(128 * d + k <= q) for d in range(4)], axis=1).astype(BF)
    Z = np.zeros((32, 32, 128), BF)
    for m in range(32):
        Z[m, m, :] = 1
    glob["Z"] = Z
    glob["iota"] = np.tile(np.arange(64, dtype=np.float32)[None, :], (128, 1))
    rmask = np.ones((32, 2048), np.float32); rmask[:, ::64] = 0
    glob["rmask"] = rmask
    j = np.arange(64)[:, None]; i = np.arange(64)[None, :]
    glob["tri8"] = np.tile((j <= i).astype(np.float32), (1, 8))
    per_layer = []
    for l in range(2):
        sfx = str(l)
        gains = np.zeros((128, 8), np.float32)
        gains[:, 0] = np.tile(inputs["a_q_gain"][l], 4); gains[:, 1] = np.tile(inputs["a_k_gain"][l], 4)
        gains[:, 2] = np.tile(inputs["b_q_gain"][l], 2); gains[:, 3] = np.tile(inputs["b_k_gain"][l], 2)
        gains[:, 4] = inputs["c_b_g"][l]
        ab = inputs["ada_b"][l]
        lam_init = 0.8 - 0.6 * math.exp(-0.3 * l)
        d = {
            "adaw" + sfx: np.ascontiguousarray(inputs["ada_w"][l]), "adabT" + sfx: pp(ab[:2048]), "n1gT" + sfx: pp(inputs["norm1_g"][l]),
            "w_in" + sfx: np.ascontiguousarray(inputs["w_in"][l]), "gains" + sfx: gains, "wg2" + sfx: np.ascontiguousarray(inputs["c_w_g2"][l]),
            "adabB" + sfx: rep128(np.stack([ab[2048:3072], ab[5120:6144]])), "adabT2" + sfx: pp(ab[3072:5120]),
            "lamv" + sfx: rep128(np.stack([inputs["a_lam_q1"][l], inputs["a_lam_k1"][l], inputs["a_lam_q2"][l], inputs["a_lam_k2"][l]])),
            "lamc" + sfx: np.tile(np.array([[lam_init, 1.0 - lam_init]], np.float32), (128, 1)),
            "aog" + sfx: rep128(inputs["a_out_gain"][l]), "cog" + sfx: rep128(inputs["c_out_gain"][l]), "n2gT" + sfx: pp(inputs["norm2_g"][l]),
            "w_out" + sfx: np.ascontiguousarray(inputs["w_out"][l]),
            "rwT" + sfx: np.ascontiguousarray(inputs["router_w"][l].reshape(8, 128, 32).transpose(1, 0, 2)), "rbB" + sfx: rep128(inputs["router_b"][l]),
            "wup" + sfx: np.ascontiguousarray(inputs["exp_w_up"][l]),
            "bupT" + sfx: np.ascontiguousarray(inputs["exp_b_up"][l].reshape(32, 16, 128).transpose(2, 0, 1)),
            "wdn" + sfx: np.ascontiguousarray(inputs["exp_w_down"][l]), "bdn" + sfx: np.ascontiguousarray(inputs["exp_b_down"][l])}
        per_layer.append(d)
    maps = []
    x = inputs["x"][0]
    for r in range(8):
        p = r % 2; nb = r % 4
        m = dict(glob)
        for d in per_layer:
            m.update(d)
        m["x"] = np.ascontiguousarray(x[r * 2048:(r + 1) * 2048])
        m["par"] = np.tile(np.array([[1.0 - p, float(p)]], np.float32), (128, 1))
        m["bmask"] = np.concatenate([(256 * p + 128 * hh + k <= q) for hh in range(2)], axis=1).astype(BF)
        for l in range(2):
            sfx = str(l)
            rw = np.zeros((64, 8), np.float32)
            sl = slice(64 * nb, 64 * nb + 64)
            rw[:, 0:4] = inputs["d_conv_w"][l][:, sl].T; rw[:, 4] = inputs["d_conv_b"][l][sl]; rw[:, 5] = inputs["d_b_a"][l][sl]
            rw[:, 6] = inputs["d_b_x"][l][sl]; rw[:, 7] = inputs["d_lambda"][l][sl]
            m["rw" + sfx] = rw
            m["rwa" + sfx] = np.ascontiguousarray(inputs["d_w_a"][l][nb]); m["rwx" + sfx] = np.ascontiguousarray(inputs["d_w_x"][l][nb])
        maps.append(m)
    return maps


def kernel(**inputs):
    inputs = {k: np.asarray(v) for k, v in inputs.items()}
    cF = build_F()
    maps = prepF(inputs)
    R = run_bass_kernel_spmd(cF.nc, maps, core_ids=list(range(8))).results
    x = np.concatenate([np.asarray(r["out"]) for r in R], axis=0)
    return np.ascontiguousarray(x[None]).astype(np.float32)
```
